# Optimizing a Trainium2 kernel written in Bass

```python
import math
import jax, jax.numpy as jnp
from jax import lax
import numpy as np

D_MODEL = 1024
BATCH = 1
SEQ = 16384
DEPTH = 2

GRID_W = 64
CTX_LEN = 256

MLA_HEADS = 8
MLA_NOPE = 64
MLA_ROPE = 32
MLA_V = 64
MLA_QK = MLA_NOPE + MLA_ROPE
MLA_Q_LORA = 384
MLA_KV_LORA = 256

NA_HEADS = 8
NA_HEAD_DIM = 64
NA_WIDTH = NA_HEADS * NA_HEAD_DIM
NA_WIN_ROWS = 8
NA_WIN_COLS = 16

RW_HEADS = 8
RW_HEAD_DIM = 64
RW_WIDTH = RW_HEADS * RW_HEAD_DIM
RW_DECAY_LORA = 64
RW_ICLR_LORA = 64
RW_GATE_LORA = 128
RW_GN_EPS = 64e-5
RW_IN = 3 * RW_WIDTH + 2 * RW_DECAY_LORA + 2 * RW_ICLR_LORA + RW_GATE_LORA

N_BRANCHES = 3
IN_SIZES = (MLA_Q_LORA, MLA_KV_LORA, MLA_ROPE, NA_WIDTH, NA_WIDTH, NA_WIDTH, RW_IN, N_BRANCHES * D_MODEL)
IN_WIDTH = sum(IN_SIZES)

FFN_DENSE = 2816
N_EXPERTS = 8
TOP_K = 2
FFN_EXPERT = 3584
MOE_BLOCK = 128
N_DENSE = (DEPTH + 1) // 2
N_MOE = DEPTH // 2

Q_BLOCK = 128
ROPE_BASE = 10000.0
NORM_EPS = 1e-6

kernel_name = "hybrid_mla_natten_rwkv7_moe_dit_block"


def rms_norm(x, g, eps=NORM_EPS):
    xf = x.astype(jnp.float32)
    y = xf * lax.rsqrt(jnp.mean(xf * xf, axis=-1, keepdims=True) + eps)
    return (y * g.astype(jnp.float32)).astype(x.dtype)


def split_last(t, sizes):
    return jnp.split(t, np.cumsum(sizes)[:-1].tolist(), axis=-1)


def to_heads(t, n_heads):
    return t.reshape(t.shape[:-1] + (n_heads, t.shape[-1] // n_heads))


def rope_axis(t, pos):
    half = t.shape[-1] // 2
    freqs = jnp.exp(-math.log(ROPE_BASE) * jnp.arange(half, dtype=jnp.float32) / half)
    ang = pos.astype(jnp.float32)[:, None] * freqs[None, :]
    cos, sin = jnp.cos(ang)[:, None, :], jnp.sin(ang)[:, None, :]
    t1 = t[..., :half].astype(jnp.float32)
    t2 = t[..., half:].astype(jnp.float32)
    return jnp.concatenate([t1 * cos - t2 * sin, t1 * sin + t2 * cos], axis=-1).astype(t.dtype)


def rope_2d_tail(t, n_pass, rows, cols):
    tail = t[..., n_pass:]
    a = tail.shape[-1] // 2
    return jnp.concatenate([t[..., :n_pass], rope_axis(tail[..., :a], rows), rope_axis(tail[..., a:], cols)], axis=-1)


def block_attention(q, k, v):
    B, Lq, H, dq = q.shape
    scale = dq ** -0.5
    nb = Lq // Q_BLOCK
    qb = q.reshape(B, nb, Q_BLOCK, H, dq).swapaxes(0, 1)

    def one(q_blk):
        s = jnp.einsum('bqhd,bkhd->bhqk', q_blk, k, preferred_element_type=jnp.float32) * scale
        p = jax.nn.softmax(s, axis=-1).astype(v.dtype)
        return jnp.einsum('bhqk,bkhd->bqhd', p, v)

    o = lax.map(one, qb)
    return o.swapaxes(0, 1).reshape(B, Lq, H, v.shape[-1])


def mla_queries(cq, cq_g, wuq, qn_g):
    q = to_heads(rms_norm(cq, cq_g) @ wuq, MLA_HEADS)
    return rms_norm(q, qn_g)


def mla_keys_values(ckv, kr, ckv_g, wukv, kn_g):
    kv = to_heads(rms_norm(ckv, ckv_g) @ wukv, MLA_HEADS)
    k_nope, v = kv[..., :MLA_NOPE], kv[..., MLA_NOPE:]
    k_rope = jnp.broadcast_to(kr[:, :, None, :], k_nope.shape[:-1] + (MLA_ROPE,))
    k = rms_norm(jnp.concatenate([k_nope, k_rope], axis=-1), kn_g)
    return k, v


def neighbourhood_attention(q, k, v, k_ctx, v_ctx, rpb):
    B, L, H, dh = q.shape
    n_rows = L // GRID_W
    kr = min(NA_WIN_ROWS, n_rows)
    kw = NA_WIN_COLS
    scale = dh ** -0.5
    kg = k.reshape(B, n_rows, GRID_W, H, dh)
    vg = v.reshape(B, n_rows, GRID_W, H, dh)
    qg = q.reshape(B, n_rows, GRID_W, H, dh).swapaxes(0, 1)
    cols = jnp.arange(GRID_W)
    col_start = jnp.clip(cols - kw // 2, 0, GRID_W - kw)
    col_idx = col_start[:, None] + jnp.arange(kw)[None, :]
    dc = col_idx - cols[:, None] + (NA_WIN_COLS - 1)

    def one_row(args):
        q_row, r = args
        rs = jnp.clip(r - kr // 2, 0, n_rows - kr)
        k_nb = lax.dynamic_slice_in_dim(kg, rs, kr, axis=1)[:, :, col_idx]
        v_nb = lax.dynamic_slice_in_dim(vg, rs, kr, axis=1)[:, :, col_idx]
        dr = rs + jnp.arange(kr) - r + (NA_WIN_ROWS - 1)
        bias = rpb[:, dr[None, :, None], dc[:, None, :]].astype(jnp.float32)
        s_loc = jnp.einsum('bqhd,brqjhd->bhqrj', q_row, k_nb, preferred_element_type=jnp.float32) * scale + bias[None]
        s_ctx = jnp.einsum('bqhd,bchd->bhqc', q_row, k_ctx, preferred_element_type=jnp.float32) * scale
        s = jnp.concatenate([s_loc.reshape(B, H, GRID_W, kr * kw), s_ctx], axis=-1)
        p = jax.nn.softmax(s, axis=-1).astype(v.dtype)
        p_loc = p[..., :kr * kw].reshape(B, H, GRID_W, kr, kw)
        p_ctx = p[..., kr * kw:]
        return jnp.einsum('bhqrj,brqjhd->bqhd', p_loc, v_nb) + jnp.einsum('bhqc,bchd->bqhd', p_ctx, v_ctx)

    o = lax.map(one_row, (qg, jnp.arange(n_rows)))
    return o.swapaxes(0, 1).reshape(B, L, H * dh)


def token_shift_centred(z, mu):
    prev = jnp.pad(z, ((0, 0), (1, 0), (0, 0)))[:, :-1]
    nxt = jnp.pad(z, ((0, 0), (0, 1), (0, 0)))[:, 1:]
    return z + mu[0] * (prev - z) + mu[1] * (nxt - z)


def rwkv_prepare(z, mu, w0, w2, a0, a2, k_k, k_a):
    B, L, _ = z.shape
    z = token_shift_centred(z.astype(jnp.float32), mu)
    r, k, v, wd, ad, gd = split_last(z, (RW_WIDTH, RW_WIDTH, RW_WIDTH, 2 * RW_DECAY_LORA, 2 * RW_ICLR_LORA, RW_GATE_LORA))
    wd = wd.reshape(B, L, 2, RW_DECAY_LORA)
    ad = ad.reshape(B, L, 2, RW_ICLR_LORA)
    w_log = -jax.nn.softplus(-(w0 + jnp.einsum('bldr,drc->bldc', jnp.tanh(wd), w2))) - 0.5
    decay = jnp.exp(-jnp.exp(w_log))
    a = jax.nn.sigmoid(a0 + jnp.einsum('bldr,drc->bldc', ad, a2))
    kk = to_heads(k * k_k, RW_HEADS)
    kk = kk / jnp.maximum(jnp.sqrt(jnp.sum(kk * kk, axis=-1, keepdims=True)), 1e-12)
    k_dir = k[:, :, None, :] * (1.0 + (a - 1.0) * k_a)
    return (to_heads(r, RW_HEADS), to_heads(v, RW_HEADS), kk, to_heads(decay, RW_HEADS),
            to_heads(a, RW_HEADS), to_heads(k_dir, RW_HEADS), gd)


def rwkv_scan(r, w, k, v, kk, a, s0, reverse):
    def step(S, inp):
        r_t, w_t, k_t, v_t, kk_t, a_t = inp
        sa = jnp.einsum('bhij,bhj->bhi', S, -kk_t)
        S = S * w_t[:, :, None, :] + sa[..., None] * (kk_t * a_t)[:, :, None, :] + v_t[..., None] * k_t[:, :, None, :]
        return S, jnp.einsum('bhij,bhj->bhi', S, r_t)

    xs = tuple(t.swapaxes(0, 1) for t in (r, w, k, v, kk, a))
    S, ys = lax.scan(step, s0, xs, reverse=reverse)
    return S, ys.swapaxes(0, 1)


def rwkv_output(r, v, k_dir, gd, y, g2, r_k, ln_g, ln_b, out_dtype):
    B, L = y.shape[:2]
    mean = jnp.mean(y, axis=-1, keepdims=True)
    var = jnp.mean(jnp.square(y - mean), axis=-1, keepdims=True)
    yn = ((y - mean) * lax.rsqrt(var + RW_GN_EPS)).reshape(B, L, RW_WIDTH) * ln_g + ln_b
    bonus = (jnp.sum(r[:, :, None] * k_dir * r_k, axis=-1, keepdims=True) * v[:, :, None]).sum(axis=2)
    g = jax.nn.sigmoid(gd) @ g2
    return ((yn + bonus.reshape(B, L, RW_WIDTH)) * g).astype(out_dtype)


def merge_branches(ya, yb, yr, gates, wo_a, wo_b, wo_r, w_o):
    g_a, g_b, g_r = jnp.split(jax.nn.sigmoid(gates), N_BRANCHES, axis=-1)
    return (g_a * (ya @ wo_a) + g_b * (yb @ wo_b) + g_r * (yr @ wo_r)) @ w_o


def swiglu(h, w1, w3, w2):
    return (jax.nn.silu(h @ w1) * (h @ w3)) @ w2


def moe_swiglu(h, router_w, w1, w3, w2):
    n_tok, d = h.shape
    logits = jnp.matmul(h, router_w, preferred_element_type=jnp.float32)
    top_logit, top_idx = lax.top_k(logits, TOP_K)
    gate = jax.nn.softmax(top_logit, axis=-1)
    n_assign = n_tok * TOP_K
    expert = top_idx.reshape(-1).astype(jnp.int32)
    token = jnp.repeat(jnp.arange(n_tok, dtype=jnp.int32), TOP_K)
    weight = gate.reshape(-1)
    order = jnp.argsort(expert)
    expert_s, token_s, weight_s = expert[order], token[order], weight[order]
    counts = jnp.bincount(expert, length=N_EXPERTS).astype(jnp.int32)
    starts = jnp.cumsum(counts) - counts
    padded = (counts + MOE_BLOCK - 1) // MOE_BLOCK * MOE_BLOCK
    pad_ends = jnp.cumsum(padded)
    pad_starts = pad_ends - padded
    dest = pad_starts[expert_s] + jnp.arange(n_assign, dtype=jnp.int32) - starts[expert_s]
    n_blocks = -(-n_assign // MOE_BLOCK) + N_EXPERTS
    buf_tok = jnp.zeros((n_blocks * MOE_BLOCK,), jnp.int32).at[dest].set(token_s)
    buf_w = jnp.zeros((n_blocks * MOE_BLOCK,), jnp.float32).at[dest].set(weight_s)
    blk_start = jnp.arange(n_blocks, dtype=jnp.int32) * MOE_BLOCK
    blk_expert = jnp.minimum(jnp.searchsorted(pad_ends, blk_start, side='right'), N_EXPERTS - 1)

    def one_block(args):
        idx, wt, e = args
        xb = h[idx]
        hid = jax.nn.silu(xb @ w1[e]) * (xb @ w3[e])
        return (hid @ w2[e]) * wt[:, None].astype(h.dtype)

    y = lax.map(one_block, (buf_tok.reshape(n_blocks, MOE_BLOCK), buf_w.reshape(n_blocks, MOE_BLOCK), blk_expert))
    return jnp.zeros_like(h).at[buf_tok].add(y.reshape(-1, d))


def setup_inputs(seed: int = 0) -> dict:
    key = jax.random.key(seed)
    ks = iter(jax.random.split(key, 64))
    D = D_MODEL

    def nrm(shape, s):
        return jax.random.normal(next(ks), shape, jnp.float32) * s

    def gain(shape):
        return 1.0 + nrm(shape, 0.05)

    return {
        "x": nrm((BATCH, SEQ, D), 1.0),
        "c": nrm((BATCH, D), 1.0),
        "ctx": nrm((BATCH, CTX_LEN, D), 1.0),
        "c_ctx": nrm((D,), 1.0),
        "mod_w": nrm((DEPTH, D, 6 * D), 0.5 * D ** -0.5),
        "mod_b": nrm((DEPTH, 6 * D), 0.02),
        "norm1_g": gain((DEPTH, D)),
        "norm2_g": gain((DEPTH, D)),
        "w_in": nrm((DEPTH, D, IN_WIDTH), D ** -0.5),
        "mla_cq_g": gain((DEPTH, MLA_Q_LORA)),
        "mla_wuq": nrm((DEPTH, MLA_Q_LORA, MLA_HEADS * MLA_QK), MLA_Q_LORA ** -0.5),
        "mla_ckv_g": gain((DEPTH, MLA_KV_LORA)),
        "mla_wukv": nrm((DEPTH, MLA_KV_LORA, MLA_HEADS * (MLA_NOPE + MLA_V)), MLA_KV_LORA ** -0.5),
        "mla_qn_g": gain((DEPTH, MLA_QK)),
        "mla_kn_g": gain((DEPTH, MLA_QK)),
        "mla_wo": nrm((DEPTH, MLA_HEADS * MLA_V, D), (MLA_HEADS * MLA_V) ** -0.5),
        "na_qn_g": gain((DEPTH, NA_HEAD_DIM)),
        "na_kn_g": gain((DEPTH, NA_HEAD_DIM)),
        "na_rpb": nrm((DEPTH, NA_HEADS, 2 * NA_WIN_ROWS - 1, 2 * NA_WIN_COLS - 1), 0.1),
        "na_wo": nrm((DEPTH, NA_WIDTH, D), NA_WIDTH ** -0.5),
        "rw_mu": jax.random.uniform(next(ks), (DEPTH, 2, RW_IN), jnp.float32, 0.0, 0.5),
        "rw_w0": jax.random.uniform(next(ks), (DEPTH, 2, RW_WIDTH), jnp.float32, -6.0, 1.0),
        "rw_w2": nrm((DEPTH, 2, RW_DECAY_LORA, RW_WIDTH), RW_DECAY_LORA ** -0.5),
        "rw_a0": nrm((DEPTH, 2, RW_WIDTH), 0.5),
        "rw_a2": nrm((DEPTH, 2, RW_ICLR_LORA, RW_WIDTH), RW_ICLR_LORA ** -0.5),
        "rw_g2": nrm((DEPTH, RW_GATE_LORA, RW_WIDTH), RW_GATE_LORA ** -0.5),
        "rw_kk": 0.85 + nrm((DEPTH, RW_WIDTH), 0.05),
        "rw_ka": gain((DEPTH, RW_WIDTH)),
        "rw_rk": nrm((DEPTH, RW_HEADS, RW_HEAD_DIM), 0.1),
        "rw_ln_g": gain((DEPTH, RW_WIDTH)),
        "rw_ln_b": nrm((DEPTH, RW_WIDTH), 0.02),
        "rw_wo": nrm((DEPTH, RW_WIDTH, D), RW_WIDTH ** -0.5),
        "w_out": nrm((DEPTH, D, D), D ** -0.5),
        "ffn_w1": nrm((N_DENSE, D, FFN_DENSE), D ** -0.5),
        "ffn_w3": nrm((N_DENSE, D, FFN_DENSE), D ** -0.5),
        "ffn_w2": nrm((N_DENSE, FFN_DENSE, D), FFN_DENSE ** -0.5),
        "moe_router": nrm((N_MOE, D, N_EXPERTS), D ** -0.5),
        "moe_w1": nrm((N_MOE, N_EXPERTS, D, FFN_EXPERT), D ** -0.5),
        "moe_w3": nrm((N_MOE, N_EXPERTS, D, FFN_EXPERT), D ** -0.5),
        "moe_w2": nrm((N_MOE, N_EXPERTS, FFN_EXPERT, D), FFN_EXPERT ** -0.5),
    }


def reference(x, c, ctx, c_ctx, mod_w, mod_b, norm1_g, norm2_g, w_in,
              mla_cq_g, mla_wuq, mla_ckv_g, mla_wukv, mla_qn_g, mla_kn_g, mla_wo,
              na_qn_g, na_kn_g, na_rpb, na_wo,
              rw_mu, rw_w0, rw_w2, rw_a0, rw_a2, rw_g2, rw_kk, rw_ka, rw_rk, rw_ln_g, rw_ln_b, rw_wo,
              w_out, ffn_w1, ffn_w3, ffn_w2, moe_router, moe_w1, moe_w3, moe_w2):
    B, L, D = x.shape
    n_ctx = ctx.shape[1]
    pos = jnp.arange(L)
    rows, cols = pos // GRID_W, pos % GRID_W
    s_lat = jax.nn.silu(c)
    s_ctx = jax.nn.silu(c_ctx)
    for l in range(DEPTH):
        need_ctx = l < DEPTH - 1
        mod = (s_lat @ mod_w[l] + mod_b[l])[:, None, :]
        mod_c = s_ctx @ mod_w[l] + mod_b[l]
        sh1, sc1, gt1, sh2, sc2, gt2 = jnp.split(mod, 6, axis=-1)
        csh1, csc1, cgt1, csh2, csc2, cgt2 = jnp.split(mod_c, 6, axis=-1)

        h = rms_norm(x, norm1_g[l]) * (1.0 + sc1) + sh1
        hc = rms_norm(ctx, norm1_g[l]) * (1.0 + csc1) + csh1
        m_cq, m_ckv, m_kr, n_q, n_k, n_v, r_z, gates = split_last(h @ w_in[l], IN_SIZES)
        mc_cq, mc_ckv, mc_kr, nc_q, nc_k, nc_v, rc_z, gates_c = split_last(hc @ w_in[l], IN_SIZES)

        qa = rope_2d_tail(mla_queries(m_cq, mla_cq_g[l], mla_wuq[l], mla_qn_g[l]), MLA_NOPE, rows, cols)
        ka, va = mla_keys_values(m_ckv, m_kr, mla_ckv_g[l], mla_wukv[l], mla_kn_g[l])
        ka = rope_2d_tail(ka, MLA_NOPE, rows, cols)
        ka_c, va_c = mla_keys_values(mc_ckv, mc_kr, mla_ckv_g[l], mla_wukv[l], mla_kn_g[l])
        ya = block_attention(qa, jnp.concatenate([ka_c, ka], axis=1),
                             jnp.concatenate([va_c, va], axis=1)).reshape(B, L, -1)

        qb = rms_norm(to_heads(n_q, NA_HEADS), na_qn_g[l])
        kb = rms_norm(to_heads(n_k, NA_HEADS), na_kn_g[l])
        vb = to_heads(n_v, NA_HEADS)
        kb_c = rms_norm(to_heads(nc_k, NA_HEADS), na_kn_g[l])
        vb_c = to_heads(nc_v, NA_HEADS)
        yb = neighbourhood_attention(qb, kb, vb, kb_c, vb_c, na_rpb[l])

        rw_p = (rw_mu[l], rw_w0[l], rw_w2[l], rw_a0[l], rw_a2[l], rw_kk[l], rw_ka[l])
        r_l, v_l, kk_l, w_l, a_l, kd_l, gd_l = rwkv_prepare(r_z, *rw_p)
        r_c, v_c, kk_c, w_c, a_c, kd_c, gd_c = rwkv_prepare(rc_z, *rw_p)
        s0 = jnp.zeros((B, RW_HEADS, RW_HEAD_DIM, RW_HEAD_DIM), jnp.float32)
        s_cf, y_cf = rwkv_scan(r_c, w_c[:, :, 0], kd_c[:, :, 0], v_c, kk_c, a_c[:, :, 0], s0, False)
        s_cb, y_cb = rwkv_scan(r_c, w_c[:, :, 1], kd_c[:, :, 1], v_c, kk_c, a_c[:, :, 1], s0, True)
        _, y_f = rwkv_scan(r_l, w_l[:, :, 0], kd_l[:, :, 0], v_l, kk_l, a_l[:, :, 0], s_cf, False)
        _, y_b = rwkv_scan(r_l, w_l[:, :, 1], kd_l[:, :, 1], v_l, kk_l, a_l[:, :, 1], s_cb, True)
        rw_o = (rw_g2[l], rw_rk[l], rw_ln_g[l], rw_ln_b[l])
        yr = rwkv_output(r_l, v_l, kd_l, gd_l, y_f + y_b, *rw_o, x.dtype)

        x_mid = x + gt1 * merge_branches(ya, yb, yr, gates, mla_wo[l], na_wo[l], rw_wo[l], w_out[l])
        if need_ctx:
            qa_c = mla_queries(mc_cq, mla_cq_g[l], mla_wuq[l], mla_qn_g[l])
            ya_c = block_attention(qa_c, ka_c, va_c).reshape(B, n_ctx, -1)
            qb_c = rms_norm(to_heads(nc_q, NA_HEADS), na_qn_g[l])
            yb_c = block_attention(qb_c, kb_c, vb_c).reshape(B, n_ctx, -1)
            yr_c = rwkv_output(r_c, v_c, kd_c, gd_c, y_cf + y_cb, *rw_o, ctx.dtype)
            ctx_mid = ctx + cgt1 * merge_branches(ya_c, yb_c, yr_c, gates_c, mla_wo[l], na_wo[l], rw_wo[l], w_out[l])

        tokens = (rms_norm(x_mid, norm2_g[l]) * (1.0 + sc2) + sh2).reshape(B * L, D)
        if need_ctx:
            h2c = rms_norm(ctx_mid, norm2_g[l]) * (1.0 + csc2) + csh2
            tokens = jnp.concatenate([tokens, h2c.reshape(B * n_ctx, D)], axis=0)
        if l % 2 == 0:
            f = swiglu(tokens, ffn_w1[l // 2], ffn_w3[l // 2], ffn_w2[l // 2])
        else:
            f = moe_swiglu(tokens, moe_router[l // 2], moe_w1[l // 2], moe_w3[l // 2], moe_w2[l // 2])
        x = x_mid + gt2 * f[:B * L].reshape(B, L, D)
        if need_ctx:
            ctx = ctx_mid + cgt2 * f[B * L:].reshape(B, n_ctx, D)
    return x
```

```python
from contextlib import ExitStack
import numpy as np
import ml_dtypes
import concourse.bass as bass
import concourse.mybir as mybir
from concourse.bass_utils import run_bass_kernel_spmd

F32 = mybir.dt.float32
BF16 = mybir.dt.bfloat16
AF = mybir.ActivationFunctionType
ALU = mybir.AluOpType
AX = mybir.AxisListType
NCORES = 8
ENGINES = ("tensor", "vector", "scalar", "gpsimd", "sync")


class Buf:
    __slots__ = ("t", "name", "lw", "rd")

    def __init__(self, t, name):
        self.t = t
        self.name = name
        self.lw = None
        self.rd = {}

    def __getitem__(self, idx):
        return self.t[idx]


class Prog:
    def __init__(self):
        self.nc = bass.Bass("TRN2", target_bir_lowering=False)
        self.ops = {e: [] for e in ENGINES}
        self.count = {}
        self.waited = {e: {} for e in ENGINES}
        self.stack = ExitStack()
        self.nbuf = 0

    def sbuf(self, shape, dt, name=None):
        self.nbuf += 1
        name = name or f"sb{self.nbuf}"
        t = self.stack.enter_context(self.nc.sbuf_tensor(name, list(shape), dt))
        return Buf(t, name)

    def psum(self, shape, dt=F32, name=None):
        self.nbuf += 1
        name = name or f"ps{self.nbuf}"
        t = self.stack.enter_context(self.nc.psum_tensor(name, list(shape), dt))
        return Buf(t, name)

    def dram(self, name, shape, dt, kind):
        t = self.nc.dram_tensor(name, list(shape), dt, kind=kind).ap()
        return Buf(t, name)

    def _deps(self, eng, reads, writes, skip_self):
        need = {}

        def add(kv):
            if kv is None:
                return
            k, v = kv
            if need.get(k, 0) < v:
                need[k] = v

        for b in reads:
            add(b.lw)
        for b in writes:
            add(b.lw)
            for k, v in b.rd.items():
                add((k, v))
        w = self.waited[eng]
        out = []
        for k, v in need.items():
            if skip_self and k == eng:
                continue
            if w.get(k, 0) < v:
                w[k] = v
                out.append((k, v))
        return out

    def _commit(self, key, inc, reads, writes):
        v = self.count.get(key, 0) + inc
        self.count[key] = v
        for b in reads:
            if b.rd.get(key, 0) < v:
                b.rd[key] = v
        for b in writes:
            b.lw = (key, v)
            b.rd = {}
        return v

    def op(self, eng, fn, reads=(), writes=(), skip_self=False):
        waits = self._deps(eng, reads, writes, skip_self)
        self._commit(eng, 1, reads, writes)
        self.ops[eng].append((waits, fn, eng, 1))

    def dma(self, eng, out_ap, in_ap, reads, writes, sem_buf):
        waits = self._deps(eng, reads, writes, False)
        key = "d_" + sem_buf.name
        self._commit(key, 16, reads, writes)
        self.ops[eng].append((waits, lambda e: e.dma_start(out=out_ap, in_=in_ap), key, 16))

    def coll(self, kind, in_buf, out_buf, op=None):
        waits = self._deps("gpsimd", [in_buf], [out_buf], False)
        key = "c_" + out_buf.name
        self._commit(key, 16, [in_buf], [out_buf])
        ia, oa = in_buf.t, out_buf.t
        op = op or ALU.bypass
        self.ops["gpsimd"].append((waits, lambda e: e.collective_compute(
            kind, op, replica_groups=[list(range(NCORES))], ins=[ia], outs=[oa]), key, 16))

    def barrier(self):
        snap = dict(self.count)
        for e in ENGINES:
            w = self.waited[e]
            waits = [(k, v) for k, v in snap.items() if w.get(k, 0) < v]
            for k, v in waits:
                w[k] = v
            if waits:
                self.ops[e].append((waits, None, None, 0))

    def scratch(self, name, shape, dt, shared=False):
        t = self.nc.dram_tensor(name, list(shape), dt, addr_space=("Shared" if shared else "Local")).ap()
        return Buf(t, name)

    def mm(self, out_ap, lhsT_ap, rhs_ap, start, stop, reads, writes):
        self.op("tensor", lambda e: e.matmul(out_ap, lhsT_ap, rhs_ap, start=start, stop=stop),
                reads, writes, skip_self=True)

    def transpose(self, out_ap, in_ap, ident_ap, reads, writes):
        self.op("tensor", lambda e: e.transpose(out_ap, in_ap, ident_ap), reads, writes, skip_self=True)

    def act(self, out_ap, in_ap, func, reads, writes, bias=None, scale=None, eng="scalar"):
        kw = {}
        if bias is not None:
            kw["bias"] = bias
        if scale is not None:
            kw["scale"] = scale
        self.op(eng, lambda e: e.activation(out_ap, in_ap, func, **kw), reads, writes)

    def tt(self, out_ap, a_ap, b_ap, op, reads, writes, eng="vector"):
        self.op(eng, lambda e: e.tensor_tensor(out_ap, a_ap, b_ap, op), reads, writes)

    def ts(self, out_ap, a_ap, s1, s2, op0, op1, reads, writes, eng="vector"):
        if s2 is None:
            self.op(eng, lambda e: e.tensor_scalar(out_ap, a_ap, s1, None, op0), reads, writes)
        else:
            self.op(eng, lambda e: e.tensor_scalar(out_ap, a_ap, s1, s2, op0, op1), reads, writes)

    def stt(self, out_ap, in0, scalar, in1, op0, op1, reads, writes):
        self.op("vector", lambda e: e.scalar_tensor_tensor(out_ap, in0, scalar, in1, op0, op1), reads, writes)

    def copy(self, out_ap, in_ap, reads, writes, eng="vector"):
        if eng == "scalar":
            self.op(eng, lambda e: e.copy(out_ap, in_ap), reads, writes)
        else:
            self.op(eng, lambda e: e.tensor_copy(out_ap, in_ap), reads, writes)

    def memset(self, ap, val, writes, eng="vector"):
        self.op(eng, lambda e: e.memset(ap, val), (), writes)

    def finish(self):
        nc = self.nc
        final_waits = []
        w = self.waited["sync"]
        for k, v in self.count.items():
            if w.get(k, 0) < v:
                final_waits.append((k, v))
        keys = list(self.count.keys())
        sems = {}
        for i, k in enumerate(keys):
            sems[k] = self.stack.enter_context(nc.semaphore(f"s{i}"))
        ops = self.ops

        def replay(e, lst):
            for waits, fn, key, inc in lst:
                for (k, v) in waits:
                    e.wait_ge(sems[k], v)
                if fn is not None:
                    fn(e).then_inc(sems[key], inc)

        with nc.Block() as block:
            @block.tensor
            def _(e):
                replay(e, ops["tensor"])

            @block.vector
            def _(e):
                replay(e, ops["vector"])

            @block.scalar
            def _(e):
                replay(e, ops["scalar"])

            @block.gpsimd
            def _(e):
                replay(e, ops["gpsimd"])

            @block.sync
            def _(e):
                replay(e, ops["sync"])
                for (k, v) in final_waits:
                    e.wait_ge(sems[k], v)
        self.stack.close()
        return nc


TRACE = False
TIMES = []


def run(prog, in_maps):
    nc = prog.finish()
    if TRACE:
        res = run_bass_kernel_spmd(nc, in_maps, core_ids=list(range(NCORES)), trace=True)
        TIMES.append(res.exec_time_ns)
        print("exec_time_ns", res.exec_time_ns, flush=True)
    else:
        res = run_bass_kernel_spmd(nc, in_maps, core_ids=list(range(NCORES)))
    return res.results


def build_L0(nch):
    P = Prog()
    w = P.dram("w", [1024, nch * 128], F32, "ExternalInput")
    b = P.dram("b", [128, nch], F32, "ExternalInput")
    cT = P.dram("cT", [128, 8, 2], F32, "ExternalInput")
    out = P.dram("out", [128, nch, 2], F32, "ExternalOutput")
    wt = P.sbuf([128, 8, nch * 128], F32, "wt")
    bt = P.sbuf([128, nch], F32, "bt")
    ct = P.sbuf([128, 8, 2], F32, "ct")
    st = P.sbuf([128, 8, 2], F32, "st")
    ot = P.sbuf([128, nch, 2], F32, "ot")
    ps = P.psum([128, nch, 2], F32, "ps0")
    P.dma("sync", ct[:], cT[:], [cT], [ct], ct)
    P.dma("sync", bt[:], b[:], [b], [bt], bt)
    for k in range(8):
        P.dma("sync", wt[:, k, :], w[k * 128:(k + 1) * 128, :], [w], [wt], wt)
    P.act(st[:], ct[:], AF.Silu, [ct], [st])
    for j in range(nch):
        for k in range(8):
            P.mm(ps[:, j, :], wt[:, k, j * 128:(j + 1) * 128], st[:, k, :], k == 0, k == 7, [wt, st], [ps])
    for j in range(nch):
        P.ts(ot[:, j, :], ps[:, j, :], bt[:, j:j + 1], None, ALU.add, None, [ps, bt], [ot])
    P.dma("sync", out[:], ot[:], [ot], [out], ot)
    return P


def fm(v):
    v = np.asarray(v, np.float32)
    return np.ascontiguousarray(v.reshape(-1, 128).T)


def run_L0(c, c_ctx, mod_w, mod_b):
    depth = mod_w.shape[0]
    nch_total = depth * 48
    nch = nch_total // NCORES
    wcat = np.concatenate([mod_w[l] for l in range(depth)], axis=1)
    bcat = np.concatenate([mod_b[l] for l in range(depth)], axis=0)
    cT = np.stack([fm(c.reshape(-1)), fm(c_ctx.reshape(-1))], axis=-1)
    in_maps = []
    for i in range(NCORES):
        sl = slice(i * nch * 128, (i + 1) * nch * 128)
        in_maps.append({"w": np.ascontiguousarray(wcat[:, sl]), "b": fm(bcat[sl]), "cT": cT})
    res = run(build_L0(nch), in_maps)
    o = np.concatenate([r["out"] for r in res], axis=1)
    return o.reshape(128, depth, 48, 2)


RW_ORDER = [12, 13, 14, 0, 4, 8, 1, 5, 9, 2, 6, 10, 3, 7, 11]


def build_L1(NL, NSUB):
    NS = NL + 260
    tiles = [(c0, min(512, NS - c0)) for c0 in range(0, NS, 512)]
    P = Prog()
    I = lambda n, s, dt=F32: P.dram(n, s, dt, "ExternalInput")
    O = lambda n, s, dt=F32: P.dram(n, s, dt, "ExternalOutput")
    xT_ = I("xT", [NSUB, 128, 8, NS])
    nv = I("nv", [128, 5, 8])
    wq = I("wq", [33, 128, 8, 128])
    ropeC_ = I("ropeC", [NSUB, 128, NS]); ropeS_ = I("ropeS", [NSUB, 128, NS]); mask_ = I("mask", [NSUB, 128, NS], BF16)
    sp = I("sp", [128, 16])
    wuq = I("wuq", [128, 3, 8, 128]); wuk = I("wuk", [128, 2, 8, 128]); wuv = I("wuv", [128, 2, 4, 128])
    rwp = I("rwp", [128, 58])
    w2d = I("w2", [128, 512]); a2d = I("a2", [128, 512]); g2d = I("g2", [128, 512])
    cst = I("cst", [128, 3, 128])
    o_qa_ = O("qa", [NSUB, 8, 128, NS], BF16); o_ka_ = O("ka", [NSUB, 8, 128, NS], BF16)
    o_va_ = O("va", [NSUB, 4, 128, NS], BF16)
    o_n_ = O("nqkv", [NSUB, 3, 4, 128, NS], BF16)
    o_r3_ = O("rw3", [NSUB, 3, 4, 128, NS])
    o_d3_ = O("rwd", [NSUB, 3, 2, 4, 128, NS])
    o_gb_ = O("rwgb", [NSUB, 2, 4, 128, NS])

    def load(d, shape, dt=F32, name=None):
        t = P.sbuf(shape, dt, name)
        P.dma("sync", t[:], d[:], [d], [t], t)
        return t
    nvt = load(nv, [128, 5, 8]); spt = load(sp, [128, 16]); rwt = load(rwp, [128, 58])
    cs = load(cst, [128, 3, 128])
    w2t = load(w2d, [128, 512]); a2t = load(a2d, [128, 512]); g2t = load(g2d, [128, 512])
    stg = P.sbuf([128, 3 * 8 * 128], F32, "stg")
    wuqb = P.sbuf([128, 3, 8, 128], BF16); wukb = P.sbuf([128, 2, 8, 128], BF16); wuvb = P.sbuf([128, 2, 4, 128], BF16)
    for (src, dstb, nel) in ((wuq, wuqb, 3 * 8 * 128), (wuk, wukb, 2 * 8 * 128), (wuv, wuvb, 2 * 4 * 128)):
        P.dma("sync", stg[:, :nel], src[:].rearrange("p a b c -> p (a b c)"), [src], [stg], stg)
        P.copy(dstb[:].rearrange("p a b c -> p (a b c)"), stg[:, :nel], [stg], [dstb], eng="gpsimd")
    ones = cs[:, 0, :]; blk = cs[:, 1, :]; rot = cs[:, 2, :]
    At = P.sbuf([128, 2, 8], F32)
    for w_, sc in ((0, 1), (1, 3)):
        P.stt(At[:, w_, :], nvt[:, sc, :], 1.0, nvt[:, 0, :], ALU.add, ALU.mult, [nvt], [At])
    m2 = P.sbuf([128, 15], F32)
    P.tt(m2[:], rwt[:, 0:15], rwt[:, 15:30], ALU.add, [rwt], [m2])
    P.ts(m2[:], m2[:], -1.0, 1.0, ALU.mult, ALU.add, [m2], [m2])

    psb = [P.psum([128, 512], F32, f"psb{i}") for i in range(6)]
    pi = [0]
    def PS():
        pi[0] += 1
        return psb[pi[0] % 6]
    slabs = [P.sbuf([128, NS], F32, f"sl{i}") for i in range(13)]
    bslabs = [P.sbuf([128, NS], BF16, f"bs{i}") for i in range(3)]
    bi = [0]
    def BS():
        bi[0] += 1
        return bslabs[bi[0] % 3]
    Ct = P.sbuf([128, NS], F32, "Ct"); St = P.sbuf([128, NS], F32, "St"); Mt = P.sbuf([128, NS], BF16, "Mt")
    hT = P.sbuf([128, 8, NS], BF16, "hT")
    x_ = P.sbuf([128, 8, 512], F32, "xt")
    sqt = [P.sbuf([128, 512], F32, f"sq{i}") for i in range(2)]
    rs = P.sbuf([128, 512], F32, "rs")
    wst = [P.sbuf([128, 8, 128], F32, f"wst{i}") for i in range(2)]
    wbf = [P.sbuf([128, 8, 128], BF16, f"wbf{i}") for i in range(2)]
    wi = [0]

    def proj(ci, dst, masked=False):
        wi[0] += 1
        ws, wb = wst[wi[0] % 2], wbf[wi[0] % 2]
        P.dma("sync", ws[:], wq[ci], [wq], [ws], ws)
        P.copy(wb[:], ws[:], [ws], [wb], eng="gpsimd")
        for (c0, n) in tiles:
            ps = PS()
            for k in range(8):
                P.mm(ps[:, :n], wb[:, k, :], hT[:, k, c0:c0 + n], k == 0, k == 7, [wb, hT], [ps])
            if masked:
                P.tt(dst[:, c0:c0 + n], ps[:, :n], Mt[:, c0:c0 + n], ALU.mult, [ps, Mt], [dst])
            else:
                P.copy(dst[:, c0:c0 + n], ps[:, :n], [ps], [dst], eng="scalar")

    def rstd_of(srcs, lhsT, dim, eps, dst, sqrt_only=False):
        tmp = slabs[12]
        for (c0, n) in tiles:
            ps = PS()
            for j, s in enumerate(srcs):
                P.act(tmp[:, c0:c0 + n], s[:, c0:c0 + n], AF.Square, [s], [tmp])
                P.mm(ps[:, :n], lhsT, tmp[:, c0:c0 + n], j == 0, j == len(srcs) - 1, [cs, tmp], [ps])
            P.act(dst[:, c0:c0 + n], ps[:, :n], AF.Sqrt, [ps], [dst], bias=eps, scale=1.0 / dim)
        if sqrt_only:
            P.ts(dst[:], dst[:], 1e-12, None, ALU.max, None, [dst], [dst])
        P.op("vector", lambda e: e.reciprocal(dst[:], dst[:]), [dst], [dst])

    def body(sub):
        D = lambda b, *idx: Buf(b.t[(sub,) + idx] if idx else b.t[sub], b.name + "_v")
        xT = D(xT_)
        o_qa, o_ka, o_va, o_n, o_r3, o_d3, o_gb = (D(o_qa_), D(o_ka_), D(o_va_), D(o_n_), D(o_r3_), D(o_d3_), D(o_gb_))
        P.dma("sync", Ct[:], ropeC_[sub], [ropeC_], [Ct], Ct)
        P.dma("sync", St[:], ropeS_[sub], [ropeS_], [St], St)
        P.dma("sync", Mt[:], mask_[sub], [mask_], [Mt], Mt)

        def out_dma(dram_ap, dram_buf, sb):
            P.dma("gpsimd", dram_ap, sb[:], [sb], [dram_buf], sb)

        for ti, (c0, n) in enumerate(tiles):
            P.dma("sync", x_[:, :, :n], xT[:, :, c0:c0 + n], [xT], [x_], x_)
            ps = PS()
            for k in range(8):
                s_ = sqt[k % 2]
                P.act(s_[:, :n], x_[:, k, :n], AF.Square, [x_], [s_])
                P.mm(ps[:, :n], ones, s_[:, :n], k == 0, k == 7, [cs, s_], [ps])
            P.act(rs[:, :n], ps[:, :n], AF.Sqrt, [ps], [rs], bias=1e-6, scale=1.0 / 1024)
            P.op("vector", lambda e, a=rs[:, :n]: e.reciprocal(a, a), [rs], [rs])
            for k in range(8):
                P.tt(x_[:, k, :n], x_[:, k, :n], rs[:, :n], ALU.mult, [x_, rs], [x_])
                for (a, b, w_, sh) in ((0, NL + 2, 0, 2), (NL + 2, NS, 1, 4)):
                    lo, hi = max(a, c0), min(b, c0 + n)
                    if lo < hi:
                        P.ts(hT[:, k, lo:hi], x_[:, k, lo - c0:hi - c0], At[:, w_, k:k + 1], nvt[:, sh, k:k + 1],
                             ALU.mult, ALU.add, [x_, At, nvt], [hT])

        cq = slabs[0:3]
        for j in range(3):
            proj(j, cq[j])
        r_ = slabs[3]
        rstd_of(cq, ones, 384.0, 1e-6, r_)
        cqn = [BS() for _ in range(3)]
        for j in range(3):
            P.stt(cqn[j][:], cq[j][:], spt[:, j:j + 1], r_[:], ALU.mult, ALU.mult, [cq[j], spt, r_], [cqn[j]])

        def head_finish(pre, gcol, dram_ap, dram_buf, ob):
            rr = slabs[4]
            rstd_of([pre], ones, 96.0, 1e-6, rr)
            P.stt(pre[:], pre[:], spt[:, gcol:gcol + 1], rr[:], ALU.mult, ALU.mult, [pre, spt, rr], [pre])
            rq = slabs[5]
            for (c0, n) in tiles:
                ps = PS()
                P.mm(ps[:, :n], rot, pre[:, c0:c0 + n], True, True, [cs, pre], [ps])
                P.tt(rq[:, c0:c0 + n], ps[:, :n], St[:, c0:c0 + n], ALU.mult, [ps, St], [rq])
            P.tt(pre[:], pre[:], Ct[:], ALU.mult, [pre, Ct], [pre])
            P.tt(ob[:], pre[:], rq[:], ALU.add, [pre, rq], [ob])
            out_dma(dram_ap, dram_buf, ob)

        obs = [slabs[8].t, slabs[9].t]
        qob = [P_q0, P_q1]
        for h in range(8):
            pre = slabs[6 + h % 2]
            for (c0, n) in tiles:
                ps = PS()
                for j in range(3):
                    P.mm(ps[:, :n], wuqb[:, j, h, :], cqn[j][:, c0:c0 + n], j == 0, j == 2, [wuqb, cqn[j]], [ps])
                P.copy(pre[:, c0:c0 + n], ps[:, :n], [ps], [pre], eng="scalar")
            head_finish(pre, 5, o_qa[h], o_qa, qob[h % 2])

        ckv = slabs[0:2]
        for j in range(2):
            proj(3 + j, ckv[j])
        krp = slabs[2]
        proj(5, krp)
        rstd_of(ckv, ones, 256.0, 1e-6, r_)
        ckvn = [BS() for _ in range(2)]
        for j in range(2):
            P.stt(ckvn[j][:], ckv[j][:], spt[:, 3 + j:4 + j], r_[:], ALU.mult, ALU.mult, [ckv[j], spt, r_], [ckvn[j]])
        for h in range(8):
            pre = slabs[6 + h % 2]
            for (c0, n) in tiles:
                ps = PS()
                for j in range(2):
                    P.mm(ps[:, :n], wukb[:, j, h, :], ckvn[j][:, c0:c0 + n], j == 0, j == 1, [wukb, ckvn[j]], [ps])
                P.tt(pre[:, c0:c0 + n], ps[:, :n], krp[:, c0:c0 + n], ALU.add, [ps, krp], [pre])
            head_finish(pre, 6, o_ka[h], o_ka, qob[h % 2])
        for c in range(4):
            ob = qob[c % 2]
            for (c0, n) in tiles:
                ps = PS()
                for j in range(2):
                    P.mm(ps[:, :n], wuvb[:, j, c, :], ckvn[j][:, c0:c0 + n], j == 0, j == 1, [wuvb, ckvn[j]], [ps])
                P.copy(ob[:, c0:c0 + n], ps[:, :n], [ps], [ob], eng="scalar")
            out_dma(o_va[c], o_va, ob)

        for which in range(3):
            for c in range(4):
                z = slabs[c % 2]
                proj(6 + which * 4 + c, z)
                ob = BS()
                if which < 2:
                    rr = slabs[4]
                    rstd_of([z], blk, 64.0, 1e-6, rr)
                    P.stt(ob[:], z[:], spt[:, 7 + which:8 + which], rr[:], ALU.mult, ALU.mult, [z, spt, rr], [ob])
                else:
                    P.copy(ob[:], z[:], [z], [ob])
                out_dma(o_n[which, c], o_n, ob)

        def shifted(ci_rw, dst, tmp):
            rwc = RW_ORDER[ci_rw]
            proj(18 + ci_rw, tmp, masked=True)
            P.ts(dst[:, 1:NS - 1], tmp[:, 1:NS - 1], m2[:, rwc:rwc + 1], None, ALU.mult, None, [tmp, m2], [dst])
            P.stt(dst[:, 1:NS - 1], tmp[:, 0:NS - 2], rwt[:, rwc:rwc + 1], dst[:, 1:NS - 1], ALU.mult, ALU.add,
                  [tmp, rwt, dst], [dst])
            P.stt(dst[:, 1:NS - 1], tmp[:, 2:NS], rwt[:, 15 + rwc:16 + rwc], dst[:, 1:NS - 1], ALU.mult, ALU.add,
                  [tmp, rwt, dst], [dst])
        tmp = slabs[11]
        wdT, adT, gdT = slabs[0], slabs[1], slabs[2]
        shifted(0, wdT, tmp); shifted(1, adT, tmp); shifted(2, gdT, tmp)
        P.act(wdT[:], wdT[:], AF.Tanh, [wdT], [wdT])
        P.act(gdT[:], gdT[:], AF.Sigmoid, [gdT], [gdT])
        for c in range(4):
            rT, kT, vT = slabs[3], slabs[4], slabs[5]
            shifted(3 + 3 * c, rT, tmp); shifted(4 + 3 * c, kT, tmp); shifted(5 + 3 * c, vT, tmp)
            P.dma("gpsimd", o_r3[0, c], rT[:], [rT], [o_r3], rT)
            P.dma("gpsimd", o_r3[1, c], vT[:], [vT], [o_r3], vT)
            kk = slabs[6]
            P.ts(kk[:], kT[:], rwt[:, 46 + c:47 + c], None, ALU.mult, None, [kT, rwt], [kk])
            rr = slabs[7]
            rstd_of([kk], blk, 1.0, 0.0, rr, sqrt_only=True)
            P.tt(kk[:], kk[:], rr[:], ALU.mult, [kk, rr], [kk])
            P.dma("gpsimd", o_r3[2, c], kk[:], [kk], [o_r3], kk)
            ksum = slabs[8]
            for d in range(2):
                lw, aa = slabs[9], slabs[10]
                pb = slice(d * 64, d * 64 + 64)
                for (c0, n) in tiles:
                    ps = PS()
                    P.mm(ps[:, :n], w2t[pb, c * 128:(c + 1) * 128], wdT[pb, c0:c0 + n], True, True, [w2t, wdT], [ps])
                    P.act(lw[:, c0:c0 + n], ps[:, :n], AF.Sigmoid, [ps, rwt], [lw],
                          bias=rwt[:, 30 + d * 4 + c:31 + d * 4 + c])
                    ps = PS()
                    P.mm(ps[:, :n], a2t[pb, c * 128:(c + 1) * 128], adT[pb, c0:c0 + n], True, True, [a2t, adT], [ps])
                    P.act(aa[:, c0:c0 + n], ps[:, :n], AF.Sigmoid, [ps, rwt], [aa],
                          bias=rwt[:, 38 + d * 4 + c:39 + d * 4 + c])
                P.ts(lw[:], lw[:], -float(np.exp(-0.5)), None, ALU.mult, None, [lw], [lw])
                P.dma("gpsimd", o_d3[0, d, c], lw[:], [lw], [o_d3], lw)
                bb = slabs[11]
                P.tt(bb[:], aa[:], kk[:], ALU.mult, [aa, kk], [bb])
                P.dma("gpsimd", o_d3[1, d, c], bb[:], [bb], [o_d3], bb)
                P.ts(aa[:], aa[:], -1.0, rwt[:, 50 + c:51 + c], ALU.add, ALU.mult, [aa, rwt], [aa])
                P.stt(aa[:], aa[:], 1.0, kT[:], ALU.add, ALU.mult, [aa, kT], [aa])
                P.dma("gpsimd", o_d3[2, d, c], aa[:], [aa], [o_d3], aa)
                if d == 0:
                    P.copy(ksum[:], aa[:], [aa], [ksum])
                else:
                    P.tt(ksum[:], ksum[:], aa[:], ALU.add, [ksum, aa], [ksum])
            P.stt(ksum[:], ksum[:], rwt[:, 54 + c:55 + c], rT[:], ALU.mult, ALU.mult, [ksum, rwt, rT], [ksum])
            gg, bo = slabs[9], slabs[10]
            for (c0, n) in tiles:
                ps = PS()
                P.mm(ps[:, :n], blk, ksum[:, c0:c0 + n], True, True, [cs, ksum], [ps])
                P.tt(bo[:, c0:c0 + n], ps[:, :n], vT[:, c0:c0 + n], ALU.mult, [ps, vT], [bo])
                ps = PS()
                P.mm(ps[:, :n], g2t[:, c * 128:(c + 1) * 128], gdT[:, c0:c0 + n], True, True, [g2t, gdT], [ps])
                P.copy(gg[:, c0:c0 + n], ps[:, :n], [ps], [gg], eng="scalar")
            P.dma("gpsimd", o_gb[0, c], gg[:], [gg], [o_gb], gg)
            P.dma("gpsimd", o_gb[1, c], bo[:], [bo], [o_gb], bo)

    P_q0 = P.sbuf([128, NS], BF16, "qo0"); P_q1 = P.sbuf([128, NS], BF16, "qo1")
    for sub in range(NSUB):
        body(sub)
    return P


def bf16(a):
    return np.asarray(a).astype(ml_dtypes.bfloat16)


def rope_tables(t0, NL, NS):
    C = np.ones((128, NS), np.float32)
    S = np.zeros((128, NS), np.float32)
    pos = t0 + np.arange(NL)
    rows, cols = (pos // 64).astype(np.float32), (pos % 64).astype(np.float32)
    fr = np.exp(-np.log(10000.0) * np.arange(8, dtype=np.float32) / 8).astype(np.float32)
    ar = rows[None, :] * fr[:, None]
    ac = cols[None, :] * fr[:, None]
    for base, ang in ((64, ar), (72, ar), (80, ac), (88, ac)):
        C[base:base + 8, 1:NL + 1] = np.cos(ang)
        S[base:base + 8, 1:NL + 1] = np.sin(ang)
    return C, S


def consts_L1():
    cst = np.zeros((128, 3, 128), np.float32)
    cst[:, 0, :] = 1.0
    cst[:64, 1, :64] = 1.0
    cst[64:, 1, 64:] = 1.0
    for i in range(8):
        cst[72 + i, 2, 64 + i] = -1.0
        cst[64 + i, 2, 72 + i] = 1.0
        cst[88 + i, 2, 80 + i] = -1.0
        cst[80 + i, 2, 88 + i] = 1.0
    return cst


def prep_L1_weights(inp, l, mod):
    W = inp["w_in"][l]
    cols = []
    z128 = np.zeros((1024, 128), np.float32)
    for j in range(3):
        cols.append(W[:, j * 128:(j + 1) * 128])
    for j in range(2):
        cols.append(W[:, 384 + j * 128:384 + (j + 1) * 128])
    kr = z128.copy(); kr[:, 64:96] = W[:, 640:672]; cols.append(kr)
    for j in range(12):
        cols.append(W[:, 672 + j * 128:672 + (j + 1) * 128])
    for j in RW_ORDER:
        cols.append(W[:, 2208 + j * 128:2208 + (j + 1) * 128])
    Wp = np.stack(cols, 0)
    wq = np.ascontiguousarray(Wp.reshape(33, 8, 128, 128).transpose(0, 2, 1, 3))
    nv = np.stack([fm(inp["norm1_g"][l]), mod[:, l, 8:16, 0], mod[:, l, 0:8, 0], mod[:, l, 8:16, 1], mod[:, l, 0:8, 1]], 1)
    sp = np.zeros((128, 16), np.float32)
    sp[:, 0:3] = fm(inp["mla_cq_g"][l]); sp[:, 3:5] = fm(inp["mla_ckv_g"][l])
    sp[:96, 5] = inp["mla_qn_g"][l]; sp[:96, 6] = inp["mla_kn_g"][l]
    sp[:, 7] = np.tile(inp["na_qn_g"][l], 2); sp[:, 8] = np.tile(inp["na_kn_g"][l], 2)
    wuq = np.zeros((384, 8, 128), np.float32)
    wuq[:, :, :96] = inp["mla_wuq"][l].reshape(384, 8, 96)
    wuq = np.ascontiguousarray(wuq.reshape(3, 128, 8, 128).transpose(1, 0, 2, 3))
    kv = inp["mla_wukv"][l].reshape(256, 8, 128)
    wuk = np.zeros((256, 8, 128), np.float32); wuk[:, :, :64] = kv[:, :, :64]
    wuk = np.ascontiguousarray(wuk.reshape(2, 128, 8, 128).transpose(1, 0, 2, 3))
    wuv = np.ascontiguousarray(kv[:, :, 64:].reshape(256, 4, 128).reshape(2, 128, 4, 128).transpose(1, 0, 2, 3))
    rwp = np.zeros((128, 58), np.float32)
    rwp[:, 0:15] = fm(inp["rw_mu"][l][0]); rwp[:, 15:30] = fm(inp["rw_mu"][l][1])
    for d in range(2):
        rwp[:, 30 + d * 4:34 + d * 4] = fm(inp["rw_w0"][l][d]); rwp[:, 38 + d * 4:42 + d * 4] = fm(inp["rw_a0"][l][d])
    rwp[:, 46:50] = fm(inp["rw_kk"][l]); rwp[:, 50:54] = fm(inp["rw_ka"][l]); rwp[:, 54:58] = fm(inp["rw_rk"][l].reshape(-1))
    return dict(nv=np.ascontiguousarray(nv, np.float32), wq=wq, sp=sp, wuq=wuq, wuk=wuk, wuv=wuv, rwp=rwp,
                w2=np.ascontiguousarray(inp["rw_w2"][l].reshape(128, 512)),
                a2=np.ascontiguousarray(inp["rw_a2"][l].reshape(128, 512)),
                g2=np.ascontiguousarray(inp["rw_g2"][l]), cst=consts_L1())


def run_L1(inp, l, mod, x, ctx, NL, NSUB):
    SEQ = x.shape[0]
    NS = NL + 260
    wts = prep_L1_weights(inp, l, mod)
    in_maps = []
    for i in range(NCORES):
        xs, Cs, Ss, Ms = [], [], [], []
        for s in range(NSUB):
            t0 = (i * NSUB + s) * NL
            slab = np.zeros((NS, 1024), np.float32)
            M = np.ones((128, NS), np.float32)
            if t0 > 0:
                slab[0] = x[t0 - 1]
            else:
                M[:, 0] = 0
            slab[1:NL + 1] = x[t0:t0 + NL]
            if t0 + NL < SEQ:
                slab[NL + 1] = x[t0 + NL]
            else:
                M[:, NL + 1] = 0
            M[:, NL + 2] = 0; M[:, NS - 1] = 0
            slab[NL + 3:NL + 259] = ctx
            xs.append(slab.T.reshape(8, 128, NS).transpose(1, 0, 2))
            C, S = rope_tables(t0, NL, NS)
            Cs.append(C); Ss.append(S); Ms.append(bf16(M))
        m = dict(wts)
        m.update(xT=np.ascontiguousarray(np.stack(xs, 0)), ropeC=np.stack(Cs, 0), ropeS=np.stack(Ss, 0), mask=np.stack(Ms, 0))
        in_maps.append(m)
    res = run(build_L1(NL, NSUB), in_maps)
    out = {}
    for key in ("qa", "ka", "va", "nqkv", "rw3", "rwd", "rwgb"):
        lat = np.concatenate([res[i][key][s][..., 1:NL + 1] for i in range(NCORES) for s in range(NSUB)], axis=-1)
        cx = res[0][key][0][..., NL + 3:NL + 259]
        out[key] = (np.asarray(lat), np.asarray(cx))
    return out


def na_plan(SEQ):
    NR = SEQ // 64
    NP_ = NR // 2
    tabs = {}
    tab_list = []
    plan = []
    qc = np.arange(64)
    cs_ = np.clip(qc - 8, 0, 48)
    for m in range(NP_):
        rows = [2 * m, 2 * m + 1]
        rs = [int(np.clip(r - 4, 0, NR - 8)) for r in rows]
        kps = sorted({(a + i) // 2 for a in rs for i in range(8)})
        ent = []
        for kp in kps:
            sig = (rs[0] - rows[0], rs[1] - rows[1], kp - m)
            if sig not in tabs:
                dr = np.full((128, 128), -1, np.int64)
                dc = np.full((128, 128), -1, np.int64)
                for kl in range(128):
                    krow, kcol = 2 * kp + kl // 64, kl % 64
                    for ql in range(128):
                        qrow, qcol = rows[ql // 64], ql % 64
                        a = rs[ql // 64]
                        if a <= krow < a + 8 and cs_[qcol] <= kcol < cs_[qcol] + 16:
                            dr[kl, ql] = krow - qrow + 7
                            dc[kl, ql] = kcol - qcol + 15
                tabs[sig] = len(tab_list)
                tab_list.append((dr, dc))
            ent.append((kp, tabs[sig]))
        plan.append(ent)
    return plan, tab_list


def build_L2(SEQ):
    NK = SEQ + 256
    NKB = NK // 128
    NCH = NKB
    plan, tab_list = na_plan(SEQ)
    NTAB = len(tab_list)
    P = Prog()
    I = lambda n, s, dt=F32: P.dram(n, s, dt, "ExternalInput")
    O = lambda n, s, dt=F32: P.dram(n, s, dt, "ExternalOutput")
    qa = I("qa", [128, SEQ + 256], BF16)
    ka = I("ka", [128, NK], BF16)
    va = I("va", [128, NKB, 65], BF16)
    nq = I("nq", [64, SEQ + 256], BF16)
    nk = I("nk", [64, NK], BF16)
    nvv = I("nv", [128, NKB, 65], BF16)
    tabs = I("tabs", [128, NTAB, 128])
    cst = I("cst", [128, 6, 128])
    rtm = I("rtm", [2, NCH, 128, 4, 64])
    rfm = I("rfm", [2, NCH, 64, 4, 128])
    o_ya = O("ya", [64, SEQ + 256]); o_yb = O("yb", [64, SEQ + 256])
    o_y = O("y", [2, NCH, 128, 64])

    psb = [P.psum([128, 512], F32, f"psb{i}") for i in range(8)]
    cs = P.sbuf([128, 6, 128], F32, "cs")
    P.dma("sync", cs[:], cst[:], [cst], [cs], cs)
    triI, triS, triL, ident, ones = (cs[:, i, :] for i in range(5))
    csb = P.sbuf([128, 2, 128], BF16, "csb")
    P.copy(csb[:, 0, :], cs[:, 3, :], [cs], [csb])
    P.copy(csb[:, 1, :], cs[:, 4, :], [cs], [csb])

    pT = [P.sbuf([128, 1024], BF16, f"pT{i}") for i in range(3)]
    osb = [P.sbuf([64, 512], F32, f"osb{i}") for i in range(2)]
    rD = P.sbuf([64, 512], F32, "rD")
    dsb = P.sbuf([128, 512], F32, "dsb")
    cnt = [0]

    def finish_od(psOD, n, out_d, out_c0):
        psB = psb[7]
        P.copy(dsb[64:65, :n], psOD[64:65, :n], [psOD], [dsb], eng="scalar")
        P.mm(psB[0:64, :n], cs[64:65, 4, 0:64], dsb[64:65, :n], True, True, [cs, dsb], [psB])
        P.op("vector", lambda e: e.reciprocal(rD[:, :n], psB[0:64, :n]), [psB], [rD])
        ob = osb[cnt[0] % 2]
        P.tt(ob[:, :n], psOD[0:64, :n], rD[:, :n], ALU.mult, [psOD, rD], [ob])
        P.dma("gpsimd", out_d[:, out_c0:out_c0 + n], ob[:, :n], [ob], [out_d], ob)

    def attn(qT, q0, n, kT, V, kblocks, KD, scale, out_d, out_c0):
        cnt[0] += 1
        psOD = psb[4 + cnt[0] % 3]
        nb = len(kblocks)

        def S(i):
            kb = kblocks[i]
            P.mm(psb[i % 4][:, :n], kT[0:KD, kb * 128:(kb + 1) * 128], qT[0:KD, q0:q0 + n], True, True, [kT, qT], [psb[i % 4]])
        S(0)
        for i, kb in enumerate(kblocks):
            if i + 1 < nb:
                S(i + 1)
            p_ = pT[i % 3]
            P.act(p_[:, :n], psb[i % 4][:, :n], AF.Exp, [psb[i % 4]], [p_], scale=scale)
            P.mm(psOD[0:65, :n], V[:, kb, :], p_[:, :n], i == 0, i == nb - 1, [V, p_], [psOD])
        finish_od(psOD, n, out_d, out_c0)

    qat = P.sbuf([128, SEQ + 256], BF16, "qat"); kat = P.sbuf([128, NK], BF16, "kat"); vat = P.sbuf([128, NKB, 65], BF16, "vat")
    for (t, d) in ((qat, qa), (kat, ka), (vat, va)):
        P.dma("sync", t[:], d[:], [d], [t], t)
    sc_a = float(96 ** -0.5)
    for q0 in range(0, SEQ, 512):
        attn(qat, q0, min(512, SEQ - q0), kat, vat, list(range(NKB)), 128, sc_a, o_ya, q0)
    attn(qat, SEQ, 256, kat, vat, [0, 1], 128, sc_a, o_ya, SEQ)

    nqt, nkt, nvt = qat, kat, vat
    P.dma("sync", nqt[0:64, :], nq[:], [nq], [nqt], nqt)
    P.dma("sync", nkt[0:64, :], nk[:], [nk], [nkt], nkt)
    P.dma("sync", nvt[:], nvv[:], [nvv], [nvt], nvt)
    tb32 = P.sbuf([128, NTAB, 128], F32, "tb32"); tbb = P.sbuf([128, NTAB, 128], BF16, "tbb")
    P.dma("sync", tb32[:], tabs[:], [tabs], [tb32], tb32)
    P.ts(tbb[:], tb32[:], 8.0, None, ALU.mult, None, [tb32], [tbb])
    sc_b = 0.125
    for m, ent in enumerate(plan):
        blocks = [(kp, tid) for (kp, tid) in ent] + [(NKB - 2, None), (NKB - 1, None)]
        cnt[0] += 1
        psOD = psb[4 + cnt[0] % 3]
        p_ = pT[cnt[0] % 3]
        qsl = nqt[0:64, m * 128:(m + 1) * 128]
        for half in range(2):
            psS = psb[(cnt[0] * 2 + half) % 4]
            sub = blocks[half * 4:half * 4 + 4]
            if not sub:
                continue
            for j, (kb, tid) in enumerate(sub):
                o_ = psS[:, j * 128:(j + 1) * 128]
                P.mm(o_, nkt[0:64, kb * 128:(kb + 1) * 128], qsl, True, tid is None, [nkt, nqt], [psS])
                if tid is not None:
                    P.mm(o_, csb[:, 0, :], tbb[:, tid, :], False, True, [csb, tbb], [psS])
            w = len(sub) * 128
            P.act(p_[:, half * 512:half * 512 + w], psS[:, :w], AF.Exp, [psS], [p_], scale=sc_b)
        for j, (kb, tid) in enumerate(blocks):
            P.mm(psOD[0:65, :128], nvt[:, kb, :], p_[:, j * 128:(j + 1) * 128], j == 0, j == len(blocks) - 1, [nvt, p_], [psOD])
        finish_od(psOD, 128, o_yb, m * 128)
    attn(nqt, SEQ, 256, nkt, nvt, [NKB - 2, NKB - 1], 64, sc_b, o_yb, SEQ)

    pi = [0]
    def PS():
        pi[0] += 1
        return psb[pi[0] % 8]
    NSET = 4
    W = lambda n, shape=(128, 128): [P.sbuf(list(shape), F32, f"{n}{i}") for i in range(NSET)]
    tmb, fmb = W("tm", (128, 4, 64)), W("fmj", (64, 4, 128))
    eLr = W("eLr", (128, 64)); Bh = W("Bh", (128, 64)); Kh = W("Kh", (128, 64))
    e1 = W("e1", (64, 128)); e2 = W("e2", (64, 128)); e3 = W("e3", (64, 128))
    Rt = W("Rt", (64, 128)); KKt = W("KKt", (64, 128)); Bt = W("Bt", (64, 128)); Kt = W("Kt", (64, 128))
    Nn = W("Nn"); NTt = W("NTt"); Mk = W("Mk"); Mbp = W("Mbp"); Mkp = W("Mkp")
    Pq = W("Pq"); PqT = W("PqT"); Tm = W("Tm")
    Zs = W("Zs", (128, 64)); nU = W("nU", (128, 64)); Ys = W("Ys", (128, 64))
    gC = W("gC", (64, 1))
    ST = [P.sbuf([64, 64], F32, f"ST{d}") for d in range(2)]
    for d in range(2):
        P.memset(ST[d][:], 0.0, [ST[d]])

    def chunk_gen(c, d, s):
        tm, fj = tmb[s], fmb[s]
        P.dma("sync", tm[:], rtm[d, c], [rtm], [tm], tm)
        P.dma("sync", fj[:], rfm[d, c], [rfm], [fj], fj)
        yield
        lw_tok = tm[:, 0, :]
        ps = PS(); P.mm(ps[:, 0:64], triL, lw_tok, True, True, [cs, tm], [ps])
        P.act(eLr[s][:], ps[:, 0:64], AF.Exp, [ps], [eLr[s]])
        yield
        ps = PS(); P.mm(ps[0:64, 0:128], lw_tok, triI, True, True, [tm, cs], [ps])
        P.act(e1[s][:], ps[0:64, 0:128], AF.Exp, [ps], [e1[s]])
        P.act(e2[s][:], ps[0:64, 0:128], AF.Exp, [ps], [e2[s]], scale=-1.0)
        yield
        ps = PS(); P.mm(ps[0:64, 0:128], lw_tok, triS, True, True, [tm, cs], [ps])
        P.act(e3[s][:], ps[0:64, 0:128], AF.Exp, [ps], [e3[s]])
        yield
        P.tt(Bh[s][:], tm[:, 1, :], eLr[s][:], ALU.mult, [tm, eLr[s]], [Bh[s]], eng="gpsimd")
        P.tt(Kh[s][:], tm[:, 2, :], eLr[s][:], ALU.mult, [tm, eLr[s]], [Kh[s]], eng="gpsimd")
        P.copy(gC[s][:], e1[s][:, 127:128], [e1[s]], [gC[s]], eng="gpsimd")
        P.tt(Bt[s][:], fj[:, 0, :], e2[s][:], ALU.mult, [fj, e2[s]], [Bt[s]])
        P.tt(Kt[s][:], fj[:, 1, :], e2[s][:], ALU.mult, [fj, e2[s]], [Kt[s]], eng="gpsimd")
        P.tt(KKt[s][:], fj[:, 2, :], e3[s][:], ALU.mult, [fj, e3[s]], [KKt[s]])
        P.tt(Rt[s][:], fj[:, 3, :], e1[s][:], ALU.mult, [fj, e1[s]], [Rt[s]], eng="gpsimd")
        yield
        for (dst, l_, r_, msk) in ((Nn[s], Bt[s], KKt[s], triS), (NTt[s], KKt[s], Bt[s], triL),
                                  (Mk[s], Kt[s], KKt[s], triS), (Mbp[s], Bt[s], Rt[s], triI), (Mkp[s], Kt[s], Rt[s], triI)):
            ps = PS(); P.mm(ps[:, 0:128], l_[:], r_[:], True, True, [l_, r_], [ps])
            P.tt(dst[:], ps[:, 0:128], msk, ALU.mult, [ps, cs], [dst])
            yield
        P.tt(Tm[s][:], ident, Nn[s][:], ALU.subtract, [cs, Nn[s]], [Tm[s]])
        Pc, PcT = Nn[s], NTt[s]
        for it in range(6):
            nxt, nxtT = (Pq[s], PqT[s]) if Pc is not Pq[s] else (Nn[s], NTt[s])
            ps = PS(); P.mm(ps[:, 0:128], Pc[:], PcT[:], True, True, [Pc, PcT], [ps])
            P.copy(nxtT[:], ps[:, 0:128], [ps], [nxtT], eng="scalar")
            if it < 5:
                ps = PS(); P.mm(ps[:, 0:128], PcT[:], Pc[:], True, True, [Pc, PcT], [ps])
                P.copy(nxt[:], ps[:, 0:128], [ps], [nxt])
            yield
            ps = PS(); P.mm(ps[:, 0:128], nxtT[:], Tm[s][:], True, True, [nxtT, Tm[s]], [ps])
            P.tt(Tm[s][:], Tm[s][:], ps[:, 0:128], ALU.add, [Tm[s], ps], [Tm[s]])
            Pc, PcT = nxt, nxtT
            yield
        vt = tm[:, 3, :]
        ps = PS()
        P.mm(ps[:, 0:64], KKt[s][:], ST[d][:], True, False, [KKt[s], ST[d]], [ps])
        P.mm(ps[:, 0:64], Mk[s][:], vt, False, True, [Mk[s], tm], [ps])
        P.copy(Zs[s][:], ps[:, 0:64], [ps], [Zs[s]])
        yield
        ps = PS(); P.mm(ps[:, 0:64], Tm[s][:], Zs[s][:], True, True, [Tm[s], Zs[s]], [ps])
        P.ts(nU[s][:], ps[:, 0:64], -1.0, None, ALU.mult, None, [ps], [nU[s]])
        yield
        ps2 = PS()
        P.mm(ps2[0:64, 0:64], Bh[s][:], nU[s][:], True, False, [Bh[s], nU[s]], [ps2])
        P.mm(ps2[0:64, 0:64], Kh[s][:], vt, False, True, [Kh[s], tm], [ps2])
        ps = PS()
        P.mm(ps[:, 0:64], Rt[s][:], ST[d][:], True, False, [Rt[s], ST[d]], [ps])
        P.mm(ps[:, 0:64], Mbp[s][:], nU[s][:], False, False, [Mbp[s], nU[s]], [ps])
        P.mm(ps[:, 0:64], Mkp[s][:], vt, False, True, [Mkp[s], tm], [ps])
        P.stt(ST[d][:], ST[d][:], gC[s][:, 0:1], ps2[0:64, 0:64], ALU.mult, ALU.add, [ST[d], gC[s], ps2], [ST[d]])
        P.copy(Ys[s][:], ps[:, 0:64], [ps], [Ys[s]], eng="scalar")
        P.dma("gpsimd", o_y[d, c], Ys[s][:], [Ys[s]], [o_y], Ys[s])
        yield

    tasks = [(c, d) for c in range(NCH) for d in range(2)]
    active = []
    nxt_task = 0
    rounds = 0
    while nxt_task < len(tasks) or active:
        if nxt_task < len(tasks) and len(active) < NSET and (rounds % 7 == 0 or not active):
            c, d = tasks[nxt_task]
            active.append(chunk_gen(c, d, nxt_task % NSET))
            nxt_task += 1
        rounds += 1
        for g in list(active):
            try:
                next(g)
            except StopIteration:
                active.remove(g)
    return P


def consts_L2():
    c = np.zeros((128, 6, 128), np.float32)
    i = np.arange(128)
    c[:, 0, :] = (i[:, None] <= i[None, :])
    c[:, 1, :] = (i[:, None] < i[None, :])
    c[:, 2, :] = (i[:, None] > i[None, :])
    c[:, 3, :] = np.eye(128)
    c[:, 4, :] = 1.0
    return c


def tokmaj(a, aug=False):
    n = a.shape[1]
    t = a.T.reshape(n // 128, 128, 64).transpose(1, 0, 2)
    if aug:
        t = np.concatenate([t, np.ones((128, n // 128, 1), t.dtype)], axis=2)
    return np.ascontiguousarray(t)


def run_L2(inp, l, o1, SEQ):
    NK = SEQ + 256
    NCH = NK // 128
    plan, tab_list = na_plan(SEQ)
    cst = consts_L2()
    in_maps = []
    f32 = lambda a: np.asarray(a, np.float32)
    hs = lambda pair, h: (pair[0].reshape(-1, pair[0].shape[-1])[h * 64:(h + 1) * 64],
                          pair[1].reshape(-1, 256)[h * 64:(h + 1) * 64])
    for h in range(NCORES):
        m = {"cst": cst}
        m["qa"] = np.ascontiguousarray(np.concatenate([o1["qa"][0][h], o1["qa"][1][h]], 1))
        m["ka"] = np.ascontiguousarray(np.concatenate([o1["ka"][1][h], o1["ka"][0][h]], 1))
        vl, vc = hs(o1["va"], h)
        m["va"] = tokmaj(np.concatenate([vc, vl], 1), aug=True)
        n_l, n_c = o1["nqkv"]
        sel = lambda w: (n_l[w].reshape(512, -1)[h * 64:(h + 1) * 64], n_c[w].reshape(512, 256)[h * 64:(h + 1) * 64])
        m["nq"] = np.ascontiguousarray(np.concatenate(sel(0), 1))
        m["nk"] = np.ascontiguousarray(np.concatenate(sel(1), 1))
        m["nv"] = tokmaj(np.concatenate(sel(2), 1), aug=True)
        rpb = inp["na_rpb"][l][h]
        tb = np.stack([np.where(dr >= 0, rpb[np.maximum(dr, 0), np.maximum(dc, 0)], np.float32(-30000.0)) for dr, dc in tab_list], 0)
        m["tabs"] = np.ascontiguousarray(tb.transpose(1, 0, 2).astype(np.float32))
        r3l, r3c = o1["rw3"]; rdl, rdc = o1["rwd"]
        def seqs(lat, cx, d):
            lat = lat.reshape(512, -1)[h * 64:(h + 1) * 64]; cx = cx.reshape(512, 256)[h * 64:(h + 1) * 64]
            return np.concatenate([cx, lat], 1) if d == 0 else np.concatenate([cx[:, ::-1], lat[:, ::-1]], 1)
        rtm = np.zeros((2, NCH, 128, 4, 64), np.float32); rfm = np.zeros((2, NCH, 64, 4, 128), np.float32)
        for d in range(2):
            lw = seqs(rdl[0, d], rdc[0, d], d); b_ = seqs(rdl[1, d], rdc[1, d], d); kd = seqs(rdl[2, d], rdc[2, d], d)
            r_ = seqs(r3l[0], r3c[0], d); v_ = seqs(r3l[1], r3c[1], d); kk = seqs(r3l[2], r3c[2], d)
            for j, a in enumerate((lw, b_, kd, v_)):
                rtm[d, :, :, j, :] = a.T.reshape(NCH, 128, 64)
            for j, a in enumerate((b_, kd, kk, r_)):
                rfm[d, :, :, j, :] = a.reshape(64, NCH, 128).transpose(1, 0, 2)
        m["rtm"] = rtm; m["rfm"] = rfm
        in_maps.append(m)
    res = run(build_L2(SEQ), in_maps)
    ya = np.concatenate([f32(res[h]["ya"]) for h in range(NCORES)], 0)
    yb = np.concatenate([f32(res[h]["yb"]) for h in range(NCORES)], 0)
    ys = []
    for d in range(2):
        yy = np.concatenate([f32(res[h]["y"][d]).reshape(NK, 64).T for h in range(NCORES)], 0)
        cx, lat = yy[:, :256], yy[:, 256:]
        if d == 1:
            cx, lat = cx[:, ::-1], lat[:, ::-1]
        ys.append((np.ascontiguousarray(lat), np.ascontiguousarray(cx)))
    return dict(ya=(ya[:, :SEQ], ya[:, SEQ:]), yb=(yb[:, :SEQ], yb[:, SEQ:]), yf=ys[0], ybk=ys[1])


def build_L3(NT, NLAT, moe):
    FC = 28 if moe else 22
    NE = 8 if moe else 1
    tiles = [(c0, min(512, NT - c0)) for c0 in range(0, NT, 512)]
    P = Prog()
    I = lambda n, s, dt=F32: P.dram(n, s, dt, "ExternalInput")
    xT = I("xT", [128, 8, NT])
    yin = I("yin", [6, 128, 4, NT])
    nv = I("nv", [128, 14, 8])
    rwo = I("rwo", [128, 2, 4])
    wg = I("wg", [24, 128, 8, 128]); wo = I("wo", [3, 8, 128, 4, 128]); wout = I("wout", [8, 128, 8, 128])
    w1 = I("w1", [NE, FC, 128, 8, 128]); w3 = I("w3", [NE, FC, 128, 8, 128]); w2 = I("w2", [NE, 8, 128, FC, 128])
    cst = I("cst", [128, 3, 128])
    if moe:
        rt = I("rt", [128, 8, 8]); sel = I("sel", [8, 8, 128])
    out = P.dram("out", [128, 8, NT], F32, "ExternalOutput")

    def load(d, shape, dt=F32, name=None):
        t = P.sbuf(shape, dt, name)
        P.dma("sync", t[:], d[:], [d], [t], t)
        return t
    nvt = load(nv, [128, 14, 8]); rwt = load(rwo, [128, 2, 4]); cs = load(cst, [128, 3, 128])
    ones, blk, ident = cs[:, 0, :], cs[:, 1, :], cs[:, 2, :]
    if moe:
        rtt = load(rt, [128, 8, 8]); selt = load(sel, [8, 8, 128])
    A1 = P.sbuf([128, 2, 8], F32); A2 = P.sbuf([128, 2, 8], F32)
    for w_, sc in ((0, 1), (1, 3)):
        P.stt(A1[:, w_, :], nvt[:, sc, :], 1.0, nvt[:, 0, :], ALU.add, ALU.mult, [nvt], [A1])
    for w_, sc in ((0, 8), (1, 10)):
        P.stt(A2[:, w_, :], nvt[:, sc, :], 1.0, nvt[:, 7, :], ALU.add, ALU.mult, [nvt], [A2])

    psb = [P.psum([128, 512], F32, f"psb{i}") for i in range(8)]
    pi = [0]
    def PS():
        pi[0] += 1
        return psb[pi[0] % 8]
    x_ = P.sbuf([128, 8, 512], F32, "x"); hT = P.sbuf([128, 8, 512], BF16, "hT"); G = P.sbuf([128, 24, 512], BF16, "G")
    stg = [P.sbuf([128, 4, 512], F32, f"stg{i}") for i in range(2)]
    ybf = [P.sbuf([128, 4, 512], BF16, f"ybf{i}") for i in range(3)]
    ysum = P.sbuf([128, 4, 512], F32, "ysum"); tmp = P.sbuf([128, 4, 512], F32, "tmp")
    Mb = P.sbuf([128, 8, 512], BF16, "Mb"); Mo = P.sbuf([128, 512], F32, "Mo"); t5 = P.sbuf([128, 512], F32, "t5")
    h2 = P.sbuf([128, 8, 512], BF16, "h2"); hid = P.sbuf([128, FC, 512], BF16, "hid")
    sq = [P.sbuf([128, 512], F32, f"sq{i}") for i in range(2)]; rs = P.sbuf([128, 512], F32, "rs")
    wst = [P.sbuf([128, 8, 128], F32, f"wst{i}") for i in range(2)]
    wbf = [P.sbuf([128, 8, 128], BF16, f"wbf{i}") for i in range(3)]
    wi = [0]
    if moe:
        lgT = P.sbuf([8, 512], F32, "lgT"); gT = P.sbuf([8, 512], F32, "gT"); gbe = P.sbuf([128, 512], F32, "gbe")
        lg = P.sbuf([128, 8], F32, "lg"); top = P.sbuf([128, 8], F32, "top"); sm = P.sbuf([128, 8], F32, "sm")
        gt_ = P.sbuf([128, 8], F32, "gt"); g2_ = P.sbuf([128, 8], F32, "g2")

    def wpiece(ap, dbuf, kc=8):
        wi[0] += 1
        ws, wb = wst[wi[0] % 2], wbf[wi[0] % 3]
        P.dma("sync", ws[:, :kc, :], ap, [dbuf], [ws], ws)
        P.copy(wb[:, :kc, :], ws[:, :kc, :], [ws], [wb], eng="gpsimd")
        return wb

    def segs(c0, n):
        for (a, b, w_) in ((0, NLAT, 0), (NLAT, NT, 1)):
            lo, hi = max(a, c0), min(b, c0 + n)
            if lo < hi:
                yield lo - c0, hi - c0, w_

    def norm_mod(src, dst, A, shl, shc, n, c0, extra=None):
        ps = PS()
        for k in range(8):
            s_ = sq[k % 2]
            P.act(s_[:, :n], src[:, k, :n], AF.Square, [src], [s_])
            P.mm(ps[:, :n], ones, s_[:, :n], k == 0, k == 7, [cs, s_], [ps])
        P.act(rs[:, :n], ps[:, :n], AF.Sqrt, [ps], [rs], bias=1e-6, scale=1.0 / 1024)
        P.op("vector", lambda e: e.reciprocal(rs[:, :n], rs[:, :n]), [rs], [rs])
        for k in range(8):
            s_ = sq[k % 2]
            P.tt(s_[:, :n], src[:, k, :n], rs[:, :n], ALU.mult, [src, rs], [s_])
            for (lo, hi, w_) in segs(c0, n):
                P.ts(dst[:, k, lo:hi], s_[:, lo:hi], A[:, w_, k:k + 1], nvt[:, (shl, shc)[w_], k:k + 1],
                     ALU.mult, ALU.add, [s_, A, nvt], [dst])
            if extra is not None:
                extra(k, s_)

    for (c0, n) in tiles:
        P.dma("sync", x_[:, :, :n], xT[:, :, c0:c0 + n], [xT], [x_], x_)
        norm_mod(x_, hT, A1, 2, 4, n, c0)
        for j in range(24):
            wb = wpiece(wg[j], wg)
            ps = PS()
            for k in range(8):
                P.mm(ps[:, :n], wb[:, k, :], hT[:, k, :n], k == 0, k == 7, [wb, hT], [ps])
            P.act(G[:, j, :n], ps[:, :n], AF.Sigmoid, [ps], [G])
        for b in range(2):
            s_ = stg[b % 2]
            P.dma("sync", s_[:, :, :n], yin[b][:, :, c0:c0 + n], [yin], [s_], s_)
            P.copy(ybf[b][:, :, :n], s_[:, :, :n], [s_], [ybf[b]])
        sa, sb_ = stg[0], stg[1]
        P.dma("sync", sa[:, :, :n], yin[2][:, :, c0:c0 + n], [yin], [sa], sa)
        P.dma("sync", sb_[:, :, :n], yin[3][:, :, c0:c0 + n], [yin], [sb_], sb_)
        P.tt(ysum[:, :, :n], sa[:, :, :n], sb_[:, :, :n], ALU.add, [sa, sb_], [ysum])
        P.dma("sync", sa[:, :, :n], yin[4][:, :, c0:c0 + n], [yin], [sa], sa)
        P.dma("sync", sb_[:, :, :n], yin[5][:, :, c0:c0 + n], [yin], [sb_], sb_)
        for c in range(4):
            ps = PS(); P.mm(ps[:, :n], blk, ysum[:, c, :n], True, True, [cs, ysum], [ps])
            P.stt(ysum[:, c, :n], ps[:, :n], -1.0 / 64, ysum[:, c, :n], ALU.mult, ALU.add, [ps, ysum], [ysum])
            P.act(tmp[:, c, :n], ysum[:, c, :n], AF.Square, [ysum], [tmp])
            ps = PS(); P.mm(ps[:, :n], blk, tmp[:, c, :n], True, True, [cs, tmp], [ps])
            P.act(tmp[:, c, :n], ps[:, :n], AF.Sqrt, [ps], [tmp], bias=64e-5, scale=1.0 / 64)
            P.op("vector", lambda e, c=c, n=n: e.reciprocal(tmp[:, c, :n], tmp[:, c, :n]), [tmp], [tmp])
            P.tt(ysum[:, c, :n], ysum[:, c, :n], tmp[:, c, :n], ALU.mult, [ysum, tmp], [ysum])
            P.ts(ysum[:, c, :n], ysum[:, c, :n], rwt[:, 0, c:c + 1], rwt[:, 1, c:c + 1], ALU.mult, ALU.add, [ysum, rwt], [ysum])
            P.tt(ysum[:, c, :n], ysum[:, c, :n], sb_[:, c, :n], ALU.add, [ysum, sb_], [ysum])
            P.tt(ybf[2][:, c, :n], ysum[:, c, :n], sa[:, c, :n], ALU.mult, [ysum, sa], [ybf[2]])
        for oc in range(8):
            for br in range(3):
                wb = wpiece(wo[br, oc], wo, 4)
                ps = PS()
                for k in range(4):
                    P.mm(ps[:, :n], wb[:, k, :], ybf[br][:, k, :n], k == 0, k == 3, [wb, ybf[br]], [ps])
                if br == 0:
                    P.tt(Mo[:, :n], ps[:, :n], G[:, oc, :n], ALU.mult, [ps, G], [Mo])
                else:
                    P.tt(t5[:, :n], ps[:, :n], G[:, br * 8 + oc, :n], ALU.mult, [ps, G], [t5])
                    P.tt(Mo[:, :n], Mo[:, :n], t5[:, :n], ALU.add, [Mo, t5], [Mo])
            P.copy(Mb[:, oc, :n], Mo[:, :n], [Mo], [Mb], eng="scalar")
        for oc in range(8):
            wb = wpiece(wout[oc], wout)
            ps = PS()
            for k in range(8):
                P.mm(ps[:, :n], wb[:, k, :], Mb[:, k, :n], k == 0, k == 7, [wb, Mb], [ps])
            for (lo, hi, w_) in segs(c0, n):
                P.stt(x_[:, oc, lo:hi], ps[:, lo:hi], nvt[:, 5 + w_, oc:oc + 1], x_[:, oc, lo:hi], ALU.mult, ALU.add,
                      [ps, nvt, x_], [x_])
        if moe:
            psr = PS()
            def extra(k, s_):
                for (lo, hi, w_) in segs(c0, n):
                    P.ts(t5[:, lo:hi], s_[:, lo:hi], A2[:, w_, k:k + 1], nvt[:, (9, 11)[w_], k:k + 1],
                         ALU.mult, ALU.add, [s_, A2, nvt], [t5])
                P.mm(psr[0:8, :n], rtt[:, k, :], t5[:, :n], k == 0, k == 7, [rtt, t5], [psr])
            norm_mod(x_, h2, A2, 9, 11, n, c0, extra)
            P.copy(lgT[:, :n], psr[0:8, :n], [psr], [lgT])
            for b0 in range(0, n, 128):
                ps = PS(); P.transpose(ps[:, 0:8], lgT[:, b0:b0 + 128], cs[0:8, 2, 0:8], [lgT, cs], [ps])
                P.copy(lg[:], ps[:, 0:8], [ps], [lg])
                P.op("vector", lambda e: e.max(top[:], lg[:]), [lg], [top])
                P.ts(sm[:, 0:1], top[:, 0:1], -1.0, None, ALU.mult, None, [top], [sm])
                P.act(sm[:, 1:2], top[:, 1:2], AF.Exp, [top, sm], [sm], bias=sm[:, 0:1])
                P.ts(sm[:, 2:3], sm[:, 1:2], 1.0, None, ALU.add, None, [sm], [sm])
                P.op("vector", lambda e: e.reciprocal(sm[:, 2:3], sm[:, 2:3]), [sm], [sm])
                P.tt(sm[:, 3:4], sm[:, 1:2], sm[:, 2:3], ALU.mult, [sm], [sm])
                P.ts(gt_[:], lg[:], top[:, 0:1], sm[:, 2:3], ALU.is_equal, ALU.mult, [lg, top, sm], [gt_])
                P.ts(g2_[:], lg[:], top[:, 1:2], sm[:, 3:4], ALU.is_equal, ALU.mult, [lg, top, sm], [g2_])
                P.tt(gt_[:], gt_[:], g2_[:], ALU.add, [gt_, g2_], [gt_])
                ps = PS(); P.transpose(ps[0:8, 0:128], gt_[:], ident, [gt_, cs], [ps])
                P.copy(gT[:, b0:b0 + 128], ps[0:8, 0:128], [ps], [gT])
        else:
            norm_mod(x_, h2, A2, 9, 11, n, c0)
        for e_ in range(NE):
            if moe:
                ps = PS(); P.mm(ps[:, :n], selt[:, e_, :], gT[:, :n], True, True, [selt, gT], [ps])
                P.copy(gbe[:, :n], ps[:, :n], [ps], [gbe], eng="scalar")
            for fc in range(FC):
                wb1 = wpiece(w1[e_, fc], w1)
                ps1 = PS()
                for k in range(8):
                    P.mm(ps1[:, :n], wb1[:, k, :], h2[:, k, :n], k == 0, k == 7, [wb1, h2], [ps1])
                wb3 = wpiece(w3[e_, fc], w3)
                ps3 = PS()
                for k in range(8):
                    P.mm(ps3[:, :n], wb3[:, k, :], h2[:, k, :n], k == 0, k == 7, [wb3, h2], [ps3])
                P.act(t5[:, :n], ps1[:, :n], AF.Silu, [ps1], [t5])
                if moe:
                    P.tt(t5[:, :n], t5[:, :n], gbe[:, :n], ALU.mult, [t5, gbe], [t5])
                P.tt(hid[:, fc, :n], t5[:, :n], ps3[:, :n], ALU.mult, [t5, ps3], [hid])
            for oc in range(8):
                ps = PS()
                for k0 in range(0, FC, 8):
                    kc = min(8, FC - k0)
                    wb = wpiece(w2[e_, oc][:, k0:k0 + kc, :], w2, kc)
                    for k in range(kc):
                        P.mm(ps[:, :n], wb[:, k, :], hid[:, k0 + k, :n], k0 + k == 0, k0 + k == FC - 1, [wb, hid], [ps])
                for (lo, hi, w_) in segs(c0, n):
                    P.stt(x_[:, oc, lo:hi], ps[:, lo:hi], nvt[:, 12 + w_, oc:oc + 1], x_[:, oc, lo:hi], ALU.mult, ALU.add,
                          [ps, nvt, x_], [x_])
        P.dma("gpsimd", out[:, :, c0:c0 + n], x_[:, :, :n], [x_], [out], x_)
    return P


def arrw(W):
    K_, M_ = W.shape[0] // 128, W.shape[1] // 128
    return np.ascontiguousarray(W.reshape(K_, 128, M_, 128).transpose(2, 1, 0, 3))


def fmT(a):
    C = a.shape[0] // 128
    return a.reshape(C, 128, a.shape[1]).transpose(1, 0, 2)


def run_L3(inp, l, mod, x, ctx, o1, o2):
    SEQ = x.shape[0]
    moe = (l % 2 == 1)
    need_ctx = l < 1
    NL3 = SEQ // NCORES
    NT = NL3 + (256 if need_ctx else 0)
    mv = lambda g, w: mod[:, l, g * 8:(g + 1) * 8, w]
    nv = np.stack([fm(inp["norm1_g"][l]), mv(1, 0), mv(0, 0), mv(1, 1), mv(0, 1), mv(2, 0), mv(2, 1),
                   fm(inp["norm2_g"][l]), mv(4, 0), mv(3, 0), mv(4, 1), mv(3, 1), mv(5, 0), mv(5, 1)], 1)
    W = dict(nv=np.ascontiguousarray(nv, np.float32),
             rwo=np.ascontiguousarray(np.stack([fm(inp["rw_ln_g"][l]), fm(inp["rw_ln_b"][l])], 1)),
             wg=arrw(inp["w_in"][l][:, 4128:7200]),
             wo=np.stack([arrw(inp["mla_wo"][l]), arrw(inp["na_wo"][l]), arrw(inp["rw_wo"][l])], 0),
             wout=arrw(inp["w_out"][l]))
    cst = np.zeros((128, 3, 128), np.float32)
    cst[:, 0, :] = 1.0; cst[:64, 1, :64] = 1.0; cst[64:, 1, 64:] = 1.0; cst[:, 2, :] = np.eye(128)
    W["cst"] = cst
    if moe:
        W["w1"] = np.stack([arrw(inp["moe_w1"][l // 2][e]) for e in range(8)], 0)
        W["w3"] = np.stack([arrw(inp["moe_w3"][l // 2][e]) for e in range(8)], 0)
        W["w2"] = np.stack([arrw(inp["moe_w2"][l // 2][e]) for e in range(8)], 0)
        W["rt"] = np.ascontiguousarray(inp["moe_router"][l // 2].reshape(8, 128, 8).transpose(1, 0, 2))
        sel = np.zeros((8, 8, 128), np.float32)
        for e in range(8):
            sel[e, e, :] = 1.0
        W["sel"] = sel
    else:
        W["w1"] = arrw(inp["ffn_w1"][l // 2])[None]; W["w3"] = arrw(inp["ffn_w3"][l // 2])[None]
        W["w2"] = arrw(inp["ffn_w2"][l // 2])[None]
    g_l, g_c = o1["rwgb"]
    srcs = [o2["ya"], o2["yb"], o2["yf"], o2["ybk"],
            (g_l[0].reshape(512, -1), g_c[0].reshape(512, 256)), (g_l[1].reshape(512, -1), g_c[1].reshape(512, 256))]
    in_maps = []
    for i in range(NCORES):
        sl = slice(i * NL3, (i + 1) * NL3)
        xs = x[sl]
        if need_ctx:
            xs = np.concatenate([xs, ctx], 0)
        m = dict(W)
        m["xT"] = np.ascontiguousarray(fmT(xs.T))
        ys = []
        for (lat, cx) in srcs:
            a = np.asarray(lat, np.float32)[:, sl]
            if need_ctx:
                a = np.concatenate([a, np.asarray(cx, np.float32)], 1)
            ys.append(fmT(a))
        m["yin"] = np.ascontiguousarray(np.stack(ys, 0))
        in_maps.append(m)
    res = run(build_L3(NT, NL3, moe), in_maps)
    outs = [np.asarray(r["out"]).transpose(2, 1, 0).reshape(NT, 1024) for r in res]
    x_new = np.concatenate([o[:NL3] for o in outs], 0)
    ctx_new = outs[0][NL3:] if need_ctx else ctx
    return x_new, ctx_new


def kernel(**inp):
    inp = {k: np.asarray(v) for k, v in inp.items()}
    SEQ = inp["x"].shape[1]
    x = np.ascontiguousarray(inp["x"][0]); ctx = np.ascontiguousarray(inp["ctx"][0])
    mod = run_L0(inp["c"], inp["c_ctx"], inp["mod_w"], inp["mod_b"])
    depth = inp["mod_w"].shape[0]
    for l in range(depth):
        o1 = run_L1(inp, l, mod, x, ctx, SEQ // 16, 2)
        o2 = run_L2(inp, l, o1, SEQ)
        del o1["qa"], o1["ka"], o1["va"], o1["nqkv"], o1["rw3"], o1["rwd"]
        x, ctx = run_L3(inp, l, mod, x, ctx, o1, o2)
    return np.ascontiguousarray(x[None].astype(np.float32))
```

```python
from contextlib import ExitStack
import numpy as np
import ml_dtypes
import concourse.bass as bass
import concourse.mybir as mybir
from concourse.bass_utils import run_bass_kernel_spmd

F32 = mybir.dt.float32
BF16 = mybir.dt.bfloat16
AF = mybir.ActivationFunctionType
ALU = mybir.AluOpType
AX = mybir.AxisListType
NCORES = 8
ENGINES = ("tensor", "vector", "scalar", "gpsimd", "sync")


class Buf:
    __slots__ = ("t", "name", "lw", "rd")

    def __init__(self, t, name):
        self.t = t
        self.name = name
        self.lw = None
        self.rd = {}

    def __getitem__(self, idx):
        return self.t[idx]


class Prog:
    def __init__(self):
        self.nc = bass.Bass("TRN2", target_bir_lowering=False)
        self.ops = {e: [] for e in ENGINES}
        self.count = {}
        self.waited = {e: {} for e in ENGINES}
        self.stack = ExitStack()
        self.nbuf = 0

    def sbuf(self, shape, dt, name=None):
        self.nbuf += 1
        name = name or f"sb{self.nbuf}"
        t = self.stack.enter_context(self.nc.sbuf_tensor(name, list(shape), dt))
        return Buf(t, name)

    def psum(self, shape, dt=F32, name=None):
        self.nbuf += 1
        name = name or f"ps{self.nbuf}"
        t = self.stack.enter_context(self.nc.psum_tensor(name, list(shape), dt))
        return Buf(t, name)

    def dram(self, name, shape, dt, kind):
        t = self.nc.dram_tensor(name, list(shape), dt, kind=kind).ap()
        return Buf(t, name)

    def _deps(self, eng, reads, writes, skip_self):
        need = {}

        def add(kv):
            if kv is None:
                return
            k, v = kv
            if need.get(k, 0) < v:
                need[k] = v

        for b in reads:
            add(b.lw)
        for b in writes:
            add(b.lw)
            for k, v in b.rd.items():
                add((k, v))
        w = self.waited[eng]
        out = []
        for k, v in need.items():
            if skip_self and k == eng:
                continue
            if w.get(k, 0) < v:
                w[k] = v
                out.append((k, v))
        return out

    def _commit(self, key, inc, reads, writes):
        v = self.count.get(key, 0) + inc
        self.count[key] = v
        for b in reads:
            if b.rd.get(key, 0) < v:
                b.rd[key] = v
        for b in writes:
            b.lw = (key, v)
            b.rd = {}
        return v

    def op(self, eng, fn, reads=(), writes=(), skip_self=False):
        waits = self._deps(eng, reads, writes, skip_self)
        self._commit(eng, 1, reads, writes)
        self.ops[eng].append((waits, fn, eng, 1))

    def dma(self, eng, out_ap, in_ap, reads, writes, sem_buf):
        waits = self._deps(eng, reads, writes, False)
        key = "d_" + sem_buf.name
        self._commit(key, 16, reads, writes)
        self.ops[eng].append((waits, lambda e: e.dma_start(out=out_ap, in_=in_ap), key, 16))

    def coll(self, kind, in_buf, out_buf, op=None):
        waits = self._deps("gpsimd", [in_buf], [out_buf], False)
        key = "c_" + out_buf.name
        self._commit(key, 16, [in_buf], [out_buf])
        ia, oa = in_buf.t, out_buf.t
        op = op or ALU.bypass
        self.ops["gpsimd"].append((waits, lambda e: e.collective_compute(
            kind, op, replica_groups=[list(range(NCORES))], ins=[ia], outs=[oa]), key, 16))

    def barrier(self):
        snap = dict(self.count)
        for e in ENGINES:
            w = self.waited[e]
            waits = [(k, v) for k, v in snap.items() if w.get(k, 0) < v]
            for k, v in waits:
                w[k] = v
            if waits:
                self.ops[e].append((waits, None, None, 0))

    def scratch(self, name, shape, dt, shared=False):
        t = self.nc.dram_tensor(name, list(shape), dt, addr_space=("Shared" if shared else "Local")).ap()
        return Buf(t, name)

    def mm(self, out_ap, lhsT_ap, rhs_ap, start, stop, reads, writes):
        self.op("tensor", lambda e: e.matmul(out_ap, lhsT_ap, rhs_ap, start=start, stop=stop),
                reads, writes, skip_self=True)

    def transpose(self, out_ap, in_ap, ident_ap, reads, writes):
        self.op("tensor", lambda e: e.transpose(out_ap, in_ap, ident_ap), reads, writes, skip_self=True)

    def act(self, out_ap, in_ap, func, reads, writes, bias=None, scale=None, eng="scalar"):
        kw = {}
        if bias is not None:
            kw["bias"] = bias
        if scale is not None:
            kw["scale"] = scale
        self.op(eng, lambda e: e.activation(out_ap, in_ap, func, **kw), reads, writes)

    def tt(self, out_ap, a_ap, b_ap, op, reads, writes, eng="vector"):
        self.op(eng, lambda e: e.tensor_tensor(out_ap, a_ap, b_ap, op), reads, writes)

    def ts(self, out_ap, a_ap, s1, s2, op0, op1, reads, writes, eng="vector"):
        if s2 is None:
            self.op(eng, lambda e: e.tensor_scalar(out_ap, a_ap, s1, None, op0), reads, writes)
        else:
            self.op(eng, lambda e: e.tensor_scalar(out_ap, a_ap, s1, s2, op0, op1), reads, writes)

    def stt(self, out_ap, in0, scalar, in1, op0, op1, reads, writes):
        self.op("vector", lambda e: e.scalar_tensor_tensor(out_ap, in0, scalar, in1, op0, op1), reads, writes)

    def copy(self, out_ap, in_ap, reads, writes, eng="vector"):
        if eng == "scalar":
            self.op(eng, lambda e: e.copy(out_ap, in_ap), reads, writes)
        else:
            self.op(eng, lambda e: e.tensor_copy(out_ap, in_ap), reads, writes)

    def memset(self, ap, val, writes, eng="vector"):
        self.op(eng, lambda e: e.memset(ap, val), (), writes)

    def finish(self):
        nc = self.nc
        final_waits = []
        w = self.waited["sync"]
        for k, v in self.count.items():
            if w.get(k, 0) < v:
                final_waits.append((k, v))
        keys = list(self.count.keys())
        sems = {}
        for i, k in enumerate(keys):
            sems[k] = self.stack.enter_context(nc.semaphore(f"s{i}"))
        ops = self.ops

        def replay(e, lst):
            for waits, fn, key, inc in lst:
                for (k, v) in waits:
                    e.wait_ge(sems[k], v)
                if fn is not None:
                    fn(e).then_inc(sems[key], inc)

        with nc.Block() as block:
            @block.tensor
            def _(e):
                replay(e, ops["tensor"])

            @block.vector
            def _(e):
                replay(e, ops["vector"])

            @block.scalar
            def _(e):
                replay(e, ops["scalar"])

            @block.gpsimd
            def _(e):
                replay(e, ops["gpsimd"])

            @block.sync
            def _(e):
                replay(e, ops["sync"])
                for (k, v) in final_waits:
                    e.wait_ge(sems[k], v)
        self.stack.close()
        return nc


TRACE = False
TIMES = []


def run(prog, in_maps):
    nc = prog.finish()
    if TRACE:
        res = run_bass_kernel_spmd(nc, in_maps, core_ids=list(range(NCORES)), trace=True)
        TIMES.append(res.exec_time_ns)
        print("exec_time_ns", res.exec_time_ns, flush=True)
    else:
        res = run_bass_kernel_spmd(nc, in_maps, core_ids=list(range(NCORES)))
    return res.results


def build_L0(nch):
    P = Prog()
    w = P.dram("w", [1024, nch * 128], F32, "ExternalInput")
    b = P.dram("b", [128, nch], F32, "ExternalInput")
    cT = P.dram("cT", [128, 8, 2], F32, "ExternalInput")
    out = P.dram("out", [128, nch, 2], F32, "ExternalOutput")
    wt = P.sbuf([128, 8, nch * 128], F32, "wt")
    bt = P.sbuf([128, nch], F32, "bt")
    ct = P.sbuf([128, 8, 2], F32, "ct")
    st = P.sbuf([128, 8, 2], F32, "st")
    ot = P.sbuf([128, nch, 2], F32, "ot")
    ps = P.psum([128, nch, 2], F32, "ps0")
    P.dma("sync", ct[:], cT[:], [cT], [ct], ct)
    P.dma("sync", bt[:], b[:], [b], [bt], bt)
    for k in range(8):
        P.dma("sync", wt[:, k, :], w[k * 128:(k + 1) * 128, :], [w], [wt], wt)
    P.act(st[:], ct[:], AF.Silu, [ct], [st])
    for j in range(nch):
        for k in range(8):
            P.mm(ps[:, j, :], wt[:, k, j * 128:(j + 1) * 128], st[:, k, :], k == 0, k == 7, [wt, st], [ps])
    for j in range(nch):
        P.ts(ot[:, j, :], ps[:, j, :], bt[:, j:j + 1], None, ALU.add, None, [ps, bt], [ot])
    P.dma("sync", out[:], ot[:], [ot], [out], ot)
    return P


def fm(v):
    v = np.asarray(v, np.float32)
    return np.ascontiguousarray(v.reshape(-1, 128).T)


def run_L0(c, c_ctx, mod_w, mod_b):
    depth = mod_w.shape[0]
    nch_total = depth * 48
    nch = nch_total // NCORES
    wcat = np.concatenate([mod_w[l] for l in range(depth)], axis=1)
    bcat = np.concatenate([mod_b[l] for l in range(depth)], axis=0)
    cT = np.stack([fm(c.reshape(-1)), fm(c_ctx.reshape(-1))], axis=-1)
    in_maps = []
    for i in range(NCORES):
        sl = slice(i * nch * 128, (i + 1) * nch * 128)
        in_maps.append({"w": np.ascontiguousarray(wcat[:, sl]), "b": fm(bcat[sl]), "cT": cT})
    res = run(build_L0(nch), in_maps)
    o = np.concatenate([r["out"] for r in res], axis=1)
    return o.reshape(128, depth, 48, 2)


RW_ORDER = [12, 13, 14, 0, 4, 8, 1, 5, 9, 2, 6, 10, 3, 7, 11]


def build_L1(NL, NSUB):
    NS = NL + 260
    tiles = [(c0, min(512, NS - c0)) for c0 in range(0, NS, 512)]
    P = Prog()
    I = lambda n, s, dt=F32: P.dram(n, s, dt, "ExternalInput")
    O = lambda n, s, dt=F32: P.dram(n, s, dt, "ExternalOutput")
    xT_ = I("xT", [NSUB, 128, 8, NS])
    nv = I("nv", [128, 5, 8])
    wq = I("wq", [33, 128, 8, 128])
    ropeC_ = I("ropeC", [NSUB, 128, NS]); ropeS_ = I("ropeS", [NSUB, 128, NS]); mask_ = I("mask", [NSUB, 128, NS], BF16)
    sp = I("sp", [128, 16])
    wuq = I("wuq", [128, 3, 8, 128]); wuk = I("wuk", [128, 2, 8, 128]); wuv = I("wuv", [128, 2, 4, 128])
    rwp = I("rwp", [128, 58])
    w2d = I("w2", [128, 512]); a2d = I("a2", [128, 512]); g2d = I("g2", [128, 512])
    cst = I("cst", [128, 3, 128])
    o_qa_ = O("qa", [NSUB, 8, 128, NS], BF16); o_ka_ = O("ka", [NSUB, 8, 128, NS], BF16)
    o_va_ = O("va", [NSUB, 4, 128, NS], BF16)
    o_n_ = O("nqkv", [NSUB, 3, 4, 128, NS], BF16)
    o_r3_ = O("rw3", [NSUB, 3, 4, 128, NS])
    o_d3_ = O("rwd", [NSUB, 3, 2, 4, 128, NS])
    o_gb_ = O("rwgb", [NSUB, 2, 4, 128, NS])

    def load(d, shape, dt=F32, name=None):
        t = P.sbuf(shape, dt, name)
        P.dma("sync", t[:], d[:], [d], [t], t)
        return t
    nvt = load(nv, [128, 5, 8]); spt = load(sp, [128, 16]); rwt = load(rwp, [128, 58])
    cs = load(cst, [128, 3, 128])
    w2t = load(w2d, [128, 512]); a2t = load(a2d, [128, 512]); g2t = load(g2d, [128, 512])
    stg = P.sbuf([128, 3 * 8 * 128], F32, "stg")
    wuqb = P.sbuf([128, 3, 8, 128], BF16); wukb = P.sbuf([128, 2, 8, 128], BF16); wuvb = P.sbuf([128, 2, 4, 128], BF16)
    for (src, dstb, nel) in ((wuq, wuqb, 3 * 8 * 128), (wuk, wukb, 2 * 8 * 128), (wuv, wuvb, 2 * 4 * 128)):
        P.dma("sync", stg[:, :nel], src[:].rearrange("p a b c -> p (a b c)"), [src], [stg], stg)
        P.copy(dstb[:].rearrange("p a b c -> p (a b c)"), stg[:, :nel], [stg], [dstb], eng="gpsimd")
    ones = cs[:, 0, :]; blk = cs[:, 1, :]; rot = cs[:, 2, :]
    At = P.sbuf([128, 2, 8], F32)
    for w_, sc in ((0, 1), (1, 3)):
        P.stt(At[:, w_, :], nvt[:, sc, :], 1.0, nvt[:, 0, :], ALU.add, ALU.mult, [nvt], [At])
    m2 = P.sbuf([128, 15], F32)
    P.tt(m2[:], rwt[:, 0:15], rwt[:, 15:30], ALU.add, [rwt], [m2])
    P.ts(m2[:], m2[:], -1.0, 1.0, ALU.mult, ALU.add, [m2], [m2])

    psb = [P.psum([128, 512], F32, f"psb{i}") for i in range(6)]
    pi = [0]
    def PS():
        pi[0] += 1
        return psb[pi[0] % 6]
    slabs = [P.sbuf([128, NS], F32, f"sl{i}") for i in range(13)]
    bslabs = [P.sbuf([128, NS], BF16, f"bs{i}") for i in range(3)]
    bi = [0]
    def BS():
        bi[0] += 1
        return bslabs[bi[0] % 3]
    Ct = P.sbuf([128, NS], F32, "Ct"); St = P.sbuf([128, NS], F32, "St"); Mt = P.sbuf([128, NS], BF16, "Mt")
    hT = P.sbuf([128, 8, NS], BF16, "hT")
    x_ = P.sbuf([128, 8, 512], F32, "xt")
    sqt = [P.sbuf([128, 512], F32, f"sq{i}") for i in range(2)]
    rs = P.sbuf([128, 512], F32, "rs")
    wst = [P.sbuf([128, 8, 128], F32, f"wst{i}") for i in range(2)]
    wbf = [P.sbuf([128, 8, 128], BF16, f"wbf{i}") for i in range(2)]
    wi = [0]

    def proj(ci, dst, masked=False):
        wi[0] += 1
        ws, wb = wst[wi[0] % 2], wbf[wi[0] % 2]
        P.dma("sync", ws[:], wq[ci], [wq], [ws], ws)
        P.copy(wb[:], ws[:], [ws], [wb], eng="gpsimd")
        for (c0, n) in tiles:
            ps = PS()
            for k in range(8):
                P.mm(ps[:, :n], wb[:, k, :], hT[:, k, c0:c0 + n], k == 0, k == 7, [wb, hT], [ps])
            if masked:
                P.tt(dst[:, c0:c0 + n], ps[:, :n], Mt[:, c0:c0 + n], ALU.mult, [ps, Mt], [dst])
            else:
                P.copy(dst[:, c0:c0 + n], ps[:, :n], [ps], [dst], eng="scalar")

    def rstd_of(srcs, lhsT, dim, eps, dst, sqrt_only=False):
        tmp = slabs[12]
        for (c0, n) in tiles:
            ps = PS()
            for j, s in enumerate(srcs):
                P.act(tmp[:, c0:c0 + n], s[:, c0:c0 + n], AF.Square, [s], [tmp])
                P.mm(ps[:, :n], lhsT, tmp[:, c0:c0 + n], j == 0, j == len(srcs) - 1, [cs, tmp], [ps])
            P.act(dst[:, c0:c0 + n], ps[:, :n], AF.Sqrt, [ps], [dst], bias=eps, scale=1.0 / dim)
        if sqrt_only:
            P.ts(dst[:], dst[:], 1e-12, None, ALU.max, None, [dst], [dst])
        P.op("vector", lambda e: e.reciprocal(dst[:], dst[:]), [dst], [dst])

    def body(sub):
        D = lambda b, *idx: Buf(b.t[(sub,) + idx] if idx else b.t[sub], b.name + "_v")
        xT = D(xT_)
        o_qa, o_ka, o_va, o_n, o_r3, o_d3, o_gb = (D(o_qa_), D(o_ka_), D(o_va_), D(o_n_), D(o_r3_), D(o_d3_), D(o_gb_))
        P.dma("sync", Ct[:], ropeC_[sub], [ropeC_], [Ct], Ct)
        P.dma("sync", St[:], ropeS_[sub], [ropeS_], [St], St)
        P.dma("sync", Mt[:], mask_[sub], [mask_], [Mt], Mt)

        def out_dma(dram_ap, dram_buf, sb):
            P.dma("gpsimd", dram_ap, sb[:], [sb], [dram_buf], sb)

        for ti, (c0, n) in enumerate(tiles):
            P.dma("sync", x_[:, :, :n], xT[:, :, c0:c0 + n], [xT], [x_], x_)
            ps = PS()
            for k in range(8):
                s_ = sqt[k % 2]
                P.act(s_[:, :n], x_[:, k, :n], AF.Square, [x_], [s_])
                P.mm(ps[:, :n], ones, s_[:, :n], k == 0, k == 7, [cs, s_], [ps])
            P.act(rs[:, :n], ps[:, :n], AF.Sqrt, [ps], [rs], bias=1e-6, scale=1.0 / 1024)
            P.op("vector", lambda e, a=rs[:, :n]: e.reciprocal(a, a), [rs], [rs])
            for k in range(8):
                P.tt(x_[:, k, :n], x_[:, k, :n], rs[:, :n], ALU.mult, [x_, rs], [x_])
                for (a, b, w_, sh) in ((0, NL + 2, 0, 2), (NL + 2, NS, 1, 4)):
                    lo, hi = max(a, c0), min(b, c0 + n)
                    if lo < hi:
                        P.ts(hT[:, k, lo:hi], x_[:, k, lo - c0:hi - c0], At[:, w_, k:k + 1], nvt[:, sh, k:k + 1],
                             ALU.mult, ALU.add, [x_, At, nvt], [hT])

        cq = slabs[0:3]
        for j in range(3):
            proj(j, cq[j])
        r_ = slabs[3]
        rstd_of(cq, ones, 384.0, 1e-6, r_)
        cqn = [BS() for _ in range(3)]
        for j in range(3):
            P.stt(cqn[j][:], cq[j][:], spt[:, j:j + 1], r_[:], ALU.mult, ALU.mult, [cq[j], spt, r_], [cqn[j]])

        def head_finish(pre, gcol, dram_ap, dram_buf, ob):
            rr = slabs[4]
            rstd_of([pre], ones, 96.0, 1e-6, rr)
            P.stt(pre[:], pre[:], spt[:, gcol:gcol + 1], rr[:], ALU.mult, ALU.mult, [pre, spt, rr], [pre])
            rq = slabs[5]
            for (c0, n) in tiles:
                ps = PS()
                P.mm(ps[:, :n], rot, pre[:, c0:c0 + n], True, True, [cs, pre], [ps])
                P.tt(rq[:, c0:c0 + n], ps[:, :n], St[:, c0:c0 + n], ALU.mult, [ps, St], [rq])
            P.tt(pre[:], pre[:], Ct[:], ALU.mult, [pre, Ct], [pre])
            P.tt(ob[:], pre[:], rq[:], ALU.add, [pre, rq], [ob])
            out_dma(dram_ap, dram_buf, ob)

        obs = [slabs[8].t, slabs[9].t]
        qob = [P_q0, P_q1]
        for h in range(8):
            pre = slabs[6 + h % 2]
            for (c0, n) in tiles:
                ps = PS()
                for j in range(3):
                    P.mm(ps[:, :n], wuqb[:, j, h, :], cqn[j][:, c0:c0 + n], j == 0, j == 2, [wuqb, cqn[j]], [ps])
                P.copy(pre[:, c0:c0 + n], ps[:, :n], [ps], [pre], eng="scalar")
            head_finish(pre, 5, o_qa[h], o_qa, qob[h % 2])

        ckv = slabs[0:2]
        for j in range(2):
            proj(3 + j, ckv[j])
        krp = slabs[2]
        proj(5, krp)
        rstd_of(ckv, ones, 256.0, 1e-6, r_)
        ckvn = [BS() for _ in range(2)]
        for j in range(2):
            P.stt(ckvn[j][:], ckv[j][:], spt[:, 3 + j:4 + j], r_[:], ALU.mult, ALU.mult, [ckv[j], spt, r_], [ckvn[j]])
        for h in range(8):
            pre = slabs[6 + h % 2]
            for (c0, n) in tiles:
                ps = PS()
                for j in range(2):
                    P.mm(ps[:, :n], wukb[:, j, h, :], ckvn[j][:, c0:c0 + n], j == 0, j == 1, [wukb, ckvn[j]], [ps])
                P.tt(pre[:, c0:c0 + n], ps[:, :n], krp[:, c0:c0 + n], ALU.add, [ps, krp], [pre])
            head_finish(pre, 6, o_ka[h], o_ka, qob[h % 2])
        for c in range(4):
            ob = qob[c % 2]
            for (c0, n) in tiles:
                ps = PS()
                for j in range(2):
                    P.mm(ps[:, :n], wuvb[:, j, c, :], ckvn[j][:, c0:c0 + n], j == 0, j == 1, [wuvb, ckvn[j]], [ps])
                P.copy(ob[:, c0:c0 + n], ps[:, :n], [ps], [ob], eng="scalar")
            out_dma(o_va[c], o_va, ob)

        for which in range(3):
            for c in range(4):
                z = slabs[c % 2]
                proj(6 + which * 4 + c, z)
                ob = BS()
                if which < 2:
                    rr = slabs[4]
                    rstd_of([z], blk, 64.0, 1e-6, rr)
                    P.stt(ob[:], z[:], spt[:, 7 + which:8 + which], rr[:], ALU.mult, ALU.mult, [z, spt, rr], [ob])
                else:
                    P.copy(ob[:], z[:], [z], [ob])
                out_dma(o_n[which, c], o_n, ob)

        def shifted(ci_rw, dst, tmp):
            rwc = RW_ORDER[ci_rw]
            proj(18 + ci_rw, tmp, masked=True)
            P.ts(dst[:, 1:NS - 1], tmp[:, 1:NS - 1], m2[:, rwc:rwc + 1], None, ALU.mult, None, [tmp, m2], [dst])
            P.stt(dst[:, 1:NS - 1], tmp[:, 0:NS - 2], rwt[:, rwc:rwc + 1], dst[:, 1:NS - 1], ALU.mult, ALU.add,
                  [tmp, rwt, dst], [dst])
            P.stt(dst[:, 1:NS - 1], tmp[:, 2:NS], rwt[:, 15 + rwc:16 + rwc], dst[:, 1:NS - 1], ALU.mult, ALU.add,
                  [tmp, rwt, dst], [dst])
        tmp = slabs[11]
        wdT, adT, gdT = slabs[0], slabs[1], slabs[2]
        shifted(0, wdT, tmp); shifted(1, adT, tmp); shifted(2, gdT, tmp)
        P.act(wdT[:], wdT[:], AF.Tanh, [wdT], [wdT])
        P.act(gdT[:], gdT[:], AF.Sigmoid, [gdT], [gdT])
        for c in range(4):
            rT, kT, vT = slabs[3], slabs[4], slabs[5]
            shifted(3 + 3 * c, rT, tmp); shifted(4 + 3 * c, kT, tmp); shifted(5 + 3 * c, vT, tmp)
            P.dma("gpsimd", o_r3[0, c], rT[:], [rT], [o_r3], rT)
            P.dma("gpsimd", o_r3[1, c], vT[:], [vT], [o_r3], vT)
            kk = slabs[6]
            P.ts(kk[:], kT[:], rwt[:, 46 + c:47 + c], None, ALU.mult, None, [kT, rwt], [kk])
            rr = slabs[7]
            rstd_of([kk], blk, 1.0, 0.0, rr, sqrt_only=True)
            P.tt(kk[:], kk[:], rr[:], ALU.mult, [kk, rr], [kk])
            P.dma("gpsimd", o_r3[2, c], kk[:], [kk], [o_r3], kk)
            ksum = slabs[8]
            for d in range(2):
                lw, aa = slabs[9], slabs[10]
                pb = slice(d * 64, d * 64 + 64)
                for (c0, n) in tiles:
                    ps = PS()
                    P.mm(ps[:, :n], w2t[pb, c * 128:(c + 1) * 128], wdT[pb, c0:c0 + n], True, True, [w2t, wdT], [ps])
                    P.act(lw[:, c0:c0 + n], ps[:, :n], AF.Sigmoid, [ps, rwt], [lw],
                          bias=rwt[:, 30 + d * 4 + c:31 + d * 4 + c])
                    ps = PS()
                    P.mm(ps[:, :n], a2t[pb, c * 128:(c + 1) * 128], adT[pb, c0:c0 + n], True, True, [a2t, adT], [ps])
                    P.act(aa[:, c0:c0 + n], ps[:, :n], AF.Sigmoid, [ps, rwt], [aa],
                          bias=rwt[:, 38 + d * 4 + c:39 + d * 4 + c])
                P.ts(lw[:], lw[:], -float(np.exp(-0.5)), None, ALU.mult, None, [lw], [lw])
                P.dma("gpsimd", o_d3[0, d, c], lw[:], [lw], [o_d3], lw)
                bb = slabs[11]
                P.tt(bb[:], aa[:], kk[:], ALU.mult, [aa, kk], [bb])
                P.dma("gpsimd", o_d3[1, d, c], bb[:], [bb], [o_d3], bb)
                P.ts(aa[:], aa[:], -1.0, rwt[:, 50 + c:51 + c], ALU.add, ALU.mult, [aa, rwt], [aa])
                P.stt(aa[:], aa[:], 1.0, kT[:], ALU.add, ALU.mult, [aa, kT], [aa])
                P.dma("gpsimd", o_d3[2, d, c], aa[:], [aa], [o_d3], aa)
                if d == 0:
                    P.copy(ksum[:], aa[:], [aa], [ksum])
                else:
                    P.tt(ksum[:], ksum[:], aa[:], ALU.add, [ksum, aa], [ksum])
            P.stt(ksum[:], ksum[:], rwt[:, 54 + c:55 + c], rT[:], ALU.mult, ALU.mult, [ksum, rwt, rT], [ksum])
            gg, bo = slabs[9], slabs[10]
            for (c0, n) in tiles:
                ps = PS()
                P.mm(ps[:, :n], blk, ksum[:, c0:c0 + n], True, True, [cs, ksum], [ps])
                P.tt(bo[:, c0:c0 + n], ps[:, :n], vT[:, c0:c0 + n], ALU.mult, [ps, vT], [bo])
                ps = PS()
                P.mm(ps[:, :n], g2t[:, c * 128:(c + 1) * 128], gdT[:, c0:c0 + n], True, True, [g2t, gdT], [ps])
                P.copy(gg[:, c0:c0 + n], ps[:, :n], [ps], [gg], eng="scalar")
            P.dma("gpsimd", o_gb[0, c], gg[:], [gg], [o_gb], gg)
            P.dma("gpsimd", o_gb[1, c], bo[:], [bo], [o_gb], bo)

    P_q0 = P.sbuf([128, NS], BF16, "qo0"); P_q1 = P.sbuf([128, NS], BF16, "qo1")
    for sub in range(NSUB):
        body(sub)
    return P


def bf16(a):
    return np.asarray(a).astype(ml_dtypes.bfloat16)


def rope_tables(t0, NL, NS):
    C = np.ones((128, NS), np.float32)
    S = np.zeros((128, NS), np.float32)
    pos = t0 + np.arange(NL)
    rows, cols = (pos // 64).astype(np.float32), (pos % 64).astype(np.float32)
    fr = np.exp(-np.log(10000.0) * np.arange(8, dtype=np.float32) / 8).astype(np.float32)
    ar = rows[None, :] * fr[:, None]
    ac = cols[None, :] * fr[:, None]
    for base, ang in ((64, ar), (72, ar), (80, ac), (88, ac)):
        C[base:base + 8, 1:NL + 1] = np.cos(ang)
        S[base:base + 8, 1:NL + 1] = np.sin(ang)
    return C, S


def consts_L1():
    cst = np.zeros((128, 3, 128), np.float32)
    cst[:, 0, :] = 1.0
    cst[:64, 1, :64] = 1.0
    cst[64:, 1, 64:] = 1.0
    for i in range(8):
        cst[72 + i, 2, 64 + i] = -1.0
        cst[64 + i, 2, 72 + i] = 1.0
        cst[88 + i, 2, 80 + i] = -1.0
        cst[80 + i, 2, 88 + i] = 1.0
    return cst


def prep_L1_weights(inp, l, mod):
    W = inp["w_in"][l]
    cols = []
    z128 = np.zeros((1024, 128), np.float32)
    for j in range(3):
        cols.append(W[:, j * 128:(j + 1) * 128])
    for j in range(2):
        cols.append(W[:, 384 + j * 128:384 + (j + 1) * 128])
    kr = z128.copy(); kr[:, 64:96] = W[:, 640:672]; cols.append(kr)
    for j in range(12):
        cols.append(W[:, 672 + j * 128:672 + (j + 1) * 128])
    for j in RW_ORDER:
        cols.append(W[:, 2208 + j * 128:2208 + (j + 1) * 128])
    Wp = np.stack(cols, 0)
    wq = np.ascontiguousarray(Wp.reshape(33, 8, 128, 128).transpose(0, 2, 1, 3))
    nv = np.stack([fm(inp["norm1_g"][l]), mod[:, l, 8:16, 0], mod[:, l, 0:8, 0], mod[:, l, 8:16, 1], mod[:, l, 0:8, 1]], 1)
    sp = np.zeros((128, 16), np.float32)
    sp[:, 0:3] = fm(inp["mla_cq_g"][l]); sp[:, 3:5] = fm(inp["mla_ckv_g"][l])
    sp[:96, 5] = inp["mla_qn_g"][l]; sp[:96, 6] = inp["mla_kn_g"][l]
    sp[:, 7] = np.tile(inp["na_qn_g"][l], 2); sp[:, 8] = np.tile(inp["na_kn_g"][l], 2)
    wuq = np.zeros((384, 8, 128), np.float32)
    wuq[:, :, :96] = inp["mla_wuq"][l].reshape(384, 8, 96)
    wuq = np.ascontiguousarray(wuq.reshape(3, 128, 8, 128).transpose(1, 0, 2, 3))
    kv = inp["mla_wukv"][l].reshape(256, 8, 128)
    wuk = np.zeros((256, 8, 128), np.float32); wuk[:, :, :64] = kv[:, :, :64]
    wuk = np.ascontiguousarray(wuk.reshape(2, 128, 8, 128).transpose(1, 0, 2, 3))
    wuv = np.ascontiguousarray(kv[:, :, 64:].reshape(256, 4, 128).reshape(2, 128, 4, 128).transpose(1, 0, 2, 3))
    rwp = np.zeros((128, 58), np.float32)
    rwp[:, 0:15] = fm(inp["rw_mu"][l][0]); rwp[:, 15:30] = fm(inp["rw_mu"][l][1])
    for d in range(2):
        rwp[:, 30 + d * 4:34 + d * 4] = fm(inp["rw_w0"][l][d]); rwp[:, 38 + d * 4:42 + d * 4] = fm(inp["rw_a0"][l][d])
    rwp[:, 46:50] = fm(inp["rw_kk"][l]); rwp[:, 50:54] = fm(inp["rw_ka"][l]); rwp[:, 54:58] = fm(inp["rw_rk"][l].reshape(-1))
    return dict(nv=np.ascontiguousarray(nv, np.float32), wq=wq, sp=sp, wuq=wuq, wuk=wuk, wuv=wuv, rwp=rwp,
                w2=np.ascontiguousarray(inp["rw_w2"][l].reshape(128, 512)),
                a2=np.ascontiguousarray(inp["rw_a2"][l].reshape(128, 512)),
                g2=np.ascontiguousarray(inp["rw_g2"][l]), cst=consts_L1())


def run_L1(inp, l, mod, x, ctx, NL, NSUB):
    SEQ = x.shape[0]
    NS = NL + 260
    wts = prep_L1_weights(inp, l, mod)
    in_maps = []
    for i in range(NCORES):
        xs, Cs, Ss, Ms = [], [], [], []
        for s in range(NSUB):
            t0 = (i * NSUB + s) * NL
            slab = np.zeros((NS, 1024), np.float32)
            M = np.ones((128, NS), np.float32)
            if t0 > 0:
                slab[0] = x[t0 - 1]
            else:
                M[:, 0] = 0
            slab[1:NL + 1] = x[t0:t0 + NL]
            if t0 + NL < SEQ:
                slab[NL + 1] = x[t0 + NL]
            else:
                M[:, NL + 1] = 0
            M[:, NL + 2] = 0; M[:, NS - 1] = 0
            slab[NL + 3:NL + 259] = ctx
            xs.append(slab.T.reshape(8, 128, NS).transpose(1, 0, 2))
            C, S = rope_tables(t0, NL, NS)
            Cs.append(C); Ss.append(S); Ms.append(bf16(M))
        m = dict(wts)
        m.update(xT=np.ascontiguousarray(np.stack(xs, 0)), ropeC=np.stack(Cs, 0), ropeS=np.stack(Ss, 0), mask=np.stack(Ms, 0))
        in_maps.append(m)
    res = run(build_L1(NL, NSUB), in_maps)
    out = {}
    for key in ("qa", "ka", "va", "nqkv", "rw3", "rwd", "rwgb"):
        lat = np.concatenate([res[i][key][s][..., 1:NL + 1] for i in range(NCORES) for s in range(NSUB)], axis=-1)
        cx = res[0][key][0][..., NL + 3:NL + 259]
        out[key] = (np.asarray(lat), np.asarray(cx))
    return out


def na_plan(SEQ):
    NR = SEQ // 64
    NP_ = NR // 2
    tabs = {}
    tab_list = []
    plan = []
    qc = np.arange(64)
    cs_ = np.clip(qc - 8, 0, 48)
    for m in range(NP_):
        rows = [2 * m, 2 * m + 1]
        rs = [int(np.clip(r - 4, 0, NR - 8)) for r in rows]
        kps = sorted({(a + i) // 2 for a in rs for i in range(8)})
        ent = []
        for kp in kps:
            sig = (rs[0] - rows[0], rs[1] - rows[1], kp - m)
            if sig not in tabs:
                dr = np.full((128, 128), -1, np.int64)
                dc = np.full((128, 128), -1, np.int64)
                for kl in range(128):
                    krow, kcol = 2 * kp + kl // 64, kl % 64
                    for ql in range(128):
                        qrow, qcol = rows[ql // 64], ql % 64
                        a = rs[ql // 64]
                        if a <= krow < a + 8 and cs_[qcol] <= kcol < cs_[qcol] + 16:
                            dr[kl, ql] = krow - qrow + 7
                            dc[kl, ql] = kcol - qcol + 15
                tabs[sig] = len(tab_list)
                tab_list.append((dr, dc))
            ent.append((kp, tabs[sig]))
        plan.append(ent)
    return plan, tab_list


def build_L2(SEQ):
    NK = SEQ + 256
    NKB = NK // 128
    NCH = NKB
    plan, tab_list = na_plan(SEQ)
    NTAB = len(tab_list)
    P = Prog()
    I = lambda n, s, dt=F32: P.dram(n, s, dt, "ExternalInput")
    O = lambda n, s, dt=F32: P.dram(n, s, dt, "ExternalOutput")
    qa = I("qa", [128, SEQ + 256], BF16)
    ka = I("ka", [128, NK], BF16)
    va = I("va", [128, NKB, 65], BF16)
    nq = I("nq", [64, SEQ + 256], BF16)
    nk = I("nk", [64, NK], BF16)
    nvv = I("nv", [128, NKB, 65], BF16)
    tabs = I("tabs", [128, NTAB, 128])
    cst = I("cst", [128, 6, 128])
    rtm = I("rtm", [2, NCH, 128, 4, 64])
    rfm = I("rfm", [2, NCH, 64, 4, 128])
    o_ya = O("ya", [64, SEQ + 256]); o_yb = O("yb", [64, SEQ + 256])
    o_y = O("y", [2, NCH, 128, 64])

    psb = [P.psum([128, 512], F32, f"psb{i}") for i in range(8)]
    cs = P.sbuf([128, 6, 128], F32, "cs")
    P.dma("sync", cs[:], cst[:], [cst], [cs], cs)
    triI, triS, triL, ident, ones = (cs[:, i, :] for i in range(5))
    csb = P.sbuf([128, 2, 128], BF16, "csb")
    P.copy(csb[:, 0, :], cs[:, 3, :], [cs], [csb])
    P.copy(csb[:, 1, :], cs[:, 4, :], [cs], [csb])

    pT = [P.sbuf([128, 1024], BF16, f"pT{i}") for i in range(3)]
    osb = [P.sbuf([64, 512], F32, f"osb{i}") for i in range(2)]
    rD = P.sbuf([64, 512], F32, "rD")
    dsb = P.sbuf([128, 512], F32, "dsb")
    cnt = [0]

    def finish_od(psOD, n, out_d, out_c0):
        psB = psb[7]
        P.copy(dsb[64:65, :n], psOD[64:65, :n], [psOD], [dsb], eng="scalar")
        P.mm(psB[0:64, :n], cs[64:65, 4, 0:64], dsb[64:65, :n], True, True, [cs, dsb], [psB])
        P.op("vector", lambda e: e.reciprocal(rD[:, :n], psB[0:64, :n]), [psB], [rD])
        ob = osb[cnt[0] % 2]
        P.tt(ob[:, :n], psOD[0:64, :n], rD[:, :n], ALU.mult, [psOD, rD], [ob])
        P.dma("gpsimd", out_d[:, out_c0:out_c0 + n], ob[:, :n], [ob], [out_d], ob)

    def attn(qT, q0, n, kT, V, kblocks, KD, scale, out_d, out_c0):
        cnt[0] += 1
        psOD = psb[4 + cnt[0] % 3]
        nb = len(kblocks)

        def S(i):
            kb = kblocks[i]
            P.mm(psb[i % 4][:, :n], kT[0:KD, kb * 128:(kb + 1) * 128], qT[0:KD, q0:q0 + n], True, True, [kT, qT], [psb[i % 4]])
        S(0)
        for i, kb in enumerate(kblocks):
            if i + 1 < nb:
                S(i + 1)
            p_ = pT[i % 3]
            P.act(p_[:, :n], psb[i % 4][:, :n], AF.Exp, [psb[i % 4]], [p_], scale=scale)
            P.mm(psOD[0:65, :n], V[:, kb, :], p_[:, :n], i == 0, i == nb - 1, [V, p_], [psOD])
        finish_od(psOD, n, out_d, out_c0)

    qat = P.sbuf([128, SEQ + 256], BF16, "qat"); kat = P.sbuf([128, NK], BF16, "kat"); vat = P.sbuf([128, NKB, 65], BF16, "vat")
    for (t, d) in ((qat, qa), (kat, ka), (vat, va)):
        P.dma("sync", t[:], d[:], [d], [t], t)
    sc_a = float(96 ** -0.5)
    for q0 in range(0, SEQ, 512):
        attn(qat, q0, min(512, SEQ - q0), kat, vat, list(range(NKB)), 128, sc_a, o_ya, q0)
    attn(qat, SEQ, 256, kat, vat, [0, 1], 128, sc_a, o_ya, SEQ)

    nqt, nkt, nvt = qat, kat, vat
    P.dma("sync", nqt[0:64, :], nq[:], [nq], [nqt], nqt)
    P.dma("sync", nkt[0:64, :], nk[:], [nk], [nkt], nkt)
    P.dma("sync", nvt[:], nvv[:], [nvv], [nvt], nvt)
    tb32 = P.sbuf([128, NTAB, 128], F32, "tb32"); tbb = P.sbuf([128, NTAB, 128], BF16, "tbb")
    P.dma("sync", tb32[:], tabs[:], [tabs], [tb32], tb32)
    P.ts(tbb[:], tb32[:], 8.0, None, ALU.mult, None, [tb32], [tbb])
    sc_b = 0.125
    for m, ent in enumerate(plan):
        blocks = [(kp, tid) for (kp, tid) in ent] + [(NKB - 2, None), (NKB - 1, None)]
        cnt[0] += 1
        psOD = psb[4 + cnt[0] % 3]
        p_ = pT[cnt[0] % 3]
        qsl = nqt[0:64, m * 128:(m + 1) * 128]
        for half in range(2):
            psS = psb[(cnt[0] * 2 + half) % 4]
            sub = blocks[half * 4:half * 4 + 4]
            if not sub:
                continue
            for j, (kb, tid) in enumerate(sub):
                o_ = psS[:, j * 128:(j + 1) * 128]
                P.mm(o_, nkt[0:64, kb * 128:(kb + 1) * 128], qsl, True, tid is None, [nkt, nqt], [psS])
                if tid is not None:
                    P.mm(o_, csb[:, 0, :], tbb[:, tid, :], False, True, [csb, tbb], [psS])
            w = len(sub) * 128
            P.act(p_[:, half * 512:half * 512 + w], psS[:, :w], AF.Exp, [psS], [p_], scale=sc_b)
        for j, (kb, tid) in enumerate(blocks):
            P.mm(psOD[0:65, :128], nvt[:, kb, :], p_[:, j * 128:(j + 1) * 128], j == 0, j == len(blocks) - 1, [nvt, p_], [psOD])
        finish_od(psOD, 128, o_yb, m * 128)
    attn(nqt, SEQ, 256, nkt, nvt, [NKB - 2, NKB - 1], 64, sc_b, o_yb, SEQ)

    pi = [0]
    def PS():
        pi[0] += 1
        return psb[pi[0] % 8]
    NSET = 4
    W = lambda n, shape=(128, 128): [P.sbuf(list(shape), F32, f"{n}{i}") for i in range(NSET)]
    tmb, fmb = W("tm", (128, 4, 64)), W("fmj", (64, 4, 128))
    eLr = W("eLr", (128, 64)); Bh = W("Bh", (128, 64)); Kh = W("Kh", (128, 64))
    e1 = W("e1", (64, 128)); e2 = W("e2", (64, 128)); e3 = W("e3", (64, 128))
    Rt = W("Rt", (64, 128)); KKt = W("KKt", (64, 128)); Bt = W("Bt", (64, 128)); Kt = W("Kt", (64, 128))
    Nn = W("Nn"); NTt = W("NTt"); Mk = W("Mk"); Mbp = W("Mbp"); Mkp = W("Mkp")
    Pq = W("Pq"); PqT = W("PqT"); Tm = W("Tm")
    Zs = W("Zs", (128, 64)); nU = W("nU", (128, 64)); Ys = W("Ys", (128, 64))
    gC = W("gC", (64, 1))
    ST = [P.sbuf([64, 64], F32, f"ST{d}") for d in range(2)]
    for d in range(2):
        P.memset(ST[d][:], 0.0, [ST[d]])

    def chunk_gen(c, d, s):
        tm, fj = tmb[s], fmb[s]
        P.dma("sync", tm[:], rtm[d, c], [rtm], [tm], tm)
        P.dma("sync", fj[:], rfm[d, c], [rfm], [fj], fj)
        yield
        lw_tok = tm[:, 0, :]
        ps = PS(); P.mm(ps[:, 0:64], triL, lw_tok, True, True, [cs, tm], [ps])
        P.act(eLr[s][:], ps[:, 0:64], AF.Exp, [ps], [eLr[s]])
        yield
        ps = PS(); P.mm(ps[0:64, 0:128], lw_tok, triI, True, True, [tm, cs], [ps])
        P.act(e1[s][:], ps[0:64, 0:128], AF.Exp, [ps], [e1[s]])
        P.act(e2[s][:], ps[0:64, 0:128], AF.Exp, [ps], [e2[s]], scale=-1.0)
        yield
        ps = PS(); P.mm(ps[0:64, 0:128], lw_tok, triS, True, True, [tm, cs], [ps])
        P.act(e3[s][:], ps[0:64, 0:128], AF.Exp, [ps], [e3[s]])
        yield
        P.tt(Bh[s][:], tm[:, 1, :], eLr[s][:], ALU.mult, [tm, eLr[s]], [Bh[s]], eng="gpsimd")
        P.tt(Kh[s][:], tm[:, 2, :], eLr[s][:], ALU.mult, [tm, eLr[s]], [Kh[s]], eng="gpsimd")
        P.copy(gC[s][:], e1[s][:, 127:128], [e1[s]], [gC[s]], eng="gpsimd")
        P.tt(Bt[s][:], fj[:, 0, :], e2[s][:], ALU.mult, [fj, e2[s]], [Bt[s]])
        P.tt(Kt[s][:], fj[:, 1, :], e2[s][:], ALU.mult, [fj, e2[s]], [Kt[s]], eng="gpsimd")
        P.tt(KKt[s][:], fj[:, 2, :], e3[s][:], ALU.mult, [fj, e3[s]], [KKt[s]])
        P.tt(Rt[s][:], fj[:, 3, :], e1[s][:], ALU.mult, [fj, e1[s]], [Rt[s]], eng="gpsimd")
        yield
        for (dst, l_, r_, msk) in ((Nn[s], Bt[s], KKt[s], triS), (NTt[s], KKt[s], Bt[s], triL),
                                  (Mk[s], Kt[s], KKt[s], triS), (Mbp[s], Bt[s], Rt[s], triI), (Mkp[s], Kt[s], Rt[s], triI)):
            ps = PS(); P.mm(ps[:, 0:128], l_[:], r_[:], True, True, [l_, r_], [ps])
            P.tt(dst[:], ps[:, 0:128], msk, ALU.mult, [ps, cs], [dst])
            yield
        P.tt(Tm[s][:], ident, Nn[s][:], ALU.subtract, [cs, Nn[s]], [Tm[s]])
        Pc, PcT = Nn[s], NTt[s]
        for it in range(6):
            nxt, nxtT = (Pq[s], PqT[s]) if Pc is not Pq[s] else (Nn[s], NTt[s])
            ps = PS(); P.mm(ps[:, 0:128], Pc[:], PcT[:], True, True, [Pc, PcT], [ps])
            P.copy(nxtT[:], ps[:, 0:128], [ps], [nxtT], eng="scalar")
            if it < 5:
                ps = PS(); P.mm(ps[:, 0:128], PcT[:], Pc[:], True, True, [Pc, PcT], [ps])
                P.copy(nxt[:], ps[:, 0:128], [ps], [nxt])
            yield
            ps = PS(); P.mm(ps[:, 0:128], nxtT[:], Tm[s][:], True, True, [nxtT, Tm[s]], [ps])
            P.tt(Tm[s][:], Tm[s][:], ps[:, 0:128], ALU.add, [Tm[s], ps], [Tm[s]])
            Pc, PcT = nxt, nxtT
            yield
        vt = tm[:, 3, :]
        ps = PS()
        P.mm(ps[:, 0:64], KKt[s][:], ST[d][:], True, False, [KKt[s], ST[d]], [ps])
        P.mm(ps[:, 0:64], Mk[s][:], vt, False, True, [Mk[s], tm], [ps])
        P.copy(Zs[s][:], ps[:, 0:64], [ps], [Zs[s]])
        yield
        ps = PS(); P.mm(ps[:, 0:64], Tm[s][:], Zs[s][:], True, True, [Tm[s], Zs[s]], [ps])
        P.ts(nU[s][:], ps[:, 0:64], -1.0, None, ALU.mult, None, [ps], [nU[s]])
        yield
        ps2 = PS()
        P.mm(ps2[0:64, 0:64], Bh[s][:], nU[s][:], True, False, [Bh[s], nU[s]], [ps2])
        P.mm(ps2[0:64, 0:64], Kh[s][:], vt, False, True, [Kh[s], tm], [ps2])
        ps = PS()
        P.mm(ps[:, 0:64], Rt[s][:], ST[d][:], True, False, [Rt[s], ST[d]], [ps])
        P.mm(ps[:, 0:64], Mbp[s][:], nU[s][:], False, False, [Mbp[s], nU[s]], [ps])
        P.mm(ps[:, 0:64], Mkp[s][:], vt, False, True, [Mkp[s], tm], [ps])
        P.stt(ST[d][:], ST[d][:], gC[s][:, 0:1], ps2[0:64, 0:64], ALU.mult, ALU.add, [ST[d], gC[s], ps2], [ST[d]])
        P.copy(Ys[s][:], ps[:, 0:64], [ps], [Ys[s]], eng="scalar")
        P.dma("gpsimd", o_y[d, c], Ys[s][:], [Ys[s]], [o_y], Ys[s])
        yield

    tasks = [(c, d) for c in range(NCH) for d in range(2)]
    active = []
    nxt_task = 0
    rounds = 0
    while nxt_task < len(tasks) or active:
        if nxt_task < len(tasks) and len(active) < NSET and (rounds % 7 == 0 or not active):
            c, d = tasks[nxt_task]
            active.append(chunk_gen(c, d, nxt_task % NSET))
            nxt_task += 1
        rounds += 1
        for g in list(active):
            try:
                next(g)
            except StopIteration:
                active.remove(g)
    return P


def consts_L2():
    c = np.zeros((128, 6, 128), np.float32)
    i = np.arange(128)
    c[:, 0, :] = (i[:, None] <= i[None, :])
    c[:, 1, :] = (i[:, None] < i[None, :])
    c[:, 2, :] = (i[:, None] > i[None, :])
    c[:, 3, :] = np.eye(128)
    c[:, 4, :] = 1.0
    return c


def tokmaj(a, aug=False):
    n = a.shape[1]
    t = a.T.reshape(n // 128, 128, 64).transpose(1, 0, 2)
    if aug:
        t = np.concatenate([t, np.ones((128, n // 128, 1), t.dtype)], axis=2)
    return np.ascontiguousarray(t)


def run_L2(inp, l, o1, SEQ):
    NK = SEQ + 256
    NCH = NK // 128
    plan, tab_list = na_plan(SEQ)
    cst = consts_L2()
    in_maps = []
    f32 = lambda a: np.asarray(a, np.float32)
    hs = lambda pair, h: (pair[0].reshape(-1, pair[0].shape[-1])[h * 64:(h + 1) * 64],
                          pair[1].reshape(-1, 256)[h * 64:(h + 1) * 64])
    for h in range(NCORES):
        m = {"cst": cst}
        m["qa"] = np.ascontiguousarray(np.concatenate([o1["qa"][0][h], o1["qa"][1][h]], 1))
        m["ka"] = np.ascontiguousarray(np.concatenate([o1["ka"][1][h], o1["ka"][0][h]], 1))
        vl, vc = hs(o1["va"], h)
        m["va"] = tokmaj(np.concatenate([vc, vl], 1), aug=True)
        n_l, n_c = o1["nqkv"]
        sel = lambda w: (n_l[w].reshape(512, -1)[h * 64:(h + 1) * 64], n_c[w].reshape(512, 256)[h * 64:(h + 1) * 64])
        m["nq"] = np.ascontiguousarray(np.concatenate(sel(0), 1))
        m["nk"] = np.ascontiguousarray(np.concatenate(sel(1), 1))
        m["nv"] = tokmaj(np.concatenate(sel(2), 1), aug=True)
        rpb = inp["na_rpb"][l][h]
        tb = np.stack([np.where(dr >= 0, rpb[np.maximum(dr, 0), np.maximum(dc, 0)], np.float32(-30000.0)) for dr, dc in tab_list], 0)
        m["tabs"] = np.ascontiguousarray(tb.transpose(1, 0, 2).astype(np.float32))
        r3l, r3c = o1["rw3"]; rdl, rdc = o1["rwd"]
        def seqs(lat, cx, d):
            lat = lat.reshape(512, -1)[h * 64:(h + 1) * 64]; cx = cx.reshape(512, 256)[h * 64:(h + 1) * 64]
            return np.concatenate([cx, lat], 1) if d == 0 else np.concatenate([cx[:, ::-1], lat[:, ::-1]], 1)
        rtm = np.zeros((2, NCH, 128, 4, 64), np.float32); rfm = np.zeros((2, NCH, 64, 4, 128), np.float32)
        for d in range(2):
            lw = seqs(rdl[0, d], rdc[0, d], d); b_ = seqs(rdl[1, d], rdc[1, d], d); kd = seqs(rdl[2, d], rdc[2, d], d)
            r_ = seqs(r3l[0], r3c[0], d); v_ = seqs(r3l[1], r3c[1], d); kk = seqs(r3l[2], r3c[2], d)
            for j, a in enumerate((lw, b_, kd, v_)):
                rtm[d, :, :, j, :] = a.T.reshape(NCH, 128, 64)
            for j, a in enumerate((b_, kd, kk, r_)):
                rfm[d, :, :, j, :] = a.reshape(64, NCH, 128).transpose(1, 0, 2)
        m["rtm"] = rtm; m["rfm"] = rfm
        in_maps.append(m)
    res = run(build_L2(SEQ), in_maps)
    ya = np.concatenate([f32(res[h]["ya"]) for h in range(NCORES)], 0)
    yb = np.concatenate([f32(res[h]["yb"]) for h in range(NCORES)], 0)
    ys = []
    for d in range(2):
        yy = np.concatenate([f32(res[h]["y"][d]).reshape(NK, 64).T for h in range(NCORES)], 0)
        cx, lat = yy[:, :256], yy[:, 256:]
        if d == 1:
            cx, lat = cx[:, ::-1], lat[:, ::-1]
        ys.append((np.ascontiguousarray(lat), np.ascontiguousarray(cx)))
    return dict(ya=(ya[:, :SEQ], ya[:, SEQ:]), yb=(yb[:, :SEQ], yb[:, SEQ:]), yf=ys[0], ybk=ys[1])


def build_L3(NT, NLAT, moe):
    FC = 28 if moe else 22
    NE = 8 if moe else 1
    tiles = [(c0, min(512, NT - c0)) for c0 in range(0, NT, 512)]
    P = Prog()
    I = lambda n, s, dt=F32: P.dram(n, s, dt, "ExternalInput")
    xT = I("xT", [128, 8, NT])
    yin = I("yin", [6, 128, 4, NT])
    nv = I("nv", [128, 14, 8])
    rwo = I("rwo", [128, 2, 4])
    wg = I("wg", [24, 128, 8, 128]); wo = I("wo", [3, 8, 128, 4, 128]); wout = I("wout", [8, 128, 8, 128])
    if not moe:
        w1 = I("w1", [NE, FC, 128, 8, 128]); w3 = I("w3", [NE, FC, 128, 8, 128]); w2 = I("w2", [NE, 8, 128, FC, 128])
    else:
        h2o = P.dram("h2o", [128, 8, NT], BF16, "ExternalOutput"); gTo = P.dram("gTo", [8, NT], F32, "ExternalOutput")
    cst = I("cst", [128, 3, 128])
    if moe:
        rt = I("rt", [128, 8, 8]); sel = I("sel", [8, 8, 128])
    out = P.dram("out", [128, 8, NT], F32, "ExternalOutput")

    def load(d, shape, dt=F32, name=None):
        t = P.sbuf(shape, dt, name)
        P.dma("sync", t[:], d[:], [d], [t], t)
        return t
    nvt = load(nv, [128, 14, 8]); rwt = load(rwo, [128, 2, 4]); cs = load(cst, [128, 3, 128])
    ones, blk, ident = cs[:, 0, :], cs[:, 1, :], cs[:, 2, :]
    if moe:
        rtt = load(rt, [128, 8, 8]); selt = load(sel, [8, 8, 128])
    A1 = P.sbuf([128, 2, 8], F32); A2 = P.sbuf([128, 2, 8], F32)
    for w_, sc in ((0, 1), (1, 3)):
        P.stt(A1[:, w_, :], nvt[:, sc, :], 1.0, nvt[:, 0, :], ALU.add, ALU.mult, [nvt], [A1])
    for w_, sc in ((0, 8), (1, 10)):
        P.stt(A2[:, w_, :], nvt[:, sc, :], 1.0, nvt[:, 7, :], ALU.add, ALU.mult, [nvt], [A2])

    psb = [P.psum([128, 512], F32, f"psb{i}") for i in range(8)]
    pi = [0]
    def PS():
        pi[0] += 1
        return psb[pi[0] % 8]
    x_ = P.sbuf([128, 8, 512], F32, "x"); hT = P.sbuf([128, 8, 512], BF16, "hT"); G = P.sbuf([128, 24, 512], BF16, "G")
    stg = [P.sbuf([128, 4, 512], F32, f"stg{i}") for i in range(2)]
    ybf = [P.sbuf([128, 4, 512], BF16, f"ybf{i}") for i in range(3)]
    ysum = P.sbuf([128, 4, 512], F32, "ysum"); tmp = P.sbuf([128, 4, 512], F32, "tmp")
    Mb = P.sbuf([128, 8, 512], BF16, "Mb"); Mo = P.sbuf([128, 512], F32, "Mo"); t5 = P.sbuf([128, 512], F32, "t5")
    h2 = P.sbuf([128, 8, 512], BF16, "h2"); hid = P.sbuf([128, 1 if moe else FC, 512], BF16, "hid")
    sq = [P.sbuf([128, 512], F32, f"sq{i}") for i in range(2)]; rs = P.sbuf([128, 512], F32, "rs")
    wst = [P.sbuf([128, 8, 128], F32, f"wst{i}") for i in range(2)]
    wbf = [P.sbuf([128, 8, 128], BF16, f"wbf{i}") for i in range(3)]
    wi = [0]
    if moe:
        lgT = P.sbuf([8, 512], F32, "lgT"); gT = P.sbuf([8, 512], F32, "gT"); gbe = P.sbuf([128, 512], F32, "gbe")
        lg = P.sbuf([128, 8], F32, "lg"); top = P.sbuf([128, 8], F32, "top"); sm = P.sbuf([128, 8], F32, "sm")
        gt_ = P.sbuf([128, 8], F32, "gt"); g2_ = P.sbuf([128, 8], F32, "g2")

    def wpiece(ap, dbuf, kc=8):
        wi[0] += 1
        ws, wb = wst[wi[0] % 2], wbf[wi[0] % 3]
        P.dma("sync", ws[:, :kc, :], ap, [dbuf], [ws], ws)
        P.copy(wb[:, :kc, :], ws[:, :kc, :], [ws], [wb], eng="gpsimd")
        return wb

    def segs(c0, n):
        for (a, b, w_) in ((0, NLAT, 0), (NLAT, NT, 1)):
            lo, hi = max(a, c0), min(b, c0 + n)
            if lo < hi:
                yield lo - c0, hi - c0, w_

    def norm_mod(src, dst, A, shl, shc, n, c0, extra=None):
        ps = PS()
        for k in range(8):
            s_ = sq[k % 2]
            P.act(s_[:, :n], src[:, k, :n], AF.Square, [src], [s_])
            P.mm(ps[:, :n], ones, s_[:, :n], k == 0, k == 7, [cs, s_], [ps])
        P.act(rs[:, :n], ps[:, :n], AF.Sqrt, [ps], [rs], bias=1e-6, scale=1.0 / 1024)
        P.op("vector", lambda e: e.reciprocal(rs[:, :n], rs[:, :n]), [rs], [rs])
        for k in range(8):
            s_ = sq[k % 2]
            P.tt(s_[:, :n], src[:, k, :n], rs[:, :n], ALU.mult, [src, rs], [s_])
            for (lo, hi, w_) in segs(c0, n):
                P.ts(dst[:, k, lo:hi], s_[:, lo:hi], A[:, w_, k:k + 1], nvt[:, (shl, shc)[w_], k:k + 1],
                     ALU.mult, ALU.add, [s_, A, nvt], [dst])
            if extra is not None:
                extra(k, s_)

    for (c0, n) in tiles:
        P.dma("sync", x_[:, :, :n], xT[:, :, c0:c0 + n], [xT], [x_], x_)
        norm_mod(x_, hT, A1, 2, 4, n, c0)
        for j in range(24):
            wb = wpiece(wg[j], wg)
            ps = PS()
            for k in range(8):
                P.mm(ps[:, :n], wb[:, k, :], hT[:, k, :n], k == 0, k == 7, [wb, hT], [ps])
            P.act(G[:, j, :n], ps[:, :n], AF.Sigmoid, [ps], [G])
        for b in range(2):
            s_ = stg[b % 2]
            P.dma("sync", s_[:, :, :n], yin[b][:, :, c0:c0 + n], [yin], [s_], s_)
            P.copy(ybf[b][:, :, :n], s_[:, :, :n], [s_], [ybf[b]])
        sa, sb_ = stg[0], stg[1]
        P.dma("sync", sa[:, :, :n], yin[2][:, :, c0:c0 + n], [yin], [sa], sa)
        P.dma("sync", sb_[:, :, :n], yin[3][:, :, c0:c0 + n], [yin], [sb_], sb_)
        P.tt(ysum[:, :, :n], sa[:, :, :n], sb_[:, :, :n], ALU.add, [sa, sb_], [ysum])
        P.dma("sync", sa[:, :, :n], yin[4][:, :, c0:c0 + n], [yin], [sa], sa)
        P.dma("sync", sb_[:, :, :n], yin[5][:, :, c0:c0 + n], [yin], [sb_], sb_)
        for c in range(4):
            ps = PS(); P.mm(ps[:, :n], blk, ysum[:, c, :n], True, True, [cs, ysum], [ps])
            P.stt(ysum[:, c, :n], ps[:, :n], -1.0 / 64, ysum[:, c, :n], ALU.mult, ALU.add, [ps, ysum], [ysum])
            P.act(tmp[:, c, :n], ysum[:, c, :n], AF.Square, [ysum], [tmp])
            ps = PS(); P.mm(ps[:, :n], blk, tmp[:, c, :n], True, True, [cs, tmp], [ps])
            P.act(tmp[:, c, :n], ps[:, :n], AF.Sqrt, [ps], [tmp], bias=64e-5, scale=1.0 / 64)
            P.op("vector", lambda e, c=c, n=n: e.reciprocal(tmp[:, c, :n], tmp[:, c, :n]), [tmp], [tmp])
            P.tt(ysum[:, c, :n], ysum[:, c, :n], tmp[:, c, :n], ALU.mult, [ysum, tmp], [ysum])
            P.ts(ysum[:, c, :n], ysum[:, c, :n], rwt[:, 0, c:c + 1], rwt[:, 1, c:c + 1], ALU.mult, ALU.add, [ysum, rwt], [ysum])
            P.tt(ysum[:, c, :n], ysum[:, c, :n], sb_[:, c, :n], ALU.add, [ysum, sb_], [ysum])
            P.tt(ybf[2][:, c, :n], ysum[:, c, :n], sa[:, c, :n], ALU.mult, [ysum, sa], [ybf[2]])
        for oc in range(8):
            for br in range(3):
                wb = wpiece(wo[br, oc], wo, 4)
                ps = PS()
                for k in range(4):
                    P.mm(ps[:, :n], wb[:, k, :], ybf[br][:, k, :n], k == 0, k == 3, [wb, ybf[br]], [ps])
                if br == 0:
                    P.tt(Mo[:, :n], ps[:, :n], G[:, oc, :n], ALU.mult, [ps, G], [Mo])
                else:
                    P.tt(t5[:, :n], ps[:, :n], G[:, br * 8 + oc, :n], ALU.mult, [ps, G], [t5])
                    P.tt(Mo[:, :n], Mo[:, :n], t5[:, :n], ALU.add, [Mo, t5], [Mo])
            P.copy(Mb[:, oc, :n], Mo[:, :n], [Mo], [Mb], eng="scalar")
        for oc in range(8):
            wb = wpiece(wout[oc], wout)
            ps = PS()
            for k in range(8):
                P.mm(ps[:, :n], wb[:, k, :], Mb[:, k, :n], k == 0, k == 7, [wb, Mb], [ps])
            for (lo, hi, w_) in segs(c0, n):
                P.stt(x_[:, oc, lo:hi], ps[:, lo:hi], nvt[:, 5 + w_, oc:oc + 1], x_[:, oc, lo:hi], ALU.mult, ALU.add,
                      [ps, nvt, x_], [x_])
        if moe:
            psr = PS()
            def extra(k, s_):
                for (lo, hi, w_) in segs(c0, n):
                    P.ts(t5[:, lo:hi], s_[:, lo:hi], A2[:, w_, k:k + 1], nvt[:, (9, 11)[w_], k:k + 1],
                         ALU.mult, ALU.add, [s_, A2, nvt], [t5])
                P.mm(psr[0:8, :n], rtt[:, k, :], t5[:, :n], k == 0, k == 7, [rtt, t5], [psr])
            norm_mod(x_, h2, A2, 9, 11, n, c0, extra)
            P.copy(lgT[:, :n], psr[0:8, :n], [psr], [lgT])
            for b0 in range(0, n, 128):
                ps = PS(); P.transpose(ps[:, 0:8], lgT[:, b0:b0 + 128], cs[0:8, 2, 0:8], [lgT, cs], [ps])
                P.copy(lg[:], ps[:, 0:8], [ps], [lg])
                P.op("vector", lambda e: e.max(top[:], lg[:]), [lg], [top])
                P.ts(sm[:, 0:1], top[:, 0:1], -1.0, None, ALU.mult, None, [top], [sm])
                P.act(sm[:, 1:2], top[:, 1:2], AF.Exp, [top, sm], [sm], bias=sm[:, 0:1])
                P.ts(sm[:, 2:3], sm[:, 1:2], 1.0, None, ALU.add, None, [sm], [sm])
                P.op("vector", lambda e: e.reciprocal(sm[:, 2:3], sm[:, 2:3]), [sm], [sm])
                P.tt(sm[:, 3:4], sm[:, 1:2], sm[:, 2:3], ALU.mult, [sm], [sm])
                P.ts(gt_[:], lg[:], top[:, 0:1], sm[:, 2:3], ALU.is_equal, ALU.mult, [lg, top, sm], [gt_])
                P.ts(g2_[:], lg[:], top[:, 1:2], sm[:, 3:4], ALU.is_equal, ALU.mult, [lg, top, sm], [g2_])
                P.tt(gt_[:], gt_[:], g2_[:], ALU.add, [gt_, g2_], [gt_])
                ps = PS(); P.transpose(ps[0:8, 0:128], gt_[:], ident, [gt_, cs], [ps])
                P.copy(gT[:, b0:b0 + 128], ps[0:8, 0:128], [ps], [gT])
        else:
            norm_mod(x_, h2, A2, 9, 11, n, c0)
        if moe:
            P.dma("gpsimd", h2o[:, :, c0:c0 + n], h2[:, :, :n], [h2], [h2o], h2)
            P.dma("gpsimd", gTo[:, c0:c0 + n], gT[:, :n], [gT], [gTo], gT)
        for e_ in range(0 if moe else NE):
            if moe:
                ps = PS(); P.mm(ps[:, :n], selt[:, e_, :], gT[:, :n], True, True, [selt, gT], [ps])
                P.copy(gbe[:, :n], ps[:, :n], [ps], [gbe], eng="scalar")
            for fc in range(FC):
                wb1 = wpiece(w1[e_, fc], w1)
                ps1 = PS()
                for k in range(8):
                    P.mm(ps1[:, :n], wb1[:, k, :], h2[:, k, :n], k == 0, k == 7, [wb1, h2], [ps1])
                wb3 = wpiece(w3[e_, fc], w3)
                ps3 = PS()
                for k in range(8):
                    P.mm(ps3[:, :n], wb3[:, k, :], h2[:, k, :n], k == 0, k == 7, [wb3, h2], [ps3])
                P.act(t5[:, :n], ps1[:, :n], AF.Silu, [ps1], [t5])
                if moe:
                    P.tt(t5[:, :n], t5[:, :n], gbe[:, :n], ALU.mult, [t5, gbe], [t5])
                P.tt(hid[:, fc, :n], t5[:, :n], ps3[:, :n], ALU.mult, [t5, ps3], [hid])
            for oc in range(8):
                ps = PS()
                for k0 in range(0, FC, 8):
                    kc = min(8, FC - k0)
                    wb = wpiece(w2[e_, oc][:, k0:k0 + kc, :], w2, kc)
                    for k in range(kc):
                        P.mm(ps[:, :n], wb[:, k, :], hid[:, k0 + k, :n], k0 + k == 0, k0 + k == FC - 1, [wb, hid], [ps])
                for (lo, hi, w_) in segs(c0, n):
                    P.stt(x_[:, oc, lo:hi], ps[:, lo:hi], nvt[:, 12 + w_, oc:oc + 1], x_[:, oc, lo:hi], ALU.mult, ALU.add,
                          [ps, nvt, x_], [x_])
        P.dma("gpsimd", out[:, :, c0:c0 + n], x_[:, :, :n], [x_], [out], x_)
    return P


def build_L4(NT):
    FC, QC = 28, 7
    tiles = [(c0, min(512, NT - c0)) for c0 in range(0, NT, 512)]
    P = Prog()
    I = lambda n, s, dt=F32: P.dram(n, s, dt, "ExternalInput")
    xm = I("xm", [128, 8, NT]); h2d = I("h2", [128, 8, NT], BF16); gTd = I("gT", [8, NT]); gt2d = I("gt2", [128, 8])
    seld = I("sel", [8, 8, 128])
    w1 = I("w1", [8, FC, 128, 8, 128]); w3 = I("w3", [8, FC, 128, 8, 128]); w2 = I("w2", [8, 8, 128, FC, 128])
    out = P.dram("out", [128, 8, NT], F32, "ExternalOutput")
    XM = P.sbuf([128, 8, NT], F32, "XM"); H2 = P.sbuf([128, 8, NT], BF16, "H2"); GT = P.sbuf([8, NT], F32, "GT")
    gt2 = P.sbuf([128, 8], F32, "gt2s"); selt = P.sbuf([8, 8, 128], F32, "selt")
    for (t, d) in ((XM, xm), (H2, h2d), (GT, gTd), (gt2, gt2d), (selt, seld)):
        P.dma("sync", t[:], d[:], [d], [t], t)
    gbe = P.sbuf([128, NT], F32, "gbe"); hid = P.sbuf([128, QC, NT], BF16, "hid")
    t5 = [P.sbuf([128, 512], F32, f"t5_{i}") for i in range(3)]
    wst = [P.sbuf([128, 8, 128], F32, f"wst{i}") for i in range(3)]
    wbf = [P.sbuf([128, 8, 128], BF16, f"wbf{i}") for i in range(4)]
    psb = [P.psum([128, 512], F32, f"psb{i}") for i in range(8)]
    pi = [0]; wi = [0]; ti = [0]
    def PS():
        pi[0] += 1
        return psb[pi[0] % 8]
    def wpiece(ap, dbuf, kc=8):
        wi[0] += 1
        ws, wb = wst[wi[0] % 3], wbf[wi[0] % 4]
        P.dma("sync", ws[:, :kc, :], ap, [dbuf], [ws], ws)
        P.copy(wb[:, :kc, :], ws[:, :kc, :], [ws], [wb], eng="gpsimd")
        return wb
    for e_ in range(8):
        for (c0, n) in tiles:
            ps = PS(); P.mm(ps[:, :n], selt[:, e_, :], GT[:, c0:c0 + n], True, True, [selt, GT], [ps])
            P.copy(gbe[:, c0:c0 + n], ps[:, :n], [ps], [gbe], eng="scalar")
        for f0 in range(0, FC, QC):
            for j in range(QC):
                wb1 = wpiece(w1[e_, f0 + j], w1)
                wb3 = wpiece(w3[e_, f0 + j], w3)
                for (c0, n) in tiles:
                    ps1 = PS()
                    for k in range(8):
                        P.mm(ps1[:, :n], wb1[:, k, :], H2[:, k, c0:c0 + n], k == 0, k == 7, [wb1, H2], [ps1])
                    ps3 = PS()
                    for k in range(8):
                        P.mm(ps3[:, :n], wb3[:, k, :], H2[:, k, c0:c0 + n], k == 0, k == 7, [wb3, H2], [ps3])
                    ti[0] += 1
                    t_ = t5[ti[0] % 3]
                    P.act(t_[:, :n], ps1[:, :n], AF.Silu, [ps1], [t_])
                    P.tt(t_[:, :n], t_[:, :n], gbe[:, c0:c0 + n], ALU.mult, [t_, gbe], [t_])
                    P.tt(hid[:, j, c0:c0 + n], t_[:, :n], ps3[:, :n], ALU.mult, [t_, ps3], [hid])
            for oc in range(8):
                wb = wpiece(w2[e_, oc][:, f0:f0 + QC, :], w2, QC)
                for (c0, n) in tiles:
                    ps = PS()
                    for k in range(QC):
                        P.mm(ps[:, :n], wb[:, k, :], hid[:, k, c0:c0 + n], k == 0, k == QC - 1, [wb, hid], [ps])
                    P.stt(XM[:, oc, c0:c0 + n], ps[:, :n], gt2[:, oc:oc + 1], XM[:, oc, c0:c0 + n], ALU.mult, ALU.add,
                          [ps, gt2, XM], [XM])
    P.dma("gpsimd", out[:], XM[:], [XM], [out], XM)
    return P


def arrw(W):
    K_, M_ = W.shape[0] // 128, W.shape[1] // 128
    return np.ascontiguousarray(W.reshape(K_, 128, M_, 128).transpose(2, 1, 0, 3))


def fmT(a):
    C = a.shape[0] // 128
    return a.reshape(C, 128, a.shape[1]).transpose(1, 0, 2)


def run_L3(inp, l, mod, x, ctx, o1, o2):
    SEQ = x.shape[0]
    moe = (l % 2 == 1)
    need_ctx = l < 1
    NL3 = SEQ // NCORES
    NT = NL3 + (256 if need_ctx else 0)
    mv = lambda g, w: mod[:, l, g * 8:(g + 1) * 8, w]
    nv = np.stack([fm(inp["norm1_g"][l]), mv(1, 0), mv(0, 0), mv(1, 1), mv(0, 1), mv(2, 0), mv(2, 1),
                   fm(inp["norm2_g"][l]), mv(4, 0), mv(3, 0), mv(4, 1), mv(3, 1), mv(5, 0), mv(5, 1)], 1)
    W = dict(nv=np.ascontiguousarray(nv, np.float32),
             rwo=np.ascontiguousarray(np.stack([fm(inp["rw_ln_g"][l]), fm(inp["rw_ln_b"][l])], 1)),
             wg=arrw(inp["w_in"][l][:, 4128:7200]),
             wo=np.stack([arrw(inp["mla_wo"][l]), arrw(inp["na_wo"][l]), arrw(inp["rw_wo"][l])], 0),
             wout=arrw(inp["w_out"][l]))
    cst = np.zeros((128, 3, 128), np.float32)
    cst[:, 0, :] = 1.0; cst[:64, 1, :64] = 1.0; cst[64:, 1, 64:] = 1.0; cst[:, 2, :] = np.eye(128)
    W["cst"] = cst
    if moe:
        W["rt"] = np.ascontiguousarray(inp["moe_router"][l // 2].reshape(8, 128, 8).transpose(1, 0, 2))
        sel = np.zeros((8, 8, 128), np.float32)
        for e in range(8):
            sel[e, e, :] = 1.0
        W["sel"] = sel
    else:
        W["w1"] = arrw(inp["ffn_w1"][l // 2])[None]; W["w3"] = arrw(inp["ffn_w3"][l // 2])[None]
        W["w2"] = arrw(inp["ffn_w2"][l // 2])[None]
    g_l, g_c = o1["rwgb"]
    srcs = [o2["ya"], o2["yb"], o2["yf"], o2["ybk"],
            (g_l[0].reshape(512, -1), g_c[0].reshape(512, 256)), (g_l[1].reshape(512, -1), g_c[1].reshape(512, 256))]
    in_maps = []
    for i in range(NCORES):
        sl = slice(i * NL3, (i + 1) * NL3)
        xs = x[sl]
        if need_ctx:
            xs = np.concatenate([xs, ctx], 0)
        m = dict(W)
        m["xT"] = np.ascontiguousarray(fmT(xs.T))
        ys = []
        for (lat, cx) in srcs:
            a = np.asarray(lat, np.float32)[:, sl]
            if need_ctx:
                a = np.concatenate([a, np.asarray(cx, np.float32)], 1)
            ys.append(fmT(a))
        m["yin"] = np.ascontiguousarray(np.stack(ys, 0))
        in_maps.append(m)
    res = run(build_L3(NT, NL3, moe), in_maps)
    if moe:
        W4 = dict(w1=np.stack([arrw(inp["moe_w1"][l // 2][e]) for e in range(8)], 0),
                  w3=np.stack([arrw(inp["moe_w3"][l // 2][e]) for e in range(8)], 0),
                  w2=np.stack([arrw(inp["moe_w2"][l // 2][e]) for e in range(8)], 0),
                  sel=W["sel"], gt2=np.ascontiguousarray(mv(5, 0), np.float32))
        maps4 = []
        for r in res:
            m4 = dict(W4)
            m4.update(xm=np.asarray(r["out"]), h2=np.asarray(r["h2o"]), gT=np.asarray(r["gTo"]))
            maps4.append(m4)
        res = run(build_L4(NT), maps4)
    outs = [np.asarray(r["out"]).transpose(2, 1, 0).reshape(NT, 1024) for r in res]
    x_new = np.concatenate([o[:NL3] for o in outs], 0)
    ctx_new = outs[0][NL3:] if need_ctx else ctx
    return x_new, ctx_new


def kernel(**inp):
    inp = {k: np.asarray(v) for k, v in inp.items()}
    SEQ = inp["x"].shape[1]
    x = np.ascontiguousarray(inp["x"][0]); ctx = np.ascontiguousarray(inp["ctx"][0])
    mod = run_L0(inp["c"], inp["c_ctx"], inp["mod_w"], inp["mod_b"])
    depth = inp["mod_w"].shape[0]
    for l in range(depth):
        o1 = run_L1(inp, l, mod, x, ctx, SEQ // 16, 2)
        o2 = run_L2(inp, l, o1, SEQ)
        del o1["qa"], o1["ka"], o1["va"], o1["nqkv"], o1["rw3"], o1["rwd"]
        x, ctx = run_L3(inp, l, mod, x, ctx, o1, o2)
    return np.ascontiguousarray(x[None].astype(np.float32))
```

```python
from contextlib import ExitStack
import numpy as np
import ml_dtypes
import concourse.bass as bass
import concourse.mybir as mybir
from concourse.bass_utils import run_bass_kernel_spmd

F32 = mybir.dt.float32
BF16 = mybir.dt.bfloat16
AF = mybir.ActivationFunctionType
ALU = mybir.AluOpType
AX = mybir.AxisListType
NCORES = 8
ENGINES = ("tensor", "vector", "scalar", "gpsimd", "sync")


class Buf:
    __slots__ = ("t", "name", "lw", "rd")

    def __init__(self, t, name):
        self.t = t
        self.name = name
        self.lw = None
        self.rd = {}

    def __getitem__(self, idx):
        return self.t[idx]


class Prog:
    def __init__(self):
        self.nc = bass.Bass("TRN2", target_bir_lowering=False)
        self.ops = {e: [] for e in ENGINES}
        self.count = {}
        self.waited = {e: {} for e in ENGINES}
        self.stack = ExitStack()
        self.nbuf = 0

    def sbuf(self, shape, dt, name=None):
        self.nbuf += 1
        name = name or f"sb{self.nbuf}"
        t = self.stack.enter_context(self.nc.sbuf_tensor(name, list(shape), dt))
        return Buf(t, name)

    def psum(self, shape, dt=F32, name=None):
        self.nbuf += 1
        name = name or f"ps{self.nbuf}"
        t = self.stack.enter_context(self.nc.psum_tensor(name, list(shape), dt))
        return Buf(t, name)

    def dram(self, name, shape, dt, kind):
        t = self.nc.dram_tensor(name, list(shape), dt, kind=kind).ap()
        return Buf(t, name)

    def _deps(self, eng, reads, writes, skip_self):
        need = {}

        def add(kv):
            if kv is None:
                return
            k, v = kv
            if need.get(k, 0) < v:
                need[k] = v

        for b in reads:
            add(b.lw)
        for b in writes:
            add(b.lw)
            for k, v in b.rd.items():
                add((k, v))
        w = self.waited[eng]
        out = []
        for k, v in need.items():
            if skip_self and k == eng:
                continue
            if w.get(k, 0) < v:
                w[k] = v
                out.append((k, v))
        return out

    def _commit(self, key, inc, reads, writes):
        v = self.count.get(key, 0) + inc
        self.count[key] = v
        for b in reads:
            if b.rd.get(key, 0) < v:
                b.rd[key] = v
        for b in writes:
            b.lw = (key, v)
            b.rd = {}
        return v

    def op(self, eng, fn, reads=(), writes=(), skip_self=False):
        waits = self._deps(eng, reads, writes, skip_self)
        self._commit(eng, 1, reads, writes)
        self.ops[eng].append((waits, fn, eng, 1))

    def dma(self, eng, out_ap, in_ap, reads, writes, sem_buf):
        waits = self._deps(eng, reads, writes, False)
        key = "d_" + sem_buf.name
        self._commit(key, 16, reads, writes)
        self.ops[eng].append((waits, lambda e: e.dma_start(out=out_ap, in_=in_ap), key, 16))

    def coll(self, kind, in_buf, out_buf, op=None):
        waits = self._deps("gpsimd", [in_buf], [out_buf], False)
        key = "c_" + out_buf.name
        self._commit(key, 16, [in_buf], [out_buf])
        ia, oa = in_buf.t, out_buf.t
        op = op or ALU.bypass
        self.ops["gpsimd"].append((waits, lambda e: e.collective_compute(
            kind, op, replica_groups=[list(range(NCORES))], ins=[ia], outs=[oa]), key, 16))

    def barrier(self):
        snap = dict(self.count)
        for e in ENGINES:
            w = self.waited[e]
            waits = [(k, v) for k, v in snap.items() if w.get(k, 0) < v]
            for k, v in waits:
                w[k] = v
            if waits:
                self.ops[e].append((waits, None, None, 0))

    def scratch(self, name, shape, dt, shared=False):
        t = self.nc.dram_tensor(name, list(shape), dt, addr_space=("Shared" if shared else "Local")).ap()
        return Buf(t, name)

    def mm(self, out_ap, lhsT_ap, rhs_ap, start, stop, reads, writes):
        self.op("tensor", lambda e: e.matmul(out_ap, lhsT_ap, rhs_ap, start=start, stop=stop),
                reads, writes, skip_self=True)

    def transpose(self, out_ap, in_ap, ident_ap, reads, writes):
        self.op("tensor", lambda e: e.transpose(out_ap, in_ap, ident_ap), reads, writes, skip_self=True)

    def act(self, out_ap, in_ap, func, reads, writes, bias=None, scale=None, eng="scalar"):
        kw = {}
        if bias is not None:
            kw["bias"] = bias
        if scale is not None:
            kw["scale"] = scale
        self.op(eng, lambda e: e.activation(out_ap, in_ap, func, **kw), reads, writes)

    def tt(self, out_ap, a_ap, b_ap, op, reads, writes, eng="vector"):
        self.op(eng, lambda e: e.tensor_tensor(out_ap, a_ap, b_ap, op), reads, writes)

    def ts(self, out_ap, a_ap, s1, s2, op0, op1, reads, writes, eng="vector"):
        if s2 is None:
            self.op(eng, lambda e: e.tensor_scalar(out_ap, a_ap, s1, None, op0), reads, writes)
        else:
            self.op(eng, lambda e: e.tensor_scalar(out_ap, a_ap, s1, s2, op0, op1), reads, writes)

    def stt(self, out_ap, in0, scalar, in1, op0, op1, reads, writes):
        self.op("vector", lambda e: e.scalar_tensor_tensor(out_ap, in0, scalar, in1, op0, op1), reads, writes)

    def copy(self, out_ap, in_ap, reads, writes, eng="vector"):
        if eng == "scalar":
            self.op(eng, lambda e: e.copy(out_ap, in_ap), reads, writes)
        else:
            self.op(eng, lambda e: e.tensor_copy(out_ap, in_ap), reads, writes)

    def memset(self, ap, val, writes, eng="vector"):
        self.op(eng, lambda e: e.memset(ap, val), (), writes)

    def finish(self):
        nc = self.nc
        final_waits = []
        w = self.waited["sync"]
        for k, v in self.count.items():
            if w.get(k, 0) < v:
                final_waits.append((k, v))
        keys = list(self.count.keys())
        sems = {}
        for i, k in enumerate(keys):
            sems[k] = self.stack.enter_context(nc.semaphore(f"s{i}"))
        ops = self.ops

        def replay(e, lst):
            for waits, fn, key, inc in lst:
                for (k, v) in waits:
                    e.wait_ge(sems[k], v)
                if fn is not None:
                    fn(e).then_inc(sems[key], inc)

        with nc.Block() as block:
            @block.tensor
            def _(e):
                replay(e, ops["tensor"])

            @block.vector
            def _(e):
                replay(e, ops["vector"])

            @block.scalar
            def _(e):
                replay(e, ops["scalar"])

            @block.gpsimd
            def _(e):
                replay(e, ops["gpsimd"])

            @block.sync
            def _(e):
                replay(e, ops["sync"])
                for (k, v) in final_waits:
                    e.wait_ge(sems[k], v)
        self.stack.close()
        return nc


TRACE = False
TIMES = []


def run(prog, in_maps):
    nc = prog.finish()
    if TRACE:
        res = run_bass_kernel_spmd(nc, in_maps, core_ids=list(range(NCORES)), trace=True)
        TIMES.append(res.exec_time_ns)
        print("exec_time_ns", res.exec_time_ns, flush=True)
    else:
        res = run_bass_kernel_spmd(nc, in_maps, core_ids=list(range(NCORES)))
    return res.results


def build_L0(nch):
    P = Prog()
    w = P.dram("w", [1024, nch * 128], F32, "ExternalInput")
    b = P.dram("b", [128, nch], F32, "ExternalInput")
    cT = P.dram("cT", [128, 8, 2], F32, "ExternalInput")
    out = P.dram("out", [128, nch, 2], F32, "ExternalOutput")
    wt = P.sbuf([128, 8, nch * 128], F32, "wt")
    bt = P.sbuf([128, nch], F32, "bt")
    ct = P.sbuf([128, 8, 2], F32, "ct")
    st = P.sbuf([128, 8, 2], F32, "st")
    ot = P.sbuf([128, nch, 2], F32, "ot")
    ps = P.psum([128, nch, 2], F32, "ps0")
    P.dma("sync", ct[:], cT[:], [cT], [ct], ct)
    P.dma("sync", bt[:], b[:], [b], [bt], bt)
    for k in range(8):
        P.dma("sync", wt[:, k, :], w[k * 128:(k + 1) * 128, :], [w], [wt], wt)
    P.act(st[:], ct[:], AF.Silu, [ct], [st])
    for j in range(nch):
        for k in range(8):
            P.mm(ps[:, j, :], wt[:, k, j * 128:(j + 1) * 128], st[:, k, :], k == 0, k == 7, [wt, st], [ps])
    for j in range(nch):
        P.ts(ot[:, j, :], ps[:, j, :], bt[:, j:j + 1], None, ALU.add, None, [ps, bt], [ot])
    P.dma("sync", out[:], ot[:], [ot], [out], ot)
    return P


def fm(v):
    v = np.asarray(v, np.float32)
    return np.ascontiguousarray(v.reshape(-1, 128).T)


def run_L0(c, c_ctx, mod_w, mod_b):
    depth = mod_w.shape[0]
    nch_total = depth * 48
    nch = nch_total // NCORES
    wcat = np.concatenate([mod_w[l] for l in range(depth)], axis=1)
    bcat = np.concatenate([mod_b[l] for l in range(depth)], axis=0)
    cT = np.stack([fm(c.reshape(-1)), fm(c_ctx.reshape(-1))], axis=-1)
    in_maps = []
    for i in range(NCORES):
        sl = slice(i * nch * 128, (i + 1) * nch * 128)
        in_maps.append({"w": np.ascontiguousarray(wcat[:, sl]), "b": fm(bcat[sl]), "cT": cT})
    res = run(build_L0(nch), in_maps)
    o = np.concatenate([r["out"] for r in res], axis=1)
    return o.reshape(128, depth, 48, 2)


RW_ORDER = [12, 13, 14, 0, 4, 8, 1, 5, 9, 2, 6, 10, 3, 7, 11]


def build_L1(NL, NSUB):
    NS = NL + 260
    tiles = [(c0, min(512, NS - c0)) for c0 in range(0, NS, 512)]
    P = Prog()
    I = lambda n, s, dt=F32: P.dram(n, s, dt, "ExternalInput")
    O = lambda n, s, dt=F32: P.dram(n, s, dt, "ExternalOutput")
    xT_ = I("xT", [NSUB, 128, 8, NS])
    nv = I("nv", [128, 5, 8])
    wq = I("wq", [33, 128, 8, 128])
    ropeC_ = I("ropeC", [NSUB, 128, NS]); ropeS_ = I("ropeS", [NSUB, 128, NS]); mask_ = I("mask", [NSUB, 128, NS], BF16)
    sp = I("sp", [128, 16])
    wuq = I("wuq", [128, 3, 8, 128]); wuk = I("wuk", [128, 2, 8, 128]); wuv = I("wuv", [128, 2, 4, 128])
    rwp = I("rwp", [128, 58])
    w2d = I("w2", [128, 512]); a2d = I("a2", [128, 512]); g2d = I("g2", [128, 512])
    cst = I("cst", [128, 3, 128])
    o_qa_ = O("qa", [NSUB, 8, 128, NS], BF16); o_ka_ = O("ka", [NSUB, 8, 128, NS], BF16)
    o_va_ = O("va", [NSUB, 4, 128, NS], BF16)
    o_n_ = O("nqkv", [NSUB, 3, 4, 128, NS], BF16)
    o_r3_ = O("rw3", [NSUB, 3, 4, 128, NS])
    o_d3_ = O("rwd", [NSUB, 3, 2, 4, 128, NS])
    o_gb_ = O("rwgb", [NSUB, 2, 4, 128, NS])

    def load(d, shape, dt=F32, name=None):
        t = P.sbuf(shape, dt, name)
        P.dma("sync", t[:], d[:], [d], [t], t)
        return t
    nvt = load(nv, [128, 5, 8]); spt = load(sp, [128, 16]); rwt = load(rwp, [128, 58])
    cs = load(cst, [128, 3, 128])
    w2t = load(w2d, [128, 512]); a2t = load(a2d, [128, 512]); g2t = load(g2d, [128, 512])
    stg = P.sbuf([128, 3 * 8 * 128], F32, "stg")
    wuqb = P.sbuf([128, 3, 8, 128], BF16); wukb = P.sbuf([128, 2, 8, 128], BF16); wuvb = P.sbuf([128, 2, 4, 128], BF16)
    for (src, dstb, nel) in ((wuq, wuqb, 3 * 8 * 128), (wuk, wukb, 2 * 8 * 128), (wuv, wuvb, 2 * 4 * 128)):
        P.dma("sync", stg[:, :nel], src[:].rearrange("p a b c -> p (a b c)"), [src], [stg], stg)
        P.copy(dstb[:].rearrange("p a b c -> p (a b c)"), stg[:, :nel], [stg], [dstb], eng="gpsimd")
    ones = cs[:, 0, :]; blk = cs[:, 1, :]; rot = cs[:, 2, :]
    At = P.sbuf([128, 2, 8], F32)
    for w_, sc in ((0, 1), (1, 3)):
        P.stt(At[:, w_, :], nvt[:, sc, :], 1.0, nvt[:, 0, :], ALU.add, ALU.mult, [nvt], [At])
    m2 = P.sbuf([128, 15], F32)
    P.tt(m2[:], rwt[:, 0:15], rwt[:, 15:30], ALU.add, [rwt], [m2])
    P.ts(m2[:], m2[:], -1.0, 1.0, ALU.mult, ALU.add, [m2], [m2])

    psb = [P.psum([128, 512], F32, f"psb{i}") for i in range(6)]
    pi = [0]
    def PS():
        pi[0] += 1
        return psb[pi[0] % 6]
    slabs = [P.sbuf([128, NS], F32, f"sl{i}") for i in range(13)]
    bslabs = [P.sbuf([128, NS], BF16, f"bs{i}") for i in range(3)]
    bi = [0]
    def BS():
        bi[0] += 1
        return bslabs[bi[0] % 3]
    Ct = P.sbuf([128, NS], F32, "Ct"); St = P.sbuf([128, NS], F32, "St"); Mt = P.sbuf([128, NS], BF16, "Mt")
    hT = P.sbuf([128, 8, NS], BF16, "hT")
    x_ = P.sbuf([128, 8, 512], F32, "xt")
    sqt = [P.sbuf([128, 512], F32, f"sq{i}") for i in range(2)]
    rs = P.sbuf([128, 512], F32, "rs")
    wst = [P.sbuf([128, 8, 128], F32, f"wst{i}") for i in range(2)]
    wbf = [P.sbuf([128, 8, 128], BF16, f"wbf{i}") for i in range(2)]
    wi = [0]

    def proj(ci, dst, masked=False):
        wi[0] += 1
        ws, wb = wst[wi[0] % 2], wbf[wi[0] % 2]
        P.dma("sync", ws[:], wq[ci], [wq], [ws], ws)
        P.copy(wb[:], ws[:], [ws], [wb], eng="gpsimd")
        for (c0, n) in tiles:
            ps = PS()
            for k in range(8):
                P.mm(ps[:, :n], wb[:, k, :], hT[:, k, c0:c0 + n], k == 0, k == 7, [wb, hT], [ps])
            if masked:
                P.tt(dst[:, c0:c0 + n], ps[:, :n], Mt[:, c0:c0 + n], ALU.mult, [ps, Mt], [dst])
            else:
                P.copy(dst[:, c0:c0 + n], ps[:, :n], [ps], [dst], eng="scalar")

    def rstd_of(srcs, lhsT, dim, eps, dst, sqrt_only=False):
        tmp = slabs[12]
        for (c0, n) in tiles:
            ps = PS()
            for j, s in enumerate(srcs):
                P.act(tmp[:, c0:c0 + n], s[:, c0:c0 + n], AF.Square, [s], [tmp])
                P.mm(ps[:, :n], lhsT, tmp[:, c0:c0 + n], j == 0, j == len(srcs) - 1, [cs, tmp], [ps])
            P.act(dst[:, c0:c0 + n], ps[:, :n], AF.Sqrt, [ps], [dst], bias=eps, scale=1.0 / dim)
        if sqrt_only:
            P.ts(dst[:], dst[:], 1e-12, None, ALU.max, None, [dst], [dst])
        P.op("vector", lambda e: e.reciprocal(dst[:], dst[:]), [dst], [dst])

    def body(sub):
        D = lambda b, *idx: Buf(b.t[(sub,) + idx] if idx else b.t[sub], b.name + "_v")
        xT = D(xT_)
        o_qa, o_ka, o_va, o_n, o_r3, o_d3, o_gb = (D(o_qa_), D(o_ka_), D(o_va_), D(o_n_), D(o_r3_), D(o_d3_), D(o_gb_))
        P.dma("sync", Ct[:], ropeC_[sub], [ropeC_], [Ct], Ct)
        P.dma("sync", St[:], ropeS_[sub], [ropeS_], [St], St)
        P.dma("sync", Mt[:], mask_[sub], [mask_], [Mt], Mt)

        def out_dma(dram_ap, dram_buf, sb):
            P.dma("gpsimd", dram_ap, sb[:], [sb], [dram_buf], sb)

        for ti, (c0, n) in enumerate(tiles):
            P.dma("sync", x_[:, :, :n], xT[:, :, c0:c0 + n], [xT], [x_], x_)
            ps = PS()
            for k in range(8):
                s_ = sqt[k % 2]
                P.act(s_[:, :n], x_[:, k, :n], AF.Square, [x_], [s_])
                P.mm(ps[:, :n], ones, s_[:, :n], k == 0, k == 7, [cs, s_], [ps])
            P.act(rs[:, :n], ps[:, :n], AF.Sqrt, [ps], [rs], bias=1e-6, scale=1.0 / 1024)
            P.op("vector", lambda e, a=rs[:, :n]: e.reciprocal(a, a), [rs], [rs])
            for k in range(8):
                P.tt(x_[:, k, :n], x_[:, k, :n], rs[:, :n], ALU.mult, [x_, rs], [x_])
                for (a, b, w_, sh) in ((0, NL + 2, 0, 2), (NL + 2, NS, 1, 4)):
                    lo, hi = max(a, c0), min(b, c0 + n)
                    if lo < hi:
                        P.ts(hT[:, k, lo:hi], x_[:, k, lo - c0:hi - c0], At[:, w_, k:k + 1], nvt[:, sh, k:k + 1],
                             ALU.mult, ALU.add, [x_, At, nvt], [hT])

        cq = slabs[0:3]
        for j in range(3):
            proj(j, cq[j])
        r_ = slabs[3]
        rstd_of(cq, ones, 384.0, 1e-6, r_)
        cqn = [BS() for _ in range(3)]
        for j in range(3):
            P.stt(cqn[j][:], cq[j][:], spt[:, j:j + 1], r_[:], ALU.mult, ALU.mult, [cq[j], spt, r_], [cqn[j]])

        def head_finish(pre, gcol, dram_ap, dram_buf, ob):
            rr = slabs[4]
            rstd_of([pre], ones, 96.0, 1e-6, rr)
            P.stt(pre[:], pre[:], spt[:, gcol:gcol + 1], rr[:], ALU.mult, ALU.mult, [pre, spt, rr], [pre])
            rq = slabs[5]
            for (c0, n) in tiles:
                ps = PS()
                P.mm(ps[:, :n], rot, pre[:, c0:c0 + n], True, True, [cs, pre], [ps])
                P.tt(rq[:, c0:c0 + n], ps[:, :n], St[:, c0:c0 + n], ALU.mult, [ps, St], [rq])
            P.tt(pre[:], pre[:], Ct[:], ALU.mult, [pre, Ct], [pre])
            P.tt(ob[:], pre[:], rq[:], ALU.add, [pre, rq], [ob])
            out_dma(dram_ap, dram_buf, ob)

        obs = [slabs[8].t, slabs[9].t]
        qob = [P_q0, P_q1]
        for h in range(8):
            pre = slabs[6 + h % 2]
            for (c0, n) in tiles:
                ps = PS()
                for j in range(3):
                    P.mm(ps[:, :n], wuqb[:, j, h, :], cqn[j][:, c0:c0 + n], j == 0, j == 2, [wuqb, cqn[j]], [ps])
                P.copy(pre[:, c0:c0 + n], ps[:, :n], [ps], [pre], eng="scalar")
            head_finish(pre, 5, o_qa[h], o_qa, qob[h % 2])

        ckv = slabs[0:2]
        for j in range(2):
            proj(3 + j, ckv[j])
        krp = slabs[2]
        proj(5, krp)
        rstd_of(ckv, ones, 256.0, 1e-6, r_)
        ckvn = [BS() for _ in range(2)]
        for j in range(2):
            P.stt(ckvn[j][:], ckv[j][:], spt[:, 3 + j:4 + j], r_[:], ALU.mult, ALU.mult, [ckv[j], spt, r_], [ckvn[j]])
        for h in range(8):
            pre = slabs[6 + h % 2]
            for (c0, n) in tiles:
                ps = PS()
                for j in range(2):
                    P.mm(ps[:, :n], wukb[:, j, h, :], ckvn[j][:, c0:c0 + n], j == 0, j == 1, [wukb, ckvn[j]], [ps])
                P.tt(pre[:, c0:c0 + n], ps[:, :n], krp[:, c0:c0 + n], ALU.add, [ps, krp], [pre])
            head_finish(pre, 6, o_ka[h], o_ka, qob[h % 2])
        for c in range(4):
            ob = qob[c % 2]
            for (c0, n) in tiles:
                ps = PS()
                for j in range(2):
                    P.mm(ps[:, :n], wuvb[:, j, c, :], ckvn[j][:, c0:c0 + n], j == 0, j == 1, [wuvb, ckvn[j]], [ps])
                P.copy(ob[:, c0:c0 + n], ps[:, :n], [ps], [ob], eng="scalar")
            out_dma(o_va[c], o_va, ob)

        for which in range(3):
            for c in range(4):
                z = slabs[c % 2]
                proj(6 + which * 4 + c, z)
                ob = BS()
                if which < 2:
                    rr = slabs[4]
                    rstd_of([z], blk, 64.0, 1e-6, rr)
                    P.stt(ob[:], z[:], spt[:, 7 + which:8 + which], rr[:], ALU.mult, ALU.mult, [z, spt, rr], [ob])
                else:
                    P.copy(ob[:], z[:], [z], [ob])
                out_dma(o_n[which, c], o_n, ob)

        def shifted(ci_rw, dst, tmp):
            rwc = RW_ORDER[ci_rw]
            proj(18 + ci_rw, tmp, masked=True)
            P.ts(dst[:, 1:NS - 1], tmp[:, 1:NS - 1], m2[:, rwc:rwc + 1], None, ALU.mult, None, [tmp, m2], [dst])
            P.stt(dst[:, 1:NS - 1], tmp[:, 0:NS - 2], rwt[:, rwc:rwc + 1], dst[:, 1:NS - 1], ALU.mult, ALU.add,
                  [tmp, rwt, dst], [dst])
            P.stt(dst[:, 1:NS - 1], tmp[:, 2:NS], rwt[:, 15 + rwc:16 + rwc], dst[:, 1:NS - 1], ALU.mult, ALU.add,
                  [tmp, rwt, dst], [dst])
        tmp = slabs[11]
        wdT, adT, gdT = slabs[0], slabs[1], slabs[2]
        shifted(0, wdT, tmp); shifted(1, adT, tmp); shifted(2, gdT, tmp)
        P.act(wdT[:], wdT[:], AF.Tanh, [wdT], [wdT])
        P.act(gdT[:], gdT[:], AF.Sigmoid, [gdT], [gdT])
        for c in range(4):
            rT, kT, vT = slabs[3], slabs[4], slabs[5]
            shifted(3 + 3 * c, rT, tmp); shifted(4 + 3 * c, kT, tmp); shifted(5 + 3 * c, vT, tmp)
            P.dma("gpsimd", o_r3[0, c], rT[:], [rT], [o_r3], rT)
            P.dma("gpsimd", o_r3[1, c], vT[:], [vT], [o_r3], vT)
            kk = slabs[6]
            P.ts(kk[:], kT[:], rwt[:, 46 + c:47 + c], None, ALU.mult, None, [kT, rwt], [kk])
            rr = slabs[7]
            rstd_of([kk], blk, 1.0, 0.0, rr, sqrt_only=True)
            P.tt(kk[:], kk[:], rr[:], ALU.mult, [kk, rr], [kk])
            P.dma("gpsimd", o_r3[2, c], kk[:], [kk], [o_r3], kk)
            ksum = slabs[8]
            for d in range(2):
                lw, aa = slabs[9], slabs[10]
                pb = slice(d * 64, d * 64 + 64)
                for (c0, n) in tiles:
                    ps = PS()
                    P.mm(ps[:, :n], w2t[pb, c * 128:(c + 1) * 128], wdT[pb, c0:c0 + n], True, True, [w2t, wdT], [ps])
                    P.act(lw[:, c0:c0 + n], ps[:, :n], AF.Sigmoid, [ps, rwt], [lw],
                          bias=rwt[:, 30 + d * 4 + c:31 + d * 4 + c])
                    ps = PS()
                    P.mm(ps[:, :n], a2t[pb, c * 128:(c + 1) * 128], adT[pb, c0:c0 + n], True, True, [a2t, adT], [ps])
                    P.act(aa[:, c0:c0 + n], ps[:, :n], AF.Sigmoid, [ps, rwt], [aa],
                          bias=rwt[:, 38 + d * 4 + c:39 + d * 4 + c])
                P.ts(lw[:], lw[:], -float(np.exp(-0.5)), None, ALU.mult, None, [lw], [lw])
                P.dma("gpsimd", o_d3[0, d, c], lw[:], [lw], [o_d3], lw)
                bb = slabs[11]
                P.tt(bb[:], aa[:], kk[:], ALU.mult, [aa, kk], [bb])
                P.dma("gpsimd", o_d3[1, d, c], bb[:], [bb], [o_d3], bb)
                P.ts(aa[:], aa[:], -1.0, rwt[:, 50 + c:51 + c], ALU.add, ALU.mult, [aa, rwt], [aa])
                P.stt(aa[:], aa[:], 1.0, kT[:], ALU.add, ALU.mult, [aa, kT], [aa])
                P.dma("gpsimd", o_d3[2, d, c], aa[:], [aa], [o_d3], aa)
                if d == 0:
                    P.copy(ksum[:], aa[:], [aa], [ksum])
                else:
                    P.tt(ksum[:], ksum[:], aa[:], ALU.add, [ksum, aa], [ksum])
            P.stt(ksum[:], ksum[:], rwt[:, 54 + c:55 + c], rT[:], ALU.mult, ALU.mult, [ksum, rwt, rT], [ksum])
            gg, bo = slabs[9], slabs[10]
            for (c0, n) in tiles:
                ps = PS()
                P.mm(ps[:, :n], blk, ksum[:, c0:c0 + n], True, True, [cs, ksum], [ps])
                P.tt(bo[:, c0:c0 + n], ps[:, :n], vT[:, c0:c0 + n], ALU.mult, [ps, vT], [bo])
                ps = PS()
                P.mm(ps[:, :n], g2t[:, c * 128:(c + 1) * 128], gdT[:, c0:c0 + n], True, True, [g2t, gdT], [ps])
                P.copy(gg[:, c0:c0 + n], ps[:, :n], [ps], [gg], eng="scalar")
            P.dma("gpsimd", o_gb[0, c], gg[:], [gg], [o_gb], gg)
            P.dma("gpsimd", o_gb[1, c], bo[:], [bo], [o_gb], bo)

    P_q0 = P.sbuf([128, NS], BF16, "qo0"); P_q1 = P.sbuf([128, NS], BF16, "qo1")
    for sub in range(NSUB):
        body(sub)
    return P


def bf16(a):
    return np.asarray(a).astype(ml_dtypes.bfloat16)


def rope_tables(t0, NL, NS):
    C = np.ones((128, NS), np.float32)
    S = np.zeros((128, NS), np.float32)
    pos = t0 + np.arange(NL)
    rows, cols = (pos // 64).astype(np.float32), (pos % 64).astype(np.float32)
    fr = np.exp(-np.log(10000.0) * np.arange(8, dtype=np.float32) / 8).astype(np.float32)
    ar = rows[None, :] * fr[:, None]
    ac = cols[None, :] * fr[:, None]
    for base, ang in ((64, ar), (72, ar), (80, ac), (88, ac)):
        C[base:base + 8, 1:NL + 1] = np.cos(ang)
        S[base:base + 8, 1:NL + 1] = np.sin(ang)
    return C, S


def consts_L1():
    cst = np.zeros((128, 3, 128), np.float32)
    cst[:, 0, :] = 1.0
    cst[:64, 1, :64] = 1.0
    cst[64:, 1, 64:] = 1.0
    for i in range(8):
        cst[72 + i, 2, 64 + i] = -1.0
        cst[64 + i, 2, 72 + i] = 1.0
        cst[88 + i, 2, 80 + i] = -1.0
        cst[80 + i, 2, 88 + i] = 1.0
    return cst


def prep_L1_weights(inp, l, mod):
    W = inp["w_in"][l]
    cols = []
    z128 = np.zeros((1024, 128), np.float32)
    for j in range(3):
        cols.append(W[:, j * 128:(j + 1) * 128])
    for j in range(2):
        cols.append(W[:, 384 + j * 128:384 + (j + 1) * 128])
    kr = z128.copy(); kr[:, 64:96] = W[:, 640:672]; cols.append(kr)
    for j in range(12):
        cols.append(W[:, 672 + j * 128:672 + (j + 1) * 128])
    for j in RW_ORDER:
        cols.append(W[:, 2208 + j * 128:2208 + (j + 1) * 128])
    Wp = np.stack(cols, 0)
    wq = np.ascontiguousarray(Wp.reshape(33, 8, 128, 128).transpose(0, 2, 1, 3))
    nv = np.stack([fm(inp["norm1_g"][l]), mod[:, l, 8:16, 0], mod[:, l, 0:8, 0], mod[:, l, 8:16, 1], mod[:, l, 0:8, 1]], 1)
    sp = np.zeros((128, 16), np.float32)
    sp[:, 0:3] = fm(inp["mla_cq_g"][l]); sp[:, 3:5] = fm(inp["mla_ckv_g"][l])
    sp[:96, 5] = inp["mla_qn_g"][l]; sp[:96, 6] = inp["mla_kn_g"][l]
    sp[:, 7] = np.tile(inp["na_qn_g"][l], 2); sp[:, 8] = np.tile(inp["na_kn_g"][l], 2)
    wuq = np.zeros((384, 8, 128), np.float32)
    wuq[:, :, :96] = inp["mla_wuq"][l].reshape(384, 8, 96)
    wuq = np.ascontiguousarray(wuq.reshape(3, 128, 8, 128).transpose(1, 0, 2, 3))
    kv = inp["mla_wukv"][l].reshape(256, 8, 128)
    wuk = np.zeros((256, 8, 128), np.float32); wuk[:, :, :64] = kv[:, :, :64]
    wuk = np.ascontiguousarray(wuk.reshape(2, 128, 8, 128).transpose(1, 0, 2, 3))
    wuv = np.ascontiguousarray(kv[:, :, 64:].reshape(256, 4, 128).reshape(2, 128, 4, 128).transpose(1, 0, 2, 3))
    rwp = np.zeros((128, 58), np.float32)
    rwp[:, 0:15] = fm(inp["rw_mu"][l][0]); rwp[:, 15:30] = fm(inp["rw_mu"][l][1])
    for d in range(2):
        rwp[:, 30 + d * 4:34 + d * 4] = fm(inp["rw_w0"][l][d]); rwp[:, 38 + d * 4:42 + d * 4] = fm(inp["rw_a0"][l][d])
    rwp[:, 46:50] = fm(inp["rw_kk"][l]); rwp[:, 50:54] = fm(inp["rw_ka"][l]); rwp[:, 54:58] = fm(inp["rw_rk"][l].reshape(-1))
    return dict(nv=np.ascontiguousarray(nv, np.float32), wq=wq, sp=sp, wuq=wuq, wuk=wuk, wuv=wuv, rwp=rwp,
                w2=np.ascontiguousarray(inp["rw_w2"][l].reshape(128, 512)),
                a2=np.ascontiguousarray(inp["rw_a2"][l].reshape(128, 512)),
                g2=np.ascontiguousarray(inp["rw_g2"][l]), cst=consts_L1())


def run_L1(inp, l, mod, x, ctx, NL, NSUB):
    SEQ = x.shape[0]
    NS = NL + 260
    wts = prep_L1_weights(inp, l, mod)
    in_maps = []
    for i in range(NCORES):
        xs, Cs, Ss, Ms = [], [], [], []
        for s in range(NSUB):
            t0 = (i * NSUB + s) * NL
            slab = np.zeros((NS, 1024), np.float32)
            M = np.ones((128, NS), np.float32)
            if t0 > 0:
                slab[0] = x[t0 - 1]
            else:
                M[:, 0] = 0
            slab[1:NL + 1] = x[t0:t0 + NL]
            if t0 + NL < SEQ:
                slab[NL + 1] = x[t0 + NL]
            else:
                M[:, NL + 1] = 0
            M[:, NL + 2] = 0; M[:, NS - 1] = 0
            slab[NL + 3:NL + 259] = ctx
            xs.append(slab.T.reshape(8, 128, NS).transpose(1, 0, 2))
            C, S = rope_tables(t0, NL, NS)
            Cs.append(C); Ss.append(S); Ms.append(bf16(M))
        m = dict(wts)
        m.update(xT=np.ascontiguousarray(np.stack(xs, 0)), ropeC=np.stack(Cs, 0), ropeS=np.stack(Ss, 0), mask=np.stack(Ms, 0))
        in_maps.append(m)
    res = run(build_L1(NL, NSUB), in_maps)
    out = {}
    for key in ("qa", "ka", "va", "nqkv", "rw3", "rwd", "rwgb"):
        lat = np.concatenate([res[i][key][s][..., 1:NL + 1] for i in range(NCORES) for s in range(NSUB)], axis=-1)
        cx = res[0][key][0][..., NL + 3:NL + 259]
        out[key] = (np.asarray(lat), np.asarray(cx))
    return out


def na_plan(SEQ):
    NR = SEQ // 64
    NP_ = NR // 2
    tabs = {}
    tab_list = []
    plan = []
    qc = np.arange(64)
    cs_ = np.clip(qc - 8, 0, 48)
    for m in range(NP_):
        rows = [2 * m, 2 * m + 1]
        rs = [int(np.clip(r - 4, 0, NR - 8)) for r in rows]
        kps = sorted({(a + i) // 2 for a in rs for i in range(8)})
        ent = []
        for kp in kps:
            sig = (rs[0] - rows[0], rs[1] - rows[1], kp - m)
            if sig not in tabs:
                dr = np.full((128, 128), -1, np.int64)
                dc = np.full((128, 128), -1, np.int64)
                for kl in range(128):
                    krow, kcol = 2 * kp + kl // 64, kl % 64
                    for ql in range(128):
                        qrow, qcol = rows[ql // 64], ql % 64
                        a = rs[ql // 64]
                        if a <= krow < a + 8 and cs_[qcol] <= kcol < cs_[qcol] + 16:
                            dr[kl, ql] = krow - qrow + 7
                            dc[kl, ql] = kcol - qcol + 15
                tabs[sig] = len(tab_list)
                tab_list.append((dr, dc))
            ent.append((kp, tabs[sig]))
        plan.append(ent)
    return plan, tab_list


def build_L2(SEQ):
    NK = SEQ + 256
    NKB = NK // 128
    NCH = NKB
    plan, tab_list = na_plan(SEQ)
    NTAB = len(tab_list)
    P = Prog()
    I = lambda n, s, dt=F32: P.dram(n, s, dt, "ExternalInput")
    O = lambda n, s, dt=F32: P.dram(n, s, dt, "ExternalOutput")
    qa = I("qa", [128, SEQ + 256], BF16)
    ka = I("ka", [128, NK], BF16)
    va = I("va", [128, NKB, 65], BF16)
    nq = I("nq", [64, SEQ + 256], BF16)
    nk = I("nk", [64, NK], BF16)
    nvv = I("nv", [128, NKB, 65], BF16)
    tabs = I("tabs", [128, NTAB, 128])
    cst = I("cst", [128, 6, 128])
    rtm = I("rtm", [2, NCH, 128, 4, 64])
    rfm = I("rfm", [2, NCH, 64, 4, 128])
    o_ya = O("ya", [64, SEQ + 256]); o_yb = O("yb", [64, SEQ + 256])
    o_y = O("y", [2, NCH, 128, 64])

    psb = [P.psum([128, 512], F32, f"psb{i}") for i in range(4)]
    psS2 = [P.psum([128, 1024], F32, f"psS{i}") for i in range(2)]
    cs = P.sbuf([128, 6, 128], F32, "cs")
    P.dma("sync", cs[:], cst[:], [cst], [cs], cs)
    triI, triS, triL, ident, ones = (cs[:, i, :] for i in range(5))
    csb = P.sbuf([128, 2, 128], BF16, "csb")
    P.copy(csb[:, 0, :], cs[:, 3, :], [cs], [csb])
    P.copy(csb[:, 1, :], cs[:, 4, :], [cs], [csb])

    pT = [P.sbuf([128, 1024], BF16, f"pT{i}") for i in range(3)]
    osb = [P.sbuf([64, 512], F32, f"osb{i}") for i in range(2)]
    rD = P.sbuf([64, 512], F32, "rD")
    dsb = P.sbuf([128, 512], F32, "dsb")
    cnt = [0]

    def finish_od(psOD, n, out_d, out_c0):
        psB = psb[2]
        P.copy(dsb[64:65, :n], psOD[64:65, :n], [psOD], [dsb], eng="scalar")
        P.mm(psB[0:64, :n], cs[64:65, 4, 0:64], dsb[64:65, :n], True, True, [cs, dsb], [psB])
        P.op("vector", lambda e: e.reciprocal(rD[:, :n], psB[0:64, :n]), [psB], [rD])
        ob = osb[cnt[0] % 2]
        P.tt(ob[:, :n], psOD[0:64, :n], rD[:, :n], ALU.mult, [psOD, rD], [ob])
        P.dma("gpsimd", out_d[:, out_c0:out_c0 + n], ob[:, :n], [ob], [out_d], ob)

    def attn(qT, q0, n, kT, V, kblocks, KD, scale, out_d, out_c0):
        cnt[0] += 1
        psOD = psb[cnt[0] % 2]
        pairs = [kblocks[i:i + 2] for i in range(0, len(kblocks), 2)]
        npair = len(pairs)

        def S(p):
            pS = psS2[p % 2]
            for j, kb in enumerate(pairs[p]):
                P.mm(pS[:, j * 512:j * 512 + n], kT[0:KD, kb * 128:(kb + 1) * 128], qT[0:KD, q0:q0 + n], True, True, [kT, qT], [pS])
        S(0)
        for p, pr in enumerate(pairs):
            if p + 1 < npair:
                S(p + 1)
            pS = psS2[p % 2]
            p_ = pT[p % 3]
            L = len(pr)
            sv = pS[:, :].rearrange("p (j c) -> p j c", c=512)[:, 0:L, 0:n]
            dv = p_[:, :].rearrange("p (j c) -> p j c", c=512)[:, 0:L, 0:n]
            P.act(dv, sv, AF.Exp, [pS], [p_], scale=scale)
            for j, kb in enumerate(pr):
                P.mm(psOD[0:65, :n], V[:, kb, :], p_[:, j * 512:j * 512 + n], p == 0 and j == 0,
                     p == npair - 1 and j == L - 1, [V, p_], [psOD])
        finish_od(psOD, n, out_d, out_c0)

    qat = P.sbuf([128, SEQ + 256], BF16, "qat"); kat = P.sbuf([128, NK], BF16, "kat"); vat = P.sbuf([128, NKB, 65], BF16, "vat")
    for (t, d) in ((qat, qa), (kat, ka), (vat, va)):
        P.dma("sync", t[:], d[:], [d], [t], t)
    sc_a = float(96 ** -0.5)
    for q0 in range(0, SEQ, 512):
        attn(qat, q0, min(512, SEQ - q0), kat, vat, list(range(NKB)), 128, sc_a, o_ya, q0)
    attn(qat, SEQ, 256, kat, vat, [0, 1], 128, sc_a, o_ya, SEQ)

    nqt, nkt, nvt = qat, kat, vat
    P.dma("sync", nqt[0:64, :], nq[:], [nq], [nqt], nqt)
    P.dma("sync", nkt[0:64, :], nk[:], [nk], [nkt], nkt)
    P.dma("sync", nvt[:], nvv[:], [nvv], [nvt], nvt)
    tb32 = P.sbuf([128, NTAB, 128], F32, "tb32"); tbb = P.sbuf([128, NTAB, 128], BF16, "tbb")
    P.dma("sync", tb32[:], tabs[:], [tabs], [tb32], tb32)
    P.ts(tbb[:], tb32[:], 8.0, None, ALU.mult, None, [tb32], [tbb])
    sc_b = 0.125
    for m, ent in enumerate(plan):
        blocks = [(kp, tid) for (kp, tid) in ent] + [(NKB - 2, None), (NKB - 1, None)]
        cnt[0] += 1
        psOD = psb[cnt[0] % 2]
        p_ = pT[cnt[0] % 3]
        pS = psS2[cnt[0] % 2]
        qsl = nqt[0:64, m * 128:(m + 1) * 128]
        for j, (kb, tid) in enumerate(blocks):
            o_ = pS[:, j * 128:(j + 1) * 128]
            P.mm(o_, nkt[0:64, kb * 128:(kb + 1) * 128], qsl, True, tid is None, [nkt, nqt], [pS])
            if tid is not None:
                P.mm(o_, csb[:, 0, :], tbb[:, tid, :], False, True, [csb, tbb], [pS])
        w = len(blocks) * 128
        P.act(p_[:, :w], pS[:, :w], AF.Exp, [pS], [p_], scale=sc_b)
        for j, (kb, tid) in enumerate(blocks):
            P.mm(psOD[0:65, :128], nvt[:, kb, :], p_[:, j * 128:(j + 1) * 128], j == 0, j == len(blocks) - 1, [nvt, p_], [psOD])
        finish_od(psOD, 128, o_yb, m * 128)
    attn(nqt, SEQ, 256, nkt, nvt, [NKB - 2, NKB - 1], 64, sc_b, o_yb, SEQ)

    pi = [0]
    rw_ps = psb + psS2
    def PS():
        pi[0] += 1
        return rw_ps[pi[0] % 6]
    NSET = 4
    W = lambda n, shape=(128, 128): [P.sbuf(list(shape), F32, f"{n}{i}") for i in range(NSET)]
    tmb, fmb = W("tm", (128, 4, 64)), W("fmj", (64, 4, 128))
    eLr = W("eLr", (128, 64)); Bh = W("Bh", (128, 64)); Kh = W("Kh", (128, 64))
    e1 = W("e1", (64, 128)); e2 = W("e2", (64, 128)); e3 = W("e3", (64, 128))
    Rt = W("Rt", (64, 128)); KKt = W("KKt", (64, 128)); Bt = W("Bt", (64, 128)); Kt = W("Kt", (64, 128))
    Nn = W("Nn"); NTt = W("NTt"); Mk = W("Mk"); Mbp = W("Mbp"); Mkp = W("Mkp")
    Pq = W("Pq"); PqT = W("PqT"); Tm = W("Tm")
    Zs = W("Zs", (128, 64)); nU = W("nU", (128, 64)); Ys = W("Ys", (128, 64))
    gC = W("gC", (64, 1))
    ST = [P.sbuf([64, 64], F32, f"ST{d}") for d in range(2)]
    for d in range(2):
        P.memset(ST[d][:], 0.0, [ST[d]])

    def chunk_gen(c, d, s):
        tm, fj = tmb[s], fmb[s]
        P.dma("sync", tm[:], rtm[d, c], [rtm], [tm], tm)
        P.dma("sync", fj[:], rfm[d, c], [rfm], [fj], fj)
        yield
        lw_tok = tm[:, 0, :]
        ps = PS(); P.mm(ps[:, 0:64], triL, lw_tok, True, True, [cs, tm], [ps])
        P.act(eLr[s][:], ps[:, 0:64], AF.Exp, [ps], [eLr[s]])
        yield
        ps = PS(); P.mm(ps[0:64, 0:128], lw_tok, triI, True, True, [tm, cs], [ps])
        P.act(e1[s][:], ps[0:64, 0:128], AF.Exp, [ps], [e1[s]])
        P.act(e2[s][:], ps[0:64, 0:128], AF.Exp, [ps], [e2[s]], scale=-1.0)
        yield
        ps = PS(); P.mm(ps[0:64, 0:128], lw_tok, triS, True, True, [tm, cs], [ps])
        P.act(e3[s][:], ps[0:64, 0:128], AF.Exp, [ps], [e3[s]])
        yield
        P.tt(Bh[s][:], tm[:, 1, :], eLr[s][:], ALU.mult, [tm, eLr[s]], [Bh[s]], eng="gpsimd")
        P.tt(Kh[s][:], tm[:, 2, :], eLr[s][:], ALU.mult, [tm, eLr[s]], [Kh[s]], eng="gpsimd")
        P.copy(gC[s][:], e1[s][:, 127:128], [e1[s]], [gC[s]], eng="gpsimd")
        P.tt(Bt[s][:], fj[:, 0, :], e2[s][:], ALU.mult, [fj, e2[s]], [Bt[s]])
        P.tt(Kt[s][:], fj[:, 1, :], e2[s][:], ALU.mult, [fj, e2[s]], [Kt[s]], eng="gpsimd")
        P.tt(KKt[s][:], fj[:, 2, :], e3[s][:], ALU.mult, [fj, e3[s]], [KKt[s]])
        P.tt(Rt[s][:], fj[:, 3, :], e1[s][:], ALU.mult, [fj, e1[s]], [Rt[s]], eng="gpsimd")
        yield
        for (dst, l_, r_, msk) in ((Nn[s], Bt[s], KKt[s], triS), (NTt[s], KKt[s], Bt[s], triL),
                                  (Mk[s], Kt[s], KKt[s], triS), (Mbp[s], Bt[s], Rt[s], triI), (Mkp[s], Kt[s], Rt[s], triI)):
            ps = PS(); P.mm(ps[:, 0:128], l_[:], r_[:], True, True, [l_, r_], [ps])
            P.tt(dst[:], ps[:, 0:128], msk, ALU.mult, [ps, cs], [dst])
            yield
        P.tt(Tm[s][:], ident, Nn[s][:], ALU.subtract, [cs, Nn[s]], [Tm[s]])
        Pc, PcT = Nn[s], NTt[s]
        for it in range(6):
            nxt, nxtT = (Pq[s], PqT[s]) if Pc is not Pq[s] else (Nn[s], NTt[s])
            ps = PS(); P.mm(ps[:, 0:128], Pc[:], PcT[:], True, True, [Pc, PcT], [ps])
            P.copy(nxtT[:], ps[:, 0:128], [ps], [nxtT], eng="scalar")
            if it < 5:
                ps = PS(); P.mm(ps[:, 0:128], PcT[:], Pc[:], True, True, [Pc, PcT], [ps])
                P.copy(nxt[:], ps[:, 0:128], [ps], [nxt])
            yield
            ps = PS(); P.mm(ps[:, 0:128], nxtT[:], Tm[s][:], True, True, [nxtT, Tm[s]], [ps])
            P.tt(Tm[s][:], Tm[s][:], ps[:, 0:128], ALU.add, [Tm[s], ps], [Tm[s]])
            Pc, PcT = nxt, nxtT
            yield
        vt = tm[:, 3, :]
        ps = PS()
        P.mm(ps[:, 0:64], KKt[s][:], ST[d][:], True, False, [KKt[s], ST[d]], [ps])
        P.mm(ps[:, 0:64], Mk[s][:], vt, False, True, [Mk[s], tm], [ps])
        P.copy(Zs[s][:], ps[:, 0:64], [ps], [Zs[s]])
        yield
        ps = PS(); P.mm(ps[:, 0:64], Tm[s][:], Zs[s][:], True, True, [Tm[s], Zs[s]], [ps])
        P.ts(nU[s][:], ps[:, 0:64], -1.0, None, ALU.mult, None, [ps], [nU[s]])
        yield
        ps2 = PS()
        P.mm(ps2[0:64, 0:64], Bh[s][:], nU[s][:], True, False, [Bh[s], nU[s]], [ps2])
        P.mm(ps2[0:64, 0:64], Kh[s][:], vt, False, True, [Kh[s], tm], [ps2])
        ps = PS()
        P.mm(ps[:, 0:64], Rt[s][:], ST[d][:], True, False, [Rt[s], ST[d]], [ps])
        P.mm(ps[:, 0:64], Mbp[s][:], nU[s][:], False, False, [Mbp[s], nU[s]], [ps])
        P.mm(ps[:, 0:64], Mkp[s][:], vt, False, True, [Mkp[s], tm], [ps])
        P.stt(ST[d][:], ST[d][:], gC[s][:, 0:1], ps2[0:64, 0:64], ALU.mult, ALU.add, [ST[d], gC[s], ps2], [ST[d]])
        P.copy(Ys[s][:], ps[:, 0:64], [ps], [Ys[s]], eng="scalar")
        P.dma("gpsimd", o_y[d, c], Ys[s][:], [Ys[s]], [o_y], Ys[s])
        yield

    tasks = [(c, d) for c in range(NCH) for d in range(2)]
    active = []
    nxt_task = 0
    rounds = 0
    while nxt_task < len(tasks) or active:
        if nxt_task < len(tasks) and len(active) < NSET and (rounds % 7 == 0 or not active):
            c, d = tasks[nxt_task]
            active.append(chunk_gen(c, d, nxt_task % NSET))
            nxt_task += 1
        rounds += 1
        for g in list(active):
            try:
                next(g)
            except StopIteration:
                active.remove(g)
    return P


def consts_L2():
    c = np.zeros((128, 6, 128), np.float32)
    i = np.arange(128)
    c[:, 0, :] = (i[:, None] <= i[None, :])
    c[:, 1, :] = (i[:, None] < i[None, :])
    c[:, 2, :] = (i[:, None] > i[None, :])
    c[:, 3, :] = np.eye(128)
    c[:, 4, :] = 1.0
    return c


def tokmaj(a, aug=False):
    n = a.shape[1]
    t = a.T.reshape(n // 128, 128, 64).transpose(1, 0, 2)
    if aug:
        t = np.concatenate([t, np.ones((128, n // 128, 1), t.dtype)], axis=2)
    return np.ascontiguousarray(t)


def run_L2(inp, l, o1, SEQ):
    NK = SEQ + 256
    NCH = NK // 128
    plan, tab_list = na_plan(SEQ)
    cst = consts_L2()
    in_maps = []
    f32 = lambda a: np.asarray(a, np.float32)
    hs = lambda pair, h: (pair[0].reshape(-1, pair[0].shape[-1])[h * 64:(h + 1) * 64],
                          pair[1].reshape(-1, 256)[h * 64:(h + 1) * 64])
    for h in range(NCORES):
        m = {"cst": cst}
        m["qa"] = np.ascontiguousarray(np.concatenate([o1["qa"][0][h], o1["qa"][1][h]], 1))
        m["ka"] = np.ascontiguousarray(np.concatenate([o1["ka"][1][h], o1["ka"][0][h]], 1))
        vl, vc = hs(o1["va"], h)
        m["va"] = tokmaj(np.concatenate([vc, vl], 1), aug=True)
        n_l, n_c = o1["nqkv"]
        sel = lambda w: (n_l[w].reshape(512, -1)[h * 64:(h + 1) * 64], n_c[w].reshape(512, 256)[h * 64:(h + 1) * 64])
        m["nq"] = np.ascontiguousarray(np.concatenate(sel(0), 1))
        m["nk"] = np.ascontiguousarray(np.concatenate(sel(1), 1))
        m["nv"] = tokmaj(np.concatenate(sel(2), 1), aug=True)
        rpb = inp["na_rpb"][l][h]
        tb = np.stack([np.where(dr >= 0, rpb[np.maximum(dr, 0), np.maximum(dc, 0)], np.float32(-30000.0)) for dr, dc in tab_list], 0)
        m["tabs"] = np.ascontiguousarray(tb.transpose(1, 0, 2).astype(np.float32))
        r3l, r3c = o1["rw3"]; rdl, rdc = o1["rwd"]
        def seqs(lat, cx, d):
            lat = lat.reshape(512, -1)[h * 64:(h + 1) * 64]; cx = cx.reshape(512, 256)[h * 64:(h + 1) * 64]
            return np.concatenate([cx, lat], 1) if d == 0 else np.concatenate([cx[:, ::-1], lat[:, ::-1]], 1)
        rtm = np.zeros((2, NCH, 128, 4, 64), np.float32); rfm = np.zeros((2, NCH, 64, 4, 128), np.float32)
        for d in range(2):
            lw = seqs(rdl[0, d], rdc[0, d], d); b_ = seqs(rdl[1, d], rdc[1, d], d); kd = seqs(rdl[2, d], rdc[2, d], d)
            r_ = seqs(r3l[0], r3c[0], d); v_ = seqs(r3l[1], r3c[1], d); kk = seqs(r3l[2], r3c[2], d)
            for j, a in enumerate((lw, b_, kd, v_)):
                rtm[d, :, :, j, :] = a.T.reshape(NCH, 128, 64)
            for j, a in enumerate((b_, kd, kk, r_)):
                rfm[d, :, :, j, :] = a.reshape(64, NCH, 128).transpose(1, 0, 2)
        m["rtm"] = rtm; m["rfm"] = rfm
        in_maps.append(m)
    res = run(build_L2(SEQ), in_maps)
    ya = np.concatenate([f32(res[h]["ya"]) for h in range(NCORES)], 0)
    yb = np.concatenate([f32(res[h]["yb"]) for h in range(NCORES)], 0)
    ys = []
    for d in range(2):
        yy = np.concatenate([f32(res[h]["y"][d]).reshape(NK, 64).T for h in range(NCORES)], 0)
        cx, lat = yy[:, :256], yy[:, 256:]
        if d == 1:
            cx, lat = cx[:, ::-1], lat[:, ::-1]
        ys.append((np.ascontiguousarray(lat), np.ascontiguousarray(cx)))
    return dict(ya=(ya[:, :SEQ], ya[:, SEQ:]), yb=(yb[:, :SEQ], yb[:, SEQ:]), yf=ys[0], ybk=ys[1])


def build_L3(NT, NLAT, moe):
    FC = 28 if moe else 22
    NE = 8 if moe else 1
    tiles = [(c0, min(512, NT - c0)) for c0 in range(0, NT, 512)]
    P = Prog()
    I = lambda n, s, dt=F32: P.dram(n, s, dt, "ExternalInput")
    xT = I("xT", [128, 8, NT])
    yin = I("yin", [6, 128, 4, NT])
    nv = I("nv", [128, 14, 8])
    rwo = I("rwo", [128, 2, 4])
    wg = I("wg", [24, 128, 8, 128]); wo = I("wo", [3, 8, 128, 4, 128]); wout = I("wout", [8, 128, 8, 128])
    if not moe:
        w1 = I("w1", [NE, FC, 128, 8, 128]); w3 = I("w3", [NE, FC, 128, 8, 128]); w2 = I("w2", [NE, 8, 128, FC, 128])
    else:
        h2o = P.dram("h2o", [128, 8, NT], BF16, "ExternalOutput"); gTo = P.dram("gTo", [8, NT], F32, "ExternalOutput")
    cst = I("cst", [128, 3, 128])
    if moe:
        rt = I("rt", [128, 8, 8]); sel = I("sel", [8, 8, 128])
    out = P.dram("out", [128, 8, NT], F32, "ExternalOutput")

    def load(d, shape, dt=F32, name=None):
        t = P.sbuf(shape, dt, name)
        P.dma("sync", t[:], d[:], [d], [t], t)
        return t
    nvt = load(nv, [128, 14, 8]); rwt = load(rwo, [128, 2, 4]); cs = load(cst, [128, 3, 128])
    ones, blk, ident = cs[:, 0, :], cs[:, 1, :], cs[:, 2, :]
    if moe:
        rtt = load(rt, [128, 8, 8]); selt = load(sel, [8, 8, 128])
    A1 = P.sbuf([128, 2, 8], F32); A2 = P.sbuf([128, 2, 8], F32)
    for w_, sc in ((0, 1), (1, 3)):
        P.stt(A1[:, w_, :], nvt[:, sc, :], 1.0, nvt[:, 0, :], ALU.add, ALU.mult, [nvt], [A1])
    for w_, sc in ((0, 8), (1, 10)):
        P.stt(A2[:, w_, :], nvt[:, sc, :], 1.0, nvt[:, 7, :], ALU.add, ALU.mult, [nvt], [A2])

    psb = [P.psum([128, 512], F32, f"psb{i}") for i in range(8)]
    pi = [0]
    def PS():
        pi[0] += 1
        return psb[pi[0] % 8]
    x_ = P.sbuf([128, 8, 512], F32, "x"); hT = P.sbuf([128, 8, 512], BF16, "hT"); G = P.sbuf([128, 24, 512], BF16, "G")
    stg = [P.sbuf([128, 4, 512], F32, f"stg{i}") for i in range(2)]
    ybf = [P.sbuf([128, 4, 512], BF16, f"ybf{i}") for i in range(3)]
    ysum = P.sbuf([128, 4, 512], F32, "ysum"); tmp = P.sbuf([128, 4, 512], F32, "tmp")
    Mb = P.sbuf([128, 8, 512], BF16, "Mb"); Mo = P.sbuf([128, 512], F32, "Mo"); t5 = P.sbuf([128, 512], F32, "t5")
    h2 = P.sbuf([128, 8, 512], BF16, "h2"); hid = P.sbuf([128, 1 if moe else FC, 512], BF16, "hid")
    sq = [P.sbuf([128, 512], F32, f"sq{i}") for i in range(2)]; rs = P.sbuf([128, 512], F32, "rs")
    wst = [P.sbuf([128, 8, 128], F32, f"wst{i}") for i in range(2)]
    wbf = [P.sbuf([128, 8, 128], BF16, f"wbf{i}") for i in range(3)]
    wi = [0]
    if moe:
        lgT = P.sbuf([8, 512], F32, "lgT"); gT = P.sbuf([8, 512], F32, "gT"); gbe = P.sbuf([128, 512], F32, "gbe")
        lg = P.sbuf([128, 8], F32, "lg"); top = P.sbuf([128, 8], F32, "top"); sm = P.sbuf([128, 8], F32, "sm")
        gt_ = P.sbuf([128, 8], F32, "gt"); g2_ = P.sbuf([128, 8], F32, "g2")

    def wpiece(ap, dbuf, kc=8):
        wi[0] += 1
        ws, wb = wst[wi[0] % 2], wbf[wi[0] % 3]
        P.dma("sync", ws[:, :kc, :], ap, [dbuf], [ws], ws)
        P.copy(wb[:, :kc, :], ws[:, :kc, :], [ws], [wb], eng="gpsimd")
        return wb

    def segs(c0, n):
        for (a, b, w_) in ((0, NLAT, 0), (NLAT, NT, 1)):
            lo, hi = max(a, c0), min(b, c0 + n)
            if lo < hi:
                yield lo - c0, hi - c0, w_

    def norm_mod(src, dst, A, shl, shc, n, c0, extra=None):
        ps = PS()
        for k in range(8):
            s_ = sq[k % 2]
            P.act(s_[:, :n], src[:, k, :n], AF.Square, [src], [s_])
            P.mm(ps[:, :n], ones, s_[:, :n], k == 0, k == 7, [cs, s_], [ps])
        P.act(rs[:, :n], ps[:, :n], AF.Sqrt, [ps], [rs], bias=1e-6, scale=1.0 / 1024)
        P.op("vector", lambda e: e.reciprocal(rs[:, :n], rs[:, :n]), [rs], [rs])
        for k in range(8):
            s_ = sq[k % 2]
            P.tt(s_[:, :n], src[:, k, :n], rs[:, :n], ALU.mult, [src, rs], [s_])
            for (lo, hi, w_) in segs(c0, n):
                P.ts(dst[:, k, lo:hi], s_[:, lo:hi], A[:, w_, k:k + 1], nvt[:, (shl, shc)[w_], k:k + 1],
                     ALU.mult, ALU.add, [s_, A, nvt], [dst])
            if extra is not None:
                extra(k, s_)

    for (c0, n) in tiles:
        P.dma("sync", x_[:, :, :n], xT[:, :, c0:c0 + n], [xT], [x_], x_)
        norm_mod(x_, hT, A1, 2, 4, n, c0)
        for j in range(24):
            wb = wpiece(wg[j], wg)
            ps = PS()
            for k in range(8):
                P.mm(ps[:, :n], wb[:, k, :], hT[:, k, :n], k == 0, k == 7, [wb, hT], [ps])
            P.act(G[:, j, :n], ps[:, :n], AF.Sigmoid, [ps], [G])
        for b in range(2):
            s_ = stg[b % 2]
            P.dma("sync", s_[:, :, :n], yin[b][:, :, c0:c0 + n], [yin], [s_], s_)
            P.copy(ybf[b][:, :, :n], s_[:, :, :n], [s_], [ybf[b]])
        sa, sb_ = stg[0], stg[1]
        P.dma("sync", sa[:, :, :n], yin[2][:, :, c0:c0 + n], [yin], [sa], sa)
        P.dma("sync", sb_[:, :, :n], yin[3][:, :, c0:c0 + n], [yin], [sb_], sb_)
        P.tt(ysum[:, :, :n], sa[:, :, :n], sb_[:, :, :n], ALU.add, [sa, sb_], [ysum])
        P.dma("sync", sa[:, :, :n], yin[4][:, :, c0:c0 + n], [yin], [sa], sa)
        P.dma("sync", sb_[:, :, :n], yin[5][:, :, c0:c0 + n], [yin], [sb_], sb_)
        for c in range(4):
            ps = PS(); P.mm(ps[:, :n], blk, ysum[:, c, :n], True, True, [cs, ysum], [ps])
            P.stt(ysum[:, c, :n], ps[:, :n], -1.0 / 64, ysum[:, c, :n], ALU.mult, ALU.add, [ps, ysum], [ysum])
            P.act(tmp[:, c, :n], ysum[:, c, :n], AF.Square, [ysum], [tmp])
            ps = PS(); P.mm(ps[:, :n], blk, tmp[:, c, :n], True, True, [cs, tmp], [ps])
            P.act(tmp[:, c, :n], ps[:, :n], AF.Sqrt, [ps], [tmp], bias=64e-5, scale=1.0 / 64)
            P.op("vector", lambda e, c=c, n=n: e.reciprocal(tmp[:, c, :n], tmp[:, c, :n]), [tmp], [tmp])
            P.tt(ysum[:, c, :n], ysum[:, c, :n], tmp[:, c, :n], ALU.mult, [ysum, tmp], [ysum])
            P.ts(ysum[:, c, :n], ysum[:, c, :n], rwt[:, 0, c:c + 1], rwt[:, 1, c:c + 1], ALU.mult, ALU.add, [ysum, rwt], [ysum])
            P.tt(ysum[:, c, :n], ysum[:, c, :n], sb_[:, c, :n], ALU.add, [ysum, sb_], [ysum])
            P.tt(ybf[2][:, c, :n], ysum[:, c, :n], sa[:, c, :n], ALU.mult, [ysum, sa], [ybf[2]])
        for oc in range(8):
            for br in range(3):
                wb = wpiece(wo[br, oc], wo, 4)
                ps = PS()
                for k in range(4):
                    P.mm(ps[:, :n], wb[:, k, :], ybf[br][:, k, :n], k == 0, k == 3, [wb, ybf[br]], [ps])
                if br == 0:
                    P.tt(Mo[:, :n], ps[:, :n], G[:, oc, :n], ALU.mult, [ps, G], [Mo])
                else:
                    P.tt(t5[:, :n], ps[:, :n], G[:, br * 8 + oc, :n], ALU.mult, [ps, G], [t5])
                    P.tt(Mo[:, :n], Mo[:, :n], t5[:, :n], ALU.add, [Mo, t5], [Mo])
            P.copy(Mb[:, oc, :n], Mo[:, :n], [Mo], [Mb], eng="scalar")
        for oc in range(8):
            wb = wpiece(wout[oc], wout)
            ps = PS()
            for k in range(8):
                P.mm(ps[:, :n], wb[:, k, :], Mb[:, k, :n], k == 0, k == 7, [wb, Mb], [ps])
            for (lo, hi, w_) in segs(c0, n):
                P.stt(x_[:, oc, lo:hi], ps[:, lo:hi], nvt[:, 5 + w_, oc:oc + 1], x_[:, oc, lo:hi], ALU.mult, ALU.add,
                      [ps, nvt, x_], [x_])
        if moe:
            psr = PS()
            def extra(k, s_):
                for (lo, hi, w_) in segs(c0, n):
                    P.ts(t5[:, lo:hi], s_[:, lo:hi], A2[:, w_, k:k + 1], nvt[:, (9, 11)[w_], k:k + 1],
                         ALU.mult, ALU.add, [s_, A2, nvt], [t5])
                P.mm(psr[0:8, :n], rtt[:, k, :], t5[:, :n], k == 0, k == 7, [rtt, t5], [psr])
            norm_mod(x_, h2, A2, 9, 11, n, c0, extra)
            P.copy(lgT[:, :n], psr[0:8, :n], [psr], [lgT])
            for b0 in range(0, n, 128):
                ps = PS(); P.transpose(ps[:, 0:8], lgT[:, b0:b0 + 128], cs[0:8, 2, 0:8], [lgT, cs], [ps])
                P.copy(lg[:], ps[:, 0:8], [ps], [lg])
                P.op("vector", lambda e: e.max(top[:], lg[:]), [lg], [top])
                P.ts(sm[:, 0:1], top[:, 0:1], -1.0, None, ALU.mult, None, [top], [sm])
                P.act(sm[:, 1:2], top[:, 1:2], AF.Exp, [top, sm], [sm], bias=sm[:, 0:1])
                P.ts(sm[:, 2:3], sm[:, 1:2], 1.0, None, ALU.add, None, [sm], [sm])
                P.op("vector", lambda e: e.reciprocal(sm[:, 2:3], sm[:, 2:3]), [sm], [sm])
                P.tt(sm[:, 3:4], sm[:, 1:2], sm[:, 2:3], ALU.mult, [sm], [sm])
                P.ts(gt_[:], lg[:], top[:, 0:1], sm[:, 2:3], ALU.is_equal, ALU.mult, [lg, top, sm], [gt_])
                P.ts(g2_[:], lg[:], top[:, 1:2], sm[:, 3:4], ALU.is_equal, ALU.mult, [lg, top, sm], [g2_])
                P.tt(gt_[:], gt_[:], g2_[:], ALU.add, [gt_, g2_], [gt_])
                ps = PS(); P.transpose(ps[0:8, 0:128], gt_[:], ident, [gt_, cs], [ps])
                P.copy(gT[:, b0:b0 + 128], ps[0:8, 0:128], [ps], [gT])
        else:
            norm_mod(x_, h2, A2, 9, 11, n, c0)
        if moe:
            P.dma("gpsimd", h2o[:, :, c0:c0 + n], h2[:, :, :n], [h2], [h2o], h2)
            P.dma("gpsimd", gTo[:, c0:c0 + n], gT[:, :n], [gT], [gTo], gT)
        for e_ in range(0 if moe else NE):
            if moe:
                ps = PS(); P.mm(ps[:, :n], selt[:, e_, :], gT[:, :n], True, True, [selt, gT], [ps])
                P.copy(gbe[:, :n], ps[:, :n], [ps], [gbe], eng="scalar")
            for fc in range(FC):
                wb1 = wpiece(w1[e_, fc], w1)
                ps1 = PS()
                for k in range(8):
                    P.mm(ps1[:, :n], wb1[:, k, :], h2[:, k, :n], k == 0, k == 7, [wb1, h2], [ps1])
                wb3 = wpiece(w3[e_, fc], w3)
                ps3 = PS()
                for k in range(8):
                    P.mm(ps3[:, :n], wb3[:, k, :], h2[:, k, :n], k == 0, k == 7, [wb3, h2], [ps3])
                P.act(t5[:, :n], ps1[:, :n], AF.Silu, [ps1], [t5])
                if moe:
                    P.tt(t5[:, :n], t5[:, :n], gbe[:, :n], ALU.mult, [t5, gbe], [t5])
                P.tt(hid[:, fc, :n], t5[:, :n], ps3[:, :n], ALU.mult, [t5, ps3], [hid])
            for oc in range(8):
                ps = PS()
                for k0 in range(0, FC, 8):
                    kc = min(8, FC - k0)
                    wb = wpiece(w2[e_, oc][:, k0:k0 + kc, :], w2, kc)
                    for k in range(kc):
                        P.mm(ps[:, :n], wb[:, k, :], hid[:, k0 + k, :n], k0 + k == 0, k0 + k == FC - 1, [wb, hid], [ps])
                for (lo, hi, w_) in segs(c0, n):
                    P.stt(x_[:, oc, lo:hi], ps[:, lo:hi], nvt[:, 12 + w_, oc:oc + 1], x_[:, oc, lo:hi], ALU.mult, ALU.add,
                          [ps, nvt, x_], [x_])
        P.dma("gpsimd", out[:, :, c0:c0 + n], x_[:, :, :n], [x_], [out], x_)
    return P


def build_L4(NT):
    FC, QC = 28, 7
    tiles = [(c0, min(512, NT - c0)) for c0 in range(0, NT, 512)]
    P = Prog()
    I = lambda n, s, dt=F32: P.dram(n, s, dt, "ExternalInput")
    xm = I("xm", [128, 8, NT]); h2d = I("h2", [128, 8, NT], BF16); gTd = I("gT", [8, NT]); gt2d = I("gt2", [128, 8])
    seld = I("sel", [8, 8, 128])
    w1 = I("w1", [8, FC, 128, 8, 128]); w3 = I("w3", [8, FC, 128, 8, 128]); w2 = I("w2", [8, 8, 128, FC, 128])
    out = P.dram("out", [128, 8, NT], F32, "ExternalOutput")
    XM = P.sbuf([128, 8, NT], F32, "XM"); H2 = P.sbuf([128, 8, NT], BF16, "H2"); GT = P.sbuf([8, NT], F32, "GT")
    gt2 = P.sbuf([128, 8], F32, "gt2s"); selt = P.sbuf([8, 8, 128], F32, "selt")
    for (t, d) in ((XM, xm), (H2, h2d), (GT, gTd), (gt2, gt2d), (selt, seld)):
        P.dma("sync", t[:], d[:], [d], [t], t)
    gbe = P.sbuf([128, NT], F32, "gbe"); hid = P.sbuf([128, QC, NT], BF16, "hid")
    t5 = [P.sbuf([128, 512], F32, f"t5_{i}") for i in range(3)]
    wst = [P.sbuf([128, 8, 128], F32, f"wst{i}") for i in range(3)]
    wbf = [P.sbuf([128, 8, 128], BF16, f"wbf{i}") for i in range(4)]
    psb = [P.psum([128, 512], F32, f"psb{i}") for i in range(8)]
    pi = [0]; wi = [0]; ti = [0]
    def PS():
        pi[0] += 1
        return psb[pi[0] % 8]
    def wpiece(ap, dbuf, kc=8):
        wi[0] += 1
        ws, wb = wst[wi[0] % 3], wbf[wi[0] % 4]
        P.dma("sync", ws[:, :kc, :], ap, [dbuf], [ws], ws)
        P.copy(wb[:, :kc, :], ws[:, :kc, :], [ws], [wb], eng="gpsimd")
        return wb
    for e_ in range(8):
        for (c0, n) in tiles:
            ps = PS(); P.mm(ps[:, :n], selt[:, e_, :], GT[:, c0:c0 + n], True, True, [selt, GT], [ps])
            P.copy(gbe[:, c0:c0 + n], ps[:, :n], [ps], [gbe], eng="scalar")
        for f0 in range(0, FC, QC):
            for j in range(QC):
                wb1 = wpiece(w1[e_, f0 + j], w1)
                wb3 = wpiece(w3[e_, f0 + j], w3)
                for (c0, n) in tiles:
                    ps1 = PS()
                    for k in range(8):
                        P.mm(ps1[:, :n], wb1[:, k, :], H2[:, k, c0:c0 + n], k == 0, k == 7, [wb1, H2], [ps1])
                    ps3 = PS()
                    for k in range(8):
                        P.mm(ps3[:, :n], wb3[:, k, :], H2[:, k, c0:c0 + n], k == 0, k == 7, [wb3, H2], [ps3])
                    ti[0] += 1
                    t_ = t5[ti[0] % 3]
                    P.act(t_[:, :n], ps1[:, :n], AF.Silu, [ps1], [t_])
                    P.tt(t_[:, :n], t_[:, :n], gbe[:, c0:c0 + n], ALU.mult, [t_, gbe], [t_])
                    P.tt(hid[:, j, c0:c0 + n], t_[:, :n], ps3[:, :n], ALU.mult, [t_, ps3], [hid])
            for oc in range(8):
                wb = wpiece(w2[e_, oc][:, f0:f0 + QC, :], w2, QC)
                for (c0, n) in tiles:
                    ps = PS()
                    for k in range(QC):
                        P.mm(ps[:, :n], wb[:, k, :], hid[:, k, c0:c0 + n], k == 0, k == QC - 1, [wb, hid], [ps])
                    P.stt(XM[:, oc, c0:c0 + n], ps[:, :n], gt2[:, oc:oc + 1], XM[:, oc, c0:c0 + n], ALU.mult, ALU.add,
                          [ps, gt2, XM], [XM])
    P.dma("gpsimd", out[:], XM[:], [XM], [out], XM)
    return P


def arrw(W):
    K_, M_ = W.shape[0] // 128, W.shape[1] // 128
    return np.ascontiguousarray(W.reshape(K_, 128, M_, 128).transpose(2, 1, 0, 3))


def fmT(a):
    C = a.shape[0] // 128
    return a.reshape(C, 128, a.shape[1]).transpose(1, 0, 2)


def run_L3(inp, l, mod, x, ctx, o1, o2):
    SEQ = x.shape[0]
    moe = (l % 2 == 1)
    need_ctx = l < 1
    NL3 = SEQ // NCORES
    NT = NL3 + (256 if need_ctx else 0)
    mv = lambda g, w: mod[:, l, g * 8:(g + 1) * 8, w]
    nv = np.stack([fm(inp["norm1_g"][l]), mv(1, 0), mv(0, 0), mv(1, 1), mv(0, 1), mv(2, 0), mv(2, 1),
                   fm(inp["norm2_g"][l]), mv(4, 0), mv(3, 0), mv(4, 1), mv(3, 1), mv(5, 0), mv(5, 1)], 1)
    W = dict(nv=np.ascontiguousarray(nv, np.float32),
             rwo=np.ascontiguousarray(np.stack([fm(inp["rw_ln_g"][l]), fm(inp["rw_ln_b"][l])], 1)),
             wg=arrw(inp["w_in"][l][:, 4128:7200]),
             wo=np.stack([arrw(inp["mla_wo"][l]), arrw(inp["na_wo"][l]), arrw(inp["rw_wo"][l])], 0),
             wout=arrw(inp["w_out"][l]))
    cst = np.zeros((128, 3, 128), np.float32)
    cst[:, 0, :] = 1.0; cst[:64, 1, :64] = 1.0; cst[64:, 1, 64:] = 1.0; cst[:, 2, :] = np.eye(128)
    W["cst"] = cst
    if moe:
        W["rt"] = np.ascontiguousarray(inp["moe_router"][l // 2].reshape(8, 128, 8).transpose(1, 0, 2))
        sel = np.zeros((8, 8, 128), np.float32)
        for e in range(8):
            sel[e, e, :] = 1.0
        W["sel"] = sel
    else:
        W["w1"] = arrw(inp["ffn_w1"][l // 2])[None]; W["w3"] = arrw(inp["ffn_w3"][l // 2])[None]
        W["w2"] = arrw(inp["ffn_w2"][l // 2])[None]
    g_l, g_c = o1["rwgb"]
    srcs = [o2["ya"], o2["yb"], o2["yf"], o2["ybk"],
            (g_l[0].reshape(512, -1), g_c[0].reshape(512, 256)), (g_l[1].reshape(512, -1), g_c[1].reshape(512, 256))]
    in_maps = []
    for i in range(NCORES):
        sl = slice(i * NL3, (i + 1) * NL3)
        xs = x[sl]
        if need_ctx:
            xs = np.concatenate([xs, ctx], 0)
        m = dict(W)
        m["xT"] = np.ascontiguousarray(fmT(xs.T))
        ys = []
        for (lat, cx) in srcs:
            a = np.asarray(lat, np.float32)[:, sl]
            if need_ctx:
                a = np.concatenate([a, np.asarray(cx, np.float32)], 1)
            ys.append(fmT(a))
        m["yin"] = np.ascontiguousarray(np.stack(ys, 0))
        in_maps.append(m)
    res = run(build_L3(NT, NL3, moe), in_maps)
    if moe:
        W4 = dict(w1=np.stack([arrw(inp["moe_w1"][l // 2][e]) for e in range(8)], 0),
                  w3=np.stack([arrw(inp["moe_w3"][l // 2][e]) for e in range(8)], 0),
                  w2=np.stack([arrw(inp["moe_w2"][l // 2][e]) for e in range(8)], 0),
                  sel=W["sel"], gt2=np.ascontiguousarray(mv(5, 0), np.float32))
        maps4 = []
        for r in res:
            m4 = dict(W4)
            m4.update(xm=np.asarray(r["out"]), h2=np.asarray(r["h2o"]), gT=np.asarray(r["gTo"]))
            maps4.append(m4)
        res = run(build_L4(NT), maps4)
    outs = [np.asarray(r["out"]).transpose(2, 1, 0).reshape(NT, 1024) for r in res]
    x_new = np.concatenate([o[:NL3] for o in outs], 0)
    ctx_new = outs[0][NL3:] if need_ctx else ctx
    return x_new, ctx_new


def kernel(**inp):
    inp = {k: np.asarray(v) for k, v in inp.items()}
    SEQ = inp["x"].shape[1]
    x = np.ascontiguousarray(inp["x"][0]); ctx = np.ascontiguousarray(inp["ctx"][0])
    mod = run_L0(inp["c"], inp["c_ctx"], inp["mod_w"], inp["mod_b"])
    depth = inp["mod_w"].shape[0]
    for l in range(depth):
        o1 = run_L1(inp, l, mod, x, ctx, SEQ // 16, 2)
        o2 = run_L2(inp, l, o1, SEQ)
        del o1["qa"], o1["ka"], o1["va"], o1["nqkv"], o1["rw3"], o1["rwd"]
        x, ctx = run_L3(inp, l, mod, x, ctx, o1, o2)
    return np.ascontiguousarray(x[None].astype(np.float32))
```

```python
from contextlib import ExitStack
import numpy as np
import ml_dtypes
import concourse.bass as bass
import concourse.mybir as mybir
from concourse.bass_utils import run_bass_kernel_spmd

F32 = mybir.dt.float32
BF16 = mybir.dt.bfloat16
AF = mybir.ActivationFunctionType
ALU = mybir.AluOpType
AX = mybir.AxisListType
NCORES = 8
ENGINES = ("tensor", "vector", "scalar", "gpsimd", "sync")


class Buf:
    __slots__ = ("t", "name", "lw", "rd")

    def __init__(self, t, name):
        self.t = t
        self.name = name
        self.lw = None
        self.rd = {}

    def __getitem__(self, idx):
        return self.t[idx]


class Prog:
    def __init__(self):
        self.nc = bass.Bass("TRN2", target_bir_lowering=False)
        self.ops = {e: [] for e in ENGINES}
        self.count = {}
        self.waited = {e: {} for e in ENGINES}
        self.stack = ExitStack()
        self.nbuf = 0

    def sbuf(self, shape, dt, name=None):
        self.nbuf += 1
        name = name or f"sb{self.nbuf}"
        t = self.stack.enter_context(self.nc.sbuf_tensor(name, list(shape), dt))
        return Buf(t, name)

    def psum(self, shape, dt=F32, name=None):
        self.nbuf += 1
        name = name or f"ps{self.nbuf}"
        t = self.stack.enter_context(self.nc.psum_tensor(name, list(shape), dt))
        return Buf(t, name)

    def dram(self, name, shape, dt, kind):
        t = self.nc.dram_tensor(name, list(shape), dt, kind=kind).ap()
        return Buf(t, name)

    def _deps(self, eng, reads, writes, skip_self):
        need = {}

        def add(kv):
            if kv is None:
                return
            k, v = kv
            if need.get(k, 0) < v:
                need[k] = v

        for b in reads:
            add(b.lw)
        for b in writes:
            add(b.lw)
            for k, v in b.rd.items():
                add((k, v))
        w = self.waited[eng]
        out = []
        for k, v in need.items():
            if skip_self and k == eng:
                continue
            if w.get(k, 0) < v:
                w[k] = v
                out.append((k, v))
        return out

    def _commit(self, key, inc, reads, writes):
        v = self.count.get(key, 0) + inc
        self.count[key] = v
        for b in reads:
            if b.rd.get(key, 0) < v:
                b.rd[key] = v
        for b in writes:
            b.lw = (key, v)
            b.rd = {}
        return v

    def op(self, eng, fn, reads=(), writes=(), skip_self=False):
        waits = self._deps(eng, reads, writes, skip_self)
        self._commit(eng, 1, reads, writes)
        self.ops[eng].append((waits, fn, eng, 1))

    def dma(self, eng, out_ap, in_ap, reads, writes, sem_buf):
        waits = self._deps(eng, reads, writes, False)
        key = "d_" + sem_buf.name
        self._commit(key, 16, reads, writes)
        self.ops[eng].append((waits, lambda e: e.dma_start(out=out_ap, in_=in_ap), key, 16))

    def coll(self, kind, in_buf, out_buf, op=None):
        waits = self._deps("gpsimd", [in_buf], [out_buf], False)
        key = "c_" + out_buf.name
        self._commit(key, 16, [in_buf], [out_buf])
        ia, oa = in_buf.t, out_buf.t
        op = op or ALU.bypass
        self.ops["gpsimd"].append((waits, lambda e: e.collective_compute(
            kind, op, replica_groups=[list(range(NCORES))], ins=[ia], outs=[oa]), key, 16))

    def barrier(self):
        snap = dict(self.count)
        for e in ENGINES:
            w = self.waited[e]
            waits = [(k, v) for k, v in snap.items() if w.get(k, 0) < v]
            for k, v in waits:
                w[k] = v
            if waits:
                self.ops[e].append((waits, None, None, 0))

    def scratch(self, name, shape, dt, shared=False):
        t = self.nc.dram_tensor(name, list(shape), dt, addr_space=("Shared" if shared else "Local")).ap()
        return Buf(t, name)

    def mm(self, out_ap, lhsT_ap, rhs_ap, start, stop, reads, writes):
        self.op("tensor", lambda e: e.matmul(out_ap, lhsT_ap, rhs_ap, start=start, stop=stop),
                reads, writes, skip_self=True)

    def transpose(self, out_ap, in_ap, ident_ap, reads, writes):
        self.op("tensor", lambda e: e.transpose(out_ap, in_ap, ident_ap), reads, writes, skip_self=True)

    def act(self, out_ap, in_ap, func, reads, writes, bias=None, scale=None, eng="scalar"):
        kw = {}
        if bias is not None:
            kw["bias"] = bias
        if scale is not None:
            kw["scale"] = scale
        self.op(eng, lambda e: e.activation(out_ap, in_ap, func, **kw), reads, writes)

    def tt(self, out_ap, a_ap, b_ap, op, reads, writes, eng="vector"):
        self.op(eng, lambda e: e.tensor_tensor(out_ap, a_ap, b_ap, op), reads, writes)

    def ts(self, out_ap, a_ap, s1, s2, op0, op1, reads, writes, eng="vector"):
        if s2 is None:
            self.op(eng, lambda e: e.tensor_scalar(out_ap, a_ap, s1, None, op0), reads, writes)
        else:
            self.op(eng, lambda e: e.tensor_scalar(out_ap, a_ap, s1, s2, op0, op1), reads, writes)

    def stt(self, out_ap, in0, scalar, in1, op0, op1, reads, writes):
        self.op("vector", lambda e: e.scalar_tensor_tensor(out_ap, in0, scalar, in1, op0, op1), reads, writes)

    def copy(self, out_ap, in_ap, reads, writes, eng="vector"):
        if eng == "scalar":
            self.op(eng, lambda e: e.copy(out_ap, in_ap), reads, writes)
        else:
            self.op(eng, lambda e: e.tensor_copy(out_ap, in_ap), reads, writes)

    def memset(self, ap, val, writes, eng="vector"):
        self.op(eng, lambda e: e.memset(ap, val), (), writes)

    def finish(self):
        nc = self.nc
        final_waits = []
        w = self.waited["sync"]
        for k, v in self.count.items():
            if w.get(k, 0) < v:
                final_waits.append((k, v))
        keys = list(self.count.keys())
        sems = {}
        for i, k in enumerate(keys):
            sems[k] = self.stack.enter_context(nc.semaphore(f"s{i}"))
        ops = self.ops

        def replay(e, lst):
            for waits, fn, key, inc in lst:
                for (k, v) in waits:
                    e.wait_ge(sems[k], v)
                if fn is not None:
                    fn(e).then_inc(sems[key], inc)

        with nc.Block() as block:
            @block.tensor
            def _(e):
                replay(e, ops["tensor"])

            @block.vector
            def _(e):
                replay(e, ops["vector"])

            @block.scalar
            def _(e):
                replay(e, ops["scalar"])

            @block.gpsimd
            def _(e):
                replay(e, ops["gpsimd"])

            @block.sync
            def _(e):
                replay(e, ops["sync"])
                for (k, v) in final_waits:
                    e.wait_ge(sems[k], v)
        self.stack.close()
        return nc


TRACE = False
TIMES = []


def run(prog, in_maps):
    nc = prog.finish()
    if TRACE:
        res = run_bass_kernel_spmd(nc, in_maps, core_ids=list(range(NCORES)), trace=True)
        TIMES.append(res.exec_time_ns)
        print("exec_time_ns", res.exec_time_ns, flush=True)
    else:
        res = run_bass_kernel_spmd(nc, in_maps, core_ids=list(range(NCORES)))
    return res.results


def build_L0(nch):
    P = Prog()
    w = P.dram("w", [1024, nch * 128], F32, "ExternalInput")
    b = P.dram("b", [128, nch], F32, "ExternalInput")
    cT = P.dram("cT", [128, 8, 2], F32, "ExternalInput")
    out = P.dram("out", [128, nch, 2], F32, "ExternalOutput")
    wt = P.sbuf([128, 8, nch * 128], F32, "wt")
    bt = P.sbuf([128, nch], F32, "bt")
    ct = P.sbuf([128, 8, 2], F32, "ct")
    st = P.sbuf([128, 8, 2], F32, "st")
    ot = P.sbuf([128, nch, 2], F32, "ot")
    ps = P.psum([128, nch, 2], F32, "ps0")
    P.dma("sync", ct[:], cT[:], [cT], [ct], ct)
    P.dma("sync", bt[:], b[:], [b], [bt], bt)
    for k in range(8):
        P.dma("sync", wt[:, k, :], w[k * 128:(k + 1) * 128, :], [w], [wt], wt)
    P.act(st[:], ct[:], AF.Silu, [ct], [st])
    for j in range(nch):
        for k in range(8):
            P.mm(ps[:, j, :], wt[:, k, j * 128:(j + 1) * 128], st[:, k, :], k == 0, k == 7, [wt, st], [ps])
    for j in range(nch):
        P.ts(ot[:, j, :], ps[:, j, :], bt[:, j:j + 1], None, ALU.add, None, [ps, bt], [ot])
    P.dma("sync", out[:], ot[:], [ot], [out], ot)
    return P


def fm(v):
    v = np.asarray(v, np.float32)
    return np.ascontiguousarray(v.reshape(-1, 128).T)


def run_L0(c, c_ctx, mod_w, mod_b):
    depth = mod_w.shape[0]
    nch_total = depth * 48
    nch = nch_total // NCORES
    wcat = np.concatenate([mod_w[l] for l in range(depth)], axis=1)
    bcat = np.concatenate([mod_b[l] for l in range(depth)], axis=0)
    cT = np.stack([fm(c.reshape(-1)), fm(c_ctx.reshape(-1))], axis=-1)
    in_maps = []
    for i in range(NCORES):
        sl = slice(i * nch * 128, (i + 1) * nch * 128)
        in_maps.append({"w": np.ascontiguousarray(wcat[:, sl]), "b": fm(bcat[sl]), "cT": cT})
    res = run(build_L0(nch), in_maps)
    o = np.concatenate([r["out"] for r in res], axis=1)
    return o.reshape(128, depth, 48, 2)


RW_ORDER = [12, 13, 14, 0, 4, 8, 1, 5, 9, 2, 6, 10, 3, 7, 11]


def build_L1(NL, NSUB):
    NS = NL + 260
    tiles = [(c0, min(512, NS - c0)) for c0 in range(0, NS, 512)]
    P = Prog()
    I = lambda n, s, dt=F32: P.dram(n, s, dt, "ExternalInput")
    O = lambda n, s, dt=F32: P.dram(n, s, dt, "ExternalOutput")
    xT_ = I("xT", [NSUB, 128, 8, NS])
    nv = I("nv", [128, 5, 8])
    wq = I("wq", [33, 128, 8, 128])
    ropeC_ = I("ropeC", [NSUB, 128, NS]); ropeS_ = I("ropeS", [NSUB, 128, NS]); mask_ = I("mask", [NSUB, 128, NS], BF16)
    sp = I("sp", [128, 16])
    wuq = I("wuq", [128, 3, 8, 128]); wuk = I("wuk", [128, 2, 8, 128]); wuv = I("wuv", [128, 2, 4, 128])
    rwp = I("rwp", [128, 58])
    w2d = I("w2", [128, 512]); a2d = I("a2", [128, 512]); g2d = I("g2", [128, 512])
    cst = I("cst", [128, 3, 128])
    o_qa_ = O("qa", [NSUB, 8, 128, NS], BF16); o_ka_ = O("ka", [NSUB, 8, 128, NS], BF16)
    o_va_ = O("va", [NSUB, 4, 128, NS], BF16)
    o_n_ = O("nqkv", [NSUB, 3, 4, 128, NS], BF16)
    o_r3_ = O("rw3", [NSUB, 3, 4, 128, NS])
    o_d3_ = O("rwd", [NSUB, 3, 2, 4, 128, NS])
    o_gb_ = O("rwgb", [NSUB, 2, 4, 128, NS])

    def load(d, shape, dt=F32, name=None):
        t = P.sbuf(shape, dt, name)
        P.dma("sync", t[:], d[:], [d], [t], t)
        return t
    nvt = load(nv, [128, 5, 8]); spt = load(sp, [128, 16]); rwt = load(rwp, [128, 58])
    cs = load(cst, [128, 3, 128])
    w2t = load(w2d, [128, 512]); a2t = load(a2d, [128, 512]); g2t = load(g2d, [128, 512])
    stg = P.sbuf([128, 3 * 8 * 128], F32, "stg")
    wuqb = P.sbuf([128, 3, 8, 128], BF16); wukb = P.sbuf([128, 2, 8, 128], BF16); wuvb = P.sbuf([128, 2, 4, 128], BF16)
    for (src, dstb, nel) in ((wuq, wuqb, 3 * 8 * 128), (wuk, wukb, 2 * 8 * 128), (wuv, wuvb, 2 * 4 * 128)):
        P.dma("sync", stg[:, :nel], src[:].rearrange("p a b c -> p (a b c)"), [src], [stg], stg)
        P.copy(dstb[:].rearrange("p a b c -> p (a b c)"), stg[:, :nel], [stg], [dstb], eng="gpsimd")
    ones = cs[:, 0, :]; blk = cs[:, 1, :]; rot = cs[:, 2, :]
    At = P.sbuf([128, 2, 8], F32)
    for w_, sc in ((0, 1), (1, 3)):
        P.stt(At[:, w_, :], nvt[:, sc, :], 1.0, nvt[:, 0, :], ALU.add, ALU.mult, [nvt], [At])
    m2 = P.sbuf([128, 15], F32)
    P.tt(m2[:], rwt[:, 0:15], rwt[:, 15:30], ALU.add, [rwt], [m2])
    P.ts(m2[:], m2[:], -1.0, 1.0, ALU.mult, ALU.add, [m2], [m2])

    psb = [P.psum([128, 512], F32, f"psb{i}") for i in range(6)]
    pi = [0]
    def PS():
        pi[0] += 1
        return psb[pi[0] % 6]
    slabs = [P.sbuf([128, NS], F32, f"sl{i}") for i in range(13)]
    bslabs = [P.sbuf([128, NS], BF16, f"bs{i}") for i in range(3)]
    bi = [0]
    def BS():
        bi[0] += 1
        return bslabs[bi[0] % 3]
    Ct = P.sbuf([128, NS], F32, "Ct"); St = P.sbuf([128, NS], F32, "St"); Mt = P.sbuf([128, NS], BF16, "Mt")
    hT = P.sbuf([128, 8, NS], BF16, "hT")
    x_ = P.sbuf([128, 8, 512], F32, "xt")
    sqt = [P.sbuf([128, 512], F32, f"sq{i}") for i in range(2)]
    rs = P.sbuf([128, 512], F32, "rs")
    wst = [P.sbuf([128, 8, 128], F32, f"wst{i}") for i in range(3)]
    wbf = [P.sbuf([128, 8, 128], BF16, f"wbf{i}") for i in range(3)]
    wi = [0]

    def proj(ci, dst, masked=False):
        wi[0] += 1
        ws, wb = wst[wi[0] % 3], wbf[wi[0] % 3]
        P.dma("sync", ws[:], wq[ci], [wq], [ws], ws)
        P.copy(wb[:], ws[:], [ws], [wb], eng=("gpsimd", "scalar")[wi[0] % 2])
        for (c0, n) in tiles:
            ps = PS()
            for k in range(8):
                P.mm(ps[:, :n], wb[:, k, :], hT[:, k, c0:c0 + n], k == 0, k == 7, [wb, hT], [ps])
            if masked:
                P.tt(dst[:, c0:c0 + n], ps[:, :n], Mt[:, c0:c0 + n], ALU.mult, [ps, Mt], [dst])
            else:
                P.copy(dst[:, c0:c0 + n], ps[:, :n], [ps], [dst], eng="scalar")

    def rstd_of(srcs, lhsT, dim, eps, dst, sqrt_only=False):
        tmp = slabs[12]
        for (c0, n) in tiles:
            ps = PS()
            for j, s in enumerate(srcs):
                P.act(tmp[:, c0:c0 + n], s[:, c0:c0 + n], AF.Square, [s], [tmp])
                P.mm(ps[:, :n], lhsT, tmp[:, c0:c0 + n], j == 0, j == len(srcs) - 1, [cs, tmp], [ps])
            P.act(dst[:, c0:c0 + n], ps[:, :n], AF.Sqrt, [ps], [dst], bias=eps, scale=1.0 / dim)
        if sqrt_only:
            P.ts(dst[:], dst[:], 1e-12, None, ALU.max, None, [dst], [dst])
        P.op("vector", lambda e: e.reciprocal(dst[:], dst[:]), [dst], [dst])

    def body(sub):
        D = lambda b, *idx: Buf(b.t[(sub,) + idx] if idx else b.t[sub], b.name + "_v")
        xT = D(xT_)
        o_qa, o_ka, o_va, o_n, o_r3, o_d3, o_gb = (D(o_qa_), D(o_ka_), D(o_va_), D(o_n_), D(o_r3_), D(o_d3_), D(o_gb_))
        P.dma("sync", Ct[:], ropeC_[sub], [ropeC_], [Ct], Ct)
        P.dma("sync", St[:], ropeS_[sub], [ropeS_], [St], St)
        P.dma("sync", Mt[:], mask_[sub], [mask_], [Mt], Mt)

        def out_dma(dram_ap, dram_buf, sb):
            P.dma("gpsimd", dram_ap, sb[:], [sb], [dram_buf], sb)

        for ti, (c0, n) in enumerate(tiles):
            P.dma("sync", x_[:, :, :n], xT[:, :, c0:c0 + n], [xT], [x_], x_)
            ps = PS()
            for k in range(8):
                s_ = sqt[k % 2]
                P.act(s_[:, :n], x_[:, k, :n], AF.Square, [x_], [s_])
                P.mm(ps[:, :n], ones, s_[:, :n], k == 0, k == 7, [cs, s_], [ps])
            P.act(rs[:, :n], ps[:, :n], AF.Sqrt, [ps], [rs], bias=1e-6, scale=1.0 / 1024)
            P.op("vector", lambda e, a=rs[:, :n]: e.reciprocal(a, a), [rs], [rs])
            for k in range(8):
                P.tt(x_[:, k, :n], x_[:, k, :n], rs[:, :n], ALU.mult, [x_, rs], [x_])
                for (a, b, w_, sh) in ((0, NL + 2, 0, 2), (NL + 2, NS, 1, 4)):
                    lo, hi = max(a, c0), min(b, c0 + n)
                    if lo < hi:
                        P.ts(hT[:, k, lo:hi], x_[:, k, lo - c0:hi - c0], At[:, w_, k:k + 1], nvt[:, sh, k:k + 1],
                             ALU.mult, ALU.add, [x_, At, nvt], [hT])

        cq = slabs[0:3]
        for j in range(3):
            proj(j, cq[j])
        r_ = slabs[3]
        rstd_of(cq, ones, 384.0, 1e-6, r_)
        cqn = [BS() for _ in range(3)]
        for j in range(3):
            P.stt(cqn[j][:], cq[j][:], spt[:, j:j + 1], r_[:], ALU.mult, ALU.mult, [cq[j], spt, r_], [cqn[j]])

        def head_finish(pre, gcol, dram_ap, dram_buf, ob):
            rr = slabs[4]
            rstd_of([pre], ones, 96.0, 1e-6, rr)
            P.stt(pre[:], pre[:], spt[:, gcol:gcol + 1], rr[:], ALU.mult, ALU.mult, [pre, spt, rr], [pre])
            rq = slabs[5]
            for (c0, n) in tiles:
                ps = PS()
                P.mm(ps[:, :n], rot, pre[:, c0:c0 + n], True, True, [cs, pre], [ps])
                P.tt(rq[:, c0:c0 + n], ps[:, :n], St[:, c0:c0 + n], ALU.mult, [ps, St], [rq])
            P.tt(pre[:], pre[:], Ct[:], ALU.mult, [pre, Ct], [pre])
            P.tt(ob[:], pre[:], rq[:], ALU.add, [pre, rq], [ob])
            out_dma(dram_ap, dram_buf, ob)

        obs = [slabs[8].t, slabs[9].t]
        qob = [P_q0, P_q1]
        for h in range(8):
            pre = slabs[6 + h % 2]
            for (c0, n) in tiles:
                ps = PS()
                for j in range(3):
                    P.mm(ps[:, :n], wuqb[:, j, h, :], cqn[j][:, c0:c0 + n], j == 0, j == 2, [wuqb, cqn[j]], [ps])
                P.copy(pre[:, c0:c0 + n], ps[:, :n], [ps], [pre], eng="scalar")
            head_finish(pre, 5, o_qa[h], o_qa, qob[h % 2])

        ckv = slabs[0:2]
        for j in range(2):
            proj(3 + j, ckv[j])
        krp = slabs[2]
        proj(5, krp)
        rstd_of(ckv, ones, 256.0, 1e-6, r_)
        ckvn = [BS() for _ in range(2)]
        for j in range(2):
            P.stt(ckvn[j][:], ckv[j][:], spt[:, 3 + j:4 + j], r_[:], ALU.mult, ALU.mult, [ckv[j], spt, r_], [ckvn[j]])
        for h in range(8):
            pre = slabs[6 + h % 2]
            for (c0, n) in tiles:
                ps = PS()
                for j in range(2):
                    P.mm(ps[:, :n], wukb[:, j, h, :], ckvn[j][:, c0:c0 + n], j == 0, j == 1, [wukb, ckvn[j]], [ps])
                P.tt(pre[:, c0:c0 + n], ps[:, :n], krp[:, c0:c0 + n], ALU.add, [ps, krp], [pre])
            head_finish(pre, 6, o_ka[h], o_ka, qob[h % 2])
        for c in range(4):
            ob = qob[c % 2]
            for (c0, n) in tiles:
                ps = PS()
                for j in range(2):
                    P.mm(ps[:, :n], wuvb[:, j, c, :], ckvn[j][:, c0:c0 + n], j == 0, j == 1, [wuvb, ckvn[j]], [ps])
                P.copy(ob[:, c0:c0 + n], ps[:, :n], [ps], [ob], eng="scalar")
            out_dma(o_va[c], o_va, ob)

        for which in range(3):
            for c in range(4):
                z = slabs[c % 2]
                proj(6 + which * 4 + c, z)
                ob = BS()
                if which < 2:
                    rr = slabs[4]
                    rstd_of([z], blk, 64.0, 1e-6, rr)
                    P.stt(ob[:], z[:], spt[:, 7 + which:8 + which], rr[:], ALU.mult, ALU.mult, [z, spt, rr], [ob])
                else:
                    P.copy(ob[:], z[:], [z], [ob])
                out_dma(o_n[which, c], o_n, ob)

        def shifted(ci_rw, dst, tmp):
            rwc = RW_ORDER[ci_rw]
            proj(18 + ci_rw, tmp, masked=True)
            P.ts(dst[:, 1:NS - 1], tmp[:, 1:NS - 1], m2[:, rwc:rwc + 1], None, ALU.mult, None, [tmp, m2], [dst])
            P.stt(dst[:, 1:NS - 1], tmp[:, 0:NS - 2], rwt[:, rwc:rwc + 1], dst[:, 1:NS - 1], ALU.mult, ALU.add,
                  [tmp, rwt, dst], [dst])
            P.stt(dst[:, 1:NS - 1], tmp[:, 2:NS], rwt[:, 15 + rwc:16 + rwc], dst[:, 1:NS - 1], ALU.mult, ALU.add,
                  [tmp, rwt, dst], [dst])
        tmp = slabs[11]
        wdT, adT, gdT = slabs[0], slabs[1], slabs[2]
        shifted(0, wdT, tmp); shifted(1, adT, tmp); shifted(2, gdT, tmp)
        P.act(wdT[:], wdT[:], AF.Tanh, [wdT], [wdT])
        P.act(gdT[:], gdT[:], AF.Sigmoid, [gdT], [gdT])
        for c in range(4):
            rT, kT, vT = slabs[3], slabs[4], slabs[5]
            shifted(3 + 3 * c, rT, tmp); shifted(4 + 3 * c, kT, tmp); shifted(5 + 3 * c, vT, tmp)
            P.dma("gpsimd", o_r3[0, c], rT[:], [rT], [o_r3], rT)
            P.dma("gpsimd", o_r3[1, c], vT[:], [vT], [o_r3], vT)
            kk = slabs[6]
            P.ts(kk[:], kT[:], rwt[:, 46 + c:47 + c], None, ALU.mult, None, [kT, rwt], [kk])
            rr = slabs[7]
            rstd_of([kk], blk, 1.0, 0.0, rr, sqrt_only=True)
            P.tt(kk[:], kk[:], rr[:], ALU.mult, [kk, rr], [kk])
            P.dma("gpsimd", o_r3[2, c], kk[:], [kk], [o_r3], kk)
            ksum = slabs[8]
            for d in range(2):
                lw, aa = slabs[9], slabs[10]
                pb = slice(d * 64, d * 64 + 64)
                for (c0, n) in tiles:
                    ps = PS()
                    P.mm(ps[:, :n], w2t[pb, c * 128:(c + 1) * 128], wdT[pb, c0:c0 + n], True, True, [w2t, wdT], [ps])
                    P.act(lw[:, c0:c0 + n], ps[:, :n], AF.Sigmoid, [ps, rwt], [lw],
                          bias=rwt[:, 30 + d * 4 + c:31 + d * 4 + c])
                    ps = PS()
                    P.mm(ps[:, :n], a2t[pb, c * 128:(c + 1) * 128], adT[pb, c0:c0 + n], True, True, [a2t, adT], [ps])
                    P.act(aa[:, c0:c0 + n], ps[:, :n], AF.Sigmoid, [ps, rwt], [aa],
                          bias=rwt[:, 38 + d * 4 + c:39 + d * 4 + c])
                P.ts(lw[:], lw[:], -float(np.exp(-0.5)), None, ALU.mult, None, [lw], [lw])
                P.dma("gpsimd", o_d3[0, d, c], lw[:], [lw], [o_d3], lw)
                bb = slabs[11]
                P.tt(bb[:], aa[:], kk[:], ALU.mult, [aa, kk], [bb])
                P.dma("gpsimd", o_d3[1, d, c], bb[:], [bb], [o_d3], bb)
                P.ts(aa[:], aa[:], -1.0, rwt[:, 50 + c:51 + c], ALU.add, ALU.mult, [aa, rwt], [aa])
                P.stt(aa[:], aa[:], 1.0, kT[:], ALU.add, ALU.mult, [aa, kT], [aa])
                P.dma("gpsimd", o_d3[2, d, c], aa[:], [aa], [o_d3], aa)
                if d == 0:
                    P.copy(ksum[:], aa[:], [aa], [ksum])
                else:
                    P.tt(ksum[:], ksum[:], aa[:], ALU.add, [ksum, aa], [ksum])
            P.stt(ksum[:], ksum[:], rwt[:, 54 + c:55 + c], rT[:], ALU.mult, ALU.mult, [ksum, rwt, rT], [ksum])
            gg, bo = slabs[9], slabs[10]
            for (c0, n) in tiles:
                ps = PS()
                P.mm(ps[:, :n], blk, ksum[:, c0:c0 + n], True, True, [cs, ksum], [ps])
                P.tt(bo[:, c0:c0 + n], ps[:, :n], vT[:, c0:c0 + n], ALU.mult, [ps, vT], [bo])
                ps = PS()
                P.mm(ps[:, :n], g2t[:, c * 128:(c + 1) * 128], gdT[:, c0:c0 + n], True, True, [g2t, gdT], [ps])
                P.copy(gg[:, c0:c0 + n], ps[:, :n], [ps], [gg], eng="scalar")
            P.dma("gpsimd", o_gb[0, c], gg[:], [gg], [o_gb], gg)
            P.dma("gpsimd", o_gb[1, c], bo[:], [bo], [o_gb], bo)

    P_q0 = P.sbuf([128, NS], BF16, "qo0"); P_q1 = P.sbuf([128, NS], BF16, "qo1")
    for sub in range(NSUB):
        body(sub)
    return P


def bf16(a):
    return np.asarray(a).astype(ml_dtypes.bfloat16)


def rope_tables(t0, NL, NS):
    C = np.ones((128, NS), np.float32)
    S = np.zeros((128, NS), np.float32)
    pos = t0 + np.arange(NL)
    rows, cols = (pos // 64).astype(np.float32), (pos % 64).astype(np.float32)
    fr = np.exp(-np.log(10000.0) * np.arange(8, dtype=np.float32) / 8).astype(np.float32)
    ar = rows[None, :] * fr[:, None]
    ac = cols[None, :] * fr[:, None]
    for base, ang in ((64, ar), (72, ar), (80, ac), (88, ac)):
        C[base:base + 8, 1:NL + 1] = np.cos(ang)
        S[base:base + 8, 1:NL + 1] = np.sin(ang)
    return C, S


def consts_L1():
    cst = np.zeros((128, 3, 128), np.float32)
    cst[:, 0, :] = 1.0
    cst[:64, 1, :64] = 1.0
    cst[64:, 1, 64:] = 1.0
    for i in range(8):
        cst[72 + i, 2, 64 + i] = -1.0
        cst[64 + i, 2, 72 + i] = 1.0
        cst[88 + i, 2, 80 + i] = -1.0
        cst[80 + i, 2, 88 + i] = 1.0
    return cst


def prep_L1_weights(inp, l, mod):
    W = inp["w_in"][l]
    cols = []
    z128 = np.zeros((1024, 128), np.float32)
    for j in range(3):
        cols.append(W[:, j * 128:(j + 1) * 128])
    for j in range(2):
        cols.append(W[:, 384 + j * 128:384 + (j + 1) * 128])
    kr = z128.copy(); kr[:, 64:96] = W[:, 640:672]; cols.append(kr)
    for j in range(12):
        cols.append(W[:, 672 + j * 128:672 + (j + 1) * 128])
    for j in RW_ORDER:
        cols.append(W[:, 2208 + j * 128:2208 + (j + 1) * 128])
    Wp = np.stack(cols, 0)
    wq = np.ascontiguousarray(Wp.reshape(33, 8, 128, 128).transpose(0, 2, 1, 3))
    nv = np.stack([fm(inp["norm1_g"][l]), mod[:, l, 8:16, 0], mod[:, l, 0:8, 0], mod[:, l, 8:16, 1], mod[:, l, 0:8, 1]], 1)
    sp = np.zeros((128, 16), np.float32)
    sp[:, 0:3] = fm(inp["mla_cq_g"][l]); sp[:, 3:5] = fm(inp["mla_ckv_g"][l])
    sp[:96, 5] = inp["mla_qn_g"][l]; sp[:96, 6] = inp["mla_kn_g"][l]
    sp[:, 7] = np.tile(inp["na_qn_g"][l], 2); sp[:, 8] = np.tile(inp["na_kn_g"][l], 2)
    wuq = np.zeros((384, 8, 128), np.float32)
    wuq[:, :, :96] = inp["mla_wuq"][l].reshape(384, 8, 96)
    wuq = np.ascontiguousarray(wuq.reshape(3, 128, 8, 128).transpose(1, 0, 2, 3))
    kv = inp["mla_wukv"][l].reshape(256, 8, 128)
    wuk = np.zeros((256, 8, 128), np.float32); wuk[:, :, :64] = kv[:, :, :64]
    wuk = np.ascontiguousarray(wuk.reshape(2, 128, 8, 128).transpose(1, 0, 2, 3))
    wuv = np.ascontiguousarray(kv[:, :, 64:].reshape(256, 4, 128).reshape(2, 128, 4, 128).transpose(1, 0, 2, 3))
    rwp = np.zeros((128, 58), np.float32)
    rwp[:, 0:15] = fm(inp["rw_mu"][l][0]); rwp[:, 15:30] = fm(inp["rw_mu"][l][1])
    for d in range(2):
        rwp[:, 30 + d * 4:34 + d * 4] = fm(inp["rw_w0"][l][d]); rwp[:, 38 + d * 4:42 + d * 4] = fm(inp["rw_a0"][l][d])
    rwp[:, 46:50] = fm(inp["rw_kk"][l]); rwp[:, 50:54] = fm(inp["rw_ka"][l]); rwp[:, 54:58] = fm(inp["rw_rk"][l].reshape(-1))
    return dict(nv=np.ascontiguousarray(nv, np.float32), wq=wq, sp=sp, wuq=wuq, wuk=wuk, wuv=wuv, rwp=rwp,
                w2=np.ascontiguousarray(inp["rw_w2"][l].reshape(128, 512)),
                a2=np.ascontiguousarray(inp["rw_a2"][l].reshape(128, 512)),
                g2=np.ascontiguousarray(inp["rw_g2"][l]), cst=consts_L1())


def run_L1(inp, l, mod, x, ctx, NL, NSUB):
    SEQ = x.shape[0]
    NS = NL + 260
    wts = prep_L1_weights(inp, l, mod)
    in_maps = []
    for i in range(NCORES):
        xs, Cs, Ss, Ms = [], [], [], []
        for s in range(NSUB):
            t0 = (i * NSUB + s) * NL
            slab = np.zeros((NS, 1024), np.float32)
            M = np.ones((128, NS), np.float32)
            if t0 > 0:
                slab[0] = x[t0 - 1]
            else:
                M[:, 0] = 0
            slab[1:NL + 1] = x[t0:t0 + NL]
            if t0 + NL < SEQ:
                slab[NL + 1] = x[t0 + NL]
            else:
                M[:, NL + 1] = 0
            M[:, NL + 2] = 0; M[:, NS - 1] = 0
            slab[NL + 3:NL + 259] = ctx
            xs.append(slab.T.reshape(8, 128, NS).transpose(1, 0, 2))
            C, S = rope_tables(t0, NL, NS)
            Cs.append(C); Ss.append(S); Ms.append(bf16(M))
        m = dict(wts)
        m.update(xT=np.ascontiguousarray(np.stack(xs, 0)), ropeC=np.stack(Cs, 0), ropeS=np.stack(Ss, 0), mask=np.stack(Ms, 0))
        in_maps.append(m)
    res = run(build_L1(NL, NSUB), in_maps)
    out = {}
    for key in ("qa", "ka", "va", "nqkv", "rw3", "rwd", "rwgb"):
        lat = np.concatenate([res[i][key][s][..., 1:NL + 1] for i in range(NCORES) for s in range(NSUB)], axis=-1)
        cx = res[0][key][0][..., NL + 3:NL + 259]
        out[key] = (np.asarray(lat), np.asarray(cx))
    return out


def na_plan(SEQ):
    NR = SEQ // 64
    NP_ = NR // 2
    tabs = {}
    tab_list = []
    plan = []
    qc = np.arange(64)
    cs_ = np.clip(qc - 8, 0, 48)
    for m in range(NP_):
        rows = [2 * m, 2 * m + 1]
        rs = [int(np.clip(r - 4, 0, NR - 8)) for r in rows]
        kps = sorted({(a + i) // 2 for a in rs for i in range(8)})
        ent = []
        for kp in kps:
            sig = (rs[0] - rows[0], rs[1] - rows[1], kp - m)
            if sig not in tabs:
                dr = np.full((128, 128), -1, np.int64)
                dc = np.full((128, 128), -1, np.int64)
                for kl in range(128):
                    krow, kcol = 2 * kp + kl // 64, kl % 64
                    for ql in range(128):
                        qrow, qcol = rows[ql // 64], ql % 64
                        a = rs[ql // 64]
                        if a <= krow < a + 8 and cs_[qcol] <= kcol < cs_[qcol] + 16:
                            dr[kl, ql] = krow - qrow + 7
                            dc[kl, ql] = kcol - qcol + 15
                tabs[sig] = len(tab_list)
                tab_list.append((dr, dc))
            ent.append((kp, tabs[sig]))
        plan.append(ent)
    return plan, tab_list


def build_L2(SEQ):
    NK = SEQ + 256
    NKB = NK // 128
    NCH = NKB
    plan, tab_list = na_plan(SEQ)
    NTAB = len(tab_list)
    P = Prog()
    I = lambda n, s, dt=F32: P.dram(n, s, dt, "ExternalInput")
    O = lambda n, s, dt=F32: P.dram(n, s, dt, "ExternalOutput")
    qa = I("qa", [128, SEQ + 256], BF16)
    ka = I("ka", [128, NK], BF16)
    va = I("va", [128, NKB, 65], BF16)
    nq = I("nq", [64, SEQ + 256], BF16)
    nk = I("nk", [64, NK], BF16)
    nvv = I("nv", [128, NKB, 65], BF16)
    tabs = I("tabs", [128, NTAB, 128])
    cst = I("cst", [128, 6, 128])
    rtm = I("rtm", [2, NCH, 128, 4, 64])
    rfm = I("rfm", [2, NCH, 64, 4, 128])
    o_ya = O("ya", [64, SEQ + 256]); o_yb = O("yb", [64, SEQ + 256])
    o_y = O("y", [2, NCH, 128, 64])

    psb = [P.psum([128, 512], F32, f"psb{i}") for i in range(4)]
    psS2 = [P.psum([128, 1024], F32, f"psS{i}") for i in range(2)]
    cs = P.sbuf([128, 6, 128], F32, "cs")
    P.dma("sync", cs[:], cst[:], [cst], [cs], cs)
    triI, triS, triL, ident, ones = (cs[:, i, :] for i in range(5))
    csb = P.sbuf([128, 2, 128], BF16, "csb")
    P.copy(csb[:, 0, :], cs[:, 3, :], [cs], [csb])
    P.copy(csb[:, 1, :], cs[:, 4, :], [cs], [csb])

    pT = [P.sbuf([128, 1024], BF16, f"pT{i}") for i in range(3)]
    osb = [P.sbuf([64, 512], F32, f"osb{i}") for i in range(2)]
    rD = P.sbuf([64, 512], F32, "rD")
    dsb = P.sbuf([128, 512], F32, "dsb")
    cnt = [0]

    def finish_od(psOD, n, out_d, out_c0):
        psB = psb[2]
        P.copy(dsb[64:65, :n], psOD[64:65, :n], [psOD], [dsb], eng="scalar")
        P.mm(psB[0:64, :n], cs[64:65, 4, 0:64], dsb[64:65, :n], True, True, [cs, dsb], [psB])
        P.op("vector", lambda e: e.reciprocal(rD[:, :n], psB[0:64, :n]), [psB], [rD])
        ob = osb[cnt[0] % 2]
        P.tt(ob[:, :n], psOD[0:64, :n], rD[:, :n], ALU.mult, [psOD, rD], [ob])
        P.dma("gpsimd", out_d[:, out_c0:out_c0 + n], ob[:, :n], [ob], [out_d], ob)

    def attn(qT, q0, n, kT, V, kblocks, KD, scale, out_d, out_c0):
        cnt[0] += 1
        psOD = psb[cnt[0] % 2]
        pairs = [kblocks[i:i + 2] for i in range(0, len(kblocks), 2)]
        npair = len(pairs)

        def S(p):
            pS = psS2[p % 2]
            for j, kb in enumerate(pairs[p]):
                P.mm(pS[:, j * 512:j * 512 + n], kT[0:KD, kb * 128:(kb + 1) * 128], qT[0:KD, q0:q0 + n], True, True, [kT, qT], [pS])
        S(0)
        for p, pr in enumerate(pairs):
            if p + 1 < npair:
                S(p + 1)
            pS = psS2[p % 2]
            p_ = pT[p % 3]
            L = len(pr)
            sv = pS[:, :].rearrange("p (j c) -> p j c", c=512)[:, 0:L, 0:n]
            dv = p_[:, :].rearrange("p (j c) -> p j c", c=512)[:, 0:L, 0:n]
            P.act(dv, sv, AF.Exp, [pS], [p_], scale=scale)
            for j, kb in enumerate(pr):
                P.mm(psOD[0:65, :n], V[:, kb, :], p_[:, j * 512:j * 512 + n], p == 0 and j == 0,
                     p == npair - 1 and j == L - 1, [V, p_], [psOD])
        finish_od(psOD, n, out_d, out_c0)

    qat = P.sbuf([128, SEQ + 256], BF16, "qat"); kat = P.sbuf([128, NK], BF16, "kat"); vat = P.sbuf([128, NKB, 65], BF16, "vat")
    for (t, d) in ((qat, qa), (kat, ka), (vat, va)):
        P.dma("sync", t[:], d[:], [d], [t], t)
    sc_a = float(96 ** -0.5)
    for q0 in range(0, SEQ, 512):
        attn(qat, q0, min(512, SEQ - q0), kat, vat, list(range(NKB)), 128, sc_a, o_ya, q0)
    attn(qat, SEQ, 256, kat, vat, [0, 1], 128, sc_a, o_ya, SEQ)

    nqt, nkt, nvt = qat, kat, vat
    P.dma("sync", nqt[0:64, :], nq[:], [nq], [nqt], nqt)
    P.dma("sync", nkt[0:64, :], nk[:], [nk], [nkt], nkt)
    P.dma("sync", nvt[:], nvv[:], [nvv], [nvt], nvt)
    tb32 = P.sbuf([128, NTAB, 128], F32, "tb32"); tbb = P.sbuf([128, NTAB, 128], BF16, "tbb")
    P.dma("sync", tb32[:], tabs[:], [tabs], [tb32], tb32)
    P.ts(tbb[:], tb32[:], 8.0, None, ALU.mult, None, [tb32], [tbb])
    sc_b = 0.125
    for m, ent in enumerate(plan):
        blocks = [(kp, tid) for (kp, tid) in ent] + [(NKB - 2, None), (NKB - 1, None)]
        cnt[0] += 1
        psOD = psb[cnt[0] % 2]
        p_ = pT[cnt[0] % 3]
        pS = psS2[cnt[0] % 2]
        qsl = nqt[0:64, m * 128:(m + 1) * 128]
        for j, (kb, tid) in enumerate(blocks):
            o_ = pS[:, j * 128:(j + 1) * 128]
            P.mm(o_, nkt[0:64, kb * 128:(kb + 1) * 128], qsl, True, tid is None, [nkt, nqt], [pS])
            if tid is not None:
                P.mm(o_, csb[:, 0, :], tbb[:, tid, :], False, True, [csb, tbb], [pS])
        w = len(blocks) * 128
        P.act(p_[:, :w], pS[:, :w], AF.Exp, [pS], [p_], scale=sc_b)
        for j, (kb, tid) in enumerate(blocks):
            P.mm(psOD[0:65, :128], nvt[:, kb, :], p_[:, j * 128:(j + 1) * 128], j == 0, j == len(blocks) - 1, [nvt, p_], [psOD])
        finish_od(psOD, 128, o_yb, m * 128)
    attn(nqt, SEQ, 256, nkt, nvt, [NKB - 2, NKB - 1], 64, sc_b, o_yb, SEQ)

    pi = [0]
    rw_ps = psb + psS2
    def PS():
        pi[0] += 1
        return rw_ps[pi[0] % 6]
    NSET = 4
    W = lambda n, shape=(128, 128): [P.sbuf(list(shape), F32, f"{n}{i}") for i in range(NSET)]
    tmb, fmb = W("tm", (128, 4, 64)), W("fmj", (64, 4, 128))
    eLr = W("eLr", (128, 64)); Bh = W("Bh", (128, 64)); Kh = W("Kh", (128, 64))
    e1 = W("e1", (64, 128)); e2 = W("e2", (64, 128)); e3 = W("e3", (64, 128))
    Rt = W("Rt", (64, 128)); KKt = W("KKt", (64, 128)); Bt = W("Bt", (64, 128)); Kt = W("Kt", (64, 128))
    Nn = W("Nn"); NTt = W("NTt"); Mk = W("Mk"); Mbp = W("Mbp"); Mkp = W("Mkp")
    Pq = W("Pq"); PqT = W("PqT"); Tm = W("Tm")
    Zs = W("Zs", (128, 64)); nU = W("nU", (128, 64)); Ys = W("Ys", (128, 64))
    gC = W("gC", (64, 1))
    ST = [P.sbuf([64, 64], F32, f"ST{d}") for d in range(2)]
    for d in range(2):
        P.memset(ST[d][:], 0.0, [ST[d]])

    def chunk_gen(c, d, s):
        tm, fj = tmb[s], fmb[s]
        P.dma("sync", tm[:], rtm[d, c], [rtm], [tm], tm)
        P.dma("sync", fj[:], rfm[d, c], [rfm], [fj], fj)
        yield
        lw_tok = tm[:, 0, :]
        ps = PS(); P.mm(ps[:, 0:64], triL, lw_tok, True, True, [cs, tm], [ps])
        P.act(eLr[s][:], ps[:, 0:64], AF.Exp, [ps], [eLr[s]])
        yield
        ps = PS(); P.mm(ps[0:64, 0:128], lw_tok, triI, True, True, [tm, cs], [ps])
        P.act(e1[s][:], ps[0:64, 0:128], AF.Exp, [ps], [e1[s]])
        P.act(e2[s][:], ps[0:64, 0:128], AF.Exp, [ps], [e2[s]], scale=-1.0)
        yield
        ps = PS(); P.mm(ps[0:64, 0:128], lw_tok, triS, True, True, [tm, cs], [ps])
        P.act(e3[s][:], ps[0:64, 0:128], AF.Exp, [ps], [e3[s]])
        yield
        P.tt(Bh[s][:], tm[:, 1, :], eLr[s][:], ALU.mult, [tm, eLr[s]], [Bh[s]], eng="gpsimd")
        P.tt(Kh[s][:], tm[:, 2, :], eLr[s][:], ALU.mult, [tm, eLr[s]], [Kh[s]], eng="gpsimd")
        P.copy(gC[s][:], e1[s][:, 127:128], [e1[s]], [gC[s]], eng="gpsimd")
        P.tt(Bt[s][:], fj[:, 0, :], e2[s][:], ALU.mult, [fj, e2[s]], [Bt[s]])
        P.tt(Kt[s][:], fj[:, 1, :], e2[s][:], ALU.mult, [fj, e2[s]], [Kt[s]], eng="gpsimd")
        P.tt(KKt[s][:], fj[:, 2, :], e3[s][:], ALU.mult, [fj, e3[s]], [KKt[s]])
        P.tt(Rt[s][:], fj[:, 3, :], e1[s][:], ALU.mult, [fj, e1[s]], [Rt[s]], eng="gpsimd")
        yield
        for (dst, l_, r_, msk) in ((Nn[s], Bt[s], KKt[s], triS), (NTt[s], KKt[s], Bt[s], triL),
                                  (Mk[s], Kt[s], KKt[s], triS), (Mbp[s], Bt[s], Rt[s], triI), (Mkp[s], Kt[s], Rt[s], triI)):
            ps = PS(); P.mm(ps[:, 0:128], l_[:], r_[:], True, True, [l_, r_], [ps])
            P.tt(dst[:], ps[:, 0:128], msk, ALU.mult, [ps, cs], [dst])
            yield
        P.tt(Tm[s][:], ident, Nn[s][:], ALU.subtract, [cs, Nn[s]], [Tm[s]])
        Pc, PcT = Nn[s], NTt[s]
        for it in range(6):
            nxt, nxtT = (Pq[s], PqT[s]) if Pc is not Pq[s] else (Nn[s], NTt[s])
            ps = PS(); P.mm(ps[:, 0:128], Pc[:], PcT[:], True, True, [Pc, PcT], [ps])
            P.copy(nxtT[:], ps[:, 0:128], [ps], [nxtT], eng="scalar")
            if it < 5:
                ps = PS(); P.mm(ps[:, 0:128], PcT[:], Pc[:], True, True, [Pc, PcT], [ps])
                P.copy(nxt[:], ps[:, 0:128], [ps], [nxt])
            yield
            ps = PS(); P.mm(ps[:, 0:128], nxtT[:], Tm[s][:], True, True, [nxtT, Tm[s]], [ps])
            P.tt(Tm[s][:], Tm[s][:], ps[:, 0:128], ALU.add, [Tm[s], ps], [Tm[s]])
            Pc, PcT = nxt, nxtT
            yield
        vt = tm[:, 3, :]
        ps = PS()
        P.mm(ps[:, 0:64], KKt[s][:], ST[d][:], True, False, [KKt[s], ST[d]], [ps])
        P.mm(ps[:, 0:64], Mk[s][:], vt, False, True, [Mk[s], tm], [ps])
        P.copy(Zs[s][:], ps[:, 0:64], [ps], [Zs[s]])
        yield
        ps = PS(); P.mm(ps[:, 0:64], Tm[s][:], Zs[s][:], True, True, [Tm[s], Zs[s]], [ps])
        P.ts(nU[s][:], ps[:, 0:64], -1.0, None, ALU.mult, None, [ps], [nU[s]])
        yield
        ps2 = PS()
        P.mm(ps2[0:64, 0:64], Bh[s][:], nU[s][:], True, False, [Bh[s], nU[s]], [ps2])
        P.mm(ps2[0:64, 0:64], Kh[s][:], vt, False, True, [Kh[s], tm], [ps2])
        ps = PS()
        P.mm(ps[:, 0:64], Rt[s][:], ST[d][:], True, False, [Rt[s], ST[d]], [ps])
        P.mm(ps[:, 0:64], Mbp[s][:], nU[s][:], False, False, [Mbp[s], nU[s]], [ps])
        P.mm(ps[:, 0:64], Mkp[s][:], vt, False, True, [Mkp[s], tm], [ps])
        P.stt(ST[d][:], ST[d][:], gC[s][:, 0:1], ps2[0:64, 0:64], ALU.mult, ALU.add, [ST[d], gC[s], ps2], [ST[d]])
        P.copy(Ys[s][:], ps[:, 0:64], [ps], [Ys[s]], eng="scalar")
        P.dma("gpsimd", o_y[d, c], Ys[s][:], [Ys[s]], [o_y], Ys[s])
        yield

    tasks = [(c, d) for c in range(NCH) for d in range(2)]
    active = []
    nxt_task = 0
    rounds = 0
    while nxt_task < len(tasks) or active:
        if nxt_task < len(tasks) and len(active) < NSET and (rounds % 7 == 0 or not active):
            c, d = tasks[nxt_task]
            active.append(chunk_gen(c, d, nxt_task % NSET))
            nxt_task += 1
        rounds += 1
        for g in list(active):
            try:
                next(g)
            except StopIteration:
                active.remove(g)
    return P


def consts_L2():
    c = np.zeros((128, 6, 128), np.float32)
    i = np.arange(128)
    c[:, 0, :] = (i[:, None] <= i[None, :])
    c[:, 1, :] = (i[:, None] < i[None, :])
    c[:, 2, :] = (i[:, None] > i[None, :])
    c[:, 3, :] = np.eye(128)
    c[:, 4, :] = 1.0
    return c


def tokmaj(a, aug=False):
    n = a.shape[1]
    t = a.T.reshape(n // 128, 128, 64).transpose(1, 0, 2)
    if aug:
        t = np.concatenate([t, np.ones((128, n // 128, 1), t.dtype)], axis=2)
    return np.ascontiguousarray(t)


def run_L2(inp, l, o1, SEQ):
    NK = SEQ + 256
    NCH = NK // 128
    plan, tab_list = na_plan(SEQ)
    cst = consts_L2()
    in_maps = []
    f32 = lambda a: np.asarray(a, np.float32)
    hs = lambda pair, h: (pair[0].reshape(-1, pair[0].shape[-1])[h * 64:(h + 1) * 64],
                          pair[1].reshape(-1, 256)[h * 64:(h + 1) * 64])
    for h in range(NCORES):
        m = {"cst": cst}
        m["qa"] = np.ascontiguousarray(np.concatenate([o1["qa"][0][h], o1["qa"][1][h]], 1))
        m["ka"] = np.ascontiguousarray(np.concatenate([o1["ka"][1][h], o1["ka"][0][h]], 1))
        vl, vc = hs(o1["va"], h)
        m["va"] = tokmaj(np.concatenate([vc, vl], 1), aug=True)
        n_l, n_c = o1["nqkv"]
        sel = lambda w: (n_l[w].reshape(512, -1)[h * 64:(h + 1) * 64], n_c[w].reshape(512, 256)[h * 64:(h + 1) * 64])
        m["nq"] = np.ascontiguousarray(np.concatenate(sel(0), 1))
        m["nk"] = np.ascontiguousarray(np.concatenate(sel(1), 1))
        m["nv"] = tokmaj(np.concatenate(sel(2), 1), aug=True)
        rpb = inp["na_rpb"][l][h]
        tb = np.stack([np.where(dr >= 0, rpb[np.maximum(dr, 0), np.maximum(dc, 0)], np.float32(-30000.0)) for dr, dc in tab_list], 0)
        m["tabs"] = np.ascontiguousarray(tb.transpose(1, 0, 2).astype(np.float32))
        r3l, r3c = o1["rw3"]; rdl, rdc = o1["rwd"]
        def seqs(lat, cx, d):
            lat = lat.reshape(512, -1)[h * 64:(h + 1) * 64]; cx = cx.reshape(512, 256)[h * 64:(h + 1) * 64]
            return np.concatenate([cx, lat], 1) if d == 0 else np.concatenate([cx[:, ::-1], lat[:, ::-1]], 1)
        rtm = np.zeros((2, NCH, 128, 4, 64), np.float32); rfm = np.zeros((2, NCH, 64, 4, 128), np.float32)
        for d in range(2):
            lw = seqs(rdl[0, d], rdc[0, d], d); b_ = seqs(rdl[1, d], rdc[1, d], d); kd = seqs(rdl[2, d], rdc[2, d], d)
            r_ = seqs(r3l[0], r3c[0], d); v_ = seqs(r3l[1], r3c[1], d); kk = seqs(r3l[2], r3c[2], d)
            for j, a in enumerate((lw, b_, kd, v_)):
                rtm[d, :, :, j, :] = a.T.reshape(NCH, 128, 64)
            for j, a in enumerate((b_, kd, kk, r_)):
                rfm[d, :, :, j, :] = a.reshape(64, NCH, 128).transpose(1, 0, 2)
        m["rtm"] = rtm; m["rfm"] = rfm
        in_maps.append(m)
    res = run(build_L2(SEQ), in_maps)
    ya = np.concatenate([f32(res[h]["ya"]) for h in range(NCORES)], 0)
    yb = np.concatenate([f32(res[h]["yb"]) for h in range(NCORES)], 0)
    ys = []
    for d in range(2):
        yy = np.concatenate([f32(res[h]["y"][d]).reshape(NK, 64).T for h in range(NCORES)], 0)
        cx, lat = yy[:, :256], yy[:, 256:]
        if d == 1:
            cx, lat = cx[:, ::-1], lat[:, ::-1]
        ys.append((np.ascontiguousarray(lat), np.ascontiguousarray(cx)))
    return dict(ya=(ya[:, :SEQ], ya[:, SEQ:]), yb=(yb[:, :SEQ], yb[:, SEQ:]), yf=ys[0], ybk=ys[1])


def build_L3(NT, NLAT, moe):
    FC = 28 if moe else 22
    NE = 8 if moe else 1
    tiles = [(c0, min(512, NT - c0)) for c0 in range(0, NT, 512)]
    P = Prog()
    I = lambda n, s, dt=F32: P.dram(n, s, dt, "ExternalInput")
    xT = I("xT", [128, 8, NT])
    yin = I("yin", [6, 128, 4, NT])
    nv = I("nv", [128, 14, 8])
    rwo = I("rwo", [128, 2, 4])
    wg = I("wg", [24, 128, 8, 128]); wo = I("wo", [3, 8, 128, 4, 128]); wout = I("wout", [8, 128, 8, 128])
    if not moe:
        w1 = I("w1", [NE, FC, 128, 8, 128]); w3 = I("w3", [NE, FC, 128, 8, 128]); w2 = I("w2", [NE, 8, 128, FC, 128])
    else:
        h2o = P.dram("h2o", [128, 8, NT], BF16, "ExternalOutput"); gTo = P.dram("gTo", [8, NT], F32, "ExternalOutput")
    cst = I("cst", [128, 3, 128])
    if moe:
        rt = I("rt", [128, 8, 8]); sel = I("sel", [8, 8, 128])
    out = P.dram("out", [128, 8, NT], F32, "ExternalOutput")

    def load(d, shape, dt=F32, name=None):
        t = P.sbuf(shape, dt, name)
        P.dma("sync", t[:], d[:], [d], [t], t)
        return t
    nvt = load(nv, [128, 14, 8]); rwt = load(rwo, [128, 2, 4]); cs = load(cst, [128, 3, 128])
    ones, blk, ident = cs[:, 0, :], cs[:, 1, :], cs[:, 2, :]
    if moe:
        rtt = load(rt, [128, 8, 8]); selt = load(sel, [8, 8, 128])
    A1 = P.sbuf([128, 2, 8], F32); A2 = P.sbuf([128, 2, 8], F32)
    for w_, sc in ((0, 1), (1, 3)):
        P.stt(A1[:, w_, :], nvt[:, sc, :], 1.0, nvt[:, 0, :], ALU.add, ALU.mult, [nvt], [A1])
    for w_, sc in ((0, 8), (1, 10)):
        P.stt(A2[:, w_, :], nvt[:, sc, :], 1.0, nvt[:, 7, :], ALU.add, ALU.mult, [nvt], [A2])

    psb = [P.psum([128, 512], F32, f"psb{i}") for i in range(8)]
    pi = [0]
    def PS():
        pi[0] += 1
        return psb[pi[0] % 8]
    x_ = P.sbuf([128, 8, 512], F32, "x"); hT = P.sbuf([128, 8, 512], BF16, "hT"); G = P.sbuf([128, 24, 512], BF16, "G")
    stg = [P.sbuf([128, 4, 512], F32, f"stg{i}") for i in range(2)]
    ybf = [P.sbuf([128, 4, 512], BF16, f"ybf{i}") for i in range(3)]
    ysum = P.sbuf([128, 4, 512], F32, "ysum"); tmp = P.sbuf([128, 4, 512], F32, "tmp")
    Mb = P.sbuf([128, 8, 512], BF16, "Mb"); Mo = P.sbuf([128, 512], F32, "Mo"); t5 = P.sbuf([128, 512], F32, "t5")
    h2 = P.sbuf([128, 8, 512], BF16, "h2"); hid = P.sbuf([128, 1 if moe else FC, 512], BF16, "hid")
    sq = [P.sbuf([128, 512], F32, f"sq{i}") for i in range(2)]; rs = P.sbuf([128, 512], F32, "rs")
    wst = [P.sbuf([128, 8, 128], F32, f"wst{i}") for i in range(6)]
    wbf = [P.sbuf([128, 8, 128], BF16, f"wbf{i}") for i in range(7)]
    wi = [0]
    if moe:
        lgT = P.sbuf([8, 512], F32, "lgT"); gT = P.sbuf([8, 512], F32, "gT"); gbe = P.sbuf([128, 512], F32, "gbe")
        lg = P.sbuf([128, 8], F32, "lg"); top = P.sbuf([128, 8], F32, "top"); sm = P.sbuf([128, 8], F32, "sm")
        gt_ = P.sbuf([128, 8], F32, "gt"); g2_ = P.sbuf([128, 8], F32, "g2")

    def wpiece(ap, dbuf, kc=8):
        wi[0] += 1
        ws, wb = wst[wi[0] % 6], wbf[wi[0] % 7]
        P.dma("sync", ws[:, :kc, :], ap, [dbuf], [ws], ws)
        P.copy(wb[:, :kc, :], ws[:, :kc, :], [ws], [wb], eng=("gpsimd", "scalar", "vector", "scalar")[wi[0] % 4])
        return wb

    def segs(c0, n):
        for (a, b, w_) in ((0, NLAT, 0), (NLAT, NT, 1)):
            lo, hi = max(a, c0), min(b, c0 + n)
            if lo < hi:
                yield lo - c0, hi - c0, w_

    def norm_mod(src, dst, A, shl, shc, n, c0, extra=None):
        ps = PS()
        for k in range(8):
            s_ = sq[k % 2]
            P.act(s_[:, :n], src[:, k, :n], AF.Square, [src], [s_])
            P.mm(ps[:, :n], ones, s_[:, :n], k == 0, k == 7, [cs, s_], [ps])
        P.act(rs[:, :n], ps[:, :n], AF.Sqrt, [ps], [rs], bias=1e-6, scale=1.0 / 1024)
        P.op("vector", lambda e: e.reciprocal(rs[:, :n], rs[:, :n]), [rs], [rs])
        for k in range(8):
            s_ = sq[k % 2]
            P.tt(s_[:, :n], src[:, k, :n], rs[:, :n], ALU.mult, [src, rs], [s_])
            for (lo, hi, w_) in segs(c0, n):
                P.ts(dst[:, k, lo:hi], s_[:, lo:hi], A[:, w_, k:k + 1], nvt[:, (shl, shc)[w_], k:k + 1],
                     ALU.mult, ALU.add, [s_, A, nvt], [dst])
            if extra is not None:
                extra(k, s_)

    for (c0, n) in tiles:
        P.dma("sync", x_[:, :, :n], xT[:, :, c0:c0 + n], [xT], [x_], x_)
        norm_mod(x_, hT, A1, 2, 4, n, c0)
        for j in range(24):
            wb = wpiece(wg[j], wg)
            ps = PS()
            for k in range(8):
                P.mm(ps[:, :n], wb[:, k, :], hT[:, k, :n], k == 0, k == 7, [wb, hT], [ps])
            P.act(G[:, j, :n], ps[:, :n], AF.Sigmoid, [ps], [G])
        for b in range(2):
            s_ = stg[b % 2]
            P.dma("sync", s_[:, :, :n], yin[b][:, :, c0:c0 + n], [yin], [s_], s_)
            P.copy(ybf[b][:, :, :n], s_[:, :, :n], [s_], [ybf[b]])
        sa, sb_ = stg[0], stg[1]
        P.dma("sync", sa[:, :, :n], yin[2][:, :, c0:c0 + n], [yin], [sa], sa)
        P.dma("sync", sb_[:, :, :n], yin[3][:, :, c0:c0 + n], [yin], [sb_], sb_)
        P.tt(ysum[:, :, :n], sa[:, :, :n], sb_[:, :, :n], ALU.add, [sa, sb_], [ysum])
        P.dma("sync", sa[:, :, :n], yin[4][:, :, c0:c0 + n], [yin], [sa], sa)
        P.dma("sync", sb_[:, :, :n], yin[5][:, :, c0:c0 + n], [yin], [sb_], sb_)
        for c in range(4):
            ps = PS(); P.mm(ps[:, :n], blk, ysum[:, c, :n], True, True, [cs, ysum], [ps])
            P.stt(ysum[:, c, :n], ps[:, :n], -1.0 / 64, ysum[:, c, :n], ALU.mult, ALU.add, [ps, ysum], [ysum])
            P.act(tmp[:, c, :n], ysum[:, c, :n], AF.Square, [ysum], [tmp])
            ps = PS(); P.mm(ps[:, :n], blk, tmp[:, c, :n], True, True, [cs, tmp], [ps])
            P.act(tmp[:, c, :n], ps[:, :n], AF.Sqrt, [ps], [tmp], bias=64e-5, scale=1.0 / 64)
            P.op("vector", lambda e, c=c, n=n: e.reciprocal(tmp[:, c, :n], tmp[:, c, :n]), [tmp], [tmp])
            P.tt(ysum[:, c, :n], ysum[:, c, :n], tmp[:, c, :n], ALU.mult, [ysum, tmp], [ysum])
            P.ts(ysum[:, c, :n], ysum[:, c, :n], rwt[:, 0, c:c + 1], rwt[:, 1, c:c + 1], ALU.mult, ALU.add, [ysum, rwt], [ysum])
            P.tt(ysum[:, c, :n], ysum[:, c, :n], sb_[:, c, :n], ALU.add, [ysum, sb_], [ysum])
            P.tt(ybf[2][:, c, :n], ysum[:, c, :n], sa[:, c, :n], ALU.mult, [ysum, sa], [ybf[2]])
        for oc in range(8):
            for br in range(3):
                wb = wpiece(wo[br, oc], wo, 4)
                ps = PS()
                for k in range(4):
                    P.mm(ps[:, :n], wb[:, k, :], ybf[br][:, k, :n], k == 0, k == 3, [wb, ybf[br]], [ps])
                if br == 0:
                    P.tt(Mo[:, :n], ps[:, :n], G[:, oc, :n], ALU.mult, [ps, G], [Mo])
                else:
                    P.tt(t5[:, :n], ps[:, :n], G[:, br * 8 + oc, :n], ALU.mult, [ps, G], [t5])
                    P.tt(Mo[:, :n], Mo[:, :n], t5[:, :n], ALU.add, [Mo, t5], [Mo])
            P.copy(Mb[:, oc, :n], Mo[:, :n], [Mo], [Mb], eng="scalar")
        for oc in range(8):
            wb = wpiece(wout[oc], wout)
            ps = PS()
            for k in range(8):
                P.mm(ps[:, :n], wb[:, k, :], Mb[:, k, :n], k == 0, k == 7, [wb, Mb], [ps])
            for (lo, hi, w_) in segs(c0, n):
                P.stt(x_[:, oc, lo:hi], ps[:, lo:hi], nvt[:, 5 + w_, oc:oc + 1], x_[:, oc, lo:hi], ALU.mult, ALU.add,
                      [ps, nvt, x_], [x_])
        if moe:
            psr = PS()
            def extra(k, s_):
                for (lo, hi, w_) in segs(c0, n):
                    P.ts(t5[:, lo:hi], s_[:, lo:hi], A2[:, w_, k:k + 1], nvt[:, (9, 11)[w_], k:k + 1],
                         ALU.mult, ALU.add, [s_, A2, nvt], [t5])
                P.mm(psr[0:8, :n], rtt[:, k, :], t5[:, :n], k == 0, k == 7, [rtt, t5], [psr])
            norm_mod(x_, h2, A2, 9, 11, n, c0, extra)
            P.copy(lgT[:, :n], psr[0:8, :n], [psr], [lgT])
            for b0 in range(0, n, 128):
                ps = PS(); P.transpose(ps[:, 0:8], lgT[:, b0:b0 + 128], cs[0:8, 2, 0:8], [lgT, cs], [ps])
                P.copy(lg[:], ps[:, 0:8], [ps], [lg])
                P.op("vector", lambda e: e.max(top[:], lg[:]), [lg], [top])
                P.ts(sm[:, 0:1], top[:, 0:1], -1.0, None, ALU.mult, None, [top], [sm])
                P.act(sm[:, 1:2], top[:, 1:2], AF.Exp, [top, sm], [sm], bias=sm[:, 0:1])
                P.ts(sm[:, 2:3], sm[:, 1:2], 1.0, None, ALU.add, None, [sm], [sm])
                P.op("vector", lambda e: e.reciprocal(sm[:, 2:3], sm[:, 2:3]), [sm], [sm])
                P.tt(sm[:, 3:4], sm[:, 1:2], sm[:, 2:3], ALU.mult, [sm], [sm])
                P.ts(gt_[:], lg[:], top[:, 0:1], sm[:, 2:3], ALU.is_equal, ALU.mult, [lg, top, sm], [gt_])
                P.ts(g2_[:], lg[:], top[:, 1:2], sm[:, 3:4], ALU.is_equal, ALU.mult, [lg, top, sm], [g2_])
                P.tt(gt_[:], gt_[:], g2_[:], ALU.add, [gt_, g2_], [gt_])
                ps = PS(); P.transpose(ps[0:8, 0:128], gt_[:], ident, [gt_, cs], [ps])
                P.copy(gT[:, b0:b0 + 128], ps[0:8, 0:128], [ps], [gT])
        else:
            norm_mod(x_, h2, A2, 9, 11, n, c0)
        if moe:
            P.dma("gpsimd", h2o[:, :, c0:c0 + n], h2[:, :, :n], [h2], [h2o], h2)
            P.dma("gpsimd", gTo[:, c0:c0 + n], gT[:, :n], [gT], [gTo], gT)
        for e_ in range(0 if moe else NE):
            if moe:
                ps = PS(); P.mm(ps[:, :n], selt[:, e_, :], gT[:, :n], True, True, [selt, gT], [ps])
                P.copy(gbe[:, :n], ps[:, :n], [ps], [gbe], eng="scalar")
            for fc in range(FC):
                wb1 = wpiece(w1[e_, fc], w1)
                ps1 = PS()
                for k in range(8):
                    P.mm(ps1[:, :n], wb1[:, k, :], h2[:, k, :n], k == 0, k == 7, [wb1, h2], [ps1])
                wb3 = wpiece(w3[e_, fc], w3)
                ps3 = PS()
                for k in range(8):
                    P.mm(ps3[:, :n], wb3[:, k, :], h2[:, k, :n], k == 0, k == 7, [wb3, h2], [ps3])
                P.act(t5[:, :n], ps1[:, :n], AF.Silu, [ps1], [t5])
                if moe:
                    P.tt(t5[:, :n], t5[:, :n], gbe[:, :n], ALU.mult, [t5, gbe], [t5])
                P.tt(hid[:, fc, :n], t5[:, :n], ps3[:, :n], ALU.mult, [t5, ps3], [hid])
            for oc in range(8):
                ps = PS()
                for k0 in range(0, FC, 8):
                    kc = min(8, FC - k0)
                    wb = wpiece(w2[e_, oc][:, k0:k0 + kc, :], w2, kc)
                    for k in range(kc):
                        P.mm(ps[:, :n], wb[:, k, :], hid[:, k0 + k, :n], k0 + k == 0, k0 + k == FC - 1, [wb, hid], [ps])
                for (lo, hi, w_) in segs(c0, n):
                    P.stt(x_[:, oc, lo:hi], ps[:, lo:hi], nvt[:, 12 + w_, oc:oc + 1], x_[:, oc, lo:hi], ALU.mult, ALU.add,
                          [ps, nvt, x_], [x_])
        P.dma("gpsimd", out[:, :, c0:c0 + n], x_[:, :, :n], [x_], [out], x_)
    return P


def build_L4(NT):
    FC, QC = 28, 7
    tiles = [(c0, min(512, NT - c0)) for c0 in range(0, NT, 512)]
    P = Prog()
    I = lambda n, s, dt=F32: P.dram(n, s, dt, "ExternalInput")
    xm = I("xm", [128, 8, NT]); h2d = I("h2", [128, 8, NT], BF16); gTd = I("gT", [8, NT]); gt2d = I("gt2", [128, 8])
    seld = I("sel", [8, 8, 128])
    w1 = I("w1", [8, FC, 128, 8, 128]); w3 = I("w3", [8, FC, 128, 8, 128]); w2 = I("w2", [8, 8, 128, FC, 128])
    out = P.dram("out", [128, 8, NT], F32, "ExternalOutput")
    XM = P.sbuf([128, 8, NT], F32, "XM"); H2 = P.sbuf([128, 8, NT], BF16, "H2"); GT = P.sbuf([8, NT], F32, "GT")
    gt2 = P.sbuf([128, 8], F32, "gt2s"); selt = P.sbuf([8, 8, 128], F32, "selt")
    for (t, d) in ((XM, xm), (H2, h2d), (GT, gTd), (gt2, gt2d), (selt, seld)):
        P.dma("sync", t[:], d[:], [d], [t], t)
    gbe = P.sbuf([128, NT], F32, "gbe"); hid = P.sbuf([128, QC, NT], BF16, "hid")
    t5 = [P.sbuf([128, 512], F32, f"t5_{i}") for i in range(3)]
    wst = [P.sbuf([128, 8, 128], F32, f"wst{i}") for i in range(4)]
    wbf = [P.sbuf([128, 8, 128], BF16, f"wbf{i}") for i in range(5)]
    psb = [P.psum([128, 512], F32, f"psb{i}") for i in range(8)]
    pi = [0]; wi = [0]; ti = [0]
    def PS():
        pi[0] += 1
        return psb[pi[0] % 8]
    def wpiece(ap, dbuf, kc=8):
        wi[0] += 1
        ws, wb = wst[wi[0] % 4], wbf[wi[0] % 5]
        P.dma("sync", ws[:, :kc, :], ap, [dbuf], [ws], ws)
        P.copy(wb[:, :kc, :], ws[:, :kc, :], [ws], [wb], eng=("gpsimd", "scalar")[wi[0] % 2])
        return wb
    for e_ in range(8):
        for (c0, n) in tiles:
            ps = PS(); P.mm(ps[:, :n], selt[:, e_, :], GT[:, c0:c0 + n], True, True, [selt, GT], [ps])
            P.copy(gbe[:, c0:c0 + n], ps[:, :n], [ps], [gbe], eng="scalar")
        for f0 in range(0, FC, QC):
            for j in range(QC):
                wb1 = wpiece(w1[e_, f0 + j], w1)
                wb3 = wpiece(w3[e_, f0 + j], w3)
                for (c0, n) in tiles:
                    ps1 = PS()
                    for k in range(8):
                        P.mm(ps1[:, :n], wb1[:, k, :], H2[:, k, c0:c0 + n], k == 0, k == 7, [wb1, H2], [ps1])
                    ps3 = PS()
                    for k in range(8):
                        P.mm(ps3[:, :n], wb3[:, k, :], H2[:, k, c0:c0 + n], k == 0, k == 7, [wb3, H2], [ps3])
                    ti[0] += 1
                    t_ = t5[ti[0] % 3]
                    P.act(t_[:, :n], ps1[:, :n], AF.Silu, [ps1], [t_])
                    P.tt(t_[:, :n], t_[:, :n], gbe[:, c0:c0 + n], ALU.mult, [t_, gbe], [t_])
                    P.tt(hid[:, j, c0:c0 + n], t_[:, :n], ps3[:, :n], ALU.mult, [t_, ps3], [hid])
            for oc in range(8):
                wb = wpiece(w2[e_, oc][:, f0:f0 + QC, :], w2, QC)
                for (c0, n) in tiles:
                    ps = PS()
                    for k in range(QC):
                        P.mm(ps[:, :n], wb[:, k, :], hid[:, k, c0:c0 + n], k == 0, k == QC - 1, [wb, hid], [ps])
                    P.stt(XM[:, oc, c0:c0 + n], ps[:, :n], gt2[:, oc:oc + 1], XM[:, oc, c0:c0 + n], ALU.mult, ALU.add,
                          [ps, gt2, XM], [XM])
    P.dma("gpsimd", out[:], XM[:], [XM], [out], XM)
    return P


def arrw(W):
    K_, M_ = W.shape[0] // 128, W.shape[1] // 128
    return np.ascontiguousarray(W.reshape(K_, 128, M_, 128).transpose(2, 1, 0, 3))


def fmT(a):
    C = a.shape[0] // 128
    return a.reshape(C, 128, a.shape[1]).transpose(1, 0, 2)


def run_L3(inp, l, mod, x, ctx, o1, o2):
    SEQ = x.shape[0]
    moe = (l % 2 == 1)
    need_ctx = l < 1
    NL3 = SEQ // NCORES
    NT = NL3 + (256 if need_ctx else 0)
    mv = lambda g, w: mod[:, l, g * 8:(g + 1) * 8, w]
    nv = np.stack([fm(inp["norm1_g"][l]), mv(1, 0), mv(0, 0), mv(1, 1), mv(0, 1), mv(2, 0), mv(2, 1),
                   fm(inp["norm2_g"][l]), mv(4, 0), mv(3, 0), mv(4, 1), mv(3, 1), mv(5, 0), mv(5, 1)], 1)
    W = dict(nv=np.ascontiguousarray(nv, np.float32),
             rwo=np.ascontiguousarray(np.stack([fm(inp["rw_ln_g"][l]), fm(inp["rw_ln_b"][l])], 1)),
             wg=arrw(inp["w_in"][l][:, 4128:7200]),
             wo=np.stack([arrw(inp["mla_wo"][l]), arrw(inp["na_wo"][l]), arrw(inp["rw_wo"][l])], 0),
             wout=arrw(inp["w_out"][l]))
    cst = np.zeros((128, 3, 128), np.float32)
    cst[:, 0, :] = 1.0; cst[:64, 1, :64] = 1.0; cst[64:, 1, 64:] = 1.0; cst[:, 2, :] = np.eye(128)
    W["cst"] = cst
    if moe:
        W["rt"] = np.ascontiguousarray(inp["moe_router"][l // 2].reshape(8, 128, 8).transpose(1, 0, 2))
        sel = np.zeros((8, 8, 128), np.float32)
        for e in range(8):
            sel[e, e, :] = 1.0
        W["sel"] = sel
    else:
        W["w1"] = arrw(inp["ffn_w1"][l // 2])[None]; W["w3"] = arrw(inp["ffn_w3"][l // 2])[None]
        W["w2"] = arrw(inp["ffn_w2"][l // 2])[None]
    g_l, g_c = o1["rwgb"]
    srcs = [o2["ya"], o2["yb"], o2["yf"], o2["ybk"],
            (g_l[0].reshape(512, -1), g_c[0].reshape(512, 256)), (g_l[1].reshape(512, -1), g_c[1].reshape(512, 256))]
    in_maps = []
    for i in range(NCORES):
        sl = slice(i * NL3, (i + 1) * NL3)
        xs = x[sl]
        if need_ctx:
            xs = np.concatenate([xs, ctx], 0)
        m = dict(W)
        m["xT"] = np.ascontiguousarray(fmT(xs.T))
        ys = []
        for (lat, cx) in srcs:
            a = np.asarray(lat, np.float32)[:, sl]
            if need_ctx:
                a = np.concatenate([a, np.asarray(cx, np.float32)], 1)
            ys.append(fmT(a))
        m["yin"] = np.ascontiguousarray(np.stack(ys, 0))
        in_maps.append(m)
    res = run(build_L3(NT, NL3, moe), in_maps)
    if moe:
        W4 = dict(w1=np.stack([arrw(inp["moe_w1"][l // 2][e]) for e in range(8)], 0),
                  w3=np.stack([arrw(inp["moe_w3"][l // 2][e]) for e in range(8)], 0),
                  w2=np.stack([arrw(inp["moe_w2"][l // 2][e]) for e in range(8)], 0),
                  sel=W["sel"], gt2=np.ascontiguousarray(mv(5, 0), np.float32))
        maps4 = []
        for r in res:
            m4 = dict(W4)
            m4.update(xm=np.asarray(r["out"]), h2=np.asarray(r["h2o"]), gT=np.asarray(r["gTo"]))
            maps4.append(m4)
        res = run(build_L4(NT), maps4)
    outs = [np.asarray(r["out"]).transpose(2, 1, 0).reshape(NT, 1024) for r in res]
    x_new = np.concatenate([o[:NL3] for o in outs], 0)
    ctx_new = outs[0][NL3:] if need_ctx else ctx
    return x_new, ctx_new


def kernel(**inp):
    inp = {k: np.asarray(v) for k, v in inp.items()}
    SEQ = inp["x"].shape[1]
    x = np.ascontiguousarray(inp["x"][0]); ctx = np.ascontiguousarray(inp["ctx"][0])
    mod = run_L0(inp["c"], inp["c_ctx"], inp["mod_w"], inp["mod_b"])
    depth = inp["mod_w"].shape[0]
    for l in range(depth):
        o1 = run_L1(inp, l, mod, x, ctx, SEQ // 16, 2)
        o2 = run_L2(inp, l, o1, SEQ)
        del o1["qa"], o1["ka"], o1["va"], o1["nqkv"], o1["rw3"], o1["rwd"]
        x, ctx = run_L3(inp, l, mod, x, ctx, o1, o2)
    return np.ascontiguousarray(x[None].astype(np.float32))
```

```python
from contextlib import ExitStack
import numpy as np
import ml_dtypes
import concourse.bass as bass
import concourse.mybir as mybir
from concourse.bass_utils import run_bass_kernel_spmd

F32 = mybir.dt.float32
BF16 = mybir.dt.bfloat16
AF = mybir.ActivationFunctionType
ALU = mybir.AluOpType
AX = mybir.AxisListType
NCORES = 8
ENGINES = ("tensor", "vector", "scalar", "gpsimd", "sync")


class Buf:
    __slots__ = ("t", "name", "lw", "rd")

    def __init__(self, t, name):
        self.t = t
        self.name = name
        self.lw = None
        self.rd = {}

    def __getitem__(self, idx):
        return self.t[idx]


class Prog:
    def __init__(self):
        self.nc = bass.Bass("TRN2", target_bir_lowering=False)
        self.ops = {e: [] for e in ENGINES}
        self.count = {}
        self.waited = {e: {} for e in ENGINES}
        self.stack = ExitStack()
        self.nbuf = 0

    def sbuf(self, shape, dt, name=None):
        self.nbuf += 1
        name = name or f"sb{self.nbuf}"
        t = self.stack.enter_context(self.nc.sbuf_tensor(name, list(shape), dt))
        return Buf(t, name)

    def psum(self, shape, dt=F32, name=None):
        self.nbuf += 1
        name = name or f"ps{self.nbuf}"
        t = self.stack.enter_context(self.nc.psum_tensor(name, list(shape), dt))
        return Buf(t, name)

    def dram(self, name, shape, dt, kind):
        t = self.nc.dram_tensor(name, list(shape), dt, kind=kind).ap()
        return Buf(t, name)

    def _deps(self, eng, reads, writes, skip_self):
        need = {}

        def add(kv):
            if kv is None:
                return
            k, v = kv
            if need.get(k, 0) < v:
                need[k] = v

        for b in reads:
            add(b.lw)
        for b in writes:
            add(b.lw)
            for k, v in b.rd.items():
                add((k, v))
        w = self.waited[eng]
        out = []
        for k, v in need.items():
            if skip_self and k == eng:
                continue
            if w.get(k, 0) < v:
                w[k] = v
                out.append((k, v))
        return out

    def _commit(self, key, inc, reads, writes):
        v = self.count.get(key, 0) + inc
        self.count[key] = v
        for b in reads:
            if b.rd.get(key, 0) < v:
                b.rd[key] = v
        for b in writes:
            b.lw = (key, v)
            b.rd = {}
        return v

    def op(self, eng, fn, reads=(), writes=(), skip_self=False):
        waits = self._deps(eng, reads, writes, skip_self)
        self._commit(eng, 1, reads, writes)
        self.ops[eng].append((waits, fn, eng, 1))

    def dma(self, eng, out_ap, in_ap, reads, writes, sem_buf):
        waits = self._deps(eng, reads, writes, False)
        key = "d_" + sem_buf.name
        self._commit(key, 16, reads, writes)
        self.ops[eng].append((waits, lambda e: e.dma_start(out=out_ap, in_=in_ap), key, 16))

    def coll(self, kind, in_buf, out_buf, op=None):
        waits = self._deps("gpsimd", [in_buf], [out_buf], False)
        key = "c_" + out_buf.name
        self._commit(key, 16, [in_buf], [out_buf])
        ia, oa = in_buf.t, out_buf.t
        op = op or ALU.bypass
        self.ops["gpsimd"].append((waits, lambda e: e.collective_compute(
            kind, op, replica_groups=[list(range(NCORES))], ins=[ia], outs=[oa]), key, 16))

    def barrier(self):
        snap = dict(self.count)
        for e in ENGINES:
            w = self.waited[e]
            waits = [(k, v) for k, v in snap.items() if w.get(k, 0) < v]
            for k, v in waits:
                w[k] = v
            if waits:
                self.ops[e].append((waits, None, None, 0))

    def scratch(self, name, shape, dt, shared=False):
        t = self.nc.dram_tensor(name, list(shape), dt, addr_space=("Shared" if shared else "Local")).ap()
        return Buf(t, name)

    def mm(self, out_ap, lhsT_ap, rhs_ap, start, stop, reads, writes):
        self.op("tensor", lambda e: e.matmul(out_ap, lhsT_ap, rhs_ap, start=start, stop=stop),
                reads, writes, skip_self=True)

    def transpose(self, out_ap, in_ap, ident_ap, reads, writes):
        self.op("tensor", lambda e: e.transpose(out_ap, in_ap, ident_ap), reads, writes, skip_self=True)

    def act(self, out_ap, in_ap, func, reads, writes, bias=None, scale=None, eng="scalar"):
        kw = {}
        if bias is not None:
            kw["bias"] = bias
        if scale is not None:
            kw["scale"] = scale
        self.op(eng, lambda e: e.activation(out_ap, in_ap, func, **kw), reads, writes)

    def tt(self, out_ap, a_ap, b_ap, op, reads, writes, eng="vector"):
        self.op(eng, lambda e: e.tensor_tensor(out_ap, a_ap, b_ap, op), reads, writes)

    def ts(self, out_ap, a_ap, s1, s2, op0, op1, reads, writes, eng="vector"):
        if s2 is None:
            self.op(eng, lambda e: e.tensor_scalar(out_ap, a_ap, s1, None, op0), reads, writes)
        else:
            self.op(eng, lambda e: e.tensor_scalar(out_ap, a_ap, s1, s2, op0, op1), reads, writes)

    def stt(self, out_ap, in0, scalar, in1, op0, op1, reads, writes):
        self.op("vector", lambda e: e.scalar_tensor_tensor(out_ap, in0, scalar, in1, op0, op1), reads, writes)

    def copy(self, out_ap, in_ap, reads, writes, eng="vector"):
        if eng == "scalar":
            self.op(eng, lambda e: e.copy(out_ap, in_ap), reads, writes)
        else:
            self.op(eng, lambda e: e.tensor_copy(out_ap, in_ap), reads, writes)

    def memset(self, ap, val, writes, eng="vector"):
        self.op(eng, lambda e: e.memset(ap, val), (), writes)

    def finish(self):
        nc = self.nc
        final_waits = []
        w = self.waited["sync"]
        for k, v in self.count.items():
            if w.get(k, 0) < v:
                final_waits.append((k, v))
        keys = list(self.count.keys())
        sems = {}
        for i, k in enumerate(keys):
            sems[k] = self.stack.enter_context(nc.semaphore(f"s{i}"))
        ops = self.ops

        def replay(e, lst):
            for waits, fn, key, inc in lst:
                for (k, v) in waits:
                    e.wait_ge(sems[k], v)
                if fn is not None:
                    fn(e).then_inc(sems[key], inc)

        with nc.Block() as block:
            @block.tensor
            def _(e):
                replay(e, ops["tensor"])

            @block.vector
            def _(e):
                replay(e, ops["vector"])

            @block.scalar
            def _(e):
                replay(e, ops["scalar"])

            @block.gpsimd
            def _(e):
                replay(e, ops["gpsimd"])

            @block.sync
            def _(e):
                replay(e, ops["sync"])
                for (k, v) in final_waits:
                    e.wait_ge(sems[k], v)
        self.stack.close()
        return nc


TRACE = False
TIMES = []


def run(prog, in_maps):
    nc = prog.finish()
    if TRACE:
        res = run_bass_kernel_spmd(nc, in_maps, core_ids=list(range(NCORES)), trace=True)
        TIMES.append(res.exec_time_ns)
        print("exec_time_ns", res.exec_time_ns, flush=True)
    else:
        res = run_bass_kernel_spmd(nc, in_maps, core_ids=list(range(NCORES)))
    return res.results


def build_L0(nch):
    P = Prog()
    w = P.dram("w", [1024, nch * 128], F32, "ExternalInput")
    b = P.dram("b", [128, nch], F32, "ExternalInput")
    cT = P.dram("cT", [128, 8, 2], F32, "ExternalInput")
    out = P.dram("out", [128, nch, 2], F32, "ExternalOutput")
    wt = P.sbuf([128, 8, nch * 128], F32, "wt")
    bt = P.sbuf([128, nch], F32, "bt")
    ct = P.sbuf([128, 8, 2], F32, "ct")
    st = P.sbuf([128, 8, 2], F32, "st")
    ot = P.sbuf([128, nch, 2], F32, "ot")
    ps = P.psum([128, nch, 2], F32, "ps0")
    P.dma("sync", ct[:], cT[:], [cT], [ct], ct)
    P.dma("sync", bt[:], b[:], [b], [bt], bt)
    for k in range(8):
        P.dma("sync", wt[:, k, :], w[k * 128:(k + 1) * 128, :], [w], [wt], wt)
    P.act(st[:], ct[:], AF.Silu, [ct], [st])
    for j in range(nch):
        for k in range(8):
            P.mm(ps[:, j, :], wt[:, k, j * 128:(j + 1) * 128], st[:, k, :], k == 0, k == 7, [wt, st], [ps])
    for j in range(nch):
        P.ts(ot[:, j, :], ps[:, j, :], bt[:, j:j + 1], None, ALU.add, None, [ps, bt], [ot])
    P.dma("sync", out[:], ot[:], [ot], [out], ot)
    return P


def fm(v):
    v = np.asarray(v, np.float32)
    return np.ascontiguousarray(v.reshape(-1, 128).T)


def run_L0(c, c_ctx, mod_w, mod_b):
    depth = mod_w.shape[0]
    nch_total = depth * 48
    nch = nch_total // NCORES
    wcat = np.concatenate([mod_w[l] for l in range(depth)], axis=1)
    bcat = np.concatenate([mod_b[l] for l in range(depth)], axis=0)
    cT = np.stack([fm(c.reshape(-1)), fm(c_ctx.reshape(-1))], axis=-1)
    in_maps = []
    for i in range(NCORES):
        sl = slice(i * nch * 128, (i + 1) * nch * 128)
        in_maps.append({"w": np.ascontiguousarray(wcat[:, sl]), "b": fm(bcat[sl]), "cT": cT})
    res = run(build_L0(nch), in_maps)
    o = np.concatenate([r["out"] for r in res], axis=1)
    return o.reshape(128, depth, 48, 2)


RW_ORDER = [12, 13, 14, 0, 4, 8, 1, 5, 9, 2, 6, 10, 3, 7, 11]


def build_L1(NL, NSUB):
    NS = NL + 260
    tiles = [(c0, min(512, NS - c0)) for c0 in range(0, NS, 512)]
    P = Prog()
    I = lambda n, s, dt=F32: P.dram(n, s, dt, "ExternalInput")
    O = lambda n, s, dt=F32: P.dram(n, s, dt, "ExternalOutput")
    xT_ = I("xT", [NSUB, 128, 8, NS])
    nv = I("nv", [128, 5, 8])
    wq = I("wq", [33, 128, 8, 128])
    ropeC_ = I("ropeC", [NSUB, 128, NS]); ropeS_ = I("ropeS", [NSUB, 128, NS]); mask_ = I("mask", [NSUB, 128, NS], BF16)
    sp = I("sp", [128, 16])
    wuq = I("wuq", [128, 3, 8, 128]); wuk = I("wuk", [128, 2, 8, 128]); wuv = I("wuv", [128, 2, 4, 128])
    rwp = I("rwp", [128, 58])
    w2d = I("w2", [128, 512]); a2d = I("a2", [128, 512]); g2d = I("g2", [128, 512])
    cst = I("cst", [128, 3, 128])
    o_qa_ = O("qa", [NSUB, 8, 128, NS], BF16); o_ka_ = O("ka", [NSUB, 8, 128, NS], BF16)
    o_va_ = O("va", [NSUB, 4, 128, NS], BF16)
    o_n_ = O("nqkv", [NSUB, 3, 4, 128, NS], BF16)
    o_r3_ = O("rw3", [NSUB, 3, 4, 128, NS])
    o_d3_ = O("rwd", [NSUB, 3, 2, 4, 128, NS])
    o_gb_ = O("rwgb", [NSUB, 2, 4, 128, NS])

    def load(d, shape, dt=F32, name=None):
        t = P.sbuf(shape, dt, name)
        P.dma("sync", t[:], d[:], [d], [t], t)
        return t
    nvt = load(nv, [128, 5, 8]); spt = load(sp, [128, 16]); rwt = load(rwp, [128, 58])
    cs = load(cst, [128, 3, 128])
    w2t = load(w2d, [128, 512]); a2t = load(a2d, [128, 512]); g2t = load(g2d, [128, 512])
    stg = P.sbuf([128, 3 * 8 * 128], F32, "stg")
    wuqb = P.sbuf([128, 3, 8, 128], BF16); wukb = P.sbuf([128, 2, 8, 128], BF16); wuvb = P.sbuf([128, 2, 4, 128], BF16)
    for (src, dstb, nel) in ((wuq, wuqb, 3 * 8 * 128), (wuk, wukb, 2 * 8 * 128), (wuv, wuvb, 2 * 4 * 128)):
        P.dma("sync", stg[:, :nel], src[:].rearrange("p a b c -> p (a b c)"), [src], [stg], stg)
        P.copy(dstb[:].rearrange("p a b c -> p (a b c)"), stg[:, :nel], [stg], [dstb], eng="gpsimd")
    ones = cs[:, 0, :]; blk = cs[:, 1, :]; rot = cs[:, 2, :]
    At = P.sbuf([128, 2, 8], F32)
    for w_, sc in ((0, 1), (1, 3)):
        P.stt(At[:, w_, :], nvt[:, sc, :], 1.0, nvt[:, 0, :], ALU.add, ALU.mult, [nvt], [At])
    m2 = P.sbuf([128, 15], F32)
    P.tt(m2[:], rwt[:, 0:15], rwt[:, 15:30], ALU.add, [rwt], [m2])
    P.ts(m2[:], m2[:], -1.0, 1.0, ALU.mult, ALU.add, [m2], [m2])

    psb = [P.psum([128, 512], F32, f"psb{i}") for i in range(6)]
    pi = [0]
    def PS():
        pi[0] += 1
        return psb[pi[0] % 6]
    slabs = [P.sbuf([128, NS], F32, f"sl{i}") for i in range(16)]
    bslabs = [P.sbuf([128, NS], BF16, f"bs{i}") for i in range(3)]
    bi = [0]
    def BS():
        bi[0] += 1
        return bslabs[bi[0] % 3]
    Ct = P.sbuf([128, NS], F32, "Ct"); St = P.sbuf([128, NS], F32, "St"); Mt = P.sbuf([128, NS], BF16, "Mt")
    hT = P.sbuf([128, 8, NS], BF16, "hT")
    x_ = P.sbuf([128, 8, 512], F32, "xt")
    sqt = [P.sbuf([128, 512], F32, f"sq{i}") for i in range(2)]
    rs = P.sbuf([128, 512], F32, "rs")
    wst = [P.sbuf([128, 8, 128], F32, f"wst{i}") for i in range(3)]
    wbf = [P.sbuf([128, 8, 128], BF16, f"wbf{i}") for i in range(3)]
    wi = [0]

    def proj(ci, dst, masked=False):
        wi[0] += 1
        ws, wb = wst[wi[0] % 3], wbf[wi[0] % 3]
        P.dma("sync", ws[:], wq[ci], [wq], [ws], ws)
        P.copy(wb[:], ws[:], [ws], [wb], eng=("gpsimd", "scalar")[wi[0] % 2])
        for (c0, n) in tiles:
            ps = PS()
            for k in range(8):
                P.mm(ps[:, :n], wb[:, k, :], hT[:, k, c0:c0 + n], k == 0, k == 7, [wb, hT], [ps])
            if masked:
                P.tt(dst[:, c0:c0 + n], ps[:, :n], Mt[:, c0:c0 + n], ALU.mult, [ps, Mt], [dst])
            else:
                P.copy(dst[:, c0:c0 + n], ps[:, :n], [ps], [dst], eng="scalar")

    def rstd_of(srcs, lhsT, dim, eps, dst, sqrt_only=False, tmp=None):
        tmp = tmp or slabs[12]
        for (c0, n) in tiles:
            ps = PS()
            for j, s in enumerate(srcs):
                P.act(tmp[:, c0:c0 + n], s[:, c0:c0 + n], AF.Square, [s], [tmp])
                P.mm(ps[:, :n], lhsT, tmp[:, c0:c0 + n], j == 0, j == len(srcs) - 1, [cs, tmp], [ps])
            P.act(dst[:, c0:c0 + n], ps[:, :n], AF.Sqrt, [ps], [dst], bias=eps, scale=1.0 / dim)
        if sqrt_only:
            P.ts(dst[:], dst[:], 1e-12, None, ALU.max, None, [dst], [dst])
        P.op("vector", lambda e: e.reciprocal(dst[:], dst[:]), [dst], [dst])

    def body(sub):
        D = lambda b, *idx: Buf(b.t[(sub,) + idx] if idx else b.t[sub], b.name + "_v")
        xT = D(xT_)
        o_qa, o_ka, o_va, o_n, o_r3, o_d3, o_gb = (D(o_qa_), D(o_ka_), D(o_va_), D(o_n_), D(o_r3_), D(o_d3_), D(o_gb_))
        P.dma("sync", Ct[:], ropeC_[sub], [ropeC_], [Ct], Ct)
        P.dma("sync", St[:], ropeS_[sub], [ropeS_], [St], St)
        P.dma("sync", Mt[:], mask_[sub], [mask_], [Mt], Mt)

        def out_dma(dram_ap, dram_buf, sb):
            P.dma("gpsimd", dram_ap, sb[:], [sb], [dram_buf], sb)

        for ti, (c0, n) in enumerate(tiles):
            P.dma("sync", x_[:, :, :n], xT[:, :, c0:c0 + n], [xT], [x_], x_)
            ps = PS()
            for k in range(8):
                s_ = sqt[k % 2]
                P.act(s_[:, :n], x_[:, k, :n], AF.Square, [x_], [s_])
                P.mm(ps[:, :n], ones, s_[:, :n], k == 0, k == 7, [cs, s_], [ps])
            P.act(rs[:, :n], ps[:, :n], AF.Sqrt, [ps], [rs], bias=1e-6, scale=1.0 / 1024)
            P.op("vector", lambda e, a=rs[:, :n]: e.reciprocal(a, a), [rs], [rs])
            for k in range(8):
                P.tt(x_[:, k, :n], x_[:, k, :n], rs[:, :n], ALU.mult, [x_, rs], [x_])
                for (a, b, w_, sh) in ((0, NL + 2, 0, 2), (NL + 2, NS, 1, 4)):
                    lo, hi = max(a, c0), min(b, c0 + n)
                    if lo < hi:
                        P.ts(hT[:, k, lo:hi], x_[:, k, lo - c0:hi - c0], At[:, w_, k:k + 1], nvt[:, sh, k:k + 1],
                             ALU.mult, ALU.add, [x_, At, nvt], [hT])

        cq = slabs[0:3]
        for j in range(3):
            proj(j, cq[j])
        r_ = slabs[3]
        rstd_of(cq, ones, 384.0, 1e-6, r_)
        cqn = [BS() for _ in range(3)]
        for j in range(3):
            P.stt(cqn[j][:], cq[j][:], spt[:, j:j + 1], r_[:], ALU.mult, ALU.mult, [cq[j], spt, r_], [cqn[j]])

        def head_finish(pre, gcol, dram_ap, dram_buf, ob, par=0):
            rr = (slabs[4], slabs[13])[par]
            rstd_of([pre], ones, 96.0, 1e-6, rr, tmp=(slabs[12], slabs[14])[par])
            P.stt(pre[:], pre[:], spt[:, gcol:gcol + 1], rr[:], ALU.mult, ALU.mult, [pre, spt, rr], [pre])
            rq = (slabs[5], slabs[15])[par]
            for (c0, n) in tiles:
                ps = PS()
                P.mm(ps[:, :n], rot, pre[:, c0:c0 + n], True, True, [cs, pre], [ps])
                P.tt(rq[:, c0:c0 + n], ps[:, :n], St[:, c0:c0 + n], ALU.mult, [ps, St], [rq])
            P.tt(pre[:], pre[:], Ct[:], ALU.mult, [pre, Ct], [pre])
            P.tt(ob[:], pre[:], rq[:], ALU.add, [pre, rq], [ob])
            out_dma(dram_ap, dram_buf, ob)

        obs = [slabs[8].t, slabs[9].t]
        qob = [P_q0, P_q1]
        for h in range(8):
            pre = slabs[6 + h % 2]
            for (c0, n) in tiles:
                ps = PS()
                for j in range(3):
                    P.mm(ps[:, :n], wuqb[:, j, h, :], cqn[j][:, c0:c0 + n], j == 0, j == 2, [wuqb, cqn[j]], [ps])
                P.copy(pre[:, c0:c0 + n], ps[:, :n], [ps], [pre], eng="scalar")
            head_finish(pre, 5, o_qa[h], o_qa, qob[h % 2], h % 2)

        ckv = slabs[0:2]
        for j in range(2):
            proj(3 + j, ckv[j])
        krp = slabs[2]
        proj(5, krp)
        rstd_of(ckv, ones, 256.0, 1e-6, r_)
        ckvn = [BS() for _ in range(2)]
        for j in range(2):
            P.stt(ckvn[j][:], ckv[j][:], spt[:, 3 + j:4 + j], r_[:], ALU.mult, ALU.mult, [ckv[j], spt, r_], [ckvn[j]])
        for h in range(8):
            pre = slabs[6 + h % 2]
            for (c0, n) in tiles:
                ps = PS()
                for j in range(2):
                    P.mm(ps[:, :n], wukb[:, j, h, :], ckvn[j][:, c0:c0 + n], j == 0, j == 1, [wukb, ckvn[j]], [ps])
                P.tt(pre[:, c0:c0 + n], ps[:, :n], krp[:, c0:c0 + n], ALU.add, [ps, krp], [pre])
            head_finish(pre, 6, o_ka[h], o_ka, qob[h % 2], h % 2)
        for c in range(4):
            ob = qob[c % 2]
            for (c0, n) in tiles:
                ps = PS()
                for j in range(2):
                    P.mm(ps[:, :n], wuvb[:, j, c, :], ckvn[j][:, c0:c0 + n], j == 0, j == 1, [wuvb, ckvn[j]], [ps])
                P.copy(ob[:, c0:c0 + n], ps[:, :n], [ps], [ob], eng="scalar")
            out_dma(o_va[c], o_va, ob)

        for which in range(3):
            for c in range(4):
                z = slabs[c % 2]
                proj(6 + which * 4 + c, z)
                ob = BS()
                if which < 2:
                    rr = (slabs[4], slabs[13])[c % 2]
                    rstd_of([z], blk, 64.0, 1e-6, rr, tmp=(slabs[12], slabs[14])[c % 2])
                    P.stt(ob[:], z[:], spt[:, 7 + which:8 + which], rr[:], ALU.mult, ALU.mult, [z, spt, rr], [ob])
                else:
                    P.copy(ob[:], z[:], [z], [ob])
                out_dma(o_n[which, c], o_n, ob)

        def shifted(ci_rw, dst, tmp):
            rwc = RW_ORDER[ci_rw]
            proj(18 + ci_rw, tmp, masked=True)
            P.ts(dst[:, 1:NS - 1], tmp[:, 1:NS - 1], m2[:, rwc:rwc + 1], None, ALU.mult, None, [tmp, m2], [dst])
            P.stt(dst[:, 1:NS - 1], tmp[:, 0:NS - 2], rwt[:, rwc:rwc + 1], dst[:, 1:NS - 1], ALU.mult, ALU.add,
                  [tmp, rwt, dst], [dst])
            P.stt(dst[:, 1:NS - 1], tmp[:, 2:NS], rwt[:, 15 + rwc:16 + rwc], dst[:, 1:NS - 1], ALU.mult, ALU.add,
                  [tmp, rwt, dst], [dst])
        tmp = slabs[11]
        wdT, adT, gdT = slabs[0], slabs[1], slabs[2]
        shifted(0, wdT, tmp); shifted(1, adT, tmp); shifted(2, gdT, tmp)
        P.act(wdT[:], wdT[:], AF.Tanh, [wdT], [wdT])
        P.act(gdT[:], gdT[:], AF.Sigmoid, [gdT], [gdT])
        for c in range(4):
            rT, kT, vT = slabs[3], slabs[4], slabs[5]
            shifted(3 + 3 * c, rT, tmp); shifted(4 + 3 * c, kT, tmp); shifted(5 + 3 * c, vT, tmp)
            P.dma("gpsimd", o_r3[0, c], rT[:], [rT], [o_r3], rT)
            P.dma("gpsimd", o_r3[1, c], vT[:], [vT], [o_r3], vT)
            kk = slabs[6]
            P.ts(kk[:], kT[:], rwt[:, 46 + c:47 + c], None, ALU.mult, None, [kT, rwt], [kk])
            rr = slabs[7]
            rstd_of([kk], blk, 1.0, 0.0, rr, sqrt_only=True)
            P.tt(kk[:], kk[:], rr[:], ALU.mult, [kk, rr], [kk])
            P.dma("gpsimd", o_r3[2, c], kk[:], [kk], [o_r3], kk)
            ksum = slabs[8]
            for d in range(2):
                lw, aa = slabs[9], slabs[10]
                pb = slice(d * 64, d * 64 + 64)
                for (c0, n) in tiles:
                    ps = PS()
                    P.mm(ps[:, :n], w2t[pb, c * 128:(c + 1) * 128], wdT[pb, c0:c0 + n], True, True, [w2t, wdT], [ps])
                    P.act(lw[:, c0:c0 + n], ps[:, :n], AF.Sigmoid, [ps, rwt], [lw],
                          bias=rwt[:, 30 + d * 4 + c:31 + d * 4 + c])
                    ps = PS()
                    P.mm(ps[:, :n], a2t[pb, c * 128:(c + 1) * 128], adT[pb, c0:c0 + n], True, True, [a2t, adT], [ps])
                    P.act(aa[:, c0:c0 + n], ps[:, :n], AF.Sigmoid, [ps, rwt], [aa],
                          bias=rwt[:, 38 + d * 4 + c:39 + d * 4 + c])
                P.ts(lw[:], lw[:], -float(np.exp(-0.5)), None, ALU.mult, None, [lw], [lw])
                P.dma("gpsimd", o_d3[0, d, c], lw[:], [lw], [o_d3], lw)
                bb = slabs[11]
                P.tt(bb[:], aa[:], kk[:], ALU.mult, [aa, kk], [bb])
                P.dma("gpsimd", o_d3[1, d, c], bb[:], [bb], [o_d3], bb)
                P.ts(aa[:], aa[:], -1.0, rwt[:, 50 + c:51 + c], ALU.add, ALU.mult, [aa, rwt], [aa])
                P.stt(aa[:], aa[:], 1.0, kT[:], ALU.add, ALU.mult, [aa, kT], [aa])
                P.dma("gpsimd", o_d3[2, d, c], aa[:], [aa], [o_d3], aa)
                if d == 0:
                    P.copy(ksum[:], aa[:], [aa], [ksum])
                else:
                    P.tt(ksum[:], ksum[:], aa[:], ALU.add, [ksum, aa], [ksum])
            P.stt(ksum[:], ksum[:], rwt[:, 54 + c:55 + c], rT[:], ALU.mult, ALU.mult, [ksum, rwt, rT], [ksum])
            gg, bo = slabs[9], slabs[10]
            for (c0, n) in tiles:
                ps = PS()
                P.mm(ps[:, :n], blk, ksum[:, c0:c0 + n], True, True, [cs, ksum], [ps])
                P.tt(bo[:, c0:c0 + n], ps[:, :n], vT[:, c0:c0 + n], ALU.mult, [ps, vT], [bo])
                ps = PS()
                P.mm(ps[:, :n], g2t[:, c * 128:(c + 1) * 128], gdT[:, c0:c0 + n], True, True, [g2t, gdT], [ps])
                P.copy(gg[:, c0:c0 + n], ps[:, :n], [ps], [gg], eng="scalar")
            P.dma("gpsimd", o_gb[0, c], gg[:], [gg], [o_gb], gg)
            P.dma("gpsimd", o_gb[1, c], bo[:], [bo], [o_gb], bo)

    P_q0 = P.sbuf([128, NS], BF16, "qo0"); P_q1 = P.sbuf([128, NS], BF16, "qo1")
    for sub in range(NSUB):
        body(sub)
    return P


def bf16(a):
    return np.asarray(a).astype(ml_dtypes.bfloat16)


def rope_tables(t0, NL, NS):
    C = np.ones((128, NS), np.float32)
    S = np.zeros((128, NS), np.float32)
    pos = t0 + np.arange(NL)
    rows, cols = (pos // 64).astype(np.float32), (pos % 64).astype(np.float32)
    fr = np.exp(-np.log(10000.0) * np.arange(8, dtype=np.float32) / 8).astype(np.float32)
    ar = rows[None, :] * fr[:, None]
    ac = cols[None, :] * fr[:, None]
    for base, ang in ((64, ar), (72, ar), (80, ac), (88, ac)):
        C[base:base + 8, 1:NL + 1] = np.cos(ang)
        S[base:base + 8, 1:NL + 1] = np.sin(ang)
    return C, S


def consts_L1():
    cst = np.zeros((128, 3, 128), np.float32)
    cst[:, 0, :] = 1.0
    cst[:64, 1, :64] = 1.0
    cst[64:, 1, 64:] = 1.0
    for i in range(8):
        cst[72 + i, 2, 64 + i] = -1.0
        cst[64 + i, 2, 72 + i] = 1.0
        cst[88 + i, 2, 80 + i] = -1.0
        cst[80 + i, 2, 88 + i] = 1.0
    return cst


def prep_L1_weights(inp, l, mod):
    W = inp["w_in"][l]
    cols = []
    z128 = np.zeros((1024, 128), np.float32)
    for j in range(3):
        cols.append(W[:, j * 128:(j + 1) * 128])
    for j in range(2):
        cols.append(W[:, 384 + j * 128:384 + (j + 1) * 128])
    kr = z128.copy(); kr[:, 64:96] = W[:, 640:672]; cols.append(kr)
    for j in range(12):
        cols.append(W[:, 672 + j * 128:672 + (j + 1) * 128])
    for j in RW_ORDER:
        cols.append(W[:, 2208 + j * 128:2208 + (j + 1) * 128])
    Wp = np.stack(cols, 0)
    wq = np.ascontiguousarray(Wp.reshape(33, 8, 128, 128).transpose(0, 2, 1, 3))
    nv = np.stack([fm(inp["norm1_g"][l]), mod[:, l, 8:16, 0], mod[:, l, 0:8, 0], mod[:, l, 8:16, 1], mod[:, l, 0:8, 1]], 1)
    sp = np.zeros((128, 16), np.float32)
    sp[:, 0:3] = fm(inp["mla_cq_g"][l]); sp[:, 3:5] = fm(inp["mla_ckv_g"][l])
    sp[:96, 5] = inp["mla_qn_g"][l]; sp[:96, 6] = inp["mla_kn_g"][l]
    sp[:, 7] = np.tile(inp["na_qn_g"][l], 2); sp[:, 8] = np.tile(inp["na_kn_g"][l], 2)
    wuq = np.zeros((384, 8, 128), np.float32)
    wuq[:, :, :96] = inp["mla_wuq"][l].reshape(384, 8, 96)
    wuq = np.ascontiguousarray(wuq.reshape(3, 128, 8, 128).transpose(1, 0, 2, 3))
    kv = inp["mla_wukv"][l].reshape(256, 8, 128)
    wuk = np.zeros((256, 8, 128), np.float32); wuk[:, :, :64] = kv[:, :, :64]
    wuk = np.ascontiguousarray(wuk.reshape(2, 128, 8, 128).transpose(1, 0, 2, 3))
    wuv = np.ascontiguousarray(kv[:, :, 64:].reshape(256, 4, 128).reshape(2, 128, 4, 128).transpose(1, 0, 2, 3))
    rwp = np.zeros((128, 58), np.float32)
    rwp[:, 0:15] = fm(inp["rw_mu"][l][0]); rwp[:, 15:30] = fm(inp["rw_mu"][l][1])
    for d in range(2):
        rwp[:, 30 + d * 4:34 + d * 4] = fm(inp["rw_w0"][l][d]); rwp[:, 38 + d * 4:42 + d * 4] = fm(inp["rw_a0"][l][d])
    rwp[:, 46:50] = fm(inp["rw_kk"][l]); rwp[:, 50:54] = fm(inp["rw_ka"][l]); rwp[:, 54:58] = fm(inp["rw_rk"][l].reshape(-1))
    return dict(nv=np.ascontiguousarray(nv, np.float32), wq=wq, sp=sp, wuq=wuq, wuk=wuk, wuv=wuv, rwp=rwp,
                w2=np.ascontiguousarray(inp["rw_w2"][l].reshape(128, 512)),
                a2=np.ascontiguousarray(inp["rw_a2"][l].reshape(128, 512)),
                g2=np.ascontiguousarray(inp["rw_g2"][l]), cst=consts_L1())


def run_L1(inp, l, mod, x, ctx, NL, NSUB):
    SEQ = x.shape[0]
    NS = NL + 260
    wts = prep_L1_weights(inp, l, mod)
    in_maps = []
    for i in range(NCORES):
        xs, Cs, Ss, Ms = [], [], [], []
        for s in range(NSUB):
            t0 = (i * NSUB + s) * NL
            slab = np.zeros((NS, 1024), np.float32)
            M = np.ones((128, NS), np.float32)
            if t0 > 0:
                slab[0] = x[t0 - 1]
            else:
                M[:, 0] = 0
            slab[1:NL + 1] = x[t0:t0 + NL]
            if t0 + NL < SEQ:
                slab[NL + 1] = x[t0 + NL]
            else:
                M[:, NL + 1] = 0
            M[:, NL + 2] = 0; M[:, NS - 1] = 0
            slab[NL + 3:NL + 259] = ctx
            xs.append(slab.T.reshape(8, 128, NS).transpose(1, 0, 2))
            C, S = rope_tables(t0, NL, NS)
            Cs.append(C); Ss.append(S); Ms.append(bf16(M))
        m = dict(wts)
        m.update(xT=np.ascontiguousarray(np.stack(xs, 0)), ropeC=np.stack(Cs, 0), ropeS=np.stack(Ss, 0), mask=np.stack(Ms, 0))
        in_maps.append(m)
    res = run(build_L1(NL, NSUB), in_maps)
    out = {}
    for key in ("qa", "ka", "va", "nqkv", "rw3", "rwd", "rwgb"):
        lat = np.concatenate([res[i][key][s][..., 1:NL + 1] for i in range(NCORES) for s in range(NSUB)], axis=-1)
        cx = res[0][key][0][..., NL + 3:NL + 259]
        out[key] = (np.asarray(lat), np.asarray(cx))
    return out


def na_plan(SEQ):
    NR = SEQ // 64
    NP_ = NR // 2
    tabs = {}
    tab_list = []
    plan = []
    qc = np.arange(64)
    cs_ = np.clip(qc - 8, 0, 48)
    for m in range(NP_):
        rows = [2 * m, 2 * m + 1]
        rs = [int(np.clip(r - 4, 0, NR - 8)) for r in rows]
        kps = sorted({(a + i) // 2 for a in rs for i in range(8)})
        ent = []
        for kp in kps:
            sig = (rs[0] - rows[0], rs[1] - rows[1], kp - m)
            if sig not in tabs:
                dr = np.full((128, 128), -1, np.int64)
                dc = np.full((128, 128), -1, np.int64)
                for kl in range(128):
                    krow, kcol = 2 * kp + kl // 64, kl % 64
                    for ql in range(128):
                        qrow, qcol = rows[ql // 64], ql % 64
                        a = rs[ql // 64]
                        if a <= krow < a + 8 and cs_[qcol] <= kcol < cs_[qcol] + 16:
                            dr[kl, ql] = krow - qrow + 7
                            dc[kl, ql] = kcol - qcol + 15
                tabs[sig] = len(tab_list)
                tab_list.append((dr, dc))
            ent.append((kp, tabs[sig]))
        plan.append(ent)
    return plan, tab_list


def build_L2(SEQ):
    NK = SEQ + 256
    NKB = NK // 128
    NCH = NKB
    plan, tab_list = na_plan(SEQ)
    NTAB = len(tab_list)
    P = Prog()
    I = lambda n, s, dt=F32: P.dram(n, s, dt, "ExternalInput")
    O = lambda n, s, dt=F32: P.dram(n, s, dt, "ExternalOutput")
    qa = I("qa", [128, SEQ + 256], BF16)
    ka = I("ka", [128, NK], BF16)
    va = I("va", [128, NKB, 65], BF16)
    nq = I("nq", [64, SEQ + 256], BF16)
    nk = I("nk", [64, NK], BF16)
    nvv = I("nv", [128, NKB, 65], BF16)
    tabs = I("tabs", [128, NTAB, 128])
    cst = I("cst", [128, 6, 128])
    rtm = I("rtm", [2, NCH, 128, 4, 64])
    rfm = I("rfm", [2, NCH, 64, 4, 128])
    o_ya = O("ya", [64, SEQ + 256]); o_yb = O("yb", [64, SEQ + 256])
    o_y = O("y", [2, NCH, 128, 64])

    psb = [P.psum([128, 512], F32, f"psb{i}") for i in range(4)]
    psS2 = [P.psum([128, 1024], F32, f"psS{i}") for i in range(2)]
    cs = P.sbuf([128, 6, 128], F32, "cs")
    P.dma("sync", cs[:], cst[:], [cst], [cs], cs)
    triI, triS, triL, ident, ones = (cs[:, i, :] for i in range(5))
    csb = P.sbuf([128, 2, 128], BF16, "csb")
    P.copy(csb[:, 0, :], cs[:, 3, :], [cs], [csb])
    P.copy(csb[:, 1, :], cs[:, 4, :], [cs], [csb])

    pT = [P.sbuf([128, 1024], BF16, f"pT{i}") for i in range(3)]
    osb = [P.sbuf([64, 512], F32, f"osb{i}") for i in range(2)]
    rD = P.sbuf([64, 512], F32, "rD")
    dsb = P.sbuf([128, 512], F32, "dsb")
    cnt = [0]

    def finish_od(psOD, n, out_d, out_c0):
        psB = psb[2]
        P.copy(dsb[64:65, :n], psOD[64:65, :n], [psOD], [dsb], eng="scalar")
        P.mm(psB[0:64, :n], cs[64:65, 4, 0:64], dsb[64:65, :n], True, True, [cs, dsb], [psB])
        P.op("vector", lambda e: e.reciprocal(rD[:, :n], psB[0:64, :n]), [psB], [rD])
        ob = osb[cnt[0] % 2]
        P.tt(ob[:, :n], psOD[0:64, :n], rD[:, :n], ALU.mult, [psOD, rD], [ob])
        P.dma("gpsimd", out_d[:, out_c0:out_c0 + n], ob[:, :n], [ob], [out_d], ob)

    def attn(qT, q0, n, kT, V, kblocks, KD, scale, out_d, out_c0):
        cnt[0] += 1
        psOD = psb[cnt[0] % 2]
        pairs = [kblocks[i:i + 2] for i in range(0, len(kblocks), 2)]
        npair = len(pairs)

        def S(p):
            pS = psS2[p % 2]
            for j, kb in enumerate(pairs[p]):
                P.mm(pS[:, j * 512:j * 512 + n], kT[0:KD, kb * 128:(kb + 1) * 128], qT[0:KD, q0:q0 + n], True, True, [kT, qT], [pS])
        S(0)
        for p, pr in enumerate(pairs):
            if p + 1 < npair:
                S(p + 1)
            pS = psS2[p % 2]
            p_ = pT[p % 3]
            L = len(pr)
            sv = pS[:, :].rearrange("p (j c) -> p j c", c=512)[:, 0:L, 0:n]
            dv = p_[:, :].rearrange("p (j c) -> p j c", c=512)[:, 0:L, 0:n]
            P.act(dv, sv, AF.Exp, [pS], [p_], scale=scale)
            for j, kb in enumerate(pr):
                P.mm(psOD[0:65, :n], V[:, kb, :], p_[:, j * 512:j * 512 + n], p == 0 and j == 0,
                     p == npair - 1 and j == L - 1, [V, p_], [psOD])
        finish_od(psOD, n, out_d, out_c0)

    qat = P.sbuf([128, SEQ + 256], BF16, "qat"); kat = P.sbuf([128, NK], BF16, "kat"); vat = P.sbuf([128, NKB, 65], BF16, "vat")
    for (t, d) in ((qat, qa), (kat, ka), (vat, va)):
        P.dma("sync", t[:], d[:], [d], [t], t)
    sc_a = float(96 ** -0.5)
    for q0 in range(0, SEQ, 512):
        attn(qat, q0, min(512, SEQ - q0), kat, vat, list(range(NKB)), 128, sc_a, o_ya, q0)
    attn(qat, SEQ, 256, kat, vat, [0, 1], 128, sc_a, o_ya, SEQ)

    nqt, nkt, nvt = qat, kat, vat
    P.dma("sync", nqt[0:64, :], nq[:], [nq], [nqt], nqt)
    P.dma("sync", nkt[0:64, :], nk[:], [nk], [nkt], nkt)
    P.dma("sync", nvt[:], nvv[:], [nvv], [nvt], nvt)
    tb32 = P.sbuf([128, NTAB, 128], F32, "tb32"); tbb = P.sbuf([128, NTAB, 128], BF16, "tbb")
    P.dma("sync", tb32[:], tabs[:], [tabs], [tb32], tb32)
    P.ts(tbb[:], tb32[:], 8.0, None, ALU.mult, None, [tb32], [tbb])
    sc_b = 0.125
    for m, ent in enumerate(plan):
        blocks = [(kp, tid) for (kp, tid) in ent] + [(NKB - 2, None), (NKB - 1, None)]
        cnt[0] += 1
        psOD = psb[cnt[0] % 2]
        p_ = pT[cnt[0] % 3]
        pS = psS2[cnt[0] % 2]
        qsl = nqt[0:64, m * 128:(m + 1) * 128]
        for j, (kb, tid) in enumerate(blocks):
            o_ = pS[:, j * 128:(j + 1) * 128]
            P.mm(o_, nkt[0:64, kb * 128:(kb + 1) * 128], qsl, True, tid is None, [nkt, nqt], [pS])
            if tid is not None:
                P.mm(o_, csb[:, 0, :], tbb[:, tid, :], False, True, [csb, tbb], [pS])
        w = len(blocks) * 128
        P.act(p_[:, :w], pS[:, :w], AF.Exp, [pS], [p_], scale=sc_b)
        for j, (kb, tid) in enumerate(blocks):
            P.mm(psOD[0:65, :128], nvt[:, kb, :], p_[:, j * 128:(j + 1) * 128], j == 0, j == len(blocks) - 1, [nvt, p_], [psOD])
        finish_od(psOD, 128, o_yb, m * 128)
    attn(nqt, SEQ, 256, nkt, nvt, [NKB - 2, NKB - 1], 64, sc_b, o_yb, SEQ)

    pi = [0]
    rw_ps = psb + psS2
    def PS():
        pi[0] += 1
        return rw_ps[pi[0] % 6]
    NSET = 4
    W = lambda n, shape=(128, 128): [P.sbuf(list(shape), F32, f"{n}{i}") for i in range(NSET)]
    tmb, fmb = W("tm", (128, 4, 64)), W("fmj", (64, 4, 128))
    eLr = W("eLr", (128, 64)); Bh = W("Bh", (128, 64)); Kh = W("Kh", (128, 64))
    e1 = W("e1", (64, 128)); e2 = W("e2", (64, 128)); e3 = W("e3", (64, 128))
    Rt = W("Rt", (64, 128)); KKt = W("KKt", (64, 128)); Bt = W("Bt", (64, 128)); Kt = W("Kt", (64, 128))
    Nn = W("Nn"); NTt = W("NTt"); Mk = W("Mk"); Mbp = W("Mbp"); Mkp = W("Mkp")
    Pq = W("Pq"); PqT = W("PqT"); Tm = W("Tm")
    Zs = W("Zs", (128, 64)); nU = W("nU", (128, 64)); Ys = W("Ys", (128, 64))
    gC = W("gC", (64, 1))
    ST = [P.sbuf([64, 64], F32, f"ST{d}") for d in range(2)]
    for d in range(2):
        P.memset(ST[d][:], 0.0, [ST[d]])

    def chunk_gen(c, d, s):
        tm, fj = tmb[s], fmb[s]
        P.dma("sync", tm[:], rtm[d, c], [rtm], [tm], tm)
        P.dma("sync", fj[:], rfm[d, c], [rfm], [fj], fj)
        yield
        lw_tok = tm[:, 0, :]
        ps = PS(); P.mm(ps[:, 0:64], triL, lw_tok, True, True, [cs, tm], [ps])
        P.act(eLr[s][:], ps[:, 0:64], AF.Exp, [ps], [eLr[s]])
        yield
        ps = PS(); P.mm(ps[0:64, 0:128], lw_tok, triI, True, True, [tm, cs], [ps])
        P.act(e1[s][:], ps[0:64, 0:128], AF.Exp, [ps], [e1[s]])
        P.act(e2[s][:], ps[0:64, 0:128], AF.Exp, [ps], [e2[s]], scale=-1.0)
        yield
        ps = PS(); P.mm(ps[0:64, 0:128], lw_tok, triS, True, True, [tm, cs], [ps])
        P.act(e3[s][:], ps[0:64, 0:128], AF.Exp, [ps], [e3[s]])
        yield
        P.tt(Bh[s][:], tm[:, 1, :], eLr[s][:], ALU.mult, [tm, eLr[s]], [Bh[s]], eng="gpsimd")
        P.tt(Kh[s][:], tm[:, 2, :], eLr[s][:], ALU.mult, [tm, eLr[s]], [Kh[s]], eng="gpsimd")
        P.copy(gC[s][:], e1[s][:, 127:128], [e1[s]], [gC[s]], eng="gpsimd")
        P.tt(Bt[s][:], fj[:, 0, :], e2[s][:], ALU.mult, [fj, e2[s]], [Bt[s]])
        P.tt(Kt[s][:], fj[:, 1, :], e2[s][:], ALU.mult, [fj, e2[s]], [Kt[s]], eng="gpsimd")
        P.tt(KKt[s][:], fj[:, 2, :], e3[s][:], ALU.mult, [fj, e3[s]], [KKt[s]])
        P.tt(Rt[s][:], fj[:, 3, :], e1[s][:], ALU.mult, [fj, e1[s]], [Rt[s]], eng="gpsimd")
        yield
        for (dst, l_, r_, msk) in ((Nn[s], Bt[s], KKt[s], triS), (NTt[s], KKt[s], Bt[s], triL),
                                  (Mk[s], Kt[s], KKt[s], triS), (Mbp[s], Bt[s], Rt[s], triI), (Mkp[s], Kt[s], Rt[s], triI)):
            ps = PS(); P.mm(ps[:, 0:128], l_[:], r_[:], True, True, [l_, r_], [ps])
            P.tt(dst[:], ps[:, 0:128], msk, ALU.mult, [ps, cs], [dst])
            yield
        P.tt(Tm[s][:], ident, Nn[s][:], ALU.subtract, [cs, Nn[s]], [Tm[s]])
        Pc, PcT = Nn[s], NTt[s]
        for it in range(6):
            nxt, nxtT = (Pq[s], PqT[s]) if Pc is not Pq[s] else (Nn[s], NTt[s])
            ps = PS(); P.mm(ps[:, 0:128], Pc[:], PcT[:], True, True, [Pc, PcT], [ps])
            P.copy(nxtT[:], ps[:, 0:128], [ps], [nxtT], eng="scalar")
            if it < 5:
                ps = PS(); P.mm(ps[:, 0:128], PcT[:], Pc[:], True, True, [Pc, PcT], [ps])
                P.copy(nxt[:], ps[:, 0:128], [ps], [nxt])
            yield
            ps = PS(); P.mm(ps[:, 0:128], nxtT[:], Tm[s][:], True, True, [nxtT, Tm[s]], [ps])
            P.tt(Tm[s][:], Tm[s][:], ps[:, 0:128], ALU.add, [Tm[s], ps], [Tm[s]])
            Pc, PcT = nxt, nxtT
            yield
        vt = tm[:, 3, :]
        ps = PS()
        P.mm(ps[:, 0:64], KKt[s][:], ST[d][:], True, False, [KKt[s], ST[d]], [ps])
        P.mm(ps[:, 0:64], Mk[s][:], vt, False, True, [Mk[s], tm], [ps])
        P.copy(Zs[s][:], ps[:, 0:64], [ps], [Zs[s]])
        yield
        ps = PS(); P.mm(ps[:, 0:64], Tm[s][:], Zs[s][:], True, True, [Tm[s], Zs[s]], [ps])
        P.ts(nU[s][:], ps[:, 0:64], -1.0, None, ALU.mult, None, [ps], [nU[s]])
        yield
        ps2 = PS()
        P.mm(ps2[0:64, 0:64], Bh[s][:], nU[s][:], True, False, [Bh[s], nU[s]], [ps2])
        P.mm(ps2[0:64, 0:64], Kh[s][:], vt, False, True, [Kh[s], tm], [ps2])
        ps = PS()
        P.mm(ps[:, 0:64], Rt[s][:], ST[d][:], True, False, [Rt[s], ST[d]], [ps])
        P.mm(ps[:, 0:64], Mbp[s][:], nU[s][:], False, False, [Mbp[s], nU[s]], [ps])
        P.mm(ps[:, 0:64], Mkp[s][:], vt, False, True, [Mkp[s], tm], [ps])
        P.stt(ST[d][:], ST[d][:], gC[s][:, 0:1], ps2[0:64, 0:64], ALU.mult, ALU.add, [ST[d], gC[s], ps2], [ST[d]])
        P.copy(Ys[s][:], ps[:, 0:64], [ps], [Ys[s]], eng="scalar")
        P.dma("gpsimd", o_y[d, c], Ys[s][:], [Ys[s]], [o_y], Ys[s])
        yield

    tasks = [(c, d) for c in range(NCH) for d in range(2)]
    active = []
    nxt_task = 0
    rounds = 0
    while nxt_task < len(tasks) or active:
        if nxt_task < len(tasks) and len(active) < NSET and (rounds % 7 == 0 or not active):
            c, d = tasks[nxt_task]
            active.append(chunk_gen(c, d, nxt_task % NSET))
            nxt_task += 1
        rounds += 1
        for g in list(active):
            try:
                next(g)
            except StopIteration:
                active.remove(g)
    return P


def consts_L2():
    c = np.zeros((128, 6, 128), np.float32)
    i = np.arange(128)
    c[:, 0, :] = (i[:, None] <= i[None, :])
    c[:, 1, :] = (i[:, None] < i[None, :])
    c[:, 2, :] = (i[:, None] > i[None, :])
    c[:, 3, :] = np.eye(128)
    c[:, 4, :] = 1.0
    return c


def tokmaj(a, aug=False):
    n = a.shape[1]
    t = a.T.reshape(n // 128, 128, 64).transpose(1, 0, 2)
    if aug:
        t = np.concatenate([t, np.ones((128, n // 128, 1), t.dtype)], axis=2)
    return np.ascontiguousarray(t)


def run_L2(inp, l, o1, SEQ):
    NK = SEQ + 256
    NCH = NK // 128
    plan, tab_list = na_plan(SEQ)
    cst = consts_L2()
    in_maps = []
    f32 = lambda a: np.asarray(a, np.float32)
    hs = lambda pair, h: (pair[0].reshape(-1, pair[0].shape[-1])[h * 64:(h + 1) * 64],
                          pair[1].reshape(-1, 256)[h * 64:(h + 1) * 64])
    for h in range(NCORES):
        m = {"cst": cst}
        m["qa"] = np.ascontiguousarray(np.concatenate([o1["qa"][0][h], o1["qa"][1][h]], 1))
        m["ka"] = np.ascontiguousarray(np.concatenate([o1["ka"][1][h], o1["ka"][0][h]], 1))
        vl, vc = hs(o1["va"], h)
        m["va"] = tokmaj(np.concatenate([vc, vl], 1), aug=True)
        n_l, n_c = o1["nqkv"]
        sel = lambda w: (n_l[w].reshape(512, -1)[h * 64:(h + 1) * 64], n_c[w].reshape(512, 256)[h * 64:(h + 1) * 64])
        m["nq"] = np.ascontiguousarray(np.concatenate(sel(0), 1))
        m["nk"] = np.ascontiguousarray(np.concatenate(sel(1), 1))
        m["nv"] = tokmaj(np.concatenate(sel(2), 1), aug=True)
        rpb = inp["na_rpb"][l][h]
        tb = np.stack([np.where(dr >= 0, rpb[np.maximum(dr, 0), np.maximum(dc, 0)], np.float32(-30000.0)) for dr, dc in tab_list], 0)
        m["tabs"] = np.ascontiguousarray(tb.transpose(1, 0, 2).astype(np.float32))
        r3l, r3c = o1["rw3"]; rdl, rdc = o1["rwd"]
        def seqs(lat, cx, d):
            lat = lat.reshape(512, -1)[h * 64:(h + 1) * 64]; cx = cx.reshape(512, 256)[h * 64:(h + 1) * 64]
            return np.concatenate([cx, lat], 1) if d == 0 else np.concatenate([cx[:, ::-1], lat[:, ::-1]], 1)
        rtm = np.zeros((2, NCH, 128, 4, 64), np.float32); rfm = np.zeros((2, NCH, 64, 4, 128), np.float32)
        for d in range(2):
            lw = seqs(rdl[0, d], rdc[0, d], d); b_ = seqs(rdl[1, d], rdc[1, d], d); kd = seqs(rdl[2, d], rdc[2, d], d)
            r_ = seqs(r3l[0], r3c[0], d); v_ = seqs(r3l[1], r3c[1], d); kk = seqs(r3l[2], r3c[2], d)
            for j, a in enumerate((lw, b_, kd, v_)):
                rtm[d, :, :, j, :] = a.T.reshape(NCH, 128, 64)
            for j, a in enumerate((b_, kd, kk, r_)):
                rfm[d, :, :, j, :] = a.reshape(64, NCH, 128).transpose(1, 0, 2)
        m["rtm"] = rtm; m["rfm"] = rfm
        in_maps.append(m)
    res = run(build_L2(SEQ), in_maps)
    ya = np.concatenate([f32(res[h]["ya"]) for h in range(NCORES)], 0)
    yb = np.concatenate([f32(res[h]["yb"]) for h in range(NCORES)], 0)
    ys = []
    for d in range(2):
        yy = np.concatenate([f32(res[h]["y"][d]).reshape(NK, 64).T for h in range(NCORES)], 0)
        cx, lat = yy[:, :256], yy[:, 256:]
        if d == 1:
            cx, lat = cx[:, ::-1], lat[:, ::-1]
        ys.append((np.ascontiguousarray(lat), np.ascontiguousarray(cx)))
    return dict(ya=(ya[:, :SEQ], ya[:, SEQ:]), yb=(yb[:, :SEQ], yb[:, SEQ:]), yf=ys[0], ybk=ys[1])


def build_L3(NT, NLAT, moe):
    FC = 28 if moe else 22
    NE = 8 if moe else 1
    tiles = [(c0, min(512, NT - c0)) for c0 in range(0, NT, 512)]
    P = Prog()
    I = lambda n, s, dt=F32: P.dram(n, s, dt, "ExternalInput")
    xT = I("xT", [128, 8, NT])
    yin = I("yin", [6, 128, 4, NT])
    nv = I("nv", [128, 14, 8])
    rwo = I("rwo", [128, 2, 4])
    wg = I("wg", [24, 128, 8, 128]); wo = I("wo", [3, 8, 128, 4, 128]); wout = I("wout", [8, 128, 8, 128])
    if not moe:
        w1 = I("w1", [NE, FC, 128, 8, 128]); w3 = I("w3", [NE, FC, 128, 8, 128]); w2 = I("w2", [NE, 8, 128, FC, 128])
    else:
        h2o = P.dram("h2o", [128, 8, NT], BF16, "ExternalOutput"); gTo = P.dram("gTo", [8, NT], F32, "ExternalOutput")
    cst = I("cst", [128, 3, 128])
    if moe:
        rt = I("rt", [128, 8, 8]); sel = I("sel", [8, 8, 128])
    out = P.dram("out", [128, 8, NT], F32, "ExternalOutput")

    def load(d, shape, dt=F32, name=None):
        t = P.sbuf(shape, dt, name)
        P.dma("sync", t[:], d[:], [d], [t], t)
        return t
    nvt = load(nv, [128, 14, 8]); rwt = load(rwo, [128, 2, 4]); cs = load(cst, [128, 3, 128])
    ones, blk, ident = cs[:, 0, :], cs[:, 1, :], cs[:, 2, :]
    if moe:
        rtt = load(rt, [128, 8, 8]); selt = load(sel, [8, 8, 128])
    A1 = P.sbuf([128, 2, 8], F32); A2 = P.sbuf([128, 2, 8], F32)
    for w_, sc in ((0, 1), (1, 3)):
        P.stt(A1[:, w_, :], nvt[:, sc, :], 1.0, nvt[:, 0, :], ALU.add, ALU.mult, [nvt], [A1])
    for w_, sc in ((0, 8), (1, 10)):
        P.stt(A2[:, w_, :], nvt[:, sc, :], 1.0, nvt[:, 7, :], ALU.add, ALU.mult, [nvt], [A2])

    psb = [P.psum([128, 512], F32, f"psb{i}") for i in range(8)]
    pi = [0]
    def PS():
        pi[0] += 1
        return psb[pi[0] % 8]
    x_ = P.sbuf([128, 8, 512], F32, "x"); hT = P.sbuf([128, 8, 512], BF16, "hT"); G = P.sbuf([128, 24, 512], BF16, "G")
    stg = [P.sbuf([128, 4, 512], F32, f"stg{i}") for i in range(2)]
    ybf = [P.sbuf([128, 4, 512], BF16, f"ybf{i}") for i in range(3)]
    ysum = P.sbuf([128, 4, 512], F32, "ysum"); tmp = P.sbuf([128, 4, 512], F32, "tmp")
    Mb = P.sbuf([128, 8, 512], BF16, "Mb"); Mo = P.sbuf([128, 512], F32, "Mo"); t5 = P.sbuf([128, 512], F32, "t5")
    h2 = P.sbuf([128, 8, 512], BF16, "h2"); hid = P.sbuf([128, 1 if moe else FC, 512], BF16, "hid")
    sq = [P.sbuf([128, 512], F32, f"sq{i}") for i in range(2)]; rs = P.sbuf([128, 512], F32, "rs")
    wst = [P.sbuf([128, 8, 128], F32, f"wst{i}") for i in range(6)]
    wbf = [P.sbuf([128, 8, 128], BF16, f"wbf{i}") for i in range(7)]
    wi = [0]
    if moe:
        lgT = P.sbuf([8, 512], F32, "lgT"); gT = P.sbuf([8, 512], F32, "gT"); gbe = P.sbuf([128, 512], F32, "gbe")
        lg = P.sbuf([128, 8], F32, "lg"); top = P.sbuf([128, 8], F32, "top"); sm = P.sbuf([128, 8], F32, "sm")
        gt_ = P.sbuf([128, 8], F32, "gt"); g2_ = P.sbuf([128, 8], F32, "g2")

    def wpiece(ap, dbuf, kc=8):
        wi[0] += 1
        ws, wb = wst[wi[0] % 6], wbf[wi[0] % 7]
        P.dma("sync", ws[:, :kc, :], ap, [dbuf], [ws], ws)
        P.copy(wb[:, :kc, :], ws[:, :kc, :], [ws], [wb], eng=("gpsimd", "scalar", "vector", "scalar")[wi[0] % 4])
        return wb

    def segs(c0, n):
        for (a, b, w_) in ((0, NLAT, 0), (NLAT, NT, 1)):
            lo, hi = max(a, c0), min(b, c0 + n)
            if lo < hi:
                yield lo - c0, hi - c0, w_

    def norm_mod(src, dst, A, shl, shc, n, c0, extra=None):
        ps = PS()
        for k in range(8):
            s_ = sq[k % 2]
            P.act(s_[:, :n], src[:, k, :n], AF.Square, [src], [s_])
            P.mm(ps[:, :n], ones, s_[:, :n], k == 0, k == 7, [cs, s_], [ps])
        P.act(rs[:, :n], ps[:, :n], AF.Sqrt, [ps], [rs], bias=1e-6, scale=1.0 / 1024)
        P.op("vector", lambda e: e.reciprocal(rs[:, :n], rs[:, :n]), [rs], [rs])
        for k in range(8):
            s_ = sq[k % 2]
            P.tt(s_[:, :n], src[:, k, :n], rs[:, :n], ALU.mult, [src, rs], [s_])
            for (lo, hi, w_) in segs(c0, n):
                P.ts(dst[:, k, lo:hi], s_[:, lo:hi], A[:, w_, k:k + 1], nvt[:, (shl, shc)[w_], k:k + 1],
                     ALU.mult, ALU.add, [s_, A, nvt], [dst])
            if extra is not None:
                extra(k, s_)

    for (c0, n) in tiles:
        P.dma("sync", x_[:, :, :n], xT[:, :, c0:c0 + n], [xT], [x_], x_)
        norm_mod(x_, hT, A1, 2, 4, n, c0)
        for j in range(24):
            wb = wpiece(wg[j], wg)
            ps = PS()
            for k in range(8):
                P.mm(ps[:, :n], wb[:, k, :], hT[:, k, :n], k == 0, k == 7, [wb, hT], [ps])
            P.act(G[:, j, :n], ps[:, :n], AF.Sigmoid, [ps], [G])
        for b in range(2):
            s_ = stg[b % 2]
            P.dma("sync", s_[:, :, :n], yin[b][:, :, c0:c0 + n], [yin], [s_], s_)
            P.copy(ybf[b][:, :, :n], s_[:, :, :n], [s_], [ybf[b]])
        sa, sb_ = stg[0], stg[1]
        P.dma("sync", sa[:, :, :n], yin[2][:, :, c0:c0 + n], [yin], [sa], sa)
        P.dma("sync", sb_[:, :, :n], yin[3][:, :, c0:c0 + n], [yin], [sb_], sb_)
        P.tt(ysum[:, :, :n], sa[:, :, :n], sb_[:, :, :n], ALU.add, [sa, sb_], [ysum])
        P.dma("sync", sa[:, :, :n], yin[4][:, :, c0:c0 + n], [yin], [sa], sa)
        P.dma("sync", sb_[:, :, :n], yin[5][:, :, c0:c0 + n], [yin], [sb_], sb_)
        for c in range(4):
            ps = PS(); P.mm(ps[:, :n], blk, ysum[:, c, :n], True, True, [cs, ysum], [ps])
            P.stt(ysum[:, c, :n], ps[:, :n], -1.0 / 64, ysum[:, c, :n], ALU.mult, ALU.add, [ps, ysum], [ysum])
            P.act(tmp[:, c, :n], ysum[:, c, :n], AF.Square, [ysum], [tmp])
            ps = PS(); P.mm(ps[:, :n], blk, tmp[:, c, :n], True, True, [cs, tmp], [ps])
            P.act(tmp[:, c, :n], ps[:, :n], AF.Sqrt, [ps], [tmp], bias=64e-5, scale=1.0 / 64)
            P.op("vector", lambda e, c=c, n=n: e.reciprocal(tmp[:, c, :n], tmp[:, c, :n]), [tmp], [tmp])
            P.tt(ysum[:, c, :n], ysum[:, c, :n], tmp[:, c, :n], ALU.mult, [ysum, tmp], [ysum])
            P.ts(ysum[:, c, :n], ysum[:, c, :n], rwt[:, 0, c:c + 1], rwt[:, 1, c:c + 1], ALU.mult, ALU.add, [ysum, rwt], [ysum])
            P.tt(ysum[:, c, :n], ysum[:, c, :n], sb_[:, c, :n], ALU.add, [ysum, sb_], [ysum])
            P.tt(ybf[2][:, c, :n], ysum[:, c, :n], sa[:, c, :n], ALU.mult, [ysum, sa], [ybf[2]])
        for oc in range(8):
            for br in range(3):
                wb = wpiece(wo[br, oc], wo, 4)
                ps = PS()
                for k in range(4):
                    P.mm(ps[:, :n], wb[:, k, :], ybf[br][:, k, :n], k == 0, k == 3, [wb, ybf[br]], [ps])
                if br == 0:
                    P.tt(Mo[:, :n], ps[:, :n], G[:, oc, :n], ALU.mult, [ps, G], [Mo])
                else:
                    P.tt(t5[:, :n], ps[:, :n], G[:, br * 8 + oc, :n], ALU.mult, [ps, G], [t5])
                    P.tt(Mo[:, :n], Mo[:, :n], t5[:, :n], ALU.add, [Mo, t5], [Mo])
            P.copy(Mb[:, oc, :n], Mo[:, :n], [Mo], [Mb], eng="scalar")
        for oc in range(8):
            wb = wpiece(wout[oc], wout)
            ps = PS()
            for k in range(8):
                P.mm(ps[:, :n], wb[:, k, :], Mb[:, k, :n], k == 0, k == 7, [wb, Mb], [ps])
            for (lo, hi, w_) in segs(c0, n):
                P.stt(x_[:, oc, lo:hi], ps[:, lo:hi], nvt[:, 5 + w_, oc:oc + 1], x_[:, oc, lo:hi], ALU.mult, ALU.add,
                      [ps, nvt, x_], [x_])
        if moe:
            psr = PS()
            def extra(k, s_):
                for (lo, hi, w_) in segs(c0, n):
                    P.ts(t5[:, lo:hi], s_[:, lo:hi], A2[:, w_, k:k + 1], nvt[:, (9, 11)[w_], k:k + 1],
                         ALU.mult, ALU.add, [s_, A2, nvt], [t5])
                P.mm(psr[0:8, :n], rtt[:, k, :], t5[:, :n], k == 0, k == 7, [rtt, t5], [psr])
            norm_mod(x_, h2, A2, 9, 11, n, c0, extra)
            P.copy(lgT[:, :n], psr[0:8, :n], [psr], [lgT])
            for b0 in range(0, n, 128):
                ps = PS(); P.transpose(ps[:, 0:8], lgT[:, b0:b0 + 128], cs[0:8, 2, 0:8], [lgT, cs], [ps])
                P.copy(lg[:], ps[:, 0:8], [ps], [lg])
                P.op("vector", lambda e: e.max(top[:], lg[:]), [lg], [top])
                P.ts(sm[:, 0:1], top[:, 0:1], -1.0, None, ALU.mult, None, [top], [sm])
                P.act(sm[:, 1:2], top[:, 1:2], AF.Exp, [top, sm], [sm], bias=sm[:, 0:1])
                P.ts(sm[:, 2:3], sm[:, 1:2], 1.0, None, ALU.add, None, [sm], [sm])
                P.op("vector", lambda e: e.reciprocal(sm[:, 2:3], sm[:, 2:3]), [sm], [sm])
                P.tt(sm[:, 3:4], sm[:, 1:2], sm[:, 2:3], ALU.mult, [sm], [sm])
                P.ts(gt_[:], lg[:], top[:, 0:1], sm[:, 2:3], ALU.is_equal, ALU.mult, [lg, top, sm], [gt_])
                P.ts(g2_[:], lg[:], top[:, 1:2], sm[:, 3:4], ALU.is_equal, ALU.mult, [lg, top, sm], [g2_])
                P.tt(gt_[:], gt_[:], g2_[:], ALU.add, [gt_, g2_], [gt_])
                ps = PS(); P.transpose(ps[0:8, 0:128], gt_[:], ident, [gt_, cs], [ps])
                P.copy(gT[:, b0:b0 + 128], ps[0:8, 0:128], [ps], [gT])
        else:
            norm_mod(x_, h2, A2, 9, 11, n, c0)
        if moe:
            P.dma("gpsimd", h2o[:, :, c0:c0 + n], h2[:, :, :n], [h2], [h2o], h2)
            P.dma("gpsimd", gTo[:, c0:c0 + n], gT[:, :n], [gT], [gTo], gT)
        for e_ in range(0 if moe else NE):
            if moe:
                ps = PS(); P.mm(ps[:, :n], selt[:, e_, :], gT[:, :n], True, True, [selt, gT], [ps])
                P.copy(gbe[:, :n], ps[:, :n], [ps], [gbe], eng="scalar")
            for fc in range(FC):
                wb1 = wpiece(w1[e_, fc], w1)
                ps1 = PS()
                for k in range(8):
                    P.mm(ps1[:, :n], wb1[:, k, :], h2[:, k, :n], k == 0, k == 7, [wb1, h2], [ps1])
                wb3 = wpiece(w3[e_, fc], w3)
                ps3 = PS()
                for k in range(8):
                    P.mm(ps3[:, :n], wb3[:, k, :], h2[:, k, :n], k == 0, k == 7, [wb3, h2], [ps3])
                P.act(t5[:, :n], ps1[:, :n], AF.Silu, [ps1], [t5])
                if moe:
                    P.tt(t5[:, :n], t5[:, :n], gbe[:, :n], ALU.mult, [t5, gbe], [t5])
                P.tt(hid[:, fc, :n], t5[:, :n], ps3[:, :n], ALU.mult, [t5, ps3], [hid])
            for oc in range(8):
                ps = PS()
                for k0 in range(0, FC, 8):
                    kc = min(8, FC - k0)
                    wb = wpiece(w2[e_, oc][:, k0:k0 + kc, :], w2, kc)
                    for k in range(kc):
                        P.mm(ps[:, :n], wb[:, k, :], hid[:, k0 + k, :n], k0 + k == 0, k0 + k == FC - 1, [wb, hid], [ps])
                for (lo, hi, w_) in segs(c0, n):
                    P.stt(x_[:, oc, lo:hi], ps[:, lo:hi], nvt[:, 12 + w_, oc:oc + 1], x_[:, oc, lo:hi], ALU.mult, ALU.add,
                          [ps, nvt, x_], [x_])
        P.dma("gpsimd", out[:, :, c0:c0 + n], x_[:, :, :n], [x_], [out], x_)
    return P


def build_L4(NT):
    FC, QC = 28, 7
    tiles = [(c0, min(512, NT - c0)) for c0 in range(0, NT, 512)]
    P = Prog()
    I = lambda n, s, dt=F32: P.dram(n, s, dt, "ExternalInput")
    xm = I("xm", [128, 8, NT]); h2d = I("h2", [128, 8, NT], BF16); gTd = I("gT", [8, NT]); gt2d = I("gt2", [128, 8])
    seld = I("sel", [8, 8, 128])
    w1 = I("w1", [8, FC, 128, 8, 128]); w3 = I("w3", [8, FC, 128, 8, 128]); w2 = I("w2", [8, 8, 128, FC, 128])
    out = P.dram("out", [128, 8, NT], F32, "ExternalOutput")
    XM = P.sbuf([128, 8, NT], F32, "XM"); H2 = P.sbuf([128, 8, NT], BF16, "H2"); GT = P.sbuf([8, NT], F32, "GT")
    gt2 = P.sbuf([128, 8], F32, "gt2s"); selt = P.sbuf([8, 8, 128], F32, "selt")
    for (t, d) in ((XM, xm), (H2, h2d), (GT, gTd), (gt2, gt2d), (selt, seld)):
        P.dma("sync", t[:], d[:], [d], [t], t)
    gbe = P.sbuf([128, NT], F32, "gbe"); hid = P.sbuf([128, QC, NT], BF16, "hid")
    t5 = [P.sbuf([128, 512], F32, f"t5_{i}") for i in range(3)]
    wst = [P.sbuf([128, 8, 128], F32, f"wst{i}") for i in range(4)]
    wbf = [P.sbuf([128, 8, 128], BF16, f"wbf{i}") for i in range(5)]
    psb = [P.psum([128, 512], F32, f"psb{i}") for i in range(8)]
    pi = [0]; wi = [0]; ti = [0]
    def PS():
        pi[0] += 1
        return psb[pi[0] % 8]
    def wpiece(ap, dbuf, kc=8):
        wi[0] += 1
        ws, wb = wst[wi[0] % 4], wbf[wi[0] % 5]
        P.dma("sync", ws[:, :kc, :], ap, [dbuf], [ws], ws)
        P.copy(wb[:, :kc, :], ws[:, :kc, :], [ws], [wb], eng=("gpsimd", "scalar")[wi[0] % 2])
        return wb
    for e_ in range(8):
        for (c0, n) in tiles:
            ps = PS(); P.mm(ps[:, :n], selt[:, e_, :], GT[:, c0:c0 + n], True, True, [selt, GT], [ps])
            P.copy(gbe[:, c0:c0 + n], ps[:, :n], [ps], [gbe], eng="scalar")
        for f0 in range(0, FC, QC):
            for j in range(QC):
                wb1 = wpiece(w1[e_, f0 + j], w1)
                wb3 = wpiece(w3[e_, f0 + j], w3)
                for (c0, n) in tiles:
                    ps1 = PS()
                    for k in range(8):
                        P.mm(ps1[:, :n], wb1[:, k, :], H2[:, k, c0:c0 + n], k == 0, k == 7, [wb1, H2], [ps1])
                    ps3 = PS()
                    for k in range(8):
                        P.mm(ps3[:, :n], wb3[:, k, :], H2[:, k, c0:c0 + n], k == 0, k == 7, [wb3, H2], [ps3])
                    ti[0] += 1
                    t_ = t5[ti[0] % 3]
                    P.act(t_[:, :n], ps1[:, :n], AF.Silu, [ps1], [t_])
                    P.tt(t_[:, :n], t_[:, :n], gbe[:, c0:c0 + n], ALU.mult, [t_, gbe], [t_])
                    P.tt(hid[:, j, c0:c0 + n], t_[:, :n], ps3[:, :n], ALU.mult, [t_, ps3], [hid])
            for oc in range(8):
                wb = wpiece(w2[e_, oc][:, f0:f0 + QC, :], w2, QC)
                for (c0, n) in tiles:
                    ps = PS()
                    for k in range(QC):
                        P.mm(ps[:, :n], wb[:, k, :], hid[:, k, c0:c0 + n], k == 0, k == QC - 1, [wb, hid], [ps])
                    P.stt(XM[:, oc, c0:c0 + n], ps[:, :n], gt2[:, oc:oc + 1], XM[:, oc, c0:c0 + n], ALU.mult, ALU.add,
                          [ps, gt2, XM], [XM])
    P.dma("gpsimd", out[:], XM[:], [XM], [out], XM)
    return P


def arrw(W):
    K_, M_ = W.shape[0] // 128, W.shape[1] // 128
    return np.ascontiguousarray(W.reshape(K_, 128, M_, 128).transpose(2, 1, 0, 3))


def fmT(a):
    C = a.shape[0] // 128
    return a.reshape(C, 128, a.shape[1]).transpose(1, 0, 2)


def run_L3(inp, l, mod, x, ctx, o1, o2):
    SEQ = x.shape[0]
    moe = (l % 2 == 1)
    need_ctx = l < 1
    NL3 = SEQ // NCORES
    NT = NL3 + (256 if need_ctx else 0)
    mv = lambda g, w: mod[:, l, g * 8:(g + 1) * 8, w]
    nv = np.stack([fm(inp["norm1_g"][l]), mv(1, 0), mv(0, 0), mv(1, 1), mv(0, 1), mv(2, 0), mv(2, 1),
                   fm(inp["norm2_g"][l]), mv(4, 0), mv(3, 0), mv(4, 1), mv(3, 1), mv(5, 0), mv(5, 1)], 1)
    W = dict(nv=np.ascontiguousarray(nv, np.float32),
             rwo=np.ascontiguousarray(np.stack([fm(inp["rw_ln_g"][l]), fm(inp["rw_ln_b"][l])], 1)),
             wg=arrw(inp["w_in"][l][:, 4128:7200]),
             wo=np.stack([arrw(inp["mla_wo"][l]), arrw(inp["na_wo"][l]), arrw(inp["rw_wo"][l])], 0),
             wout=arrw(inp["w_out"][l]))
    cst = np.zeros((128, 3, 128), np.float32)
    cst[:, 0, :] = 1.0; cst[:64, 1, :64] = 1.0; cst[64:, 1, 64:] = 1.0; cst[:, 2, :] = np.eye(128)
    W["cst"] = cst
    if moe:
        W["rt"] = np.ascontiguousarray(inp["moe_router"][l // 2].reshape(8, 128, 8).transpose(1, 0, 2))
        sel = np.zeros((8, 8, 128), np.float32)
        for e in range(8):
            sel[e, e, :] = 1.0
        W["sel"] = sel
    else:
        W["w1"] = arrw(inp["ffn_w1"][l // 2])[None]; W["w3"] = arrw(inp["ffn_w3"][l // 2])[None]
        W["w2"] = arrw(inp["ffn_w2"][l // 2])[None]
    g_l, g_c = o1["rwgb"]
    srcs = [o2["ya"], o2["yb"], o2["yf"], o2["ybk"],
            (g_l[0].reshape(512, -1), g_c[0].reshape(512, 256)), (g_l[1].reshape(512, -1), g_c[1].reshape(512, 256))]
    in_maps = []
    for i in range(NCORES):
        sl = slice(i * NL3, (i + 1) * NL3)
        xs = x[sl]
        if need_ctx:
            xs = np.concatenate([xs, ctx], 0)
        m = dict(W)
        m["xT"] = np.ascontiguousarray(fmT(xs.T))
        ys = []
        for (lat, cx) in srcs:
            a = np.asarray(lat, np.float32)[:, sl]
            if need_ctx:
                a = np.concatenate([a, np.asarray(cx, np.float32)], 1)
            ys.append(fmT(a))
        m["yin"] = np.ascontiguousarray(np.stack(ys, 0))
        in_maps.append(m)
    res = run(build_L3(NT, NL3, moe), in_maps)
    if moe:
        W4 = dict(w1=np.stack([arrw(inp["moe_w1"][l // 2][e]) for e in range(8)], 0),
                  w3=np.stack([arrw(inp["moe_w3"][l // 2][e]) for e in range(8)], 0),
                  w2=np.stack([arrw(inp["moe_w2"][l // 2][e]) for e in range(8)], 0),
                  sel=W["sel"], gt2=np.ascontiguousarray(mv(5, 0), np.float32))
        maps4 = []
        for r in res:
            m4 = dict(W4)
            m4.update(xm=np.asarray(r["out"]), h2=np.asarray(r["h2o"]), gT=np.asarray(r["gTo"]))
            maps4.append(m4)
        res = run(build_L4(NT), maps4)
    outs = [np.asarray(r["out"]).transpose(2, 1, 0).reshape(NT, 1024) for r in res]
    x_new = np.concatenate([o[:NL3] for o in outs], 0)
    ctx_new = outs[0][NL3:] if need_ctx else ctx
    return x_new, ctx_new


def kernel(**inp):
    inp = {k: np.asarray(v) for k, v in inp.items()}
    SEQ = inp["x"].shape[1]
    x = np.ascontiguousarray(inp["x"][0]); ctx = np.ascontiguousarray(inp["ctx"][0])
    mod = run_L0(inp["c"], inp["c_ctx"], inp["mod_w"], inp["mod_b"])
    depth = inp["mod_w"].shape[0]
    for l in range(depth):
        o1 = run_L1(inp, l, mod, x, ctx, SEQ // 16, 2)
        o2 = run_L2(inp, l, o1, SEQ)
        del o1["qa"], o1["ka"], o1["va"], o1["nqkv"], o1["rw3"], o1["rwd"]
        x, ctx = run_L3(inp, l, mod, x, ctx, o1, o2)
    return np.ascontiguousarray(x[None].astype(np.float32))
```

```python
from contextlib import ExitStack
import numpy as np
import ml_dtypes
import concourse.bass as bass
import concourse.mybir as mybir
from concourse.bass_utils import run_bass_kernel_spmd

F32 = mybir.dt.float32
BF16 = mybir.dt.bfloat16
AF = mybir.ActivationFunctionType
ALU = mybir.AluOpType
AX = mybir.AxisListType
NCORES = 8
ENGINES = ("tensor", "vector", "scalar", "gpsimd", "sync")


class Buf:
    __slots__ = ("t", "name", "lw", "rd")

    def __init__(self, t, name):
        self.t = t
        self.name = name
        self.lw = None
        self.rd = {}

    def __getitem__(self, idx):
        return self.t[idx]


class Prog:
    def __init__(self):
        self.nc = bass.Bass("TRN2", target_bir_lowering=False)
        self.ops = {e: [] for e in ENGINES}
        self.count = {}
        self.waited = {e: {} for e in ENGINES}
        self.stack = ExitStack()
        self.nbuf = 0

    def sbuf(self, shape, dt, name=None):
        self.nbuf += 1
        name = name or f"sb{self.nbuf}"
        t = self.stack.enter_context(self.nc.sbuf_tensor(name, list(shape), dt))
        return Buf(t, name)

    def psum(self, shape, dt=F32, name=None):
        self.nbuf += 1
        name = name or f"ps{self.nbuf}"
        t = self.stack.enter_context(self.nc.psum_tensor(name, list(shape), dt))
        return Buf(t, name)

    def dram(self, name, shape, dt, kind):
        t = self.nc.dram_tensor(name, list(shape), dt, kind=kind).ap()
        return Buf(t, name)

    def _deps(self, eng, reads, writes, skip_self):
        need = {}

        def add(kv):
            if kv is None:
                return
            k, v = kv
            if need.get(k, 0) < v:
                need[k] = v

        for b in reads:
            add(b.lw)
        for b in writes:
            add(b.lw)
            for k, v in b.rd.items():
                add((k, v))
        w = self.waited[eng]
        out = []
        for k, v in need.items():
            if skip_self and k == eng:
                continue
            if w.get(k, 0) < v:
                w[k] = v
                out.append((k, v))
        return out

    def _commit(self, key, inc, reads, writes):
        v = self.count.get(key, 0) + inc
        self.count[key] = v
        for b in reads:
            if b.rd.get(key, 0) < v:
                b.rd[key] = v
        for b in writes:
            b.lw = (key, v)
            b.rd = {}
        return v

    def op(self, eng, fn, reads=(), writes=(), skip_self=False):
        waits = self._deps(eng, reads, writes, skip_self)
        self._commit(eng, 1, reads, writes)
        self.ops[eng].append((waits, fn, eng, 1))

    def dma(self, eng, out_ap, in_ap, reads, writes, sem_buf):
        waits = self._deps(eng, reads, writes, False)
        key = "d_" + sem_buf.name
        self._commit(key, 16, reads, writes)
        self.ops[eng].append((waits, lambda e: e.dma_start(out=out_ap, in_=in_ap), key, 16))

    def coll(self, kind, in_buf, out_buf, op=None):
        waits = self._deps("gpsimd", [in_buf], [out_buf], False)
        key = "c_" + out_buf.name
        self._commit(key, 16, [in_buf], [out_buf])
        ia, oa = in_buf.t, out_buf.t
        op = op or ALU.bypass
        self.ops["gpsimd"].append((waits, lambda e: e.collective_compute(
            kind, op, replica_groups=[list(range(NCORES))], ins=[ia], outs=[oa]), key, 16))

    def barrier(self):
        snap = dict(self.count)
        for e in ENGINES:
            w = self.waited[e]
            waits = [(k, v) for k, v in snap.items() if w.get(k, 0) < v]
            for k, v in waits:
                w[k] = v
            if waits:
                self.ops[e].append((waits, None, None, 0))

    def scratch(self, name, shape, dt, shared=False):
        t = self.nc.dram_tensor(name, list(shape), dt, addr_space=("Shared" if shared else "Local")).ap()
        return Buf(t, name)

    def mm(self, out_ap, lhsT_ap, rhs_ap, start, stop, reads, writes):
        self.op("tensor", lambda e: e.matmul(out_ap, lhsT_ap, rhs_ap, start=start, stop=stop),
                reads, writes, skip_self=True)

    def transpose(self, out_ap, in_ap, ident_ap, reads, writes):
        self.op("tensor", lambda e: e.transpose(out_ap, in_ap, ident_ap), reads, writes, skip_self=True)

    def act(self, out_ap, in_ap, func, reads, writes, bias=None, scale=None, eng="scalar"):
        kw = {}
        if bias is not None:
            kw["bias"] = bias
        if scale is not None:
            kw["scale"] = scale
        self.op(eng, lambda e: e.activation(out_ap, in_ap, func, **kw), reads, writes)

    def tt(self, out_ap, a_ap, b_ap, op, reads, writes, eng="vector"):
        self.op(eng, lambda e: e.tensor_tensor(out_ap, a_ap, b_ap, op), reads, writes)

    def ts(self, out_ap, a_ap, s1, s2, op0, op1, reads, writes, eng="vector"):
        if s2 is None:
            self.op(eng, lambda e: e.tensor_scalar(out_ap, a_ap, s1, None, op0), reads, writes)
        else:
            self.op(eng, lambda e: e.tensor_scalar(out_ap, a_ap, s1, s2, op0, op1), reads, writes)

    def stt(self, out_ap, in0, scalar, in1, op0, op1, reads, writes):
        self.op("vector", lambda e: e.scalar_tensor_tensor(out_ap, in0, scalar, in1, op0, op1), reads, writes)

    def copy(self, out_ap, in_ap, reads, writes, eng="vector"):
        if eng == "scalar":
            self.op(eng, lambda e: e.copy(out_ap, in_ap), reads, writes)
        else:
            self.op(eng, lambda e: e.tensor_copy(out_ap, in_ap), reads, writes)

    def memset(self, ap, val, writes, eng="vector"):
        self.op(eng, lambda e: e.memset(ap, val), (), writes)

    def finish(self):
        nc = self.nc
        final_waits = []
        w = self.waited["sync"]
        for k, v in self.count.items():
            if w.get(k, 0) < v:
                final_waits.append((k, v))
        keys = list(self.count.keys())
        sems = {}
        for i, k in enumerate(keys):
            sems[k] = self.stack.enter_context(nc.semaphore(f"s{i}"))
        ops = self.ops

        def replay(e, lst):
            for waits, fn, key, inc in lst:
                for (k, v) in waits:
                    e.wait_ge(sems[k], v)
                if fn is not None:
                    fn(e).then_inc(sems[key], inc)

        with nc.Block() as block:
            @block.tensor
            def _(e):
                replay(e, ops["tensor"])

            @block.vector
            def _(e):
                replay(e, ops["vector"])

            @block.scalar
            def _(e):
                replay(e, ops["scalar"])

            @block.gpsimd
            def _(e):
                replay(e, ops["gpsimd"])

            @block.sync
            def _(e):
                replay(e, ops["sync"])
                for (k, v) in final_waits:
                    e.wait_ge(sems[k], v)
        self.stack.close()
        return nc


TRACE = False
TIMES = []


def run(prog, in_maps):
    nc = prog.finish()
    if TRACE:
        res = run_bass_kernel_spmd(nc, in_maps, core_ids=list(range(NCORES)), trace=True)
        TIMES.append(res.exec_time_ns)
        print("exec_time_ns", res.exec_time_ns, flush=True)
    else:
        res = run_bass_kernel_spmd(nc, in_maps, core_ids=list(range(NCORES)))
    return res.results


def build_L0(nch):
    P = Prog()
    w = P.dram("w", [1024, nch * 128], F32, "ExternalInput")
    b = P.dram("b", [128, nch], F32, "ExternalInput")
    cT = P.dram("cT", [128, 8, 2], F32, "ExternalInput")
    out = P.dram("out", [128, nch, 2], F32, "ExternalOutput")
    wt = P.sbuf([128, 8, nch * 128], F32, "wt")
    bt = P.sbuf([128, nch], F32, "bt")
    ct = P.sbuf([128, 8, 2], F32, "ct")
    st = P.sbuf([128, 8, 2], F32, "st")
    ot = P.sbuf([128, nch, 2], F32, "ot")
    ps = P.psum([128, nch, 2], F32, "ps0")
    P.dma("sync", ct[:], cT[:], [cT], [ct], ct)
    P.dma("sync", bt[:], b[:], [b], [bt], bt)
    for k in range(8):
        P.dma("sync", wt[:, k, :], w[k * 128:(k + 1) * 128, :], [w], [wt], wt)
    P.act(st[:], ct[:], AF.Silu, [ct], [st])
    for j in range(nch):
        for k in range(8):
            P.mm(ps[:, j, :], wt[:, k, j * 128:(j + 1) * 128], st[:, k, :], k == 0, k == 7, [wt, st], [ps])
    for j in range(nch):
        P.ts(ot[:, j, :], ps[:, j, :], bt[:, j:j + 1], None, ALU.add, None, [ps, bt], [ot])
    P.dma("sync", out[:], ot[:], [ot], [out], ot)
    return P


def fm(v):
    v = np.asarray(v, np.float32)
    return np.ascontiguousarray(v.reshape(-1, 128).T)


def run_L0(c, c_ctx, mod_w, mod_b):
    depth = mod_w.shape[0]
    nch_total = depth * 48
    nch = nch_total // NCORES
    wcat = np.concatenate([mod_w[l] for l in range(depth)], axis=1)
    bcat = np.concatenate([mod_b[l] for l in range(depth)], axis=0)
    cT = np.stack([fm(c.reshape(-1)), fm(c_ctx.reshape(-1))], axis=-1)
    in_maps = []
    for i in range(NCORES):
        sl = slice(i * nch * 128, (i + 1) * nch * 128)
        in_maps.append({"w": np.ascontiguousarray(wcat[:, sl]), "b": fm(bcat[sl]), "cT": cT})
    res = run(build_L0(nch), in_maps)
    o = np.concatenate([r["out"] for r in res], axis=1)
    return o.reshape(128, depth, 48, 2)


RW_ORDER = [12, 13, 14, 0, 4, 8, 1, 5, 9, 2, 6, 10, 3, 7, 11]


def build_L1(NL, NSUB):
    NS = NL + 260
    tiles = [(c0, min(512, NS - c0)) for c0 in range(0, NS, 512)]
    P = Prog()
    I = lambda n, s, dt=F32: P.dram(n, s, dt, "ExternalInput")
    O = lambda n, s, dt=F32: P.dram(n, s, dt, "ExternalOutput")
    xT_ = I("xT", [NSUB, 128, 8, NS])
    nv = I("nv", [128, 5, 8])
    wq = I("wq", [33, 128, 8, 128])
    ropeC_ = I("ropeC", [NSUB, 128, NS]); ropeS_ = I("ropeS", [NSUB, 128, NS]); mask_ = I("mask", [NSUB, 128, NS], BF16)
    sp = I("sp", [128, 16])
    wuq = I("wuq", [128, 3, 8, 128]); wuk = I("wuk", [128, 2, 8, 128]); wuv = I("wuv", [128, 2, 4, 128])
    rwp = I("rwp", [128, 58])
    w2d = I("w2", [128, 512]); a2d = I("a2", [128, 512]); g2d = I("g2", [128, 512])
    cst = I("cst", [128, 3, 128])
    o_qa_ = O("qa", [NSUB, 8, 128, NS], BF16); o_ka_ = O("ka", [NSUB, 8, 128, NS], BF16)
    o_va_ = O("va", [NSUB, 4, 128, NS], BF16)
    o_n_ = O("nqkv", [NSUB, 3, 4, 128, NS], BF16)
    o_r3_ = O("rw3", [NSUB, 3, 4, 128, NS])
    o_d3_ = O("rwd", [NSUB, 3, 2, 4, 128, NS])
    o_gb_ = O("rwgb", [NSUB, 2, 4, 128, NS])

    def load(d, shape, dt=F32, name=None):
        t = P.sbuf(shape, dt, name)
        P.dma("sync", t[:], d[:], [d], [t], t)
        return t
    nvt = load(nv, [128, 5, 8]); spt = load(sp, [128, 16]); rwt = load(rwp, [128, 58])
    cs = load(cst, [128, 3, 128])
    w2t = load(w2d, [128, 512]); a2t = load(a2d, [128, 512]); g2t = load(g2d, [128, 512])
    stg = P.sbuf([128, 3 * 8 * 128], F32, "stg")
    wuqb = P.sbuf([128, 3, 8, 128], BF16); wukb = P.sbuf([128, 2, 8, 128], BF16); wuvb = P.sbuf([128, 2, 4, 128], BF16)
    for (src, dstb, nel) in ((wuq, wuqb, 3 * 8 * 128), (wuk, wukb, 2 * 8 * 128), (wuv, wuvb, 2 * 4 * 128)):
        P.dma("sync", stg[:, :nel], src[:].rearrange("p a b c -> p (a b c)"), [src], [stg], stg)
        P.copy(dstb[:].rearrange("p a b c -> p (a b c)"), stg[:, :nel], [stg], [dstb], eng="gpsimd")
    ones = cs[:, 0, :]; blk = cs[:, 1, :]; rot = cs[:, 2, :]
    At = P.sbuf([128, 2, 8], F32)
    for w_, sc in ((0, 1), (1, 3)):
        P.stt(At[:, w_, :], nvt[:, sc, :], 1.0, nvt[:, 0, :], ALU.add, ALU.mult, [nvt], [At])
    m2 = P.sbuf([128, 15], F32)
    P.tt(m2[:], rwt[:, 0:15], rwt[:, 15:30], ALU.add, [rwt], [m2])
    P.ts(m2[:], m2[:], -1.0, 1.0, ALU.mult, ALU.add, [m2], [m2])

    psb = [P.psum([128, 512], F32, f"psb{i}") for i in range(6)]
    pi = [0]
    def PS():
        pi[0] += 1
        return psb[pi[0] % 6]
    slabs = [P.sbuf([128, NS], F32, f"sl{i}") for i in range(16)]
    bslabs = [P.sbuf([128, NS], BF16, f"bs{i}") for i in range(3)]
    bi = [0]
    def BS():
        bi[0] += 1
        return bslabs[bi[0] % 3]
    Ct = P.sbuf([128, NS], F32, "Ct"); St = P.sbuf([128, NS], F32, "St"); Mt = P.sbuf([128, NS], BF16, "Mt")
    hT = P.sbuf([128, 8, NS], BF16, "hT")
    x_ = P.sbuf([128, 8, 512], F32, "xt")
    sqt = [P.sbuf([128, 512], F32, f"sq{i}") for i in range(2)]
    rs = P.sbuf([128, 512], F32, "rs")
    wst = [P.sbuf([128, 8, 128], F32, f"wst{i}") for i in range(3)]
    wbf = [P.sbuf([128, 8, 128], BF16, f"wbf{i}") for i in range(3)]
    wi = [0]

    def proj(ci, dst, masked=False):
        wi[0] += 1
        ws, wb = wst[wi[0] % 3], wbf[wi[0] % 3]
        P.dma("sync", ws[:], wq[ci], [wq], [ws], ws)
        P.copy(wb[:], ws[:], [ws], [wb], eng=("gpsimd", "scalar")[wi[0] % 2])
        for (c0, n) in tiles:
            ps = PS()
            for k in range(8):
                P.mm(ps[:, :n], wb[:, k, :], hT[:, k, c0:c0 + n], k == 0, k == 7, [wb, hT], [ps])
            if masked:
                P.tt(dst[:, c0:c0 + n], ps[:, :n], Mt[:, c0:c0 + n], ALU.mult, [ps, Mt], [dst])
            else:
                P.copy(dst[:, c0:c0 + n], ps[:, :n], [ps], [dst], eng="scalar")

    def rstd_of(srcs, lhsT, dim, eps, dst, sqrt_only=False, tmp=None):
        tmp = tmp or slabs[12]
        for (c0, n) in tiles:
            ps = PS()
            for j, s in enumerate(srcs):
                P.act(tmp[:, c0:c0 + n], s[:, c0:c0 + n], AF.Square, [s], [tmp])
                P.mm(ps[:, :n], lhsT, tmp[:, c0:c0 + n], j == 0, j == len(srcs) - 1, [cs, tmp], [ps])
            P.act(dst[:, c0:c0 + n], ps[:, :n], AF.Sqrt, [ps], [dst], bias=eps, scale=1.0 / dim)
        if sqrt_only:
            P.ts(dst[:], dst[:], 1e-12, None, ALU.max, None, [dst], [dst])
        P.op("vector", lambda e: e.reciprocal(dst[:], dst[:]), [dst], [dst])

    def body(sub):
        D = lambda b, *idx: Buf(b.t[(sub,) + idx] if idx else b.t[sub], b.name + "_v")
        xT = D(xT_)
        o_qa, o_ka, o_va, o_n, o_r3, o_d3, o_gb = (D(o_qa_), D(o_ka_), D(o_va_), D(o_n_), D(o_r3_), D(o_d3_), D(o_gb_))
        P.dma("sync", Ct[:], ropeC_[sub], [ropeC_], [Ct], Ct)
        P.dma("sync", St[:], ropeS_[sub], [ropeS_], [St], St)
        P.dma("sync", Mt[:], mask_[sub], [mask_], [Mt], Mt)

        def out_dma(dram_ap, dram_buf, sb):
            P.dma("gpsimd", dram_ap, sb[:], [sb], [dram_buf], sb)

        for ti, (c0, n) in enumerate(tiles):
            P.dma("sync", x_[:, :, :n], xT[:, :, c0:c0 + n], [xT], [x_], x_)
            ps = PS()
            for k in range(8):
                s_ = sqt[k % 2]
                P.act(s_[:, :n], x_[:, k, :n], AF.Square, [x_], [s_])
                P.mm(ps[:, :n], ones, s_[:, :n], k == 0, k == 7, [cs, s_], [ps])
            P.act(rs[:, :n], ps[:, :n], AF.Sqrt, [ps], [rs], bias=1e-6, scale=1.0 / 1024)
            P.op("vector", lambda e, a=rs[:, :n]: e.reciprocal(a, a), [rs], [rs])
            for k in range(8):
                P.tt(x_[:, k, :n], x_[:, k, :n], rs[:, :n], ALU.mult, [x_, rs], [x_])
                for (a, b, w_, sh) in ((0, NL + 2, 0, 2), (NL + 2, NS, 1, 4)):
                    lo, hi = max(a, c0), min(b, c0 + n)
                    if lo < hi:
                        P.ts(hT[:, k, lo:hi], x_[:, k, lo - c0:hi - c0], At[:, w_, k:k + 1], nvt[:, sh, k:k + 1],
                             ALU.mult, ALU.add, [x_, At, nvt], [hT])

        cq = slabs[0:3]
        for j in range(3):
            proj(j, cq[j])
        r_ = slabs[3]
        rstd_of(cq, ones, 384.0, 1e-6, r_)
        cqn = [BS() for _ in range(3)]
        for j in range(3):
            P.stt(cqn[j][:], cq[j][:], spt[:, j:j + 1], r_[:], ALU.mult, ALU.mult, [cq[j], spt, r_], [cqn[j]])

        def head_finish(pre, gcol, dram_ap, dram_buf, ob, par=0):
            rr = (slabs[4], slabs[13])[par]
            rstd_of([pre], ones, 96.0, 1e-6, rr, tmp=(slabs[12], slabs[14])[par])
            P.stt(pre[:], pre[:], spt[:, gcol:gcol + 1], rr[:], ALU.mult, ALU.mult, [pre, spt, rr], [pre])
            rq = (slabs[5], slabs[15])[par]
            for (c0, n) in tiles:
                ps = PS()
                P.mm(ps[:, :n], rot, pre[:, c0:c0 + n], True, True, [cs, pre], [ps])
                P.tt(rq[:, c0:c0 + n], ps[:, :n], St[:, c0:c0 + n], ALU.mult, [ps, St], [rq])
            P.tt(pre[:], pre[:], Ct[:], ALU.mult, [pre, Ct], [pre])
            P.tt(ob[:], pre[:], rq[:], ALU.add, [pre, rq], [ob])
            out_dma(dram_ap, dram_buf, ob)

        obs = [slabs[8].t, slabs[9].t]
        qob = [P_q0, P_q1]
        for h in range(8):
            pre = slabs[6 + h % 2]
            for (c0, n) in tiles:
                ps = PS()
                for j in range(3):
                    P.mm(ps[:, :n], wuqb[:, j, h, :], cqn[j][:, c0:c0 + n], j == 0, j == 2, [wuqb, cqn[j]], [ps])
                P.copy(pre[:, c0:c0 + n], ps[:, :n], [ps], [pre], eng="scalar")
            head_finish(pre, 5, o_qa[h], o_qa, qob[h % 2], h % 2)

        ckv = slabs[0:2]
        for j in range(2):
            proj(3 + j, ckv[j])
        krp = slabs[2]
        proj(5, krp)
        rstd_of(ckv, ones, 256.0, 1e-6, r_)
        ckvn = [BS() for _ in range(2)]
        for j in range(2):
            P.stt(ckvn[j][:], ckv[j][:], spt[:, 3 + j:4 + j], r_[:], ALU.mult, ALU.mult, [ckv[j], spt, r_], [ckvn[j]])
        for h in range(8):
            pre = slabs[6 + h % 2]
            for (c0, n) in tiles:
                ps = PS()
                for j in range(2):
                    P.mm(ps[:, :n], wukb[:, j, h, :], ckvn[j][:, c0:c0 + n], j == 0, j == 1, [wukb, ckvn[j]], [ps])
                P.tt(pre[:, c0:c0 + n], ps[:, :n], krp[:, c0:c0 + n], ALU.add, [ps, krp], [pre])
            head_finish(pre, 6, o_ka[h], o_ka, qob[h % 2], h % 2)
        for c in range(4):
            ob = qob[c % 2]
            for (c0, n) in tiles:
                ps = PS()
                for j in range(2):
                    P.mm(ps[:, :n], wuvb[:, j, c, :], ckvn[j][:, c0:c0 + n], j == 0, j == 1, [wuvb, ckvn[j]], [ps])
                P.copy(ob[:, c0:c0 + n], ps[:, :n], [ps], [ob], eng="scalar")
            out_dma(o_va[c], o_va, ob)

        for which in range(3):
            for c in range(4):
                z = slabs[c % 2]
                proj(6 + which * 4 + c, z)
                ob = BS()
                if which < 2:
                    rr = (slabs[4], slabs[13])[c % 2]
                    rstd_of([z], blk, 64.0, 1e-6, rr, tmp=(slabs[12], slabs[14])[c % 2])
                    P.stt(ob[:], z[:], spt[:, 7 + which:8 + which], rr[:], ALU.mult, ALU.mult, [z, spt, rr], [ob])
                else:
                    P.copy(ob[:], z[:], [z], [ob])
                out_dma(o_n[which, c], o_n, ob)

        def shifted(ci_rw, dst, tmp):
            rwc = RW_ORDER[ci_rw]
            proj(18 + ci_rw, tmp, masked=True)
            P.ts(dst[:, 1:NS - 1], tmp[:, 1:NS - 1], m2[:, rwc:rwc + 1], None, ALU.mult, None, [tmp, m2], [dst])
            P.stt(dst[:, 1:NS - 1], tmp[:, 0:NS - 2], rwt[:, rwc:rwc + 1], dst[:, 1:NS - 1], ALU.mult, ALU.add,
                  [tmp, rwt, dst], [dst])
            P.stt(dst[:, 1:NS - 1], tmp[:, 2:NS], rwt[:, 15 + rwc:16 + rwc], dst[:, 1:NS - 1], ALU.mult, ALU.add,
                  [tmp, rwt, dst], [dst])
        tmp = slabs[11]
        wdT, adT, gdT = slabs[0], slabs[1], slabs[2]
        shifted(0, wdT, tmp); shifted(1, adT, tmp); shifted(2, gdT, tmp)
        P.act(wdT[:], wdT[:], AF.Tanh, [wdT], [wdT])
        P.act(gdT[:], gdT[:], AF.Sigmoid, [gdT], [gdT])
        for c in range(4):
            rT, kT, vT = slabs[3], slabs[4], slabs[5]
            shifted(3 + 3 * c, rT, tmp); shifted(4 + 3 * c, kT, tmp); shifted(5 + 3 * c, vT, tmp)
            P.dma("gpsimd", o_r3[0, c], rT[:], [rT], [o_r3], rT)
            P.dma("gpsimd", o_r3[1, c], vT[:], [vT], [o_r3], vT)
            kk = slabs[6]
            P.ts(kk[:], kT[:], rwt[:, 46 + c:47 + c], None, ALU.mult, None, [kT, rwt], [kk])
            rr = slabs[7]
            rstd_of([kk], blk, 1.0, 0.0, rr, sqrt_only=True)
            P.tt(kk[:], kk[:], rr[:], ALU.mult, [kk, rr], [kk])
            P.dma("gpsimd", o_r3[2, c], kk[:], [kk], [o_r3], kk)
            ksum = slabs[8]
            for d in range(2):
                lw, aa = slabs[9], slabs[10]
                pb = slice(d * 64, d * 64 + 64)
                for (c0, n) in tiles:
                    ps = PS()
                    P.mm(ps[:, :n], w2t[pb, c * 128:(c + 1) * 128], wdT[pb, c0:c0 + n], True, True, [w2t, wdT], [ps])
                    P.act(lw[:, c0:c0 + n], ps[:, :n], AF.Sigmoid, [ps, rwt], [lw],
                          bias=rwt[:, 30 + d * 4 + c:31 + d * 4 + c])
                    ps = PS()
                    P.mm(ps[:, :n], a2t[pb, c * 128:(c + 1) * 128], adT[pb, c0:c0 + n], True, True, [a2t, adT], [ps])
                    P.act(aa[:, c0:c0 + n], ps[:, :n], AF.Sigmoid, [ps, rwt], [aa],
                          bias=rwt[:, 38 + d * 4 + c:39 + d * 4 + c])
                P.ts(lw[:], lw[:], -float(np.exp(-0.5)), None, ALU.mult, None, [lw], [lw])
                P.dma("gpsimd", o_d3[0, d, c], lw[:], [lw], [o_d3], lw)
                bb = slabs[11]
                P.tt(bb[:], aa[:], kk[:], ALU.mult, [aa, kk], [bb])
                P.dma("gpsimd", o_d3[1, d, c], bb[:], [bb], [o_d3], bb)
                P.ts(aa[:], aa[:], -1.0, rwt[:, 50 + c:51 + c], ALU.add, ALU.mult, [aa, rwt], [aa])
                P.stt(aa[:], aa[:], 1.0, kT[:], ALU.add, ALU.mult, [aa, kT], [aa])
                P.dma("gpsimd", o_d3[2, d, c], aa[:], [aa], [o_d3], aa)
                if d == 0:
                    P.copy(ksum[:], aa[:], [aa], [ksum])
                else:
                    P.tt(ksum[:], ksum[:], aa[:], ALU.add, [ksum, aa], [ksum])
            P.stt(ksum[:], ksum[:], rwt[:, 54 + c:55 + c], rT[:], ALU.mult, ALU.mult, [ksum, rwt, rT], [ksum])
            gg, bo = slabs[9], slabs[10]
            for (c0, n) in tiles:
                ps = PS()
                P.mm(ps[:, :n], blk, ksum[:, c0:c0 + n], True, True, [cs, ksum], [ps])
                P.tt(bo[:, c0:c0 + n], ps[:, :n], vT[:, c0:c0 + n], ALU.mult, [ps, vT], [bo])
                ps = PS()
                P.mm(ps[:, :n], g2t[:, c * 128:(c + 1) * 128], gdT[:, c0:c0 + n], True, True, [g2t, gdT], [ps])
                P.copy(gg[:, c0:c0 + n], ps[:, :n], [ps], [gg], eng="scalar")
            P.dma("gpsimd", o_gb[0, c], gg[:], [gg], [o_gb], gg)
            P.dma("gpsimd", o_gb[1, c], bo[:], [bo], [o_gb], bo)

    P_q0 = P.sbuf([128, NS], BF16, "qo0"); P_q1 = P.sbuf([128, NS], BF16, "qo1")
    for sub in range(NSUB):
        body(sub)
    return P


def bf16(a):
    return np.asarray(a).astype(ml_dtypes.bfloat16)


def rope_tables(t0, NL, NS):
    C = np.ones((128, NS), np.float32)
    S = np.zeros((128, NS), np.float32)
    pos = t0 + np.arange(NL)
    rows, cols = (pos // 64).astype(np.float32), (pos % 64).astype(np.float32)
    fr = np.exp(-np.log(10000.0) * np.arange(8, dtype=np.float32) / 8).astype(np.float32)
    ar = rows[None, :] * fr[:, None]
    ac = cols[None, :] * fr[:, None]
    for base, ang in ((64, ar), (72, ar), (80, ac), (88, ac)):
        C[base:base + 8, 1:NL + 1] = np.cos(ang)
        S[base:base + 8, 1:NL + 1] = np.sin(ang)
    return C, S


def consts_L1():
    cst = np.zeros((128, 3, 128), np.float32)
    cst[:, 0, :] = 1.0
    cst[:64, 1, :64] = 1.0
    cst[64:, 1, 64:] = 1.0
    for i in range(8):
        cst[72 + i, 2, 64 + i] = -1.0
        cst[64 + i, 2, 72 + i] = 1.0
        cst[88 + i, 2, 80 + i] = -1.0
        cst[80 + i, 2, 88 + i] = 1.0
    return cst


def prep_L1_weights(inp, l, mod):
    W = inp["w_in"][l]
    cols = []
    z128 = np.zeros((1024, 128), np.float32)
    for j in range(3):
        cols.append(W[:, j * 128:(j + 1) * 128])
    for j in range(2):
        cols.append(W[:, 384 + j * 128:384 + (j + 1) * 128])
    kr = z128.copy(); kr[:, 64:96] = W[:, 640:672]; cols.append(kr)
    for j in range(12):
        cols.append(W[:, 672 + j * 128:672 + (j + 1) * 128])
    for j in RW_ORDER:
        cols.append(W[:, 2208 + j * 128:2208 + (j + 1) * 128])
    Wp = np.stack(cols, 0)
    wq = np.ascontiguousarray(Wp.reshape(33, 8, 128, 128).transpose(0, 2, 1, 3))
    nv = np.stack([fm(inp["norm1_g"][l]), mod[:, l, 8:16, 0], mod[:, l, 0:8, 0], mod[:, l, 8:16, 1], mod[:, l, 0:8, 1]], 1)
    sp = np.zeros((128, 16), np.float32)
    sp[:, 0:3] = fm(inp["mla_cq_g"][l]); sp[:, 3:5] = fm(inp["mla_ckv_g"][l])
    sp[:96, 5] = inp["mla_qn_g"][l]; sp[:96, 6] = inp["mla_kn_g"][l]
    sp[:, 7] = np.tile(inp["na_qn_g"][l], 2); sp[:, 8] = np.tile(inp["na_kn_g"][l], 2)
    wuq = np.zeros((384, 8, 128), np.float32)
    wuq[:, :, :96] = inp["mla_wuq"][l].reshape(384, 8, 96)
    wuq = np.ascontiguousarray(wuq.reshape(3, 128, 8, 128).transpose(1, 0, 2, 3))
    kv = inp["mla_wukv"][l].reshape(256, 8, 128)
    wuk = np.zeros((256, 8, 128), np.float32); wuk[:, :, :64] = kv[:, :, :64]
    wuk = np.ascontiguousarray(wuk.reshape(2, 128, 8, 128).transpose(1, 0, 2, 3))
    wuv = np.ascontiguousarray(kv[:, :, 64:].reshape(256, 4, 128).reshape(2, 128, 4, 128).transpose(1, 0, 2, 3))
    rwp = np.zeros((128, 58), np.float32)
    rwp[:, 0:15] = fm(inp["rw_mu"][l][0]); rwp[:, 15:30] = fm(inp["rw_mu"][l][1])
    for d in range(2):
        rwp[:, 30 + d * 4:34 + d * 4] = fm(inp["rw_w0"][l][d]); rwp[:, 38 + d * 4:42 + d * 4] = fm(inp["rw_a0"][l][d])
    rwp[:, 46:50] = fm(inp["rw_kk"][l]); rwp[:, 50:54] = fm(inp["rw_ka"][l]); rwp[:, 54:58] = fm(inp["rw_rk"][l].reshape(-1))
    return dict(nv=np.ascontiguousarray(nv, np.float32), wq=wq, sp=sp, wuq=wuq, wuk=wuk, wuv=wuv, rwp=rwp,
                w2=np.ascontiguousarray(inp["rw_w2"][l].reshape(128, 512)),
                a2=np.ascontiguousarray(inp["rw_a2"][l].reshape(128, 512)),
                g2=np.ascontiguousarray(inp["rw_g2"][l]), cst=consts_L1())


def run_L1(inp, l, mod, x, ctx, NL, NSUB):
    SEQ = x.shape[0]
    NS = NL + 260
    wts = prep_L1_weights(inp, l, mod)
    in_maps = []
    for i in range(NCORES):
        xs, Cs, Ss, Ms = [], [], [], []
        for s in range(NSUB):
            t0 = (i * NSUB + s) * NL
            slab = np.zeros((NS, 1024), np.float32)
            M = np.ones((128, NS), np.float32)
            if t0 > 0:
                slab[0] = x[t0 - 1]
            else:
                M[:, 0] = 0
            slab[1:NL + 1] = x[t0:t0 + NL]
            if t0 + NL < SEQ:
                slab[NL + 1] = x[t0 + NL]
            else:
                M[:, NL + 1] = 0
            M[:, NL + 2] = 0; M[:, NS - 1] = 0
            slab[NL + 3:NL + 259] = ctx
            xs.append(slab.T.reshape(8, 128, NS).transpose(1, 0, 2))
            C, S = rope_tables(t0, NL, NS)
            Cs.append(C); Ss.append(S); Ms.append(bf16(M))
        m = dict(wts)
        m.update(xT=np.ascontiguousarray(np.stack(xs, 0)), ropeC=np.stack(Cs, 0), ropeS=np.stack(Ss, 0), mask=np.stack(Ms, 0))
        in_maps.append(m)
    res = run(build_L1(NL, NSUB), in_maps)
    out = {}
    for key in ("qa", "ka", "va", "nqkv", "rw3", "rwd", "rwgb"):
        lat = np.concatenate([res[i][key][s][..., 1:NL + 1] for i in range(NCORES) for s in range(NSUB)], axis=-1)
        cx = res[0][key][0][..., NL + 3:NL + 259]
        out[key] = (np.asarray(lat), np.asarray(cx))
    return out


def na_plan(SEQ):
    NR = SEQ // 64
    NP_ = NR // 2
    tabs = {}
    tab_list = []
    plan = []
    qc = np.arange(64)
    cs_ = np.clip(qc - 8, 0, 48)
    for m in range(NP_):
        rows = [2 * m, 2 * m + 1]
        rs = [int(np.clip(r - 4, 0, NR - 8)) for r in rows]
        kps = sorted({(a + i) // 2 for a in rs for i in range(8)})
        ent = []
        for kp in kps:
            sig = (rs[0] - rows[0], rs[1] - rows[1], kp - m)
            if sig not in tabs:
                dr = np.full((128, 128), -1, np.int64)
                dc = np.full((128, 128), -1, np.int64)
                for kl in range(128):
                    krow, kcol = 2 * kp + kl // 64, kl % 64
                    for ql in range(128):
                        qrow, qcol = rows[ql // 64], ql % 64
                        a = rs[ql // 64]
                        if a <= krow < a + 8 and cs_[qcol] <= kcol < cs_[qcol] + 16:
                            dr[kl, ql] = krow - qrow + 7
                            dc[kl, ql] = kcol - qcol + 15
                tabs[sig] = len(tab_list)
                tab_list.append((dr, dc))
            ent.append((kp, tabs[sig]))
        plan.append(ent)
    return plan, tab_list


def build_L2(SEQ):
    NK = SEQ + 256
    NKB = NK // 128
    NCH = NKB
    plan, tab_list = na_plan(SEQ)
    NTAB = len(tab_list)
    P = Prog()
    I = lambda n, s, dt=F32: P.dram(n, s, dt, "ExternalInput")
    O = lambda n, s, dt=F32: P.dram(n, s, dt, "ExternalOutput")
    qa = I("qa", [128, SEQ + 256], BF16)
    ka = I("ka", [128, NK], BF16)
    va = I("va", [128, NKB, 65], BF16)
    nq = I("nq", [64, SEQ + 256], BF16)
    nk = I("nk", [64, NK], BF16)
    nvv = I("nv", [128, NKB, 65], BF16)
    tabs = I("tabs", [128, NTAB, 128])
    cst = I("cst", [128, 6, 128])
    rtm = I("rtm", [2, NCH, 128, 4, 64])
    rfm = I("rfm", [2, NCH, 64, 4, 128])
    o_ya = O("ya", [64, SEQ + 256]); o_yb = O("yb", [64, SEQ + 256])
    o_y = O("y", [2, NCH, 128, 64])

    psb = [P.psum([128, 512], F32, f"psb{i}") for i in range(4)]
    psS2 = [P.psum([128, 1024], F32, f"psS{i}") for i in range(2)]
    cs = P.sbuf([128, 6, 128], F32, "cs")
    P.dma("sync", cs[:], cst[:], [cst], [cs], cs)
    triI, triS, triL, ident, ones = (cs[:, i, :] for i in range(5))
    csb = P.sbuf([128, 2, 128], BF16, "csb")
    P.copy(csb[:, 0, :], cs[:, 3, :], [cs], [csb])
    P.copy(csb[:, 1, :], cs[:, 4, :], [cs], [csb])

    pT = [P.sbuf([128, 1024], BF16, f"pT{i}") for i in range(3)]
    osb = [P.sbuf([64, 512], F32, f"osb{i}") for i in range(2)]
    rD = P.sbuf([64, 512], F32, "rD")
    dsb = P.sbuf([128, 512], F32, "dsb")
    cnt = [0]

    def finish_od(psOD, n, out_d, out_c0):
        psB = psb[2]
        P.copy(dsb[64:65, :n], psOD[64:65, :n], [psOD], [dsb], eng="scalar")
        P.mm(psB[0:64, :n], cs[64:65, 4, 0:64], dsb[64:65, :n], True, True, [cs, dsb], [psB])
        P.op("vector", lambda e: e.reciprocal(rD[:, :n], psB[0:64, :n]), [psB], [rD])
        ob = osb[cnt[0] % 2]
        P.tt(ob[:, :n], psOD[0:64, :n], rD[:, :n], ALU.mult, [psOD, rD], [ob])
        P.dma("gpsimd", out_d[:, out_c0:out_c0 + n], ob[:, :n], [ob], [out_d], ob)

    def attn(qT, q0, n, kT, V, kblocks, KD, scale, out_d, out_c0):
        cnt[0] += 1
        psOD = psb[cnt[0] % 2]
        pairs = [kblocks[i:i + 2] for i in range(0, len(kblocks), 2)]
        npair = len(pairs)

        def S(p):
            pS = psS2[p % 2]
            for j, kb in enumerate(pairs[p]):
                P.mm(pS[:, j * 512:j * 512 + n], kT[0:KD, kb * 128:(kb + 1) * 128], qT[0:KD, q0:q0 + n], True, True, [kT, qT], [pS])
        S(0)
        for p, pr in enumerate(pairs):
            if p + 1 < npair:
                S(p + 1)
            pS = psS2[p % 2]
            p_ = pT[p % 3]
            L = len(pr)
            sv = pS[:, :].rearrange("p (j c) -> p j c", c=512)[:, 0:L, 0:n]
            dv = p_[:, :].rearrange("p (j c) -> p j c", c=512)[:, 0:L, 0:n]
            P.act(dv, sv, AF.Exp, [pS], [p_], scale=scale)
            for j, kb in enumerate(pr):
                P.mm(psOD[0:65, :n], V[:, kb, :], p_[:, j * 512:j * 512 + n], p == 0 and j == 0,
                     p == npair - 1 and j == L - 1, [V, p_], [psOD])
        finish_od(psOD, n, out_d, out_c0)

    qat = P.sbuf([128, SEQ + 256], BF16, "qat"); kat = P.sbuf([128, NK], BF16, "kat"); vat = P.sbuf([128, NKB, 65], BF16, "vat")
    for (t, d) in ((qat, qa), (kat, ka), (vat, va)):
        P.dma("sync", t[:], d[:], [d], [t], t)
    sc_a = float(96 ** -0.5)
    for q0 in range(0, SEQ, 512):
        attn(qat, q0, min(512, SEQ - q0), kat, vat, list(range(NKB)), 128, sc_a, o_ya, q0)
    attn(qat, SEQ, 256, kat, vat, [0, 1], 128, sc_a, o_ya, SEQ)

    nqt, nkt, nvt = qat, kat, vat
    P.dma("sync", nqt[0:64, :], nq[:], [nq], [nqt], nqt)
    P.dma("sync", nkt[0:64, :], nk[:], [nk], [nkt], nkt)
    P.dma("sync", nvt[:], nvv[:], [nvv], [nvt], nvt)
    tb32 = P.sbuf([128, NTAB, 128], F32, "tb32"); tbb = P.sbuf([128, NTAB, 128], BF16, "tbb")
    P.dma("sync", tb32[:], tabs[:], [tabs], [tb32], tb32)
    P.ts(tbb[:], tb32[:], 8.0, None, ALU.mult, None, [tb32], [tbb])
    sc_b = 0.125
    def na_blocks(m):
        return [(kp, tid) for (kp, tid) in plan[m]] + [(NKB - 2, None), (NKB - 1, None)]

    def na_scores(m):
        pS = psS2[m % 2]
        qsl = nqt[0:64, m * 128:(m + 1) * 128]
        for j, (kb, tid) in enumerate(na_blocks(m)):
            o_ = pS[:, j * 128:(j + 1) * 128]
            P.mm(o_, nkt[0:64, kb * 128:(kb + 1) * 128], qsl, True, tid is None, [nkt, nqt], [pS])
            if tid is not None:
                P.mm(o_, csb[:, 0, :], tbb[:, tid, :], False, True, [csb, tbb], [pS])

    na_scores(0)
    for m in range(len(plan)):
        if m + 1 < len(plan):
            na_scores(m + 1)
        blocks = na_blocks(m)
        cnt[0] += 1
        psOD = psb[cnt[0] % 2]
        p_ = pT[cnt[0] % 3]
        pS = psS2[m % 2]
        w = len(blocks) * 128
        P.act(p_[:, :w], pS[:, :w], AF.Exp, [pS], [p_], scale=sc_b)
        for j, (kb, tid) in enumerate(blocks):
            P.mm(psOD[0:65, :128], nvt[:, kb, :], p_[:, j * 128:(j + 1) * 128], j == 0, j == len(blocks) - 1, [nvt, p_], [psOD])
        finish_od(psOD, 128, o_yb, m * 128)
    attn(nqt, SEQ, 256, nkt, nvt, [NKB - 2, NKB - 1], 64, sc_b, o_yb, SEQ)

    pi = [0]
    rw_ps = psb + psS2
    def PS():
        pi[0] += 1
        return rw_ps[pi[0] % 6]
    NSET = 4
    W = lambda n, shape=(128, 128): [P.sbuf(list(shape), F32, f"{n}{i}") for i in range(NSET)]
    tmb, fmb = W("tm", (128, 4, 64)), W("fmj", (64, 4, 128))
    eLr = W("eLr", (128, 64)); Bh = W("Bh", (128, 64)); Kh = W("Kh", (128, 64))
    e1 = W("e1", (64, 128)); e2 = W("e2", (64, 128)); e3 = W("e3", (64, 128))
    Rt = W("Rt", (64, 128)); KKt = W("KKt", (64, 128)); Bt = W("Bt", (64, 128)); Kt = W("Kt", (64, 128))
    Nn = W("Nn"); NTt = W("NTt"); Mk = W("Mk"); Mbp = W("Mbp"); Mkp = W("Mkp")
    Pq = W("Pq"); PqT = W("PqT"); Tm = W("Tm")
    Zs = W("Zs", (128, 64)); nU = W("nU", (128, 64)); Ys = W("Ys", (128, 64))
    gC = W("gC", (64, 1))
    ST = [P.sbuf([64, 64], F32, f"ST{d}") for d in range(2)]
    for d in range(2):
        P.memset(ST[d][:], 0.0, [ST[d]])

    def chunk_gen(c, d, s):
        tm, fj = tmb[s], fmb[s]
        P.dma("sync", tm[:], rtm[d, c], [rtm], [tm], tm)
        P.dma("sync", fj[:], rfm[d, c], [rfm], [fj], fj)
        yield
        lw_tok = tm[:, 0, :]
        ps = PS(); P.mm(ps[:, 0:64], triL, lw_tok, True, True, [cs, tm], [ps])
        P.act(eLr[s][:], ps[:, 0:64], AF.Exp, [ps], [eLr[s]])
        yield
        ps = PS(); P.mm(ps[0:64, 0:128], lw_tok, triI, True, True, [tm, cs], [ps])
        P.act(e1[s][:], ps[0:64, 0:128], AF.Exp, [ps], [e1[s]])
        P.act(e2[s][:], ps[0:64, 0:128], AF.Exp, [ps], [e2[s]], scale=-1.0)
        yield
        ps = PS(); P.mm(ps[0:64, 0:128], lw_tok, triS, True, True, [tm, cs], [ps])
        P.act(e3[s][:], ps[0:64, 0:128], AF.Exp, [ps], [e3[s]])
        yield
        P.tt(Bh[s][:], tm[:, 1, :], eLr[s][:], ALU.mult, [tm, eLr[s]], [Bh[s]], eng="gpsimd")
        P.tt(Kh[s][:], tm[:, 2, :], eLr[s][:], ALU.mult, [tm, eLr[s]], [Kh[s]], eng="gpsimd")
        P.copy(gC[s][:], e1[s][:, 127:128], [e1[s]], [gC[s]], eng="gpsimd")
        P.tt(Bt[s][:], fj[:, 0, :], e2[s][:], ALU.mult, [fj, e2[s]], [Bt[s]])
        P.tt(Kt[s][:], fj[:, 1, :], e2[s][:], ALU.mult, [fj, e2[s]], [Kt[s]], eng="gpsimd")
        P.tt(KKt[s][:], fj[:, 2, :], e3[s][:], ALU.mult, [fj, e3[s]], [KKt[s]])
        P.tt(Rt[s][:], fj[:, 3, :], e1[s][:], ALU.mult, [fj, e1[s]], [Rt[s]], eng="gpsimd")
        yield
        for (dst, l_, r_, msk) in ((Nn[s], Bt[s], KKt[s], triS), (NTt[s], KKt[s], Bt[s], triL),
                                  (Mk[s], Kt[s], KKt[s], triS), (Mbp[s], Bt[s], Rt[s], triI), (Mkp[s], Kt[s], Rt[s], triI)):
            ps = PS(); P.mm(ps[:, 0:128], l_[:], r_[:], True, True, [l_, r_], [ps])
            P.tt(dst[:], ps[:, 0:128], msk, ALU.mult, [ps, cs], [dst])
            yield
        P.tt(Tm[s][:], ident, Nn[s][:], ALU.subtract, [cs, Nn[s]], [Tm[s]])
        Pc, PcT = Nn[s], NTt[s]
        for it in range(6):
            nxt, nxtT = (Pq[s], PqT[s]) if Pc is not Pq[s] else (Nn[s], NTt[s])
            ps = PS(); P.mm(ps[:, 0:128], Pc[:], PcT[:], True, True, [Pc, PcT], [ps])
            P.copy(nxtT[:], ps[:, 0:128], [ps], [nxtT], eng="scalar")
            if it < 5:
                ps = PS(); P.mm(ps[:, 0:128], PcT[:], Pc[:], True, True, [Pc, PcT], [ps])
                P.copy(nxt[:], ps[:, 0:128], [ps], [nxt])
            yield
            ps = PS(); P.mm(ps[:, 0:128], nxtT[:], Tm[s][:], True, True, [nxtT, Tm[s]], [ps])
            P.tt(Tm[s][:], Tm[s][:], ps[:, 0:128], ALU.add, [Tm[s], ps], [Tm[s]])
            Pc, PcT = nxt, nxtT
            yield
        vt = tm[:, 3, :]
        ps = PS()
        P.mm(ps[:, 0:64], KKt[s][:], ST[d][:], True, False, [KKt[s], ST[d]], [ps])
        P.mm(ps[:, 0:64], Mk[s][:], vt, False, True, [Mk[s], tm], [ps])
        P.copy(Zs[s][:], ps[:, 0:64], [ps], [Zs[s]])
        yield
        ps = PS(); P.mm(ps[:, 0:64], Tm[s][:], Zs[s][:], True, True, [Tm[s], Zs[s]], [ps])
        P.ts(nU[s][:], ps[:, 0:64], -1.0, None, ALU.mult, None, [ps], [nU[s]])
        yield
        ps2 = PS()
        P.mm(ps2[0:64, 0:64], Bh[s][:], nU[s][:], True, False, [Bh[s], nU[s]], [ps2])
        P.mm(ps2[0:64, 0:64], Kh[s][:], vt, False, True, [Kh[s], tm], [ps2])
        ps = PS()
        P.mm(ps[:, 0:64], Rt[s][:], ST[d][:], True, False, [Rt[s], ST[d]], [ps])
        P.mm(ps[:, 0:64], Mbp[s][:], nU[s][:], False, False, [Mbp[s], nU[s]], [ps])
        P.mm(ps[:, 0:64], Mkp[s][:], vt, False, True, [Mkp[s], tm], [ps])
        P.stt(ST[d][:], ST[d][:], gC[s][:, 0:1], ps2[0:64, 0:64], ALU.mult, ALU.add, [ST[d], gC[s], ps2], [ST[d]])
        P.copy(Ys[s][:], ps[:, 0:64], [ps], [Ys[s]], eng="scalar")
        P.dma("gpsimd", o_y[d, c], Ys[s][:], [Ys[s]], [o_y], Ys[s])
        yield

    tasks = [(c, d) for c in range(NCH) for d in range(2)]
    active = []
    nxt_task = 0
    rounds = 0
    while nxt_task < len(tasks) or active:
        if nxt_task < len(tasks) and len(active) < NSET and (rounds % 7 == 0 or not active):
            c, d = tasks[nxt_task]
            active.append(chunk_gen(c, d, nxt_task % NSET))
            nxt_task += 1
        rounds += 1
        for g in list(active):
            try:
                next(g)
            except StopIteration:
                active.remove(g)
    return P


def consts_L2():
    c = np.zeros((128, 6, 128), np.float32)
    i = np.arange(128)
    c[:, 0, :] = (i[:, None] <= i[None, :])
    c[:, 1, :] = (i[:, None] < i[None, :])
    c[:, 2, :] = (i[:, None] > i[None, :])
    c[:, 3, :] = np.eye(128)
    c[:, 4, :] = 1.0
    return c


def tokmaj(a, aug=False):
    n = a.shape[1]
    t = a.T.reshape(n // 128, 128, 64).transpose(1, 0, 2)
    if aug:
        t = np.concatenate([t, np.ones((128, n // 128, 1), t.dtype)], axis=2)
    return np.ascontiguousarray(t)


def run_L2(inp, l, o1, SEQ):
    NK = SEQ + 256
    NCH = NK // 128
    plan, tab_list = na_plan(SEQ)
    cst = consts_L2()
    in_maps = []
    f32 = lambda a: np.asarray(a, np.float32)
    hs = lambda pair, h: (pair[0].reshape(-1, pair[0].shape[-1])[h * 64:(h + 1) * 64],
                          pair[1].reshape(-1, 256)[h * 64:(h + 1) * 64])
    for h in range(NCORES):
        m = {"cst": cst}
        m["qa"] = np.ascontiguousarray(np.concatenate([o1["qa"][0][h], o1["qa"][1][h]], 1))
        m["ka"] = np.ascontiguousarray(np.concatenate([o1["ka"][1][h], o1["ka"][0][h]], 1))
        vl, vc = hs(o1["va"], h)
        m["va"] = tokmaj(np.concatenate([vc, vl], 1), aug=True)
        n_l, n_c = o1["nqkv"]
        sel = lambda w: (n_l[w].reshape(512, -1)[h * 64:(h + 1) * 64], n_c[w].reshape(512, 256)[h * 64:(h + 1) * 64])
        m["nq"] = np.ascontiguousarray(np.concatenate(sel(0), 1))
        m["nk"] = np.ascontiguousarray(np.concatenate(sel(1), 1))
        m["nv"] = tokmaj(np.concatenate(sel(2), 1), aug=True)
        rpb = inp["na_rpb"][l][h]
        tb = np.stack([np.where(dr >= 0, rpb[np.maximum(dr, 0), np.maximum(dc, 0)], np.float32(-30000.0)) for dr, dc in tab_list], 0)
        m["tabs"] = np.ascontiguousarray(tb.transpose(1, 0, 2).astype(np.float32))
        r3l, r3c = o1["rw3"]; rdl, rdc = o1["rwd"]
        def seqs(lat, cx, d):
            lat = lat.reshape(512, -1)[h * 64:(h + 1) * 64]; cx = cx.reshape(512, 256)[h * 64:(h + 1) * 64]
            return np.concatenate([cx, lat], 1) if d == 0 else np.concatenate([cx[:, ::-1], lat[:, ::-1]], 1)
        rtm = np.zeros((2, NCH, 128, 4, 64), np.float32); rfm = np.zeros((2, NCH, 64, 4, 128), np.float32)
        for d in range(2):
            lw = seqs(rdl[0, d], rdc[0, d], d); b_ = seqs(rdl[1, d], rdc[1, d], d); kd = seqs(rdl[2, d], rdc[2, d], d)
            r_ = seqs(r3l[0], r3c[0], d); v_ = seqs(r3l[1], r3c[1], d); kk = seqs(r3l[2], r3c[2], d)
            for j, a in enumerate((lw, b_, kd, v_)):
                rtm[d, :, :, j, :] = a.T.reshape(NCH, 128, 64)
            for j, a in enumerate((b_, kd, kk, r_)):
                rfm[d, :, :, j, :] = a.reshape(64, NCH, 128).transpose(1, 0, 2)
        m["rtm"] = rtm; m["rfm"] = rfm
        in_maps.append(m)
    res = run(build_L2(SEQ), in_maps)
    ya = np.concatenate([f32(res[h]["ya"]) for h in range(NCORES)], 0)
    yb = np.concatenate([f32(res[h]["yb"]) for h in range(NCORES)], 0)
    ys = []
    for d in range(2):
        yy = np.concatenate([f32(res[h]["y"][d]).reshape(NK, 64).T for h in range(NCORES)], 0)
        cx, lat = yy[:, :256], yy[:, 256:]
        if d == 1:
            cx, lat = cx[:, ::-1], lat[:, ::-1]
        ys.append((np.ascontiguousarray(lat), np.ascontiguousarray(cx)))
    return dict(ya=(ya[:, :SEQ], ya[:, SEQ:]), yb=(yb[:, :SEQ], yb[:, SEQ:]), yf=ys[0], ybk=ys[1])


def build_L3(NT, NLAT, moe):
    FC = 28 if moe else 22
    NE = 8 if moe else 1
    tiles = [(c0, min(512, NT - c0)) for c0 in range(0, NT, 512)]
    P = Prog()
    I = lambda n, s, dt=F32: P.dram(n, s, dt, "ExternalInput")
    xT = I("xT", [128, 8, NT])
    yin = I("yin", [6, 128, 4, NT])
    nv = I("nv", [128, 14, 8])
    rwo = I("rwo", [128, 2, 4])
    wg = I("wg", [24, 128, 8, 128]); wo = I("wo", [3, 8, 128, 4, 128]); wout = I("wout", [8, 128, 8, 128])
    if not moe:
        w1 = I("w1", [NE, FC, 128, 8, 128]); w3 = I("w3", [NE, FC, 128, 8, 128]); w2 = I("w2", [NE, 8, 128, FC, 128])
    else:
        h2o = P.dram("h2o", [128, 8, NT], BF16, "ExternalOutput"); gTo = P.dram("gTo", [8, NT], F32, "ExternalOutput")
    cst = I("cst", [128, 3, 128])
    if moe:
        rt = I("rt", [128, 8, 8]); sel = I("sel", [8, 8, 128])
    out = P.dram("out", [128, 8, NT], F32, "ExternalOutput")

    def load(d, shape, dt=F32, name=None):
        t = P.sbuf(shape, dt, name)
        P.dma("sync", t[:], d[:], [d], [t], t)
        return t
    nvt = load(nv, [128, 14, 8]); rwt = load(rwo, [128, 2, 4]); cs = load(cst, [128, 3, 128])
    ones, blk, ident = cs[:, 0, :], cs[:, 1, :], cs[:, 2, :]
    if moe:
        rtt = load(rt, [128, 8, 8]); selt = load(sel, [8, 8, 128])
    A1 = P.sbuf([128, 2, 8], F32); A2 = P.sbuf([128, 2, 8], F32)
    for w_, sc in ((0, 1), (1, 3)):
        P.stt(A1[:, w_, :], nvt[:, sc, :], 1.0, nvt[:, 0, :], ALU.add, ALU.mult, [nvt], [A1])
    for w_, sc in ((0, 8), (1, 10)):
        P.stt(A2[:, w_, :], nvt[:, sc, :], 1.0, nvt[:, 7, :], ALU.add, ALU.mult, [nvt], [A2])

    psb = [P.psum([128, 512], F32, f"psb{i}") for i in range(8)]
    pi = [0]
    def PS():
        pi[0] += 1
        return psb[pi[0] % 8]
    x_ = P.sbuf([128, 8, 512], F32, "x"); hT = P.sbuf([128, 8, 512], BF16, "hT"); G = P.sbuf([128, 24, 512], BF16, "G")
    stg = [P.sbuf([128, 4, 512], F32, f"stg{i}") for i in range(2)]
    ybf = [P.sbuf([128, 4, 512], BF16, f"ybf{i}") for i in range(3)]
    ysum = P.sbuf([128, 4, 512], F32, "ysum"); tmp = P.sbuf([128, 4, 512], F32, "tmp")
    Mb = P.sbuf([128, 8, 512], BF16, "Mb"); Mo = P.sbuf([128, 512], F32, "Mo"); t5 = P.sbuf([128, 512], F32, "t5")
    h2 = P.sbuf([128, 8, 512], BF16, "h2"); hid = P.sbuf([128, 1 if moe else FC, 512], BF16, "hid")
    sq = [P.sbuf([128, 512], F32, f"sq{i}") for i in range(2)]; rs = P.sbuf([128, 512], F32, "rs")
    wst = [P.sbuf([128, 8, 128], F32, f"wst{i}") for i in range(6)]
    wbf = [P.sbuf([128, 8, 128], BF16, f"wbf{i}") for i in range(7)]
    wi = [0]
    if moe:
        lgT = P.sbuf([8, 512], F32, "lgT"); gT = P.sbuf([8, 512], F32, "gT"); gbe = P.sbuf([128, 512], F32, "gbe")
        lg = P.sbuf([128, 8], F32, "lg"); top = P.sbuf([128, 8], F32, "top"); sm = P.sbuf([128, 8], F32, "sm")
        gt_ = P.sbuf([128, 8], F32, "gt"); g2_ = P.sbuf([128, 8], F32, "g2")

    def wpiece(ap, dbuf, kc=8):
        wi[0] += 1
        ws, wb = wst[wi[0] % 6], wbf[wi[0] % 7]
        P.dma("sync", ws[:, :kc, :], ap, [dbuf], [ws], ws)
        P.copy(wb[:, :kc, :], ws[:, :kc, :], [ws], [wb], eng=("gpsimd", "scalar", "vector", "scalar")[wi[0] % 4])
        return wb

    def segs(c0, n):
        for (a, b, w_) in ((0, NLAT, 0), (NLAT, NT, 1)):
            lo, hi = max(a, c0), min(b, c0 + n)
            if lo < hi:
                yield lo - c0, hi - c0, w_

    def norm_mod(src, dst, A, shl, shc, n, c0, extra=None):
        ps = PS()
        for k in range(8):
            s_ = sq[k % 2]
            P.act(s_[:, :n], src[:, k, :n], AF.Square, [src], [s_])
            P.mm(ps[:, :n], ones, s_[:, :n], k == 0, k == 7, [cs, s_], [ps])
        P.act(rs[:, :n], ps[:, :n], AF.Sqrt, [ps], [rs], bias=1e-6, scale=1.0 / 1024)
        P.op("vector", lambda e: e.reciprocal(rs[:, :n], rs[:, :n]), [rs], [rs])
        for k in range(8):
            s_ = sq[k % 2]
            P.tt(s_[:, :n], src[:, k, :n], rs[:, :n], ALU.mult, [src, rs], [s_])
            for (lo, hi, w_) in segs(c0, n):
                P.ts(dst[:, k, lo:hi], s_[:, lo:hi], A[:, w_, k:k + 1], nvt[:, (shl, shc)[w_], k:k + 1],
                     ALU.mult, ALU.add, [s_, A, nvt], [dst])
            if extra is not None:
                extra(k, s_)

    for (c0, n) in tiles:
        P.dma("sync", x_[:, :, :n], xT[:, :, c0:c0 + n], [xT], [x_], x_)
        norm_mod(x_, hT, A1, 2, 4, n, c0)
        for j in range(24):
            wb = wpiece(wg[j], wg)
            ps = PS()
            for k in range(8):
                P.mm(ps[:, :n], wb[:, k, :], hT[:, k, :n], k == 0, k == 7, [wb, hT], [ps])
            P.act(G[:, j, :n], ps[:, :n], AF.Sigmoid, [ps], [G])
        for b in range(2):
            s_ = stg[b % 2]
            P.dma("sync", s_[:, :, :n], yin[b][:, :, c0:c0 + n], [yin], [s_], s_)
            P.copy(ybf[b][:, :, :n], s_[:, :, :n], [s_], [ybf[b]])
        sa, sb_ = stg[0], stg[1]
        P.dma("sync", sa[:, :, :n], yin[2][:, :, c0:c0 + n], [yin], [sa], sa)
        P.dma("sync", sb_[:, :, :n], yin[3][:, :, c0:c0 + n], [yin], [sb_], sb_)
        P.tt(ysum[:, :, :n], sa[:, :, :n], sb_[:, :, :n], ALU.add, [sa, sb_], [ysum])
        P.dma("sync", sa[:, :, :n], yin[4][:, :, c0:c0 + n], [yin], [sa], sa)
        P.dma("sync", sb_[:, :, :n], yin[5][:, :, c0:c0 + n], [yin], [sb_], sb_)
        for c in range(4):
            ps = PS(); P.mm(ps[:, :n], blk, ysum[:, c, :n], True, True, [cs, ysum], [ps])
            P.stt(ysum[:, c, :n], ps[:, :n], -1.0 / 64, ysum[:, c, :n], ALU.mult, ALU.add, [ps, ysum], [ysum])
            P.act(tmp[:, c, :n], ysum[:, c, :n], AF.Square, [ysum], [tmp])
            ps = PS(); P.mm(ps[:, :n], blk, tmp[:, c, :n], True, True, [cs, tmp], [ps])
            P.act(tmp[:, c, :n], ps[:, :n], AF.Sqrt, [ps], [tmp], bias=64e-5, scale=1.0 / 64)
            P.op("vector", lambda e, c=c, n=n: e.reciprocal(tmp[:, c, :n], tmp[:, c, :n]), [tmp], [tmp])
            P.tt(ysum[:, c, :n], ysum[:, c, :n], tmp[:, c, :n], ALU.mult, [ysum, tmp], [ysum])
            P.ts(ysum[:, c, :n], ysum[:, c, :n], rwt[:, 0, c:c + 1], rwt[:, 1, c:c + 1], ALU.mult, ALU.add, [ysum, rwt], [ysum])
            P.tt(ysum[:, c, :n], ysum[:, c, :n], sb_[:, c, :n], ALU.add, [ysum, sb_], [ysum])
            P.tt(ybf[2][:, c, :n], ysum[:, c, :n], sa[:, c, :n], ALU.mult, [ysum, sa], [ybf[2]])
        for oc in range(8):
            for br in range(3):
                wb = wpiece(wo[br, oc], wo, 4)
                ps = PS()
                for k in range(4):
                    P.mm(ps[:, :n], wb[:, k, :], ybf[br][:, k, :n], k == 0, k == 3, [wb, ybf[br]], [ps])
                if br == 0:
                    P.tt(Mo[:, :n], ps[:, :n], G[:, oc, :n], ALU.mult, [ps, G], [Mo])
                else:
                    P.tt(t5[:, :n], ps[:, :n], G[:, br * 8 + oc, :n], ALU.mult, [ps, G], [t5])
                    P.tt(Mo[:, :n], Mo[:, :n], t5[:, :n], ALU.add, [Mo, t5], [Mo])
            P.copy(Mb[:, oc, :n], Mo[:, :n], [Mo], [Mb], eng="scalar")
        for oc in range(8):
            wb = wpiece(wout[oc], wout)
            ps = PS()
            for k in range(8):
                P.mm(ps[:, :n], wb[:, k, :], Mb[:, k, :n], k == 0, k == 7, [wb, Mb], [ps])
            for (lo, hi, w_) in segs(c0, n):
                P.stt(x_[:, oc, lo:hi], ps[:, lo:hi], nvt[:, 5 + w_, oc:oc + 1], x_[:, oc, lo:hi], ALU.mult, ALU.add,
                      [ps, nvt, x_], [x_])
        if moe:
            psr = PS()
            def extra(k, s_):
                for (lo, hi, w_) in segs(c0, n):
                    P.ts(t5[:, lo:hi], s_[:, lo:hi], A2[:, w_, k:k + 1], nvt[:, (9, 11)[w_], k:k + 1],
                         ALU.mult, ALU.add, [s_, A2, nvt], [t5])
                P.mm(psr[0:8, :n], rtt[:, k, :], t5[:, :n], k == 0, k == 7, [rtt, t5], [psr])
            norm_mod(x_, h2, A2, 9, 11, n, c0, extra)
            P.copy(lgT[:, :n], psr[0:8, :n], [psr], [lgT])
            for b0 in range(0, n, 128):
                ps = PS(); P.transpose(ps[:, 0:8], lgT[:, b0:b0 + 128], cs[0:8, 2, 0:8], [lgT, cs], [ps])
                P.copy(lg[:], ps[:, 0:8], [ps], [lg])
                P.op("vector", lambda e: e.max(top[:], lg[:]), [lg], [top])
                P.ts(sm[:, 0:1], top[:, 0:1], -1.0, None, ALU.mult, None, [top], [sm])
                P.act(sm[:, 1:2], top[:, 1:2], AF.Exp, [top, sm], [sm], bias=sm[:, 0:1])
                P.ts(sm[:, 2:3], sm[:, 1:2], 1.0, None, ALU.add, None, [sm], [sm])
                P.op("vector", lambda e: e.reciprocal(sm[:, 2:3], sm[:, 2:3]), [sm], [sm])
                P.tt(sm[:, 3:4], sm[:, 1:2], sm[:, 2:3], ALU.mult, [sm], [sm])
                P.ts(gt_[:], lg[:], top[:, 0:1], sm[:, 2:3], ALU.is_equal, ALU.mult, [lg, top, sm], [gt_])
                P.ts(g2_[:], lg[:], top[:, 1:2], sm[:, 3:4], ALU.is_equal, ALU.mult, [lg, top, sm], [g2_])
                P.tt(gt_[:], gt_[:], g2_[:], ALU.add, [gt_, g2_], [gt_])
                ps = PS(); P.transpose(ps[0:8, 0:128], gt_[:], ident, [gt_, cs], [ps])
                P.copy(gT[:, b0:b0 + 128], ps[0:8, 0:128], [ps], [gT])
        else:
            norm_mod(x_, h2, A2, 9, 11, n, c0)
        if moe:
            P.dma("gpsimd", h2o[:, :, c0:c0 + n], h2[:, :, :n], [h2], [h2o], h2)
            P.dma("gpsimd", gTo[:, c0:c0 + n], gT[:, :n], [gT], [gTo], gT)
        for e_ in range(0 if moe else NE):
            if moe:
                ps = PS(); P.mm(ps[:, :n], selt[:, e_, :], gT[:, :n], True, True, [selt, gT], [ps])
                P.copy(gbe[:, :n], ps[:, :n], [ps], [gbe], eng="scalar")
            for fc in range(FC):
                wb1 = wpiece(w1[e_, fc], w1)
                ps1 = PS()
                for k in range(8):
                    P.mm(ps1[:, :n], wb1[:, k, :], h2[:, k, :n], k == 0, k == 7, [wb1, h2], [ps1])
                wb3 = wpiece(w3[e_, fc], w3)
                ps3 = PS()
                for k in range(8):
                    P.mm(ps3[:, :n], wb3[:, k, :], h2[:, k, :n], k == 0, k == 7, [wb3, h2], [ps3])
                P.act(t5[:, :n], ps1[:, :n], AF.Silu, [ps1], [t5])
                if moe:
                    P.tt(t5[:, :n], t5[:, :n], gbe[:, :n], ALU.mult, [t5, gbe], [t5])
                P.tt(hid[:, fc, :n], t5[:, :n], ps3[:, :n], ALU.mult, [t5, ps3], [hid])
            for oc in range(8):
                ps = PS()
                for k0 in range(0, FC, 8):
                    kc = min(8, FC - k0)
                    wb = wpiece(w2[e_, oc][:, k0:k0 + kc, :], w2, kc)
                    for k in range(kc):
                        P.mm(ps[:, :n], wb[:, k, :], hid[:, k0 + k, :n], k0 + k == 0, k0 + k == FC - 1, [wb, hid], [ps])
                for (lo, hi, w_) in segs(c0, n):
                    P.stt(x_[:, oc, lo:hi], ps[:, lo:hi], nvt[:, 12 + w_, oc:oc + 1], x_[:, oc, lo:hi], ALU.mult, ALU.add,
                          [ps, nvt, x_], [x_])
        P.dma("gpsimd", out[:, :, c0:c0 + n], x_[:, :, :n], [x_], [out], x_)
    return P


def build_L4(NT):
    FC, QC = 28, 7
    tiles = [(c0, min(512, NT - c0)) for c0 in range(0, NT, 512)]
    P = Prog()
    I = lambda n, s, dt=F32: P.dram(n, s, dt, "ExternalInput")
    xm = I("xm", [128, 8, NT]); h2d = I("h2", [128, 8, NT], BF16); gTd = I("gT", [8, NT]); gt2d = I("gt2", [128, 8])
    seld = I("sel", [8, 8, 128])
    w1 = I("w1", [8, FC, 128, 8, 128]); w3 = I("w3", [8, FC, 128, 8, 128]); w2 = I("w2", [8, 8, 128, FC, 128])
    out = P.dram("out", [128, 8, NT], F32, "ExternalOutput")
    XM = P.sbuf([128, 8, NT], F32, "XM"); H2 = P.sbuf([128, 8, NT], BF16, "H2"); GT = P.sbuf([8, NT], F32, "GT")
    gt2 = P.sbuf([128, 8], F32, "gt2s"); selt = P.sbuf([8, 8, 128], F32, "selt")
    for (t, d) in ((XM, xm), (H2, h2d), (GT, gTd), (gt2, gt2d), (selt, seld)):
        P.dma("sync", t[:], d[:], [d], [t], t)
    gbe = P.sbuf([128, NT], F32, "gbe"); hid = P.sbuf([128, QC, NT], BF16, "hid")
    t5 = [P.sbuf([128, 512], F32, f"t5_{i}") for i in range(3)]
    wst = [P.sbuf([128, 8, 128], F32, f"wst{i}") for i in range(4)]
    wbf = [P.sbuf([128, 8, 128], BF16, f"wbf{i}") for i in range(5)]
    psb = [P.psum([128, 512], F32, f"psb{i}") for i in range(8)]
    pi = [0]; wi = [0]; ti = [0]
    def PS():
        pi[0] += 1
        return psb[pi[0] % 8]
    def wpiece(ap, dbuf, kc=8):
        wi[0] += 1
        ws, wb = wst[wi[0] % 4], wbf[wi[0] % 5]
        P.dma("sync", ws[:, :kc, :], ap, [dbuf], [ws], ws)
        P.copy(wb[:, :kc, :], ws[:, :kc, :], [ws], [wb], eng=("gpsimd", "scalar")[wi[0] % 2])
        return wb
    for e_ in range(8):
        for (c0, n) in tiles:
            ps = PS(); P.mm(ps[:, :n], selt[:, e_, :], GT[:, c0:c0 + n], True, True, [selt, GT], [ps])
            P.copy(gbe[:, c0:c0 + n], ps[:, :n], [ps], [gbe], eng="scalar")
        for f0 in range(0, FC, QC):
            for j in range(QC):
                wb1 = wpiece(w1[e_, f0 + j], w1)
                wb3 = wpiece(w3[e_, f0 + j], w3)
                for (c0, n) in tiles:
                    ps1 = PS()
                    for k in range(8):
                        P.mm(ps1[:, :n], wb1[:, k, :], H2[:, k, c0:c0 + n], k == 0, k == 7, [wb1, H2], [ps1])
                    ps3 = PS()
                    for k in range(8):
                        P.mm(ps3[:, :n], wb3[:, k, :], H2[:, k, c0:c0 + n], k == 0, k == 7, [wb3, H2], [ps3])
                    ti[0] += 1
                    t_ = t5[ti[0] % 3]
                    P.act(t_[:, :n], ps1[:, :n], AF.Silu, [ps1], [t_])
                    P.tt(t_[:, :n], t_[:, :n], gbe[:, c0:c0 + n], ALU.mult, [t_, gbe], [t_])
                    P.tt(hid[:, j, c0:c0 + n], t_[:, :n], ps3[:, :n], ALU.mult, [t_, ps3], [hid])
            for oc in range(8):
                wb = wpiece(w2[e_, oc][:, f0:f0 + QC, :], w2, QC)
                for (c0, n) in tiles:
                    ps = PS()
                    for k in range(QC):
                        P.mm(ps[:, :n], wb[:, k, :], hid[:, k, c0:c0 + n], k == 0, k == QC - 1, [wb, hid], [ps])
                    P.stt(XM[:, oc, c0:c0 + n], ps[:, :n], gt2[:, oc:oc + 1], XM[:, oc, c0:c0 + n], ALU.mult, ALU.add,
                          [ps, gt2, XM], [XM])
    P.dma("gpsimd", out[:], XM[:], [XM], [out], XM)
    return P


def arrw(W):
    K_, M_ = W.shape[0] // 128, W.shape[1] // 128
    return np.ascontiguousarray(W.reshape(K_, 128, M_, 128).transpose(2, 1, 0, 3))


def fmT(a):
    C = a.shape[0] // 128
    return a.reshape(C, 128, a.shape[1]).transpose(1, 0, 2)


def run_L3(inp, l, mod, x, ctx, o1, o2):
    SEQ = x.shape[0]
    moe = (l % 2 == 1)
    need_ctx = l < 1
    NL3 = SEQ // NCORES
    NT = NL3 + (256 if need_ctx else 0)
    mv = lambda g, w: mod[:, l, g * 8:(g + 1) * 8, w]
    nv = np.stack([fm(inp["norm1_g"][l]), mv(1, 0), mv(0, 0), mv(1, 1), mv(0, 1), mv(2, 0), mv(2, 1),
                   fm(inp["norm2_g"][l]), mv(4, 0), mv(3, 0), mv(4, 1), mv(3, 1), mv(5, 0), mv(5, 1)], 1)
    W = dict(nv=np.ascontiguousarray(nv, np.float32),
             rwo=np.ascontiguousarray(np.stack([fm(inp["rw_ln_g"][l]), fm(inp["rw_ln_b"][l])], 1)),
             wg=arrw(inp["w_in"][l][:, 4128:7200]),
             wo=np.stack([arrw(inp["mla_wo"][l]), arrw(inp["na_wo"][l]), arrw(inp["rw_wo"][l])], 0),
             wout=arrw(inp["w_out"][l]))
    cst = np.zeros((128, 3, 128), np.float32)
    cst[:, 0, :] = 1.0; cst[:64, 1, :64] = 1.0; cst[64:, 1, 64:] = 1.0; cst[:, 2, :] = np.eye(128)
    W["cst"] = cst
    if moe:
        W["rt"] = np.ascontiguousarray(inp["moe_router"][l // 2].reshape(8, 128, 8).transpose(1, 0, 2))
        sel = np.zeros((8, 8, 128), np.float32)
        for e in range(8):
            sel[e, e, :] = 1.0
        W["sel"] = sel
    else:
        W["w1"] = arrw(inp["ffn_w1"][l // 2])[None]; W["w3"] = arrw(inp["ffn_w3"][l // 2])[None]
        W["w2"] = arrw(inp["ffn_w2"][l // 2])[None]
    g_l, g_c = o1["rwgb"]
    srcs = [o2["ya"], o2["yb"], o2["yf"], o2["ybk"],
            (g_l[0].reshape(512, -1), g_c[0].reshape(512, 256)), (g_l[1].reshape(512, -1), g_c[1].reshape(512, 256))]
    in_maps = []
    for i in range(NCORES):
        sl = slice(i * NL3, (i + 1) * NL3)
        xs = x[sl]
        if need_ctx:
            xs = np.concatenate([xs, ctx], 0)
        m = dict(W)
        m["xT"] = np.ascontiguousarray(fmT(xs.T))
        ys = []
        for (lat, cx) in srcs:
            a = np.asarray(lat, np.float32)[:, sl]
            if need_ctx:
                a = np.concatenate([a, np.asarray(cx, np.float32)], 1)
            ys.append(fmT(a))
        m["yin"] = np.ascontiguousarray(np.stack(ys, 0))
        in_maps.append(m)
    res = run(build_L3(NT, NL3, moe), in_maps)
    if moe:
        W4 = dict(w1=np.stack([arrw(inp["moe_w1"][l // 2][e]) for e in range(8)], 0),
                  w3=np.stack([arrw(inp["moe_w3"][l // 2][e]) for e in range(8)], 0),
                  w2=np.stack([arrw(inp["moe_w2"][l // 2][e]) for e in range(8)], 0),
                  sel=W["sel"], gt2=np.ascontiguousarray(mv(5, 0), np.float32))
        maps4 = []
        for r in res:
            m4 = dict(W4)
            m4.update(xm=np.asarray(r["out"]), h2=np.asarray(r["h2o"]), gT=np.asarray(r["gTo"]))
            maps4.append(m4)
        res = run(build_L4(NT), maps4)
    outs = [np.asarray(r["out"]).transpose(2, 1, 0).reshape(NT, 1024) for r in res]
    x_new = np.concatenate([o[:NL3] for o in outs], 0)
    ctx_new = outs[0][NL3:] if need_ctx else ctx
    return x_new, ctx_new


def kernel(**inp):
    inp = {k: np.asarray(v) for k, v in inp.items()}
    SEQ = inp["x"].shape[1]
    x = np.ascontiguousarray(inp["x"][0]); ctx = np.ascontiguousarray(inp["ctx"][0])
    mod = run_L0(inp["c"], inp["c_ctx"], inp["mod_w"], inp["mod_b"])
    depth = inp["mod_w"].shape[0]
    for l in range(depth):
        o1 = run_L1(inp, l, mod, x, ctx, SEQ // 16, 2)
        o2 = run_L2(inp, l, o1, SEQ)
        del o1["qa"], o1["ka"], o1["va"], o1["nqkv"], o1["rw3"], o1["rwd"]
        x, ctx = run_L3(inp, l, mod, x, ctx, o1, o2)
    return np.ascontiguousarray(x[None].astype(np.float32))
```

```python
from contextlib import ExitStack
import numpy as np
import ml_dtypes
import concourse.bass as bass
import concourse.mybir as mybir
from concourse.bass_utils import run_bass_kernel_spmd

F32 = mybir.dt.float32
BF16 = mybir.dt.bfloat16
AF = mybir.ActivationFunctionType
ALU = mybir.AluOpType
AX = mybir.AxisListType
NCORES = 8
ENGINES = ("tensor", "vector", "scalar", "gpsimd", "sync")


class Buf:
    __slots__ = ("t", "name", "lw", "rd")

    def __init__(self, t, name):
        self.t = t
        self.name = name
        self.lw = None
        self.rd = {}

    def __getitem__(self, idx):
        return self.t[idx]


class Prog:
    def __init__(self):
        self.nc = bass.Bass("TRN2", target_bir_lowering=False)
        self.ops = {e: [] for e in ENGINES}
        self.count = {}
        self.waited = {e: {} for e in ENGINES}
        self.stack = ExitStack()
        self.nbuf = 0

    def sbuf(self, shape, dt, name=None):
        self.nbuf += 1
        name = name or f"sb{self.nbuf}"
        t = self.stack.enter_context(self.nc.sbuf_tensor(name, list(shape), dt))
        return Buf(t, name)

    def psum(self, shape, dt=F32, name=None):
        self.nbuf += 1
        name = name or f"ps{self.nbuf}"
        t = self.stack.enter_context(self.nc.psum_tensor(name, list(shape), dt))
        return Buf(t, name)

    def dram(self, name, shape, dt, kind):
        t = self.nc.dram_tensor(name, list(shape), dt, kind=kind).ap()
        return Buf(t, name)

    def _deps(self, eng, reads, writes, skip_self):
        need = {}

        def add(kv):
            if kv is None:
                return
            k, v = kv
            if need.get(k, 0) < v:
                need[k] = v

        for b in reads:
            add(b.lw)
        for b in writes:
            add(b.lw)
            for k, v in b.rd.items():
                add((k, v))
        w = self.waited[eng]
        out = []
        for k, v in need.items():
            if skip_self and k == eng:
                continue
            if w.get(k, 0) < v:
                w[k] = v
                out.append((k, v))
        return out

    def _commit(self, key, inc, reads, writes):
        v = self.count.get(key, 0) + inc
        self.count[key] = v
        for b in reads:
            if b.rd.get(key, 0) < v:
                b.rd[key] = v
        for b in writes:
            b.lw = (key, v)
            b.rd = {}
        return v

    def op(self, eng, fn, reads=(), writes=(), skip_self=False):
        waits = self._deps(eng, reads, writes, skip_self)
        self._commit(eng, 1, reads, writes)
        self.ops[eng].append((waits, fn, eng, 1))

    def dma(self, eng, out_ap, in_ap, reads, writes, sem_buf):
        waits = self._deps(eng, reads, writes, False)
        key = "d_" + sem_buf.name
        self._commit(key, 16, reads, writes)
        self.ops[eng].append((waits, lambda e: e.dma_start(out=out_ap, in_=in_ap), key, 16))

    def coll(self, kind, in_buf, out_buf, op=None):
        waits = self._deps("gpsimd", [in_buf], [out_buf], False)
        key = "c_" + out_buf.name
        self._commit(key, 16, [in_buf], [out_buf])
        ia, oa = in_buf.t, out_buf.t
        op = op or ALU.bypass
        self.ops["gpsimd"].append((waits, lambda e: e.collective_compute(
            kind, op, replica_groups=[list(range(NCORES))], ins=[ia], outs=[oa]), key, 16))

    def barrier(self):
        snap = dict(self.count)
        for e in ENGINES:
            w = self.waited[e]
            waits = [(k, v) for k, v in snap.items() if w.get(k, 0) < v]
            for k, v in waits:
                w[k] = v
            if waits:
                self.ops[e].append((waits, None, None, 0))

    def scratch(self, name, shape, dt, shared=False):
        t = self.nc.dram_tensor(name, list(shape), dt, addr_space=("Shared" if shared else "Local")).ap()
        return Buf(t, name)

    def mm(self, out_ap, lhsT_ap, rhs_ap, start, stop, reads, writes):
        self.op("tensor", lambda e: e.matmul(out_ap, lhsT_ap, rhs_ap, start=start, stop=stop),
                reads, writes, skip_self=True)

    def transpose(self, out_ap, in_ap, ident_ap, reads, writes):
        self.op("tensor", lambda e: e.transpose(out_ap, in_ap, ident_ap), reads, writes, skip_self=True)

    def act(self, out_ap, in_ap, func, reads, writes, bias=None, scale=None, eng="scalar"):
        kw = {}
        if bias is not None:
            kw["bias"] = bias
        if scale is not None:
            kw["scale"] = scale
        self.op(eng, lambda e: e.activation(out_ap, in_ap, func, **kw), reads, writes)

    def tt(self, out_ap, a_ap, b_ap, op, reads, writes, eng="vector"):
        self.op(eng, lambda e: e.tensor_tensor(out_ap, a_ap, b_ap, op), reads, writes)

    def ts(self, out_ap, a_ap, s1, s2, op0, op1, reads, writes, eng="vector"):
        if s2 is None:
            self.op(eng, lambda e: e.tensor_scalar(out_ap, a_ap, s1, None, op0), reads, writes)
        else:
            self.op(eng, lambda e: e.tensor_scalar(out_ap, a_ap, s1, s2, op0, op1), reads, writes)

    def stt(self, out_ap, in0, scalar, in1, op0, op1, reads, writes):
        self.op("vector", lambda e: e.scalar_tensor_tensor(out_ap, in0, scalar, in1, op0, op1), reads, writes)

    def copy(self, out_ap, in_ap, reads, writes, eng="vector"):
        if eng == "scalar":
            self.op(eng, lambda e: e.copy(out_ap, in_ap), reads, writes)
        else:
            self.op(eng, lambda e: e.tensor_copy(out_ap, in_ap), reads, writes)

    def memset(self, ap, val, writes, eng="vector"):
        self.op(eng, lambda e: e.memset(ap, val), (), writes)

    def finish(self):
        nc = self.nc
        final_waits = []
        w = self.waited["sync"]
        for k, v in self.count.items():
            if w.get(k, 0) < v:
                final_waits.append((k, v))
        keys = list(self.count.keys())
        sems = {}
        for i, k in enumerate(keys):
            sems[k] = self.stack.enter_context(nc.semaphore(f"s{i}"))
        ops = self.ops

        def replay(e, lst):
            for waits, fn, key, inc in lst:
                for (k, v) in waits:
                    e.wait_ge(sems[k], v)
                if fn is not None:
                    fn(e).then_inc(sems[key], inc)

        with nc.Block() as block:
            @block.tensor
            def _(e):
                replay(e, ops["tensor"])

            @block.vector
            def _(e):
                replay(e, ops["vector"])

            @block.scalar
            def _(e):
                replay(e, ops["scalar"])

            @block.gpsimd
            def _(e):
                replay(e, ops["gpsimd"])

            @block.sync
            def _(e):
                replay(e, ops["sync"])
                for (k, v) in final_waits:
                    e.wait_ge(sems[k], v)
        self.stack.close()
        return nc


TRACE = False
TIMES = []


def run(prog, in_maps):
    nc = prog.finish()
    if TRACE:
        res = run_bass_kernel_spmd(nc, in_maps, core_ids=list(range(NCORES)), trace=True)
        TIMES.append(res.exec_time_ns)
        print("exec_time_ns", res.exec_time_ns, flush=True)
    else:
        res = run_bass_kernel_spmd(nc, in_maps, core_ids=list(range(NCORES)))
    return res.results


def build_L0(nch):
    P = Prog()
    w = P.dram("w", [1024, nch * 128], F32, "ExternalInput")
    b = P.dram("b", [128, nch], F32, "ExternalInput")
    cT = P.dram("cT", [128, 8, 2], F32, "ExternalInput")
    out = P.dram("out", [128, nch, 2], F32, "ExternalOutput")
    wt = P.sbuf([128, 8, nch * 128], F32, "wt")
    bt = P.sbuf([128, nch], F32, "bt")
    ct = P.sbuf([128, 8, 2], F32, "ct")
    st = P.sbuf([128, 8, 2], F32, "st")
    ot = P.sbuf([128, nch, 2], F32, "ot")
    ps = P.psum([128, nch, 2], F32, "ps0")
    P.dma("sync", ct[:], cT[:], [cT], [ct], ct)
    P.dma("sync", bt[:], b[:], [b], [bt], bt)
    for k in range(8):
        P.dma("sync", wt[:, k, :], w[k * 128:(k + 1) * 128, :], [w], [wt], wt)
    P.act(st[:], ct[:], AF.Silu, [ct], [st])
    for j in range(nch):
        for k in range(8):
            P.mm(ps[:, j, :], wt[:, k, j * 128:(j + 1) * 128], st[:, k, :], k == 0, k == 7, [wt, st], [ps])
    for j in range(nch):
        P.ts(ot[:, j, :], ps[:, j, :], bt[:, j:j + 1], None, ALU.add, None, [ps, bt], [ot])
    P.dma("sync", out[:], ot[:], [ot], [out], ot)
    return P


def fm(v):
    v = np.asarray(v, np.float32)
    return np.ascontiguousarray(v.reshape(-1, 128).T)


def run_L0(c, c_ctx, mod_w, mod_b):
    depth = mod_w.shape[0]
    nch_total = depth * 48
    nch = nch_total // NCORES
    wcat = np.concatenate([mod_w[l] for l in range(depth)], axis=1)
    bcat = np.concatenate([mod_b[l] for l in range(depth)], axis=0)
    cT = np.stack([fm(c.reshape(-1)), fm(c_ctx.reshape(-1))], axis=-1)
    in_maps = []
    for i in range(NCORES):
        sl = slice(i * nch * 128, (i + 1) * nch * 128)
        in_maps.append({"w": np.ascontiguousarray(wcat[:, sl]), "b": fm(bcat[sl]), "cT": cT})
    res = run(build_L0(nch), in_maps)
    o = np.concatenate([r["out"] for r in res], axis=1)
    return o.reshape(128, depth, 48, 2)


RW_ORDER = [12, 13, 14, 0, 4, 8, 1, 5, 9, 2, 6, 10, 3, 7, 11]


def build_L1(NL, NSUB):
    NS = NL + 260
    tiles = [(c0, min(512, NS - c0)) for c0 in range(0, NS, 512)]
    P = Prog()
    I = lambda n, s, dt=F32: P.dram(n, s, dt, "ExternalInput")
    O = lambda n, s, dt=F32: P.dram(n, s, dt, "ExternalOutput")
    xT_ = I("xT", [NSUB, 128, 8, NS])
    nv = I("nv", [128, 5, 8])
    wq = I("wq", [33, 128, 8, 128])
    ropeC_ = I("ropeC", [NSUB, 128, NS]); ropeS_ = I("ropeS", [NSUB, 128, NS]); mask_ = I("mask", [NSUB, 128, NS], BF16)
    sp = I("sp", [128, 16])
    wuq = I("wuq", [128, 3, 8, 128]); wuk = I("wuk", [128, 2, 8, 128]); wuv = I("wuv", [128, 2, 4, 128])
    rwp = I("rwp", [128, 58])
    w2d = I("w2", [128, 512]); a2d = I("a2", [128, 512]); g2d = I("g2", [128, 512])
    cst = I("cst", [128, 3, 128])
    o_qa_ = O("qa", [NSUB, 8, 128, NS], BF16); o_ka_ = O("ka", [NSUB, 8, 128, NS], BF16)
    o_va_ = O("va", [NSUB, 4, 128, NS], BF16)
    o_n_ = O("nqkv", [NSUB, 3, 4, 128, NS], BF16)
    o_r3_ = O("rw3", [NSUB, 3, 4, 128, NS])
    o_d3_ = O("rwd", [NSUB, 3, 2, 4, 128, NS])
    o_gb_ = O("rwgb", [NSUB, 2, 4, 128, NS])

    def load(d, shape, dt=F32, name=None):
        t = P.sbuf(shape, dt, name)
        P.dma("sync", t[:], d[:], [d], [t], t)
        return t
    nvt = load(nv, [128, 5, 8]); spt = load(sp, [128, 16]); rwt = load(rwp, [128, 58])
    cs = load(cst, [128, 3, 128])
    w2t = load(w2d, [128, 512]); a2t = load(a2d, [128, 512]); g2t = load(g2d, [128, 512])
    stg = P.sbuf([128, 3 * 8 * 128], F32, "stg")
    wuqb = P.sbuf([128, 3, 8, 128], BF16); wukb = P.sbuf([128, 2, 8, 128], BF16); wuvb = P.sbuf([128, 2, 4, 128], BF16)
    for (src, dstb, nel) in ((wuq, wuqb, 3 * 8 * 128), (wuk, wukb, 2 * 8 * 128), (wuv, wuvb, 2 * 4 * 128)):
        P.dma("sync", stg[:, :nel], src[:].rearrange("p a b c -> p (a b c)"), [src], [stg], stg)
        P.copy(dstb[:].rearrange("p a b c -> p (a b c)"), stg[:, :nel], [stg], [dstb], eng="gpsimd")
    ones = cs[:, 0, :]; blk = cs[:, 1, :]; rot = cs[:, 2, :]
    At = P.sbuf([128, 2, 8], F32)
    for w_, sc in ((0, 1), (1, 3)):
        P.stt(At[:, w_, :], nvt[:, sc, :], 1.0, nvt[:, 0, :], ALU.add, ALU.mult, [nvt], [At])
    m2 = P.sbuf([128, 15], F32)
    P.tt(m2[:], rwt[:, 0:15], rwt[:, 15:30], ALU.add, [rwt], [m2])
    P.ts(m2[:], m2[:], -1.0, 1.0, ALU.mult, ALU.add, [m2], [m2])

    psb = [P.psum([128, 512], F32, f"psb{i}") for i in range(6)]
    pi = [0]
    def PS():
        pi[0] += 1
        return psb[pi[0] % 6]
    slabs = [P.sbuf([128, NS], F32, f"sl{i}") for i in range(16)]
    bslabs = [P.sbuf([128, NS], BF16, f"bs{i}") for i in range(3)]
    bi = [0]
    def BS():
        bi[0] += 1
        return bslabs[bi[0] % 3]
    Ct = P.sbuf([128, NS], F32, "Ct"); St = P.sbuf([128, NS], F32, "St"); Mt = P.sbuf([128, NS], BF16, "Mt")
    hT = P.sbuf([128, 8, NS], BF16, "hT")
    x_ = P.sbuf([128, 8, 512], F32, "xt")
    sqt = [P.sbuf([128, 512], F32, f"sq{i}") for i in range(2)]
    rs = P.sbuf([128, 512], F32, "rs")
    wst = [P.sbuf([128, 8, 128], F32, f"wst{i}") for i in range(3)]
    wbf = [P.sbuf([128, 8, 128], BF16, f"wbf{i}") for i in range(3)]
    wi = [0]

    def proj(ci, dst, masked=False):
        wi[0] += 1
        ws, wb = wst[wi[0] % 3], wbf[wi[0] % 3]
        P.dma("sync", ws[:], wq[ci], [wq], [ws], ws)
        P.copy(wb[:], ws[:], [ws], [wb], eng=("gpsimd", "scalar")[wi[0] % 2])
        for (c0, n) in tiles:
            ps = PS()
            for k in range(8):
                P.mm(ps[:, :n], wb[:, k, :], hT[:, k, c0:c0 + n], k == 0, k == 7, [wb, hT], [ps])
            if masked:
                P.tt(dst[:, c0:c0 + n], ps[:, :n], Mt[:, c0:c0 + n], ALU.mult, [ps, Mt], [dst])
            else:
                P.copy(dst[:, c0:c0 + n], ps[:, :n], [ps], [dst], eng="scalar")

    def rstd_of(srcs, lhsT, dim, eps, dst, sqrt_only=False, tmp=None):
        tmp = tmp or slabs[12]
        for (c0, n) in tiles:
            ps = PS()
            for j, s in enumerate(srcs):
                P.act(tmp[:, c0:c0 + n], s[:, c0:c0 + n], AF.Square, [s], [tmp])
                P.mm(ps[:, :n], lhsT, tmp[:, c0:c0 + n], j == 0, j == len(srcs) - 1, [cs, tmp], [ps])
            P.act(dst[:, c0:c0 + n], ps[:, :n], AF.Sqrt, [ps], [dst], bias=eps, scale=1.0 / dim)
        if sqrt_only:
            P.ts(dst[:], dst[:], 1e-12, None, ALU.max, None, [dst], [dst])
        P.op("vector", lambda e: e.reciprocal(dst[:], dst[:]), [dst], [dst])

    def body(sub):
        D = lambda b, *idx: Buf(b.t[(sub,) + idx] if idx else b.t[sub], b.name + "_v")
        xT = D(xT_)
        o_qa, o_ka, o_va, o_n, o_r3, o_d3, o_gb = (D(o_qa_), D(o_ka_), D(o_va_), D(o_n_), D(o_r3_), D(o_d3_), D(o_gb_))
        P.dma("sync", Ct[:], ropeC_[sub], [ropeC_], [Ct], Ct)
        P.dma("sync", St[:], ropeS_[sub], [ropeS_], [St], St)
        P.dma("sync", Mt[:], mask_[sub], [mask_], [Mt], Mt)

        def out_dma(dram_ap, dram_buf, sb):
            P.dma("gpsimd", dram_ap, sb[:], [sb], [dram_buf], sb)

        for ti, (c0, n) in enumerate(tiles):
            P.dma("sync", x_[:, :, :n], xT[:, :, c0:c0 + n], [xT], [x_], x_)
            ps = PS()
            for k in range(8):
                s_ = sqt[k % 2]
                P.act(s_[:, :n], x_[:, k, :n], AF.Square, [x_], [s_])
                P.mm(ps[:, :n], ones, s_[:, :n], k == 0, k == 7, [cs, s_], [ps])
            P.act(rs[:, :n], ps[:, :n], AF.Sqrt, [ps], [rs], bias=1e-6, scale=1.0 / 1024)
            P.op("vector", lambda e, a=rs[:, :n]: e.reciprocal(a, a), [rs], [rs])
            for k in range(8):
                P.tt(x_[:, k, :n], x_[:, k, :n], rs[:, :n], ALU.mult, [x_, rs], [x_])
                for (a, b, w_, sh) in ((0, NL + 2, 0, 2), (NL + 2, NS, 1, 4)):
                    lo, hi = max(a, c0), min(b, c0 + n)
                    if lo < hi:
                        P.ts(hT[:, k, lo:hi], x_[:, k, lo - c0:hi - c0], At[:, w_, k:k + 1], nvt[:, sh, k:k + 1],
                             ALU.mult, ALU.add, [x_, At, nvt], [hT])

        cq = slabs[0:3]
        for j in range(3):
            proj(j, cq[j])
        r_ = slabs[3]
        rstd_of(cq, ones, 384.0, 1e-6, r_)
        cqn = [BS() for _ in range(3)]
        for j in range(3):
            P.stt(cqn[j][:], cq[j][:], spt[:, j:j + 1], r_[:], ALU.mult, ALU.mult, [cq[j], spt, r_], [cqn[j]])

        def head_finish(pre, gcol, dram_ap, dram_buf, ob, par=0):
            rr = (slabs[4], slabs[13])[par]
            rstd_of([pre], ones, 96.0, 1e-6, rr, tmp=(slabs[12], slabs[14])[par])
            P.stt(pre[:], pre[:], spt[:, gcol:gcol + 1], rr[:], ALU.mult, ALU.mult, [pre, spt, rr], [pre])
            rq = (slabs[5], slabs[15])[par]
            for (c0, n) in tiles:
                ps = PS()
                P.mm(ps[:, :n], rot, pre[:, c0:c0 + n], True, True, [cs, pre], [ps])
                P.tt(rq[:, c0:c0 + n], ps[:, :n], St[:, c0:c0 + n], ALU.mult, [ps, St], [rq])
            P.tt(pre[:], pre[:], Ct[:], ALU.mult, [pre, Ct], [pre])
            P.tt(ob[:], pre[:], rq[:], ALU.add, [pre, rq], [ob])
            out_dma(dram_ap, dram_buf, ob)

        obs = [slabs[8].t, slabs[9].t]
        qob = [P_q0, P_q1]
        for h in range(8):
            pre = slabs[6 + h % 2]
            for (c0, n) in tiles:
                ps = PS()
                for j in range(3):
                    P.mm(ps[:, :n], wuqb[:, j, h, :], cqn[j][:, c0:c0 + n], j == 0, j == 2, [wuqb, cqn[j]], [ps])
                P.copy(pre[:, c0:c0 + n], ps[:, :n], [ps], [pre], eng="scalar")
            head_finish(pre, 5, o_qa[h], o_qa, qob[h % 2], h % 2)

        ckv = slabs[0:2]
        for j in range(2):
            proj(3 + j, ckv[j])
        krp = slabs[2]
        proj(5, krp)
        rstd_of(ckv, ones, 256.0, 1e-6, r_)
        ckvn = [BS() for _ in range(2)]
        for j in range(2):
            P.stt(ckvn[j][:], ckv[j][:], spt[:, 3 + j:4 + j], r_[:], ALU.mult, ALU.mult, [ckv[j], spt, r_], [ckvn[j]])
        for h in range(8):
            pre = slabs[6 + h % 2]
            for (c0, n) in tiles:
                ps = PS()
                for j in range(2):
                    P.mm(ps[:, :n], wukb[:, j, h, :], ckvn[j][:, c0:c0 + n], j == 0, j == 1, [wukb, ckvn[j]], [ps])
                P.tt(pre[:, c0:c0 + n], ps[:, :n], krp[:, c0:c0 + n], ALU.add, [ps, krp], [pre])
            head_finish(pre, 6, o_ka[h], o_ka, qob[h % 2], h % 2)
        for c in range(4):
            ob = qob[c % 2]
            for (c0, n) in tiles:
                ps = PS()
                for j in range(2):
                    P.mm(ps[:, :n], wuvb[:, j, c, :], ckvn[j][:, c0:c0 + n], j == 0, j == 1, [wuvb, ckvn[j]], [ps])
                P.copy(ob[:, c0:c0 + n], ps[:, :n], [ps], [ob], eng="scalar")
            out_dma(o_va[c], o_va, ob)

        for which in range(3):
            for c in range(4):
                z = slabs[c % 2]
                proj(6 + which * 4 + c, z)
                ob = BS()
                if which < 2:
                    rr = (slabs[4], slabs[13])[c % 2]
                    rstd_of([z], blk, 64.0, 1e-6, rr, tmp=(slabs[12], slabs[14])[c % 2])
                    P.stt(ob[:], z[:], spt[:, 7 + which:8 + which], rr[:], ALU.mult, ALU.mult, [z, spt, rr], [ob])
                else:
                    P.copy(ob[:], z[:], [z], [ob])
                out_dma(o_n[which, c], o_n, ob)

        def shifted(ci_rw, dst, tmp):
            rwc = RW_ORDER[ci_rw]
            proj(18 + ci_rw, tmp, masked=True)
            P.ts(dst[:, 1:NS - 1], tmp[:, 1:NS - 1], m2[:, rwc:rwc + 1], None, ALU.mult, None, [tmp, m2], [dst])
            P.stt(dst[:, 1:NS - 1], tmp[:, 0:NS - 2], rwt[:, rwc:rwc + 1], dst[:, 1:NS - 1], ALU.mult, ALU.add,
                  [tmp, rwt, dst], [dst])
            P.stt(dst[:, 1:NS - 1], tmp[:, 2:NS], rwt[:, 15 + rwc:16 + rwc], dst[:, 1:NS - 1], ALU.mult, ALU.add,
                  [tmp, rwt, dst], [dst])
        tmp = slabs[11]
        wdT, adT, gdT = slabs[0], slabs[1], slabs[2]
        shifted(0, wdT, tmp); shifted(1, adT, tmp); shifted(2, gdT, tmp)
        P.act(wdT[:], wdT[:], AF.Tanh, [wdT], [wdT])
        P.act(gdT[:], gdT[:], AF.Sigmoid, [gdT], [gdT])
        for c in range(4):
            rT, kT, vT = slabs[3], slabs[4], slabs[5]
            shifted(3 + 3 * c, rT, tmp); shifted(4 + 3 * c, kT, tmp); shifted(5 + 3 * c, vT, tmp)
            P.dma("gpsimd", o_r3[0, c], rT[:], [rT], [o_r3], rT)
            P.dma("gpsimd", o_r3[1, c], vT[:], [vT], [o_r3], vT)
            kk = slabs[6]
            P.ts(kk[:], kT[:], rwt[:, 46 + c:47 + c], None, ALU.mult, None, [kT, rwt], [kk])
            rr = slabs[7]
            rstd_of([kk], blk, 1.0, 0.0, rr, sqrt_only=True)
            P.tt(kk[:], kk[:], rr[:], ALU.mult, [kk, rr], [kk])
            P.dma("gpsimd", o_r3[2, c], kk[:], [kk], [o_r3], kk)
            ksum = slabs[8]
            for d in range(2):
                lw, aa = slabs[9], slabs[10]
                pb = slice(d * 64, d * 64 + 64)
                for (c0, n) in tiles:
                    ps = PS()
                    P.mm(ps[:, :n], w2t[pb, c * 128:(c + 1) * 128], wdT[pb, c0:c0 + n], True, True, [w2t, wdT], [ps])
                    P.act(lw[:, c0:c0 + n], ps[:, :n], AF.Sigmoid, [ps, rwt], [lw],
                          bias=rwt[:, 30 + d * 4 + c:31 + d * 4 + c])
                    ps = PS()
                    P.mm(ps[:, :n], a2t[pb, c * 128:(c + 1) * 128], adT[pb, c0:c0 + n], True, True, [a2t, adT], [ps])
                    P.act(aa[:, c0:c0 + n], ps[:, :n], AF.Sigmoid, [ps, rwt], [aa],
                          bias=rwt[:, 38 + d * 4 + c:39 + d * 4 + c])
                P.ts(lw[:], lw[:], -float(np.exp(-0.5)), None, ALU.mult, None, [lw], [lw])
                P.dma("gpsimd", o_d3[0, d, c], lw[:], [lw], [o_d3], lw)
                bb = slabs[11]
                P.tt(bb[:], aa[:], kk[:], ALU.mult, [aa, kk], [bb])
                P.dma("gpsimd", o_d3[1, d, c], bb[:], [bb], [o_d3], bb)
                P.ts(aa[:], aa[:], -1.0, rwt[:, 50 + c:51 + c], ALU.add, ALU.mult, [aa, rwt], [aa])
                P.stt(aa[:], aa[:], 1.0, kT[:], ALU.add, ALU.mult, [aa, kT], [aa])
                P.dma("gpsimd", o_d3[2, d, c], aa[:], [aa], [o_d3], aa)
                if d == 0:
                    P.copy(ksum[:], aa[:], [aa], [ksum])
                else:
                    P.tt(ksum[:], ksum[:], aa[:], ALU.add, [ksum, aa], [ksum])
            P.stt(ksum[:], ksum[:], rwt[:, 54 + c:55 + c], rT[:], ALU.mult, ALU.mult, [ksum, rwt, rT], [ksum])
            gg, bo = slabs[9], slabs[10]
            for (c0, n) in tiles:
                ps = PS()
                P.mm(ps[:, :n], blk, ksum[:, c0:c0 + n], True, True, [cs, ksum], [ps])
                P.tt(bo[:, c0:c0 + n], ps[:, :n], vT[:, c0:c0 + n], ALU.mult, [ps, vT], [bo])
                ps = PS()
                P.mm(ps[:, :n], g2t[:, c * 128:(c + 1) * 128], gdT[:, c0:c0 + n], True, True, [g2t, gdT], [ps])
                P.copy(gg[:, c0:c0 + n], ps[:, :n], [ps], [gg], eng="scalar")
            P.dma("gpsimd", o_gb[0, c], gg[:], [gg], [o_gb], gg)
            P.dma("gpsimd", o_gb[1, c], bo[:], [bo], [o_gb], bo)

    P_q0 = P.sbuf([128, NS], BF16, "qo0"); P_q1 = P.sbuf([128, NS], BF16, "qo1")
    for sub in range(NSUB):
        body(sub)
    return P


def bf16(a):
    return np.asarray(a).astype(ml_dtypes.bfloat16)


def rope_tables(t0, NL, NS):
    C = np.ones((128, NS), np.float32)
    S = np.zeros((128, NS), np.float32)
    pos = t0 + np.arange(NL)
    rows, cols = (pos // 64).astype(np.float32), (pos % 64).astype(np.float32)
    fr = np.exp(-np.log(10000.0) * np.arange(8, dtype=np.float32) / 8).astype(np.float32)
    ar = rows[None, :] * fr[:, None]
    ac = cols[None, :] * fr[:, None]
    for base, ang in ((64, ar), (72, ar), (80, ac), (88, ac)):
        C[base:base + 8, 1:NL + 1] = np.cos(ang)
        S[base:base + 8, 1:NL + 1] = np.sin(ang)
    return C, S


def consts_L1():
    cst = np.zeros((128, 3, 128), np.float32)
    cst[:, 0, :] = 1.0
    cst[:64, 1, :64] = 1.0
    cst[64:, 1, 64:] = 1.0
    for i in range(8):
        cst[72 + i, 2, 64 + i] = -1.0
        cst[64 + i, 2, 72 + i] = 1.0
        cst[88 + i, 2, 80 + i] = -1.0
        cst[80 + i, 2, 88 + i] = 1.0
    return cst


def prep_L1_weights(inp, l, mod):
    W = inp["w_in"][l]
    cols = []
    z128 = np.zeros((1024, 128), np.float32)
    for j in range(3):
        cols.append(W[:, j * 128:(j + 1) * 128])
    for j in range(2):
        cols.append(W[:, 384 + j * 128:384 + (j + 1) * 128])
    kr = z128.copy(); kr[:, 64:96] = W[:, 640:672]; cols.append(kr)
    for j in range(12):
        cols.append(W[:, 672 + j * 128:672 + (j + 1) * 128])
    for j in RW_ORDER:
        cols.append(W[:, 2208 + j * 128:2208 + (j + 1) * 128])
    Wp = np.stack(cols, 0)
    wq = np.ascontiguousarray(Wp.reshape(33, 8, 128, 128).transpose(0, 2, 1, 3))
    nv = np.stack([fm(inp["norm1_g"][l]), mod[:, l, 8:16, 0], mod[:, l, 0:8, 0], mod[:, l, 8:16, 1], mod[:, l, 0:8, 1]], 1)
    sp = np.zeros((128, 16), np.float32)
    sp[:, 0:3] = fm(inp["mla_cq_g"][l]); sp[:, 3:5] = fm(inp["mla_ckv_g"][l])
    sp[:96, 5] = inp["mla_qn_g"][l]; sp[:96, 6] = inp["mla_kn_g"][l]
    sp[:, 7] = np.tile(inp["na_qn_g"][l], 2); sp[:, 8] = np.tile(inp["na_kn_g"][l], 2)
    wuq = np.zeros((384, 8, 128), np.float32)
    wuq[:, :, :96] = inp["mla_wuq"][l].reshape(384, 8, 96)
    wuq = np.ascontiguousarray(wuq.reshape(3, 128, 8, 128).transpose(1, 0, 2, 3))
    kv = inp["mla_wukv"][l].reshape(256, 8, 128)
    wuk = np.zeros((256, 8, 128), np.float32); wuk[:, :, :64] = kv[:, :, :64]
    wuk = np.ascontiguousarray(wuk.reshape(2, 128, 8, 128).transpose(1, 0, 2, 3))
    wuv = np.ascontiguousarray(kv[:, :, 64:].reshape(256, 4, 128).reshape(2, 128, 4, 128).transpose(1, 0, 2, 3))
    rwp = np.zeros((128, 58), np.float32)
    rwp[:, 0:15] = fm(inp["rw_mu"][l][0]); rwp[:, 15:30] = fm(inp["rw_mu"][l][1])
    for d in range(2):
        rwp[:, 30 + d * 4:34 + d * 4] = fm(inp["rw_w0"][l][d]); rwp[:, 38 + d * 4:42 + d * 4] = fm(inp["rw_a0"][l][d])
    rwp[:, 46:50] = fm(inp["rw_kk"][l]); rwp[:, 50:54] = fm(inp["rw_ka"][l]); rwp[:, 54:58] = fm(inp["rw_rk"][l].reshape(-1))
    return dict(nv=np.ascontiguousarray(nv, np.float32), wq=wq, sp=sp, wuq=wuq, wuk=wuk, wuv=wuv, rwp=rwp,
                w2=np.ascontiguousarray(inp["rw_w2"][l].reshape(128, 512)),
                a2=np.ascontiguousarray(inp["rw_a2"][l].reshape(128, 512)),
                g2=np.ascontiguousarray(inp["rw_g2"][l]), cst=consts_L1())


def run_L1(inp, l, mod, x, ctx, NL, NSUB):
    SEQ = x.shape[0]
    NS = NL + 260
    wts = prep_L1_weights(inp, l, mod)
    in_maps = []
    for i in range(NCORES):
        xs, Cs, Ss, Ms = [], [], [], []
        for s in range(NSUB):
            t0 = (i * NSUB + s) * NL
            slab = np.zeros((NS, 1024), np.float32)
            M = np.ones((128, NS), np.float32)
            if t0 > 0:
                slab[0] = x[t0 - 1]
            else:
                M[:, 0] = 0
            slab[1:NL + 1] = x[t0:t0 + NL]
            if t0 + NL < SEQ:
                slab[NL + 1] = x[t0 + NL]
            else:
                M[:, NL + 1] = 0
            M[:, NL + 2] = 0; M[:, NS - 1] = 0
            slab[NL + 3:NL + 259] = ctx
            xs.append(slab.T.reshape(8, 128, NS).transpose(1, 0, 2))
            C, S = rope_tables(t0, NL, NS)
            Cs.append(C); Ss.append(S); Ms.append(bf16(M))
        m = dict(wts)
        m.update(xT=np.ascontiguousarray(np.stack(xs, 0)), ropeC=np.stack(Cs, 0), ropeS=np.stack(Ss, 0), mask=np.stack(Ms, 0))
        in_maps.append(m)
    res = run(build_L1(NL, NSUB), in_maps)
    out = {}
    for key in ("qa", "ka", "va", "nqkv", "rw3", "rwd", "rwgb"):
        lat = np.concatenate([res[i][key][s][..., 1:NL + 1] for i in range(NCORES) for s in range(NSUB)], axis=-1)
        cx = res[0][key][0][..., NL + 3:NL + 259]
        out[key] = (np.asarray(lat), np.asarray(cx))
    return out


def na_plan(SEQ):
    NR = SEQ // 64
    NP_ = NR // 2
    tabs = {}
    tab_list = []
    plan = []
    qc = np.arange(64)
    cs_ = np.clip(qc - 8, 0, 48)
    for m in range(NP_):
        rows = [2 * m, 2 * m + 1]
        rs = [int(np.clip(r - 4, 0, NR - 8)) for r in rows]
        kps = sorted({(a + i) // 2 for a in rs for i in range(8)})
        ent = []
        for kp in kps:
            sig = (rs[0] - rows[0], rs[1] - rows[1], kp - m)
            if sig not in tabs:
                dr = np.full((128, 128), -1, np.int64)
                dc = np.full((128, 128), -1, np.int64)
                for kl in range(128):
                    krow, kcol = 2 * kp + kl // 64, kl % 64
                    for ql in range(128):
                        qrow, qcol = rows[ql // 64], ql % 64
                        a = rs[ql // 64]
                        if a <= krow < a + 8 and cs_[qcol] <= kcol < cs_[qcol] + 16:
                            dr[kl, ql] = krow - qrow + 7
                            dc[kl, ql] = kcol - qcol + 15
                tabs[sig] = len(tab_list)
                tab_list.append((dr, dc))
            ent.append((kp, tabs[sig]))
        plan.append(ent)
    return plan, tab_list


def build_L2(SEQ):
    NK = SEQ + 256
    NKB = NK // 128
    NCH = NKB
    plan, tab_list = na_plan(SEQ)
    NTAB = len(tab_list)
    P = Prog()
    I = lambda n, s, dt=F32: P.dram(n, s, dt, "ExternalInput")
    O = lambda n, s, dt=F32: P.dram(n, s, dt, "ExternalOutput")
    qa = I("qa", [128, SEQ + 256], BF16)
    ka = I("ka", [128, NK], BF16)
    va = I("va", [128, NKB, 65], BF16)
    nq = I("nq", [64, SEQ + 256], BF16)
    nk = I("nk", [64, NK], BF16)
    nvv = I("nv", [128, NKB, 65], BF16)
    tabs = I("tabs", [128, NTAB, 128])
    cst = I("cst", [128, 6, 128])
    rtm = I("rtm", [2, NCH, 128, 4, 64])
    rfm = I("rfm", [2, NCH, 64, 4, 128])
    o_ya = O("ya", [64, SEQ + 256]); o_yb = O("yb", [64, SEQ + 256])
    o_y = O("y", [2, NCH, 128, 64])

    psb = [P.psum([128, 512], F32, f"psb{i}") for i in range(2)]
    psS2 = [P.psum([128, 1024], F32, f"psS{i}") for i in range(3)]
    cs = P.sbuf([128, 6, 128], F32, "cs")
    P.dma("sync", cs[:], cst[:], [cst], [cs], cs)
    triI, triS, triL, ident, ones = (cs[:, i, :] for i in range(5))
    csb = P.sbuf([128, 2, 128], BF16, "csb")
    P.copy(csb[:, 0, :], cs[:, 3, :], [cs], [csb])
    P.copy(csb[:, 1, :], cs[:, 4, :], [cs], [csb])

    pT = [P.sbuf([128, 1024], BF16, f"pT{i}") for i in range(3)]
    osb = [P.sbuf([64, 512], F32, f"osb{i}") for i in range(2)]
    rD = P.sbuf([64, 512], F32, "rD")
    dsb = P.sbuf([128, 512], F32, "dsb")
    cnt = [0]

    def finish_od(psOD, n, out_d, out_c0):
        psB = psb[1]
        P.copy(dsb[64:65, :n], psOD[64:65, :n], [psOD], [dsb], eng="scalar")
        P.mm(psB[0:64, :n], cs[64:65, 4, 0:64], dsb[64:65, :n], True, True, [cs, dsb], [psB])
        P.op("vector", lambda e: e.reciprocal(rD[:, :n], psB[0:64, :n]), [psB], [rD])
        ob = osb[cnt[0] % 2]
        P.tt(ob[:, :n], psOD[0:64, :n], rD[:, :n], ALU.mult, [psOD, rD], [ob])
        P.dma("gpsimd", out_d[:, out_c0:out_c0 + n], ob[:, :n], [ob], [out_d], ob)

    def attn(qT, q0, n, kT, V, kblocks, KD, scale, out_d, out_c0):
        cnt[0] += 1
        psOD = psb[0]
        pairs = [kblocks[i:i + 2] for i in range(0, len(kblocks), 2)]
        npair = len(pairs)

        def S(p):
            pS = psS2[p % 3]
            for j, kb in enumerate(pairs[p]):
                P.mm(pS[:, j * 512:j * 512 + n], kT[0:KD, kb * 128:(kb + 1) * 128], qT[0:KD, q0:q0 + n], True, True, [kT, qT], [pS])
        S(0)
        if npair > 1:
            S(1)
        for p, pr in enumerate(pairs):
            if p + 2 < npair:
                S(p + 2)
            pS = psS2[p % 3]
            p_ = pT[p % 3]
            L = len(pr)
            sv = pS[:, :].rearrange("p (j c) -> p j c", c=512)[:, 0:L, 0:n]
            dv = p_[:, :].rearrange("p (j c) -> p j c", c=512)[:, 0:L, 0:n]
            P.act(dv, sv, AF.Exp, [pS], [p_], scale=scale)
            for j, kb in enumerate(pr):
                P.mm(psOD[0:65, :n], V[:, kb, :], p_[:, j * 512:j * 512 + n], p == 0 and j == 0,
                     p == npair - 1 and j == L - 1, [V, p_], [psOD])
        finish_od(psOD, n, out_d, out_c0)

    qat = P.sbuf([128, SEQ + 256], BF16, "qat"); kat = P.sbuf([128, NK], BF16, "kat"); vat = P.sbuf([128, NKB, 65], BF16, "vat")
    for (t, d) in ((qat, qa), (kat, ka), (vat, va)):
        P.dma("sync", t[:], d[:], [d], [t], t)
    sc_a = float(96 ** -0.5)
    for q0 in range(0, SEQ, 512):
        attn(qat, q0, min(512, SEQ - q0), kat, vat, list(range(NKB)), 128, sc_a, o_ya, q0)
    attn(qat, SEQ, 256, kat, vat, [0, 1], 128, sc_a, o_ya, SEQ)

    nqt, nkt, nvt = qat, kat, vat
    P.dma("sync", nqt[0:64, :], nq[:], [nq], [nqt], nqt)
    P.dma("sync", nkt[0:64, :], nk[:], [nk], [nkt], nkt)
    P.dma("sync", nvt[:], nvv[:], [nvv], [nvt], nvt)
    tb32 = P.sbuf([128, NTAB, 128], F32, "tb32"); tbb = P.sbuf([128, NTAB, 128], BF16, "tbb")
    P.dma("sync", tb32[:], tabs[:], [tabs], [tb32], tb32)
    P.ts(tbb[:], tb32[:], 8.0, None, ALU.mult, None, [tb32], [tbb])
    sc_b = 0.125
    def na_blocks(m):
        return [(kp, tid) for (kp, tid) in plan[m]] + [(NKB - 2, None), (NKB - 1, None)]

    def na_scores(m):
        pS = psS2[m % 3]
        qsl = nqt[0:64, m * 128:(m + 1) * 128]
        for j, (kb, tid) in enumerate(na_blocks(m)):
            o_ = pS[:, j * 128:(j + 1) * 128]
            P.mm(o_, nkt[0:64, kb * 128:(kb + 1) * 128], qsl, True, tid is None, [nkt, nqt], [pS])
            if tid is not None:
                P.mm(o_, csb[:, 0, :], tbb[:, tid, :], False, True, [csb, tbb], [pS])

    na_scores(0)
    for m in range(len(plan)):
        if m + 1 < len(plan):
            na_scores(m + 1)
        blocks = na_blocks(m)
        cnt[0] += 1
        psOD = psb[0]
        p_ = pT[cnt[0] % 3]
        pS = psS2[m % 3]
        w = len(blocks) * 128
        P.act(p_[:, :w], pS[:, :w], AF.Exp, [pS], [p_], scale=sc_b)
        for j, (kb, tid) in enumerate(blocks):
            P.mm(psOD[0:65, :128], nvt[:, kb, :], p_[:, j * 128:(j + 1) * 128], j == 0, j == len(blocks) - 1, [nvt, p_], [psOD])
        finish_od(psOD, 128, o_yb, m * 128)
    attn(nqt, SEQ, 256, nkt, nvt, [NKB - 2, NKB - 1], 64, sc_b, o_yb, SEQ)

    pi = [0]
    rw_ps = psb + psS2
    def PS():
        pi[0] += 1
        return rw_ps[pi[0] % 5]
    NSET = 4
    W = lambda n, shape=(128, 128): [P.sbuf(list(shape), F32, f"{n}{i}") for i in range(NSET)]
    tmb, fmb = W("tm", (128, 4, 64)), W("fmj", (64, 4, 128))
    eLr = W("eLr", (128, 64)); Bh = W("Bh", (128, 64)); Kh = W("Kh", (128, 64))
    e1 = W("e1", (64, 128)); e2 = W("e2", (64, 128)); e3 = W("e3", (64, 128))
    Rt = W("Rt", (64, 128)); KKt = W("KKt", (64, 128)); Bt = W("Bt", (64, 128)); Kt = W("Kt", (64, 128))
    Nn = W("Nn"); NTt = W("NTt"); Mk = W("Mk"); Mbp = W("Mbp"); Mkp = W("Mkp")
    Pq = W("Pq"); PqT = W("PqT"); Tm = W("Tm")
    Zs = W("Zs", (128, 64)); nU = W("nU", (128, 64)); Ys = W("Ys", (128, 64))
    gC = W("gC", (64, 1))
    ST = [P.sbuf([64, 64], F32, f"ST{d}") for d in range(2)]
    for d in range(2):
        P.memset(ST[d][:], 0.0, [ST[d]])

    def chunk_gen(c, d, s):
        tm, fj = tmb[s], fmb[s]
        P.dma("sync", tm[:], rtm[d, c], [rtm], [tm], tm)
        P.dma("sync", fj[:], rfm[d, c], [rfm], [fj], fj)
        yield
        lw_tok = tm[:, 0, :]
        ps = PS(); P.mm(ps[:, 0:64], triL, lw_tok, True, True, [cs, tm], [ps])
        P.act(eLr[s][:], ps[:, 0:64], AF.Exp, [ps], [eLr[s]])
        yield
        ps = PS(); P.mm(ps[0:64, 0:128], lw_tok, triI, True, True, [tm, cs], [ps])
        P.act(e1[s][:], ps[0:64, 0:128], AF.Exp, [ps], [e1[s]])
        P.act(e2[s][:], ps[0:64, 0:128], AF.Exp, [ps], [e2[s]], scale=-1.0)
        yield
        ps = PS(); P.mm(ps[0:64, 0:128], lw_tok, triS, True, True, [tm, cs], [ps])
        P.act(e3[s][:], ps[0:64, 0:128], AF.Exp, [ps], [e3[s]])
        yield
        P.tt(Bh[s][:], tm[:, 1, :], eLr[s][:], ALU.mult, [tm, eLr[s]], [Bh[s]], eng="gpsimd")
        P.tt(Kh[s][:], tm[:, 2, :], eLr[s][:], ALU.mult, [tm, eLr[s]], [Kh[s]], eng="gpsimd")
        P.copy(gC[s][:], e1[s][:, 127:128], [e1[s]], [gC[s]], eng="gpsimd")
        P.tt(Bt[s][:], fj[:, 0, :], e2[s][:], ALU.mult, [fj, e2[s]], [Bt[s]])
        P.tt(Kt[s][:], fj[:, 1, :], e2[s][:], ALU.mult, [fj, e2[s]], [Kt[s]], eng="gpsimd")
        P.tt(KKt[s][:], fj[:, 2, :], e3[s][:], ALU.mult, [fj, e3[s]], [KKt[s]])
        P.tt(Rt[s][:], fj[:, 3, :], e1[s][:], ALU.mult, [fj, e1[s]], [Rt[s]], eng="gpsimd")
        yield
        for (dst, l_, r_, msk) in ((Nn[s], Bt[s], KKt[s], triS), (NTt[s], KKt[s], Bt[s], triL),
                                  (Mk[s], Kt[s], KKt[s], triS), (Mbp[s], Bt[s], Rt[s], triI), (Mkp[s], Kt[s], Rt[s], triI)):
            ps = PS(); P.mm(ps[:, 0:128], l_[:], r_[:], True, True, [l_, r_], [ps])
            P.tt(dst[:], ps[:, 0:128], msk, ALU.mult, [ps, cs], [dst])
            yield
        P.tt(Tm[s][:], ident, Nn[s][:], ALU.subtract, [cs, Nn[s]], [Tm[s]])
        Pc, PcT = Nn[s], NTt[s]
        for it in range(6):
            nxt, nxtT = (Pq[s], PqT[s]) if Pc is not Pq[s] else (Nn[s], NTt[s])
            ps = PS(); P.mm(ps[:, 0:128], Pc[:], PcT[:], True, True, [Pc, PcT], [ps])
            P.copy(nxtT[:], ps[:, 0:128], [ps], [nxtT], eng="scalar")
            if it < 5:
                ps = PS(); P.mm(ps[:, 0:128], PcT[:], Pc[:], True, True, [Pc, PcT], [ps])
                P.copy(nxt[:], ps[:, 0:128], [ps], [nxt])
            yield
            ps = PS(); P.mm(ps[:, 0:128], nxtT[:], Tm[s][:], True, True, [nxtT, Tm[s]], [ps])
            P.tt(Tm[s][:], Tm[s][:], ps[:, 0:128], ALU.add, [Tm[s], ps], [Tm[s]])
            Pc, PcT = nxt, nxtT
            yield
        vt = tm[:, 3, :]
        ps = PS()
        P.mm(ps[:, 0:64], KKt[s][:], ST[d][:], True, False, [KKt[s], ST[d]], [ps])
        P.mm(ps[:, 0:64], Mk[s][:], vt, False, True, [Mk[s], tm], [ps])
        P.copy(Zs[s][:], ps[:, 0:64], [ps], [Zs[s]])
        yield
        ps = PS(); P.mm(ps[:, 0:64], Tm[s][:], Zs[s][:], True, True, [Tm[s], Zs[s]], [ps])
        P.ts(nU[s][:], ps[:, 0:64], -1.0, None, ALU.mult, None, [ps], [nU[s]])
        yield
        ps2 = PS()
        P.mm(ps2[0:64, 0:64], Bh[s][:], nU[s][:], True, False, [Bh[s], nU[s]], [ps2])
        P.mm(ps2[0:64, 0:64], Kh[s][:], vt, False, True, [Kh[s], tm], [ps2])
        ps = PS()
        P.mm(ps[:, 0:64], Rt[s][:], ST[d][:], True, False, [Rt[s], ST[d]], [ps])
        P.mm(ps[:, 0:64], Mbp[s][:], nU[s][:], False, False, [Mbp[s], nU[s]], [ps])
        P.mm(ps[:, 0:64], Mkp[s][:], vt, False, True, [Mkp[s], tm], [ps])
        P.stt(ST[d][:], ST[d][:], gC[s][:, 0:1], ps2[0:64, 0:64], ALU.mult, ALU.add, [ST[d], gC[s], ps2], [ST[d]])
        P.copy(Ys[s][:], ps[:, 0:64], [ps], [Ys[s]], eng="scalar")
        P.dma("gpsimd", o_y[d, c], Ys[s][:], [Ys[s]], [o_y], Ys[s])
        yield

    tasks = [(c, d) for c in range(NCH) for d in range(2)]
    active = []
    nxt_task = 0
    rounds = 0
    while nxt_task < len(tasks) or active:
        if nxt_task < len(tasks) and len(active) < NSET and (rounds % 7 == 0 or not active):
            c, d = tasks[nxt_task]
            active.append(chunk_gen(c, d, nxt_task % NSET))
            nxt_task += 1
        rounds += 1
        for g in list(active):
            try:
                next(g)
            except StopIteration:
                active.remove(g)
    return P


def consts_L2():
    c = np.zeros((128, 6, 128), np.float32)
    i = np.arange(128)
    c[:, 0, :] = (i[:, None] <= i[None, :])
    c[:, 1, :] = (i[:, None] < i[None, :])
    c[:, 2, :] = (i[:, None] > i[None, :])
    c[:, 3, :] = np.eye(128)
    c[:, 4, :] = 1.0
    return c


def tokmaj(a, aug=False):
    n = a.shape[1]
    t = a.T.reshape(n // 128, 128, 64).transpose(1, 0, 2)
    if aug:
        t = np.concatenate([t, np.ones((128, n // 128, 1), t.dtype)], axis=2)
    return np.ascontiguousarray(t)


def run_L2(inp, l, o1, SEQ):
    NK = SEQ + 256
    NCH = NK // 128
    plan, tab_list = na_plan(SEQ)
    cst = consts_L2()
    in_maps = []
    f32 = lambda a: np.asarray(a, np.float32)
    hs = lambda pair, h: (pair[0].reshape(-1, pair[0].shape[-1])[h * 64:(h + 1) * 64],
                          pair[1].reshape(-1, 256)[h * 64:(h + 1) * 64])
    for h in range(NCORES):
        m = {"cst": cst}
        m["qa"] = np.ascontiguousarray(np.concatenate([o1["qa"][0][h], o1["qa"][1][h]], 1))
        m["ka"] = np.ascontiguousarray(np.concatenate([o1["ka"][1][h], o1["ka"][0][h]], 1))
        vl, vc = hs(o1["va"], h)
        m["va"] = tokmaj(np.concatenate([vc, vl], 1), aug=True)
        n_l, n_c = o1["nqkv"]
        sel = lambda w: (n_l[w].reshape(512, -1)[h * 64:(h + 1) * 64], n_c[w].reshape(512, 256)[h * 64:(h + 1) * 64])
        m["nq"] = np.ascontiguousarray(np.concatenate(sel(0), 1))
        m["nk"] = np.ascontiguousarray(np.concatenate(sel(1), 1))
        m["nv"] = tokmaj(np.concatenate(sel(2), 1), aug=True)
        rpb = inp["na_rpb"][l][h]
        tb = np.stack([np.where(dr >= 0, rpb[np.maximum(dr, 0), np.maximum(dc, 0)], np.float32(-30000.0)) for dr, dc in tab_list], 0)
        m["tabs"] = np.ascontiguousarray(tb.transpose(1, 0, 2).astype(np.float32))
        r3l, r3c = o1["rw3"]; rdl, rdc = o1["rwd"]
        def seqs(lat, cx, d):
            lat = lat.reshape(512, -1)[h * 64:(h + 1) * 64]; cx = cx.reshape(512, 256)[h * 64:(h + 1) * 64]
            return np.concatenate([cx, lat], 1) if d == 0 else np.concatenate([cx[:, ::-1], lat[:, ::-1]], 1)
        rtm = np.zeros((2, NCH, 128, 4, 64), np.float32); rfm = np.zeros((2, NCH, 64, 4, 128), np.float32)
        for d in range(2):
            lw = seqs(rdl[0, d], rdc[0, d], d); b_ = seqs(rdl[1, d], rdc[1, d], d); kd = seqs(rdl[2, d], rdc[2, d], d)
            r_ = seqs(r3l[0], r3c[0], d); v_ = seqs(r3l[1], r3c[1], d); kk = seqs(r3l[2], r3c[2], d)
            for j, a in enumerate((lw, b_, kd, v_)):
                rtm[d, :, :, j, :] = a.T.reshape(NCH, 128, 64)
            for j, a in enumerate((b_, kd, kk, r_)):
                rfm[d, :, :, j, :] = a.reshape(64, NCH, 128).transpose(1, 0, 2)
        m["rtm"] = rtm; m["rfm"] = rfm
        in_maps.append(m)
    res = run(build_L2(SEQ), in_maps)
    ya = np.concatenate([f32(res[h]["ya"]) for h in range(NCORES)], 0)
    yb = np.concatenate([f32(res[h]["yb"]) for h in range(NCORES)], 0)
    ys = []
    for d in range(2):
        yy = np.concatenate([f32(res[h]["y"][d]).reshape(NK, 64).T for h in range(NCORES)], 0)
        cx, lat = yy[:, :256], yy[:, 256:]
        if d == 1:
            cx, lat = cx[:, ::-1], lat[:, ::-1]
        ys.append((np.ascontiguousarray(lat), np.ascontiguousarray(cx)))
    return dict(ya=(ya[:, :SEQ], ya[:, SEQ:]), yb=(yb[:, :SEQ], yb[:, SEQ:]), yf=ys[0], ybk=ys[1])


def build_L3(NT, NLAT, moe):
    FC = 28 if moe else 22
    NE = 8 if moe else 1
    tiles = [(c0, min(512, NT - c0)) for c0 in range(0, NT, 512)]
    P = Prog()
    I = lambda n, s, dt=F32: P.dram(n, s, dt, "ExternalInput")
    xT = I("xT", [128, 8, NT])
    yin = I("yin", [6, 128, 4, NT])
    nv = I("nv", [128, 14, 8])
    rwo = I("rwo", [128, 2, 4])
    wg = I("wg", [24, 128, 8, 128]); wo = I("wo", [3, 8, 128, 4, 128]); wout = I("wout", [8, 128, 8, 128])
    if not moe:
        w1 = I("w1", [NE, FC, 128, 8, 128]); w3 = I("w3", [NE, FC, 128, 8, 128]); w2 = I("w2", [NE, 8, 128, FC, 128])
    else:
        h2o = P.dram("h2o", [128, 8, NT], BF16, "ExternalOutput"); gTo = P.dram("gTo", [8, NT], F32, "ExternalOutput")
    cst = I("cst", [128, 3, 128])
    if moe:
        rt = I("rt", [128, 8, 8]); sel = I("sel", [8, 8, 128])
    out = P.dram("out", [128, 8, NT], F32, "ExternalOutput")

    def load(d, shape, dt=F32, name=None):
        t = P.sbuf(shape, dt, name)
        P.dma("sync", t[:], d[:], [d], [t], t)
        return t
    nvt = load(nv, [128, 14, 8]); rwt = load(rwo, [128, 2, 4]); cs = load(cst, [128, 3, 128])
    ones, blk, ident = cs[:, 0, :], cs[:, 1, :], cs[:, 2, :]
    if moe:
        rtt = load(rt, [128, 8, 8]); selt = load(sel, [8, 8, 128])
    A1 = P.sbuf([128, 2, 8], F32); A2 = P.sbuf([128, 2, 8], F32)
    for w_, sc in ((0, 1), (1, 3)):
        P.stt(A1[:, w_, :], nvt[:, sc, :], 1.0, nvt[:, 0, :], ALU.add, ALU.mult, [nvt], [A1])
    for w_, sc in ((0, 8), (1, 10)):
        P.stt(A2[:, w_, :], nvt[:, sc, :], 1.0, nvt[:, 7, :], ALU.add, ALU.mult, [nvt], [A2])

    psb = [P.psum([128, 512], F32, f"psb{i}") for i in range(8)]
    pi = [0]
    def PS():
        pi[0] += 1
        return psb[pi[0] % 8]
    x_ = P.sbuf([128, 8, 512], F32, "x"); hT = P.sbuf([128, 8, 512], BF16, "hT"); G = P.sbuf([128, 24, 512], BF16, "G")
    stg = [P.sbuf([128, 4, 512], F32, f"stg{i}") for i in range(2)]
    ybf = [P.sbuf([128, 4, 512], BF16, f"ybf{i}") for i in range(3)]
    ysum = P.sbuf([128, 4, 512], F32, "ysum"); tmp = P.sbuf([128, 4, 512], F32, "tmp")
    Mb = P.sbuf([128, 8, 512], BF16, "Mb"); Mo = P.sbuf([128, 512], F32, "Mo"); t5 = P.sbuf([128, 512], F32, "t5")
    h2 = P.sbuf([128, 8, 512], BF16, "h2"); hid = P.sbuf([128, 1 if moe else FC, 512], BF16, "hid")
    sq = [P.sbuf([128, 512], F32, f"sq{i}") for i in range(2)]; rs = P.sbuf([128, 512], F32, "rs")
    wst = [P.sbuf([128, 8, 128], F32, f"wst{i}") for i in range(6)]
    wbf = [P.sbuf([128, 8, 128], BF16, f"wbf{i}") for i in range(7)]
    wi = [0]
    if moe:
        lgT = P.sbuf([8, 512], F32, "lgT"); gT = P.sbuf([8, 512], F32, "gT"); gbe = P.sbuf([128, 512], F32, "gbe")
        lg = P.sbuf([128, 8], F32, "lg"); top = P.sbuf([128, 8], F32, "top"); sm = P.sbuf([128, 8], F32, "sm")
        gt_ = P.sbuf([128, 8], F32, "gt"); g2_ = P.sbuf([128, 8], F32, "g2")

    def wpiece(ap, dbuf, kc=8):
        wi[0] += 1
        ws, wb = wst[wi[0] % 6], wbf[wi[0] % 7]
        P.dma("sync", ws[:, :kc, :], ap, [dbuf], [ws], ws)
        P.copy(wb[:, :kc, :], ws[:, :kc, :], [ws], [wb], eng=("gpsimd", "scalar", "vector", "scalar")[wi[0] % 4])
        return wb

    def segs(c0, n):
        for (a, b, w_) in ((0, NLAT, 0), (NLAT, NT, 1)):
            lo, hi = max(a, c0), min(b, c0 + n)
            if lo < hi:
                yield lo - c0, hi - c0, w_

    def norm_mod(src, dst, A, shl, shc, n, c0, extra=None):
        ps = PS()
        for k in range(8):
            s_ = sq[k % 2]
            P.act(s_[:, :n], src[:, k, :n], AF.Square, [src], [s_])
            P.mm(ps[:, :n], ones, s_[:, :n], k == 0, k == 7, [cs, s_], [ps])
        P.act(rs[:, :n], ps[:, :n], AF.Sqrt, [ps], [rs], bias=1e-6, scale=1.0 / 1024)
        P.op("vector", lambda e: e.reciprocal(rs[:, :n], rs[:, :n]), [rs], [rs])
        for k in range(8):
            s_ = sq[k % 2]
            P.tt(s_[:, :n], src[:, k, :n], rs[:, :n], ALU.mult, [src, rs], [s_])
            for (lo, hi, w_) in segs(c0, n):
                P.ts(dst[:, k, lo:hi], s_[:, lo:hi], A[:, w_, k:k + 1], nvt[:, (shl, shc)[w_], k:k + 1],
                     ALU.mult, ALU.add, [s_, A, nvt], [dst])
            if extra is not None:
                extra(k, s_)

    for (c0, n) in tiles:
        P.dma("sync", x_[:, :, :n], xT[:, :, c0:c0 + n], [xT], [x_], x_)
        norm_mod(x_, hT, A1, 2, 4, n, c0)
        for j in range(24):
            wb = wpiece(wg[j], wg)
            ps = PS()
            for k in range(8):
                P.mm(ps[:, :n], wb[:, k, :], hT[:, k, :n], k == 0, k == 7, [wb, hT], [ps])
            P.act(G[:, j, :n], ps[:, :n], AF.Sigmoid, [ps], [G])
        for b in range(2):
            s_ = stg[b % 2]
            P.dma("sync", s_[:, :, :n], yin[b][:, :, c0:c0 + n], [yin], [s_], s_)
            P.copy(ybf[b][:, :, :n], s_[:, :, :n], [s_], [ybf[b]])
        sa, sb_ = stg[0], stg[1]
        P.dma("sync", sa[:, :, :n], yin[2][:, :, c0:c0 + n], [yin], [sa], sa)
        P.dma("sync", sb_[:, :, :n], yin[3][:, :, c0:c0 + n], [yin], [sb_], sb_)
        P.tt(ysum[:, :, :n], sa[:, :, :n], sb_[:, :, :n], ALU.add, [sa, sb_], [ysum])
        P.dma("sync", sa[:, :, :n], yin[4][:, :, c0:c0 + n], [yin], [sa], sa)
        P.dma("sync", sb_[:, :, :n], yin[5][:, :, c0:c0 + n], [yin], [sb_], sb_)
        for c in range(4):
            ps = PS(); P.mm(ps[:, :n], blk, ysum[:, c, :n], True, True, [cs, ysum], [ps])
            P.stt(ysum[:, c, :n], ps[:, :n], -1.0 / 64, ysum[:, c, :n], ALU.mult, ALU.add, [ps, ysum], [ysum])
            P.act(tmp[:, c, :n], ysum[:, c, :n], AF.Square, [ysum], [tmp])
            ps = PS(); P.mm(ps[:, :n], blk, tmp[:, c, :n], True, True, [cs, tmp], [ps])
            P.act(tmp[:, c, :n], ps[:, :n], AF.Sqrt, [ps], [tmp], bias=64e-5, scale=1.0 / 64)
            P.op("vector", lambda e, c=c, n=n: e.reciprocal(tmp[:, c, :n], tmp[:, c, :n]), [tmp], [tmp])
            P.tt(ysum[:, c, :n], ysum[:, c, :n], tmp[:, c, :n], ALU.mult, [ysum, tmp], [ysum])
            P.ts(ysum[:, c, :n], ysum[:, c, :n], rwt[:, 0, c:c + 1], rwt[:, 1, c:c + 1], ALU.mult, ALU.add, [ysum, rwt], [ysum])
            P.tt(ysum[:, c, :n], ysum[:, c, :n], sb_[:, c, :n], ALU.add, [ysum, sb_], [ysum])
            P.tt(ybf[2][:, c, :n], ysum[:, c, :n], sa[:, c, :n], ALU.mult, [ysum, sa], [ybf[2]])
        for oc in range(8):
            for br in range(3):
                wb = wpiece(wo[br, oc], wo, 4)
                ps = PS()
                for k in range(4):
                    P.mm(ps[:, :n], wb[:, k, :], ybf[br][:, k, :n], k == 0, k == 3, [wb, ybf[br]], [ps])
                if br == 0:
                    P.tt(Mo[:, :n], ps[:, :n], G[:, oc, :n], ALU.mult, [ps, G], [Mo])
                else:
                    P.tt(t5[:, :n], ps[:, :n], G[:, br * 8 + oc, :n], ALU.mult, [ps, G], [t5])
                    P.tt(Mo[:, :n], Mo[:, :n], t5[:, :n], ALU.add, [Mo, t5], [Mo])
            P.copy(Mb[:, oc, :n], Mo[:, :n], [Mo], [Mb], eng="scalar")
        for oc in range(8):
            wb = wpiece(wout[oc], wout)
            ps = PS()
            for k in range(8):
                P.mm(ps[:, :n], wb[:, k, :], Mb[:, k, :n], k == 0, k == 7, [wb, Mb], [ps])
            for (lo, hi, w_) in segs(c0, n):
                P.stt(x_[:, oc, lo:hi], ps[:, lo:hi], nvt[:, 5 + w_, oc:oc + 1], x_[:, oc, lo:hi], ALU.mult, ALU.add,
                      [ps, nvt, x_], [x_])
        if moe:
            psr = PS()
            def extra(k, s_):
                for (lo, hi, w_) in segs(c0, n):
                    P.ts(t5[:, lo:hi], s_[:, lo:hi], A2[:, w_, k:k + 1], nvt[:, (9, 11)[w_], k:k + 1],
                         ALU.mult, ALU.add, [s_, A2, nvt], [t5])
                P.mm(psr[0:8, :n], rtt[:, k, :], t5[:, :n], k == 0, k == 7, [rtt, t5], [psr])
            norm_mod(x_, h2, A2, 9, 11, n, c0, extra)
            P.copy(lgT[:, :n], psr[0:8, :n], [psr], [lgT])
            for b0 in range(0, n, 128):
                ps = PS(); P.transpose(ps[:, 0:8], lgT[:, b0:b0 + 128], cs[0:8, 2, 0:8], [lgT, cs], [ps])
                P.copy(lg[:], ps[:, 0:8], [ps], [lg])
                P.op("vector", lambda e: e.max(top[:], lg[:]), [lg], [top])
                P.ts(sm[:, 0:1], top[:, 0:1], -1.0, None, ALU.mult, None, [top], [sm])
                P.act(sm[:, 1:2], top[:, 1:2], AF.Exp, [top, sm], [sm], bias=sm[:, 0:1])
                P.ts(sm[:, 2:3], sm[:, 1:2], 1.0, None, ALU.add, None, [sm], [sm])
                P.op("vector", lambda e: e.reciprocal(sm[:, 2:3], sm[:, 2:3]), [sm], [sm])
                P.tt(sm[:, 3:4], sm[:, 1:2], sm[:, 2:3], ALU.mult, [sm], [sm])
                P.ts(gt_[:], lg[:], top[:, 0:1], sm[:, 2:3], ALU.is_equal, ALU.mult, [lg, top, sm], [gt_])
                P.ts(g2_[:], lg[:], top[:, 1:2], sm[:, 3:4], ALU.is_equal, ALU.mult, [lg, top, sm], [g2_])
                P.tt(gt_[:], gt_[:], g2_[:], ALU.add, [gt_, g2_], [gt_])
                ps = PS(); P.transpose(ps[0:8, 0:128], gt_[:], ident, [gt_, cs], [ps])
                P.copy(gT[:, b0:b0 + 128], ps[0:8, 0:128], [ps], [gT])
        else:
            norm_mod(x_, h2, A2, 9, 11, n, c0)
        if moe:
            P.dma("gpsimd", h2o[:, :, c0:c0 + n], h2[:, :, :n], [h2], [h2o], h2)
            P.dma("gpsimd", gTo[:, c0:c0 + n], gT[:, :n], [gT], [gTo], gT)
        for e_ in range(0 if moe else NE):
            if moe:
                ps = PS(); P.mm(ps[:, :n], selt[:, e_, :], gT[:, :n], True, True, [selt, gT], [ps])
                P.copy(gbe[:, :n], ps[:, :n], [ps], [gbe], eng="scalar")
            for fc in range(FC):
                wb1 = wpiece(w1[e_, fc], w1)
                ps1 = PS()
                for k in range(8):
                    P.mm(ps1[:, :n], wb1[:, k, :], h2[:, k, :n], k == 0, k == 7, [wb1, h2], [ps1])
                wb3 = wpiece(w3[e_, fc], w3)
                ps3 = PS()
                for k in range(8):
                    P.mm(ps3[:, :n], wb3[:, k, :], h2[:, k, :n], k == 0, k == 7, [wb3, h2], [ps3])
                P.act(t5[:, :n], ps1[:, :n], AF.Silu, [ps1], [t5])
                if moe:
                    P.tt(t5[:, :n], t5[:, :n], gbe[:, :n], ALU.mult, [t5, gbe], [t5])
                P.tt(hid[:, fc, :n], t5[:, :n], ps3[:, :n], ALU.mult, [t5, ps3], [hid])
            for oc in range(8):
                ps = PS()
                for k0 in range(0, FC, 8):
                    kc = min(8, FC - k0)
                    wb = wpiece(w2[e_, oc][:, k0:k0 + kc, :], w2, kc)
                    for k in range(kc):
                        P.mm(ps[:, :n], wb[:, k, :], hid[:, k0 + k, :n], k0 + k == 0, k0 + k == FC - 1, [wb, hid], [ps])
                for (lo, hi, w_) in segs(c0, n):
                    P.stt(x_[:, oc, lo:hi], ps[:, lo:hi], nvt[:, 12 + w_, oc:oc + 1], x_[:, oc, lo:hi], ALU.mult, ALU.add,
                          [ps, nvt, x_], [x_])
        P.dma("gpsimd", out[:, :, c0:c0 + n], x_[:, :, :n], [x_], [out], x_)
    return P


def build_L4(NT):
    FC, QC = 28, 7
    tiles = [(c0, min(512, NT - c0)) for c0 in range(0, NT, 512)]
    P = Prog()
    I = lambda n, s, dt=F32: P.dram(n, s, dt, "ExternalInput")
    xm = I("xm", [128, 8, NT]); h2d = I("h2", [128, 8, NT], BF16); gTd = I("gT", [8, NT]); gt2d = I("gt2", [128, 8])
    seld = I("sel", [8, 8, 128])
    w1 = I("w1", [8, FC, 128, 8, 128]); w3 = I("w3", [8, FC, 128, 8, 128]); w2 = I("w2", [8, 8, 128, FC, 128])
    out = P.dram("out", [128, 8, NT], F32, "ExternalOutput")
    XM = P.sbuf([128, 8, NT], F32, "XM"); H2 = P.sbuf([128, 8, NT], BF16, "H2"); GT = P.sbuf([8, NT], F32, "GT")
    gt2 = P.sbuf([128, 8], F32, "gt2s"); selt = P.sbuf([8, 8, 128], F32, "selt")
    for (t, d) in ((XM, xm), (H2, h2d), (GT, gTd), (gt2, gt2d), (selt, seld)):
        P.dma("sync", t[:], d[:], [d], [t], t)
    gbe = P.sbuf([128, NT], F32, "gbe"); hid = P.sbuf([128, QC, NT], BF16, "hid")
    t5 = [P.sbuf([128, 512], F32, f"t5_{i}") for i in range(3)]
    wst = [P.sbuf([128, 8, 128], F32, f"wst{i}") for i in range(4)]
    wbf = [P.sbuf([128, 8, 128], BF16, f"wbf{i}") for i in range(5)]
    psb = [P.psum([128, 512], F32, f"psb{i}") for i in range(8)]
    pi = [0]; wi = [0]; ti = [0]
    def PS():
        pi[0] += 1
        return psb[pi[0] % 8]
    def wpiece(ap, dbuf, kc=8):
        wi[0] += 1
        ws, wb = wst[wi[0] % 4], wbf[wi[0] % 5]
        P.dma("sync", ws[:, :kc, :], ap, [dbuf], [ws], ws)
        P.copy(wb[:, :kc, :], ws[:, :kc, :], [ws], [wb], eng=("gpsimd", "scalar")[wi[0] % 2])
        return wb
    for e_ in range(8):
        for (c0, n) in tiles:
            ps = PS(); P.mm(ps[:, :n], selt[:, e_, :], GT[:, c0:c0 + n], True, True, [selt, GT], [ps])
            P.copy(gbe[:, c0:c0 + n], ps[:, :n], [ps], [gbe], eng="scalar")
        for f0 in range(0, FC, QC):
            for j in range(QC):
                wb1 = wpiece(w1[e_, f0 + j], w1)
                wb3 = wpiece(w3[e_, f0 + j], w3)
                for (c0, n) in tiles:
                    ps1 = PS()
                    for k in range(8):
                        P.mm(ps1[:, :n], wb1[:, k, :], H2[:, k, c0:c0 + n], k == 0, k == 7, [wb1, H2], [ps1])
                    ps3 = PS()
                    for k in range(8):
                        P.mm(ps3[:, :n], wb3[:, k, :], H2[:, k, c0:c0 + n], k == 0, k == 7, [wb3, H2], [ps3])
                    ti[0] += 1
                    t_ = t5[ti[0] % 3]
                    P.act(t_[:, :n], ps1[:, :n], AF.Silu, [ps1], [t_])
                    P.tt(t_[:, :n], t_[:, :n], gbe[:, c0:c0 + n], ALU.mult, [t_, gbe], [t_])
                    P.tt(hid[:, j, c0:c0 + n], t_[:, :n], ps3[:, :n], ALU.mult, [t_, ps3], [hid])
            for oc in range(8):
                wb = wpiece(w2[e_, oc][:, f0:f0 + QC, :], w2, QC)
                for (c0, n) in tiles:
                    ps = PS()
                    for k in range(QC):
                        P.mm(ps[:, :n], wb[:, k, :], hid[:, k, c0:c0 + n], k == 0, k == QC - 1, [wb, hid], [ps])
                    P.stt(XM[:, oc, c0:c0 + n], ps[:, :n], gt2[:, oc:oc + 1], XM[:, oc, c0:c0 + n], ALU.mult, ALU.add,
                          [ps, gt2, XM], [XM])
    P.dma("gpsimd", out[:], XM[:], [XM], [out], XM)
    return P


def arrw(W):
    K_, M_ = W.shape[0] // 128, W.shape[1] // 128
    return np.ascontiguousarray(W.reshape(K_, 128, M_, 128).transpose(2, 1, 0, 3))


def fmT(a):
    C = a.shape[0] // 128
    return a.reshape(C, 128, a.shape[1]).transpose(1, 0, 2)


def run_L3(inp, l, mod, x, ctx, o1, o2):
    SEQ = x.shape[0]
    moe = (l % 2 == 1)
    need_ctx = l < 1
    NL3 = SEQ // NCORES
    NT = NL3 + (256 if need_ctx else 0)
    mv = lambda g, w: mod[:, l, g * 8:(g + 1) * 8, w]
    nv = np.stack([fm(inp["norm1_g"][l]), mv(1, 0), mv(0, 0), mv(1, 1), mv(0, 1), mv(2, 0), mv(2, 1),
                   fm(inp["norm2_g"][l]), mv(4, 0), mv(3, 0), mv(4, 1), mv(3, 1), mv(5, 0), mv(5, 1)], 1)
    W = dict(nv=np.ascontiguousarray(nv, np.float32),
             rwo=np.ascontiguousarray(np.stack([fm(inp["rw_ln_g"][l]), fm(inp["rw_ln_b"][l])], 1)),
             wg=arrw(inp["w_in"][l][:, 4128:7200]),
             wo=np.stack([arrw(inp["mla_wo"][l]), arrw(inp["na_wo"][l]), arrw(inp["rw_wo"][l])], 0),
             wout=arrw(inp["w_out"][l]))
    cst = np.zeros((128, 3, 128), np.float32)
    cst[:, 0, :] = 1.0; cst[:64, 1, :64] = 1.0; cst[64:, 1, 64:] = 1.0; cst[:, 2, :] = np.eye(128)
    W["cst"] = cst
    if moe:
        W["rt"] = np.ascontiguousarray(inp["moe_router"][l // 2].reshape(8, 128, 8).transpose(1, 0, 2))
        sel = np.zeros((8, 8, 128), np.float32)
        for e in range(8):
            sel[e, e, :] = 1.0
        W["sel"] = sel
    else:
        W["w1"] = arrw(inp["ffn_w1"][l // 2])[None]; W["w3"] = arrw(inp["ffn_w3"][l // 2])[None]
        W["w2"] = arrw(inp["ffn_w2"][l // 2])[None]
    g_l, g_c = o1["rwgb"]
    srcs = [o2["ya"], o2["yb"], o2["yf"], o2["ybk"],
            (g_l[0].reshape(512, -1), g_c[0].reshape(512, 256)), (g_l[1].reshape(512, -1), g_c[1].reshape(512, 256))]
    in_maps = []
    for i in range(NCORES):
        sl = slice(i * NL3, (i + 1) * NL3)
        xs = x[sl]
        if need_ctx:
            xs = np.concatenate([xs, ctx], 0)
        m = dict(W)
        m["xT"] = np.ascontiguousarray(fmT(xs.T))
        ys = []
        for (lat, cx) in srcs:
            a = np.asarray(lat, np.float32)[:, sl]
            if need_ctx:
                a = np.concatenate([a, np.asarray(cx, np.float32)], 1)
            ys.append(fmT(a))
        m["yin"] = np.ascontiguousarray(np.stack(ys, 0))
        in_maps.append(m)
    res = run(build_L3(NT, NL3, moe), in_maps)
    if moe:
        W4 = dict(w1=np.stack([arrw(inp["moe_w1"][l // 2][e]) for e in range(8)], 0),
                  w3=np.stack([arrw(inp["moe_w3"][l // 2][e]) for e in range(8)], 0),
                  w2=np.stack([arrw(inp["moe_w2"][l // 2][e]) for e in range(8)], 0),
                  sel=W["sel"], gt2=np.ascontiguousarray(mv(5, 0), np.float32))
        maps4 = []
        for r in res:
            m4 = dict(W4)
            m4.update(xm=np.asarray(r["out"]), h2=np.asarray(r["h2o"]), gT=np.asarray(r["gTo"]))
            maps4.append(m4)
        res = run(build_L4(NT), maps4)
    outs = [np.asarray(r["out"]).transpose(2, 1, 0).reshape(NT, 1024) for r in res]
    x_new = np.concatenate([o[:NL3] for o in outs], 0)
    ctx_new = outs[0][NL3:] if need_ctx else ctx
    return x_new, ctx_new


def kernel(**inp):
    inp = {k: np.asarray(v) for k, v in inp.items()}
    SEQ = inp["x"].shape[1]
    x = np.ascontiguousarray(inp["x"][0]); ctx = np.ascontiguousarray(inp["ctx"][0])
    mod = run_L0(inp["c"], inp["c_ctx"], inp["mod_w"], inp["mod_b"])
    depth = inp["mod_w"].shape[0]
    for l in range(depth):
        o1 = run_L1(inp, l, mod, x, ctx, SEQ // 16, 2)
        o2 = run_L2(inp, l, o1, SEQ)
        del o1["qa"], o1["ka"], o1["va"], o1["nqkv"], o1["rw3"], o1["rwd"]
        x, ctx = run_L3(inp, l, mod, x, ctx, o1, o2)
    return np.ascontiguousarray(x[None].astype(np.float32))
```
